# Optimizing a Trainium2 kernel written in Bass

```python
import jax, jax.numpy as jnp
from jax import lax
import numpy as np

D_MODEL = 1024
BATCH = 16
SEQ = 2048
DEPTH = 4

N_EVEN = (DEPTH + 1) // 2
N_ODD = DEPTH // 2

NSA_HEADS = 8
NSA_KV_HEADS = 2
NSA_GROUP = NSA_HEADS // NSA_KV_HEADS
NSA_HEAD_DIM = 64
NSA_WIDTH = NSA_HEADS * NSA_HEAD_DIM
NSA_KV_WIDTH = NSA_KV_HEADS * NSA_HEAD_DIM
CMP_BLOCK = 32
CMP_STRIDE = 16
SLC_BLOCK = 64
SLC_TOPK = 8
WINDOW = 512
Q_BLOCK = 128
FORCE_BONUS = 1e4
NEG_INF = -1e30

SGU_GROUPS = 8
SGU_GROUP_DIM = 64
SGU_WIDTH = SGU_GROUPS * SGU_GROUP_DIM
SGU_CHUNK = 128

CONV_WIDTH = D_MODEL
CONV_KERNEL = 31

RMS_EPS = 1e-6
LN_EPS = 1e-5

EVEN_SPLIT_SIZES = (NSA_WIDTH, NSA_KV_WIDTH, NSA_KV_WIDTH, NSA_KV_WIDTH, NSA_KV_WIDTH,
                    NSA_KV_WIDTH, NSA_KV_WIDTH, 3 * NSA_HEADS, NSA_WIDTH,
                    SGU_WIDTH, SGU_WIDTH, SGU_WIDTH)
EVEN_IN_WIDTH = sum(EVEN_SPLIT_SIZES)
EVEN_MIX_WIDTH = NSA_WIDTH + SGU_WIDTH

kernel_name = "nsa_sgu_conformer_hybrid"


def _offsets(sizes):
    out, acc = [], 0
    for s in sizes[:-1]:
        acc += s
        out.append(acc)
    return out


def rms_norm(x, g):
    x32 = x.astype(jnp.float32)
    y = x32 * lax.rsqrt(jnp.mean(x32 * x32, axis=-1, keepdims=True) + RMS_EPS)
    return (y * g.astype(jnp.float32)).astype(x.dtype)


def layer_norm(x, g, b):
    x32 = x.astype(jnp.float32)
    mu = jnp.mean(x32, axis=-1, keepdims=True)
    xc = x32 - mu
    y = xc * lax.rsqrt(jnp.mean(xc * xc, axis=-1, keepdims=True) + LN_EPS)
    return (y * g.astype(jnp.float32) + b.astype(jnp.float32)).astype(x.dtype)


def masked_softmax(s, mask):
    p = jax.nn.softmax(jnp.where(mask, s, NEG_INF), axis=-1)
    return jnp.where(mask, p, 0.0)


def compress_blocks(k, pe, w1, w2):
    B, S, G, dh = k.shape
    n_cmp = (S - CMP_BLOCK) // CMP_STRIDE + 1
    idx = np.arange(n_cmp)[:, None] * CMP_STRIDE + np.arange(CMP_BLOCK)[None, :]
    blk = k[:, idx] + pe[None, None, :, None, :]
    blk = jnp.swapaxes(blk, 2, 3).reshape(B, n_cmp, G, CMP_BLOCK * dh)
    return jax.nn.silu(blk @ w1) @ w2


def nsa_attention(q, kc, vc, ks, vs, kw, vw, gate_logits,
                  cmp_k_pe, cmp_k_w1, cmp_k_w2, cmp_v_pe, cmp_v_w1, cmp_v_w2):
    B, S, _ = q.shape
    G, R, dh = NSA_KV_HEADS, NSA_GROUP, NSA_HEAD_DIM
    q = q.reshape(B, S, G, R, dh) * (dh ** -0.5)
    kc, vc, ks, vs, kw, vw = [a.reshape(B, S, G, dh) for a in (kc, vc, ks, vs, kw, vw)]
    t = np.arange(S)

    k_cmp = compress_blocks(kc, cmp_k_pe, cmp_k_w1, cmp_k_w2)
    v_cmp = compress_blocks(vc, cmp_v_pe, cmp_v_w1, cmp_v_w2)
    n_cmp = k_cmp.shape[1]
    cmp_end = np.arange(n_cmp) * CMP_STRIDE + CMP_BLOCK - 1
    cmp_mask = cmp_end[None, :] <= t[:, None]
    s_cmp = jnp.einsum('bsgrd,bngd->bgrsn', q, k_cmp).astype(jnp.float32)
    p_cmp = masked_softmax(s_cmp, cmp_mask)
    o_cmp = jnp.einsum('bgrsn,bngd->bsgrd', p_cmp.astype(v_cmp.dtype), v_cmp)

    n_slc = S // SLC_BLOCK
    k_top = min(SLC_TOPK, n_slc)
    tok = np.arange(n_cmp)[:, None] * CMP_STRIDE + np.arange(CMP_BLOCK)[None, :]
    overlap = ((tok[:, :, None] // SLC_BLOCK) == np.arange(n_slc)[None, None, :]).sum(1)
    overlap = overlap.astype(np.float32) / np.float32(CMP_BLOCK)
    imp = jnp.einsum('bgrsn,nj->bgsj', p_cmp, overlap)
    cur = t // SLC_BLOCK
    j = np.arange(n_slc)
    forced = (j[None, :] == 0) | (j[None, :] == cur[:, None]) | (j[None, :] == cur[:, None] - 1)
    causal_blk = j[None, :] <= cur[:, None]
    imp = jnp.where(causal_blk, imp + forced.astype(np.float32) * np.float32(FORCE_BONUS), NEG_INF)
    _, sel = lax.top_k(imp, k_top)

    ks_blk = ks.reshape(B, n_slc, SLC_BLOCK, G, dh).transpose(0, 3, 1, 2, 4)
    vs_blk = vs.reshape(B, n_slc, SLC_BLOCK, G, dh).transpose(0, 3, 1, 2, 4)
    kw_pad = jnp.pad(kw, ((0, 0), (WINDOW, 0), (0, 0), (0, 0)))
    vw_pad = jnp.pad(vw, ((0, 0), (WINDOW, 0), (0, 0), (0, 0)))
    bi = jnp.arange(B)[:, None, None, None]
    gi = jnp.arange(G)[None, :, None, None]

    def block_fn(qb):
        s0 = qb * Q_BLOCK
        q_blk = lax.dynamic_slice_in_dim(q, s0, Q_BLOCK, axis=1)
        tq = s0 + jnp.arange(Q_BLOCK)
        sel_blk = lax.dynamic_slice_in_dim(sel, s0, Q_BLOCK, axis=2)
        k_sel = ks_blk[bi, gi, sel_blk].reshape(B, G, Q_BLOCK, k_top * SLC_BLOCK, dh)
        v_sel = vs_blk[bi, gi, sel_blk].reshape(B, G, Q_BLOCK, k_top * SLC_BLOCK, dh)
        pos = (sel_blk[..., None] * SLC_BLOCK + jnp.arange(SLC_BLOCK)).reshape(B, G, Q_BLOCK, -1)
        m_sel = (pos <= tq[None, None, :, None])[:, :, None]
        s_sel = jnp.einsum('bqgrd,bgqkd->bgrqk', q_blk, k_sel).astype(jnp.float32)
        p_sel = masked_softmax(s_sel, m_sel)
        o_sel = jnp.einsum('bgrqk,bgqkd->bqgrd', p_sel.astype(v_sel.dtype), v_sel)
        k_win = lax.dynamic_slice_in_dim(kw_pad, s0, Q_BLOCK + WINDOW, axis=1)
        v_win = lax.dynamic_slice_in_dim(vw_pad, s0, Q_BLOCK + WINDOW, axis=1)
        kp = s0 - WINDOW + jnp.arange(Q_BLOCK + WINDOW)
        m_win = ((kp[None, :] <= tq[:, None]) & (kp[None, :] > tq[:, None] - WINDOW)
                 & (kp[None, :] >= 0))
        s_win = jnp.einsum('bqgrd,bkgd->bgrqk', q_blk, k_win).astype(jnp.float32)
        p_win = masked_softmax(s_win, m_win)
        o_win = jnp.einsum('bgrqk,bkgd->bqgrd', p_win.astype(v_win.dtype), v_win)
        return o_sel, o_win

    o_sel, o_win = lax.map(block_fn, jnp.arange(S // Q_BLOCK))
    o_sel = jnp.moveaxis(o_sel, 0, 1).reshape(B, S, G, R, dh)
    o_win = jnp.moveaxis(o_win, 0, 1).reshape(B, S, G, R, dh)

    gates = jax.nn.sigmoid(gate_logits.astype(jnp.float32)).reshape(B, S, 3, G, R, 1).astype(q.dtype)
    o = gates[:, :, 0] * o_cmp + gates[:, :, 1] * o_sel + gates[:, :, 2] * o_win
    return o.reshape(B, S, NSA_WIDTH)


def spatial_gating(u, v, ln_g, ln_b, w_s, b_s):
    B, S, _ = u.shape
    v = layer_norm(v, ln_g, ln_b)
    v = v.reshape(B, S // SGU_CHUNK, SGU_CHUNK, SGU_GROUPS, SGU_GROUP_DIM)
    mixed = jnp.einsum('gij,bnjgd->bnigd', jnp.tril(w_s), v) + b_s.T[None, None, :, :, None]
    return u * mixed.reshape(B, S, SGU_WIDTH)


def even_layer(h, w_in, cmp_k_pe, cmp_k_w1, cmp_k_w2, cmp_v_pe, cmp_v_w1, cmp_v_w2,
               sgu_ln_g, sgu_ln_b, sgu_w, sgu_b, w_out):
    parts = jnp.split(h @ w_in, _offsets(EVEN_SPLIT_SIZES), axis=-1)
    q, kc, vc, ks, vs, kw, vw, g_logit, z_a, u, v, z_b = parts
    a = nsa_attention(q, kc, vc, ks, vs, kw, vw, g_logit,
                      cmp_k_pe, cmp_k_w1, cmp_k_w2, cmp_v_pe, cmp_v_w1, cmp_v_w2) * jax.nn.silu(z_a)
    b = spatial_gating(jax.nn.gelu(u), jax.nn.gelu(v), sgu_ln_g, sgu_ln_b, sgu_w, sgu_b) * jax.nn.silu(z_b)
    return jnp.concatenate([a, b], axis=-1) @ w_out


def conv_layer(h, w_in, dw_w, dw_b, ln_g, ln_b, w_out):
    a, gl, z = jnp.split(h @ w_in, 3, axis=-1)
    y = a * jax.nn.sigmoid(gl)
    y = lax.conv_general_dilated(y, dw_w[:, None, :], window_strides=(1,),
                                 padding=((CONV_KERNEL - 1, 0),),
                                 dimension_numbers=('NWC', 'WIO', 'NWC'),
                                 feature_group_count=CONV_WIDTH) + dw_b
    y = jax.nn.silu(layer_norm(y, ln_g, ln_b))
    return (y * jax.nn.silu(z)) @ w_out


def setup_inputs(seed: int = 0) -> dict:
    key = jax.random.key(seed)
    ks = jax.random.split(key, 24)

    def nrm(k, shape, scale):
        return jax.random.normal(k, shape, jnp.float32) * scale

    dh = NSA_HEAD_DIM
    return {
        "x": nrm(ks[0], (BATCH, SEQ, D_MODEL), 1.0),
        "norm_pre": 1.0 + nrm(ks[1], (DEPTH, D_MODEL), 0.01),
        "norm_post": 1.0 + nrm(ks[2], (DEPTH, D_MODEL), 0.01),
        "even_w_in": nrm(ks[3], (N_EVEN, D_MODEL, EVEN_IN_WIDTH), D_MODEL ** -0.5),
        "even_cmp_k_pe": nrm(ks[4], (N_EVEN, CMP_BLOCK, dh), 0.02),
        "even_cmp_k_w1": nrm(ks[5], (N_EVEN, CMP_BLOCK * dh, dh), (CMP_BLOCK * dh) ** -0.5),
        "even_cmp_k_w2": nrm(ks[6], (N_EVEN, dh, dh), dh ** -0.5),
        "even_cmp_v_pe": nrm(ks[7], (N_EVEN, CMP_BLOCK, dh), 0.02),
        "even_cmp_v_w1": nrm(ks[8], (N_EVEN, CMP_BLOCK * dh, dh), (CMP_BLOCK * dh) ** -0.5),
        "even_cmp_v_w2": nrm(ks[9], (N_EVEN, dh, dh), dh ** -0.5),
        "even_sgu_ln_g": 1.0 + nrm(ks[10], (N_EVEN, SGU_WIDTH), 0.01),
        "even_sgu_ln_b": nrm(ks[11], (N_EVEN, SGU_WIDTH), 0.01),
        "even_sgu_w": nrm(ks[12], (N_EVEN, SGU_GROUPS, SGU_CHUNK, SGU_CHUNK), SGU_CHUNK ** -0.5),
        "even_sgu_b": 1.0 + nrm(ks[13], (N_EVEN, SGU_GROUPS, SGU_CHUNK), 0.01),
        "even_w_out": nrm(ks[14], (N_EVEN, EVEN_MIX_WIDTH, D_MODEL), EVEN_MIX_WIDTH ** -0.5),
        "odd_w_in": nrm(ks[15], (N_ODD, D_MODEL, 3 * CONV_WIDTH), D_MODEL ** -0.5),
        "odd_dw_w": nrm(ks[16], (N_ODD, CONV_KERNEL, CONV_WIDTH), CONV_KERNEL ** -0.5),
        "odd_dw_b": nrm(ks[17], (N_ODD, CONV_WIDTH), 0.01),
        "odd_ln_g": 1.0 + nrm(ks[18], (N_ODD, CONV_WIDTH), 0.01),
        "odd_ln_b": nrm(ks[19], (N_ODD, CONV_WIDTH), 0.01),
        "odd_w_out": nrm(ks[20], (N_ODD, CONV_WIDTH, D_MODEL), CONV_WIDTH ** -0.5),
    }


def reference(x, norm_pre, norm_post, even_w_in, even_cmp_k_pe, even_cmp_k_w1, even_cmp_k_w2,
              even_cmp_v_pe, even_cmp_v_w1, even_cmp_v_w2, even_sgu_ln_g, even_sgu_ln_b,
              even_sgu_w, even_sgu_b, even_w_out, odd_w_in, odd_dw_w, odd_dw_b, odd_ln_g,
              odd_ln_b, odd_w_out):
    for i in range(DEPTH):
        h = rms_norm(x, norm_pre[i])
        li = i // 2
        if i % 2 == 0:
            y = even_layer(h, even_w_in[li], even_cmp_k_pe[li], even_cmp_k_w1[li], even_cmp_k_w2[li],
                           even_cmp_v_pe[li], even_cmp_v_w1[li], even_cmp_v_w2[li],
                           even_sgu_ln_g[li], even_sgu_ln_b[li], even_sgu_w[li], even_sgu_b[li],
                           even_w_out[li])
        else:
            y = conv_layer(h, odd_w_in[li], odd_dw_w[li], odd_dw_b[li], odd_ln_g[li],
                           odd_ln_b[li], odd_w_out[li])
        x = x + rms_norm(y, norm_post[i])
    return x
```

```python
from contextlib import ExitStack

import numpy as np
import concourse.bass as bass
import concourse.mybir as mybir
from concourse.bass_utils import run_bass_kernel_spmd

F32 = mybir.dt.float32
BF16 = mybir.dt.bfloat16
AF = mybir.ActivationFunctionType
ALU = mybir.AluOpType

NCORES = 8
SEQ = 2048
D = 1024
NSEQ = 2
NBLK = 4
EVEN_W = 3352
ODD_W = 3072
RMS_EPS = 1e-6
LN_EPS = 1e-5
BIG = 30000.0
import os as _os
STOP = _os.environ.get("KSTOP", "")

C_Q, C_KC, C_VC, C_KS, C_VS, C_KW, C_VW, C_GATE, C_ZA, C_U, C_V, C_ZB = (
    0, 512, 640, 768, 896, 1024, 1152, 1280, 1304, 1816, 2328, 2840)


class Sem:
    __slots__ = ("h", "val")

    def __init__(self, h):
        self.h = h
        self.val = 0


class Buf:
    __slots__ = ("name", "w", "r", "dsem")

    def __init__(self, name):
        self.name = name
        self.w = None
        self.r = {}
        self.dsem = None


class Trk:
    def __init__(self, nc, stack):
        self.nc = nc
        self.stack = stack
        self.eng = {"pe": nc.tensor, "act": nc.scalar, "dve": nc.vector, "pool": nc.gpsimd, "sp": nc.sync}
        self.nsem = 0
        self.esem = {e: self.newsem("e_" + e) for e in self.eng}
        self.waited = {e: {} for e in self.eng}
        self.nops = 0
        self.dsems = []

    def newsem(self, name):
        self.nsem += 1
        return Sem(self.stack.enter_context(self.nc.semaphore(f"{name}_{self.nsem}")))

    def _sync(self, E, reads, writes):
        deps = {}
        for b in reads:
            if b.w is not None:
                s, v = b.w
                if deps.get(s, 0) < v:
                    deps[s] = v
        for b in writes:
            if b.w is not None:
                s, v = b.w
                if deps.get(s, 0) < v:
                    deps[s] = v
            for s, v in b.r.items():
                if deps.get(s, 0) < v:
                    deps[s] = v
        eng = self.eng[E]
        w = self.waited[E]
        own = self.esem[E]
        for s, v in deps.items():
            if E == "pe" and s is own:
                continue
            if w.get(s, 0) < v:
                eng.wait_ge(s.h, v)
                w[s] = v

    def op(self, E, fn, reads=(), writes=(), signal=True):
        self._sync(E, reads, writes)
        ins = fn(self.eng[E])
        s = self.esem[E]
        if signal:
            s.val += 1
            ins.then_inc(s.h, 1)
            tag = (s, s.val)
        else:
            tag = (s, s.val + 1)
        for b in reads:
            if b.r.get(s, 0) < tag[1]:
                b.r[s] = tag[1]
        for b in writes:
            b.w = tag
            b.r = {}
        self.nops += 1
        return ins

    def barrier(self):
        sems = list(self.esem.values()) + self.dsems
        for E, eng in self.eng.items():
            w = self.waited[E]
            for s in sems:
                if s.val > 0 and w.get(s, 0) < s.val:
                    eng.wait_ge(s.h, s.val)
                    w[s] = s.val

    def dma(self, E, out, in_, reads, writes, dbuf, **kw):
        self._sync(E, reads, writes)
        if dbuf.dsem is None:
            dbuf.dsem = self.newsem("d_" + dbuf.name)
            self.dsems.append(dbuf.dsem)
        s = dbuf.dsem
        ins = self.eng[E].dma_start(out=out, in_=in_, **kw)
        s.val += 16
        ins.then_inc(s.h, 16)
        tag = (s, s.val)
        for b in reads:
            if b.r.get(s, 0) < tag[1]:
                b.r[s] = tag[1]
        for b in writes:
            b.w = tag
            b.r = {}
        self.nops += 1
        return ins


class Rot:
    def __init__(self, items):
        self.items = items
        self.i = 0

    def next(self):
        it = self.items[self.i % len(self.items)]
        self.i += 1
        return it


def build_program(n_layers=4):
    nc = bass.Bass("TRN2", target_bir_lowering=False, dynamic_dma_scratch_size=16384)
    stack = ExitStack()
    K = Trk(nc, stack)

    def dram(name, shape, dt=F32, kind="ExternalInput"):
        return nc.dram_tensor(name, list(shape), dt, kind=kind).ap()

    x_in = dram("x", [NSEQ, SEQ, D])
    out = dram("out", [NSEQ, SEQ, D], kind="ExternalOutput")
    norm_pre = dram("norm_pre", [4, D])
    norm_post = dram("norm_post", [4, D])
    e_w_in = dram("even_w_in", [2, D, EVEN_W])
    e_k_pe = dram("even_cmp_k_pe", [2, 32, 64])
    e_k_w1 = dram("even_cmp_k_w1", [2, 2048, 64])
    e_k_w2 = dram("even_cmp_k_w2", [2, 64, 64])
    e_v_pe = dram("even_cmp_v_pe", [2, 32, 64])
    e_v_w1 = dram("even_cmp_v_w1", [2, 2048, 64])
    e_v_w2 = dram("even_cmp_v_w2", [2, 64, 64])
    e_ln_g = dram("even_sgu_ln_g", [2, 512])
    e_ln_b = dram("even_sgu_ln_b", [2, 512])
    e_sgu_w = dram("even_sgu_w", [2, 8, 128, 128])
    e_sgu_b = dram("even_sgu_b", [2, 8, 128])
    e_w_out = dram("even_w_out", [2, D, D])
    o_w_in = dram("odd_w_in", [2, D, ODD_W])
    o_dw_w = dram("odd_dw_w", [2, 31, D])
    o_dw_b = dram("odd_dw_b", [2, D])
    o_ln_g = dram("odd_ln_g", [2, D])
    o_ln_b = dram("odd_ln_b", [2, D])
    o_w_out = dram("odd_w_out", [2, D, D])
    c_ident = dram("c_ident", [128, 128])
    c_tril = dram("c_tril", [128, 128])
    c_far = dram("c_far", [128, 128])
    c_ctab = dram("c_ctab", [128, 16, 32])
    c_ebig = dram("c_ebig", [32, 2048])
    c_ovl = dram("c_ovl", [127, 32])

    def sb(name, shape, dt=F32):
        t = stack.enter_context(nc.sbuf_tensor(name, list(shape), dt))
        return t, Buf(name)

    def sbn(name, shape, dt, n):
        return Rot([sb(f"{name}{i}", shape, dt) for i in range(n)])

    w_in = stack.enter_context(nc.sbuf_tensor("w_in", [128, 8, EVEN_W], BF16))
    w_inB = [Buf(f"w_in{k}") for k in range(8)]
    w_out = stack.enter_context(nc.sbuf_tensor("w_out", [128, 8, D], BF16))
    w_outB = [Buf(f"w_out{k}") for k in range(8)]
    identb, identbB = sb("identb", [128, 128], BF16)
    identf, identfB = sb("identf", [128, 128], F32)
    trilk, trilkB = sb("trilk", [128, 128], BF16)
    fark, farkB = sb("fark", [128, 128], BF16)
    neghalf, neghalfB = sb("neghalf", [128, 8], F32)
    gpost, gpostB = sb("gpost", [128, D], F32)
    xa = sbn("xa", [128, D], F32, 2)
    xr = sbn("xr", [128, D], F32, 2)
    hp = sbn("hp", [128, D], BF16, 2)
    hT = stack.enter_context(nc.sbuf_tensor("hT", [128, 8, 512], BF16))
    hTB = [Buf(f"hT{t}") for t in range(4)]
    mixT = stack.enter_context(nc.sbuf_tensor("mixT", [128, 8, 512], BF16))
    mixTB = [Buf(f"mixT{t}") for t in range(4)]
    junks = sbn("junk", [128, D], BF16, 2)
    ss, _ = sb("ss", [128, 8], F32)
    ssB = [Buf(f"ss{t}") for t in range(8)]
    ms, _ = sb("ms", [128, 8], F32)
    msB = [Buf(f"ms{t}") for t in range(8)]
    rstd, _ = sb("rstd", [128, 8], F32)
    rstdB = [Buf(f"rstd{t}") for t in range(8)]
    ssy = Rot([sb(f"ssy{i}", [128, 4], F32) + (Buf(f"ssy{i}a"), Buf(f"ssy{i}b")) for i in range(2)])
    rsy = sbn("rsy", [128, 4], F32, 2)
    par, parB = sb("par", [128, 8, 36], F32)
    tmpf = sbn("tmpf", [128, 512], F32, 2)

    PS = []
    for i in range(8):
        t = stack.enter_context(nc.psum_tensor(f"ps{i}", [128, 512], F32))
        PS.append((t, Buf(f"ps{i}")))
    psA = Rot(PS[0:4])
    psB = Rot(PS[4:8])

    xd = [[Buf(f"xd{s}_{t}") for t in range(16)] for s in range(NSEQ)]

    K.dma("pool", identb[:, :], c_ident[:, :], [], [identbB], identbB)
    K.dma("sp", identf[:, :], c_ident[:, :], [], [identfB], identfB)
    K.dma("pool", trilk[:, :], c_tril[:, :], [], [trilkB], trilkB)
    K.dma("pool", fark[:, :], c_far[:, :], [], [farkB], farkB)
    K.op("dve", lambda e: e.memset(neghalf[:, :], -0.5), [], [neghalfB])

    def load_common(layer, w_in_dram, w_in_cols, w_out_dram, rows):
        stg, stgB = xa.next()
        rows = [(norm_pre[layer:layer + 1, :], 1)] + rows
        if sum(r for _, r in rows) % 2:
            rows = rows + [(norm_post[layer:layer + 1, :], 1)]
        r0 = 0
        for ap, r in rows:
            K.dma("sp", stg[r0:r0 + r, :], ap, [], [stgB], stgB)
            r0 += r
        R = r0
        for c in range(8):
            pt, pB = psA.next()
            K.op("pe", lambda e, c=c, pt=pt: e.transpose(pt[:, 0:R], stg[0:R, c * 128:(c + 1) * 128], identf[0:R, 0:R]),
                 [stgB, identfB], [pB])
            K.op("dve", lambda e, c=c, pt=pt: e.tensor_copy(out=par[:, c, 0:R], in_=pt[:, 0:R]), [pB], [parB])
        for kc in range(8):
            K.dma("pool", w_in[:, kc, 0:w_in_cols], w_in_dram[kc * 128:(kc + 1) * 128, :], [], [w_inB[kc]], w_inB[kc],
                  max_dma_last_dim=4096)
        for kc in range(8):
            K.dma("pool", w_out[:, kc, :], w_out_dram[kc * 128:(kc + 1) * 128, :], [], [w_outB[kc]], w_outB[kc],
                  max_dma_last_dim=4096)
        K.dma("sp", gpost[:, :], norm_post[layer:layer + 1, :].to_broadcast([128, D]), [], [gpostB], gpostB)

    def phase_A(layer, s, b):
        src = x_in if layer == 0 else out
        xts = []
        for tt in range(4):
            xt, xB = xa.next()
            tile = b * 4 + tt
            K.dma("sp", xt[:, :], src[s, tile * 128:(tile + 1) * 128, :], [xd[s][tile]], [xB], xB)
            jk, jkB = junks.next()
            K.op("act", lambda e, xt=xt, tt=tt, jk=jk: e.activation(out=jk[:, :], in_=xt[:, :], func=AF.Square,
                                                                    accum_out=ss[:, tt:tt + 1]), [xB], [ssB[tt], jkB])
            K.op("dve", lambda e, tt=tt: e.tensor_scalar(out=ms[:, tt:tt + 1], in0=ss[:, tt:tt + 1], scalar1=1.0 / D,
                                                        scalar2=RMS_EPS, op0=ALU.mult, op1=ALU.add), [ssB[tt]], [msB[tt]])
            K.op("pool", lambda e, tt=tt: e.tensor_tensor(out=rstd[:, tt:tt + 1], in0=ms[:, tt:tt + 1],
                                                          in1=neghalf[:, 0:1], op=ALU.pow), [msB[tt], neghalfB], [rstdB[tt]])
            ht, hB = hp.next()
            K.op("dve", lambda e, ht=ht, xt=xt, tt=tt: e.tensor_scalar(out=ht[:, :], in0=xt[:, :], scalar1=rstd[:, tt:tt + 1],
                                                                       scalar2=None, op0=ALU.mult), [xB, rstdB[tt]], [hB])
            pt, pB = psA.next()
            pb = pt[:].bitcast(BF16)
            for kc in range(8):
                K.op("pe", lambda e, kc=kc, pb=pb, ht=ht: e.transpose(pb[:, kc * 128:(kc + 1) * 128],
                                                                      ht[:, kc * 128:(kc + 1) * 128], identb[:, :]),
                     [hB, identbB], [pB], signal=(kc == 7))
            K.op("dve", lambda e, pb=pb, tt=tt: e.tensor_tensor(
                out=hT[:, :, tt * 128:(tt + 1) * 128], in0=pb.rearrange("p (k t) -> p k t", k=8),
                in1=par[:, :, 0:1].to_broadcast([128, 8, 128]), op=ALU.mult), [pB, parB], [hTB[tt]])

    def fm_proj(col0, M):
        pt, pB = psA.next()
        for kc in range(8):
            K.op("pe", lambda e, kc=kc, pt=pt: e.matmul(pt[0:M, 0:512], w_in[:, kc, col0:col0 + M], hT[:, kc, :],
                                                        start=(kc == 0), stop=(kc == 7)),
                 [w_inB[kc]] + hTB, [pB], signal=(kc == 7))
        return pt, pB

    def tm_proj(tt, col0, N):
        pt, pB = psA.next()
        for kc in range(8):
            K.op("pe", lambda e, kc=kc, pt=pt: e.matmul(pt[:, 0:N], hT[:, kc, tt * 128:(tt + 1) * 128],
                                                        w_in[:, kc, col0:col0 + N], start=(kc == 0), stop=(kc == 7)),
                 [w_inB[kc], hTB[tt]], [pB], signal=(kc == 7))
        return pt, pB

    def phase_G(layer, s, b):
        src = x_in if layer == 0 else out
        for tt in range(4):
            tile = b * 4 + tt
            xt, xB = xr.next()
            K.dma("sp", xt[:, :], src[s, tile * 128:(tile + 1) * 128, :], [xd[s][tile]], [xB], xB)
            halves = []
            sy, syB, syB0, syB1 = ssy.next()
            ry, ryB = rsy.next()
            for hf in range(2):
                pt, pB = psB.next()
                for fc in range(8):
                    K.op("pe", lambda e, fc=fc, pt=pt, hf=hf: e.matmul(pt[:, 0:512], mixT[:, fc, tt * 128:(tt + 1) * 128],
                                                                        w_out[:, fc, hf * 512:(hf + 1) * 512],
                                                                        start=(fc == 0), stop=(fc == 7)),
                         [mixTB[tt], w_outB[fc]], [pB], signal=(fc == 7))
                halves.append((pt, pB))
            for hf, sB_ in ((0, syB0), (1, syB1)):
                pt, pB = halves[hf]
                jk, jkB = junks.next()
                K.op("act", lambda e, pt=pt, hf=hf, sy=sy, jk=jk: e.activation(out=jk[:, 0:512], in_=pt[:, 0:512], func=AF.Square,
                                                                               accum_out=sy[:, hf:hf + 1]), [pB], [sB_, jkB])
            K.op("dve", lambda e, sy=sy: e.tensor_tensor(out=sy[:, 2:3], in0=sy[:, 0:1], in1=sy[:, 1:2], op=ALU.add),
                 [syB0, syB1, syB], [syB])
            K.op("dve", lambda e, sy=sy: e.tensor_scalar(out=sy[:, 3:4], in0=sy[:, 2:3], scalar1=1.0 / D, scalar2=RMS_EPS,
                                                        op0=ALU.mult, op1=ALU.add), [syB], [syB])
            K.op("pool", lambda e, sy=sy, ry=ry: e.tensor_tensor(out=ry[:, 0:1], in0=sy[:, 3:4], in1=neghalf[:, 0:1],
                                                                 op=ALU.pow), [syB, neghalfB], [ryB])
            for hf in range(2):
                pt, pB = halves[hf]
                tf, tfB = tmpf.next()
                K.op("dve", lambda e, pt=pt, tf=tf, hf=hf, ry=ry: e.scalar_tensor_tensor(
                    out=tf[:, :], in0=pt[:, 0:512], scalar=ry[:, 0:1], in1=gpost[:, hf * 512:(hf + 1) * 512],
                    op0=ALU.mult, op1=ALU.mult), [pB, ryB, gpostB], [tfB])
                K.op("pool", lambda e, tf=tf, xt=xt, hf=hf: e.tensor_tensor(
                    out=xt[:, hf * 512:(hf + 1) * 512], in0=xt[:, hf * 512:(hf + 1) * 512], in1=tf[:, :], op=ALU.add),
                     [tfB, xB], [xB])
            K.dma("sp", out[s, tile * 128:(tile + 1) * 128, :], xt[:, :], [xB], [xd[s][tile]], xd[s][tile])

    def even_layer(layer, li):
        st = ExitStack()

        def sbl(name, shape, dt=F32):
            t = st.enter_context(nc.sbuf_tensor(f"{name}_{layer}", list(shape), dt))
            return t, Buf(name)

        def sbln(name, shape, dt, n):
            return Rot([sbl(f"{name}{i}", shape, dt) for i in range(n)])

        qT = st.enter_context(nc.sbuf_tensor(f"qT_{layer}", [128, 8, 512], BF16))
        qTB = [Buf(f"qT{h}") for h in range(8)]
        qSB = [Buf(f"qS{h}") for h in range(8)]
        ksT = st.enter_context(nc.sbuf_tensor(f"ksT_{layer}", [128, 2, 2048], BF16))
        ksTB = [[Buf(f"ksT{g}_{b}") for b in range(4)] for g in range(2)]
        ebB = Buf("ebig")
        kwT = st.enter_context(nc.sbuf_tensor(f"kwT_{layer}", [64, 2, 1024], BF16))
        kwTB = [[Buf(f"kwT{g}_{r}") for r in range(2)] for g in range(2)]
        kcT, kcTB = sbl("kcT", [128, 528], BF16)
        vcT, vcTB = sbl("vcT", [128, 528], BF16)
        vsA = st.enter_context(nc.sbuf_tensor(f"vsA_{layer}", [128, 16, 2, 65], BF16))
        vsAB = [Buf(f"vsA{k}") for k in range(16)]
        vwA = st.enter_context(nc.sbuf_tensor(f"vwA_{layer}", [128, 8, 2, 65], BF16))
        vwAB = [Buf(f"vwA{k}") for k in range(8)]
        w1k, w1kB = sbl("w1k", [128, 32, 64], BF16)
        w1v, w1vB = sbl("w1v", [128, 32, 64], BF16)
        w2k, w2kB = sbl("w2k", [64, 64], BF16)
        w2v, w2vB = sbl("w2v", [64, 64], BF16)
        pek, pekB = sbl("pek", [32, 64], BF16)
        pev, pevB = sbl("pev", [32, 64], BF16)
        peT, peTB = sbl("peT", [64, 2, 32], BF16)
        pebias, pebiasB = sbl("pebias", [64, 2], F32)
        hidk, hidkB = sbl("hidk", [64, 2, 32], BF16)
        hidv, hidvB = sbl("hidv", [64, 2, 128], BF16)
        kcmpT, kcmpTB = sbl("kcmpT", [64, 2, 128], BF16)
        vcmp, vcmpB = sbl("vcmp", [128, 2, 97], BF16)
        ctab, ctabB = sbl("ctab", [128, 16, 32], F32)
        WsT, WsTB = sbl("WsT", [128, 8, 128], BF16)
        bs, bsB = sbl("bs", [128, 8], F32)
        lng, lngB = sbl("lng", [128, 512], F32)
        lnb, lnbB = sbl("lnb", [128, 512], F32)
        gates = st.enter_context(nc.sbuf_tensor(f"gates_{layer}", [128, 4, 24], F32))
        gatesB = [Buf(f"gates{t}") for t in range(4)]
        sza = st.enter_context(nc.sbuf_tensor(f"sza_{layer}", [128, 4, 512], BF16))
        szaB = [Buf(f"sza{t}") for t in range(4)]
        ug = sbln("ug", [128, 512], BF16, 2)
        szb = sbln("szb", [128, 512], BF16, 2)
        vg = sbln("vg", [128, 512], F32, 2)
        vln = sbln("vln", [128, 512], BF16, 2)
        lnst = sbln("lnst", [128, 16], F32, 2)
        acc_o = st.enter_context(nc.sbuf_tensor(f"acc_o_{layer}", [128, 4, 512], F32))
        acc_oB = [Buf(f"acc_o{h}") for h in range(8)]
        imp = sbln("imp", [128, 4, 32], F32, 2)
        top8 = sbln("top8", [128, 4, 8], F32, 2)
        selpad, selpadB = sbl("selpad", [128, 4, 96], BF16)
        pTs = sbln("pT", [128, 512], BF16, 4)
        rcs = sbln("rc", [128, 16], F32, 4)
        amix = sbln("amix", [128, 512], BF16, 2)
        tmpb = sbln("tmpb", [128, 512], BF16, 2)

        for g in range(2):
            K.dma("pool", ksT[64:96, g, :], c_ebig[:, :], [], [ebB], ebB, max_dma_last_dim=4096)
        for (w1, w1B, src) in ((w1k, w1kB, e_k_w1), (w1v, w1vB, e_v_w1)):
            for half in range(2):
                K.dma("pool", w1[half * 64:(half + 1) * 64, :, :], src[li].rearrange("(l d) e -> d l e", d=64), [], [w1B], w1B)
        K.dma("pool", w2k[:, :], e_k_w2[li], [], [w2kB], w2kB)
        K.dma("pool", w2v[:, :], e_v_w2[li], [], [w2vB], w2vB)
        K.dma("pool", pek[:, :], e_k_pe[li], [], [pekB], pekB)
        K.dma("pool", pev[:, :], e_v_pe[li], [], [pevB], pevB)
        K.dma("sp", ctab[:, :, :], c_ctab[:, :, :], [], [ctabB], ctabB)
        wsst_t, wsstB = xa.next()
        wsst = wsst_t[:, :].rearrange("p (g j) -> p g j", g=8)
        K.dma("sp", wsst, e_sgu_w[li].rearrange("g i j -> i g j"), [], [wsstB], wsstB)
        bst, bstB = xa.next()
        K.dma("sp", bst[0:8, 0:128], e_sgu_b[li], [], [bstB], bstB)
        pt, pB = psA.next()
        K.op("pe", lambda e: e.transpose(pt[:, 0:8], bst[0:8, 0:128], identf[0:8, 0:8]), [bstB, identfB], [pB])
        K.op("dve", lambda e: e.tensor_copy(out=bs[:, :], in_=pt[:, 0:8]), [pB], [bsB])
        K.dma("sp", lng[:, :], e_ln_g[li:li + 1, :].to_broadcast([128, 512]), [], [lngB], lngB)
        K.dma("sp", lnb[:, :], e_ln_b[li:li + 1, :].to_broadcast([128, 512]), [], [lnbB], lnbB)
        K.op("pool", lambda e: e.memset(vsA[:, :, :, 64:65], 1.0), [], vsAB)
        K.op("pool", lambda e: e.memset(vwA[:, :, :, 64:65], 1.0), [], vwAB)
        K.op("pool", lambda e: e.memset(vcmp[:, :, :], 0.0), [], [vcmpB])
        K.op("pool", lambda e: e.memset(vcmp[:, :, 64:65], 1.0), [vcmpB], [vcmpB])
        for g in range(2):
            K.dma("pool", vcmp[0:127, g, 65:97], c_ovl[:, :], [], [vcmpB], vcmpB)
        K.op("pool", lambda e: e.memset(hidv[:, :, :], 0.0), [], [hidvB])
        K.op("pool", lambda e: e.memset(kcmpT[:, :, :], 0.0), [], [kcmpTB])
        K.op("pool", lambda e: e.memset(selpad[:, :, :], 0.0), [], [selpadB])
        K.op("dve", lambda e: e.memset(kcT[:, 0:16], 0.0), [], [kcTB])
        K.op("dve", lambda e: e.memset(vcT[:, 0:16], 0.0), [], [vcTB])
        for g in range(8):
            pt, pB = psA.next()
            K.op("pe", lambda e, g=g, pt=pt: e.transpose(pt[:, 0:128], wsst[:, g, :], identf[:, :]), [wsstB, identfB], [pB])
            K.op("dve", lambda e, g=g, pt=pt: e.tensor_tensor(out=WsT[:, g, :], in0=pt[:, 0:128], in1=trilk[:, :], op=ALU.mult),
                 [pB, trilkB], [WsTB])
        for xi, (pe_, peB_, w1, w1B) in enumerate(((pek, pekB, w1k, w1kB), (pev, pevB, w1v, w1vB))):
            pt, pB = psA.next()
            pb = pt[:].bitcast(BF16)
            K.op("pe", lambda e, pb=pb, pe_=pe_: e.transpose(pb[0:64, 0:32], pe_[:, :], identb[0:32, 0:32]), [peB_, identbB], [pB])
            K.op("dve", lambda e, pb=pb, xi=xi: e.tensor_copy(out=peT[:, xi, :], in_=pb[0:64, 0:32]), [pB], [peTB])
            pt2, pB2 = psA.next()
            for l in range(32):
                K.op("pe", lambda e, l=l, pt2=pt2, w1=w1, xi=xi: e.matmul(pt2[0:64, 0:1], w1[0:64, l, :], peT[:, xi, l:l + 1],
                                                                          start=(l == 0), stop=(l == 31)),
                     [w1B, peTB], [pB2], signal=(l == 31))
            K.op("dve", lambda e, pt2=pt2, xi=xi: e.tensor_copy(out=pebias[:, xi:xi + 1], in_=pt2[0:64, 0:1]), [pB2], [pebiasB])

        def evac_copy(i, out_ap, in_ap, reads, writes, scale=None):
            if i % 2 == 0:
                if scale is None:
                    K.op("act", lambda e: e.copy(out=out_ap, in_=in_ap), reads, writes)
                else:
                    K.op("act", lambda e: e.mul(out=out_ap, in_=in_ap, mul=scale), reads, writes)
            else:
                if scale is None:
                    K.op("dve", lambda e: e.tensor_copy(out=out_ap, in_=in_ap), reads, writes)
                else:
                    K.op("dve", lambda e: e.tensor_scalar(out=out_ap, in0=in_ap, scalar1=scale, scalar2=None, op0=ALU.mult),
                         reads, writes)

        if STOP == "params":
            st.close()
            return
        for s in range(NSEQ):
            for b in range(NBLK):
                t0 = b * 512
                phase_A(layer, s, b)
                if STOP == "A":
                    st.close()
                    return
                for h in range(8):
                    pt, pB = fm_proj(C_Q + h * 64, 64)
                    evac_copy(h, qT[0:64, h, :], pt[0:64, 0:512], [pB], [qTB[h]], scale=0.125)
                for g in range(2):
                    pt, pB = fm_proj(C_KS + g * 64, 64)
                    evac_copy(g, ksT[0:64, g, t0:t0 + 512], pt[0:64, 0:512], [pB], [ksTB[g][b]])
                for g in range(2):
                    pt, pB = fm_proj(C_KW + g * 64, 64)
                    r = b % 2
                    evac_copy(g + 1, kwT[0:64, g, r * 512:(r + 1) * 512], pt[0:64, 0:512], [pB], [kwTB[g][r]])
                for i, (cX, XT, XTB) in enumerate(((C_KC, kcT, kcTB), (C_VC, vcT, vcTB))):
                    if b > 0:
                        K.op("pool", lambda e, XT=XT: e.tensor_copy(out=XT[:, 0:16], in_=XT[:, 512:528]), [XTB], [XTB])
                    pt, pB = fm_proj(cX, 128)
                    evac_copy(i, XT[:, 16:528], pt[:, 0:512], [pB], [XTB])
                if STOP == "B":
                    st.close()
                    return
                for tt in range(4):
                    kt = b * 4 + tt
                    pt, pB = tm_proj(tt, C_VS, 408)
                    K.op("dve", lambda e, pt=pt, kt=kt: e.tensor_copy(out=vsA[:, kt, :, 0:64],
                                                                      in_=pt[:, 0:128].rearrange("p (g d) -> p g d", g=2)),
                         [pB], [vsAB[kt]])
                    K.op("dve", lambda e, pt=pt, kt=kt: e.tensor_copy(out=vwA[:, kt % 8, :, 0:64],
                                                                      in_=pt[:, 256:384].rearrange("p (g d) -> p g d", g=2)),
                         [pB], [vwAB[kt % 8]])
                    K.op("act", lambda e, pt=pt, tt=tt: e.activation(out=gates[:, tt, :], in_=pt[:, 384:408], func=AF.Sigmoid),
                         [pB], [gatesB[tt]])
                    pt, pB = tm_proj(tt, C_ZA, 512)
                    K.op("act", lambda e, pt=pt, tt=tt: e.activation(out=sza[:, tt, :], in_=pt[:, 0:512], func=AF.Silu),
                         [pB], [szaB[tt]])
                    pt, pB = tm_proj(tt, C_ZB, 512)
                    zt, ztB = szb.next()
                    K.op("act", lambda e, pt=pt, zt=zt: e.activation(out=zt[:, :], in_=pt[:, 0:512], func=AF.Silu), [pB], [ztB])
                    pt, pB = tm_proj(tt, C_U, 512)
                    ut, utB = ug.next()
                    K.op("act", lambda e, pt=pt, ut=ut: e.activation(out=ut[:, :], in_=pt[:, 0:512], func=AF.Gelu_apprx_tanh),
                         [pB], [utB])
                    pt, pB = tm_proj(tt, C_V, 512)
                    vt, vtB = vg.next()
                    K.op("act", lambda e, pt=pt, vt=vt: e.activation(out=vt[:, :], in_=pt[:, 0:512], func=AF.Gelu_apprx_tanh),
                         [pB], [vtB])
                    K.op("pool", lambda e, ut=ut, zt=zt: e.tensor_tensor(out=ut[:, :], in0=ut[:, :], in1=zt[:, :], op=ALU.mult),
                         [utB, ztB], [utB])
                    ls, lsB = lnst.next()
                    K.op("dve", lambda e, ls=ls, vt=vt: e.bn_stats(out=ls[:, 0:6], in_=vt[:, :]), [vtB], [lsB])
                    K.op("dve", lambda e, ls=ls: e.bn_aggr(out=ls[:, 6:8], in_=ls[:, 0:6]), [lsB], [lsB])
                    K.op("dve", lambda e, ls=ls: e.tensor_scalar(out=ls[:, 8:9], in0=ls[:, 7:8], scalar1=LN_EPS, scalar2=None,
                                                                op0=ALU.add), [lsB], [lsB])
                    K.op("pool", lambda e, ls=ls: e.tensor_tensor(out=ls[:, 9:10], in0=ls[:, 8:9], in1=neghalf[:, 0:1], op=ALU.pow),
                         [lsB, neghalfB], [lsB])
                    K.op("dve", lambda e, ls=ls, vt=vt: e.tensor_scalar(out=vt[:, :], in0=vt[:, :], scalar1=ls[:, 6:7],
                                                                        scalar2=ls[:, 9:10], op0=ALU.subtract, op1=ALU.mult),
                         [lsB, vtB], [vtB])
                    K.op("pool", lambda e, vt=vt: e.tensor_tensor(out=vt[:, :], in0=vt[:, :], in1=lng[:, :], op=ALU.mult),
                         [vtB, lngB], [vtB])
                    vl, vlB = vln.next()
                    K.op("pool", lambda e, vt=vt, vl=vl: e.tensor_tensor(out=vl[:, :], in0=vt[:, :], in1=lnb[:, :], op=ALU.add),
                         [vtB, lnbB], [vlB])
                    pt, pB = psB.next()
                    for g in range(8):
                        K.op("pe", lambda e, g=g, pt=pt, vl=vl: e.matmul(pt[:, g * 64:(g + 1) * 64], WsT[:, g, :],
                                                                          vl[:, g * 64:(g + 1) * 64], start=True, stop=True),
                             [WsTB, vlB], [pB], signal=(g == 7))
                    tb, tbB = tmpb.next()
                    K.op("dve", lambda e, pt=pt, tb=tb: e.tensor_tensor(
                        out=tb[:, :].rearrange("p (g d) -> p g d", g=8), in0=pt[:, 0:512].rearrange("p (g d) -> p g d", g=8),
                        in1=bs[:, :].unsqueeze(2).to_broadcast([128, 8, 64]), op=ALU.add), [pB, bsB], [tbB])
                    K.op("pool", lambda e, tb=tb, ut=ut: e.tensor_tensor(out=tb[:, :], in0=tb[:, :], in1=ut[:, :], op=ALU.mult),
                         [tbB, utB], [tbB])
                    pt, pB = psA.next()
                    pb = pt[:].bitcast(BF16)
                    for fc in range(4):
                        K.op("pe", lambda e, fc=fc, pb=pb, tb=tb: e.transpose(pb[:, fc * 128:(fc + 1) * 128],
                                                                              tb[:, fc * 128:(fc + 1) * 128], identb[:, :]),
                             [tbB, identbB], [pB], signal=(fc == 3))
                    K.op("dve", lambda e, pb=pb, tt=tt: e.tensor_copy(out=mixT[:, 4:8, tt * 128:(tt + 1) * 128],
                                                                      in_=pb[:, 0:512].rearrange("p (k t) -> p k t", k=4)),
                         [pB], [mixTB[tt]])
                if STOP == "C":
                    st.close()
                    return
                if b == 0:
                    n0, nn, c0 = 0, 31, 16
                else:
                    n0, nn, c0 = 32 * b - 1, 32, 0
                nk = 32 * (b + 1) - 1
                for g in range(2):
                    for xi, (XT, XTB, w1, w1B, hid, hidB, hcol) in enumerate((
                            (kcT, kcTB, w1k, w1kB, hidk, hidkB, 0), (vcT, vcTB, w1v, w1vB, hidv, hidvB, n0))):
                        pt, pB = psA.next()
                        for l in range(32):
                            K.op("pe", lambda e, l=l, pt=pt, XT=XT, w1=w1: e.matmul(
                                pt[0:64, 0:nn], w1[g * 64:(g + 1) * 64, l, :],
                                XT[g * 64:(g + 1) * 64, c0 + l:c0 + l + 16 * (nn - 1) + 1:16], start=(l == 0), stop=(l == 31)),
                                 [w1B, XTB], [pB], signal=(l == 31))
                        K.op("act", lambda e, pt=pt, hid=hid, hcol=hcol, xi=xi: e.activation(
                            out=hid[:, g, hcol:hcol + nn], in_=pt[0:64, 0:nn], func=AF.Silu, bias=pebias[:, xi:xi + 1]),
                             [pB, pebiasB], [hidB])
                    pt, pB = psA.next()
                    K.op("pe", lambda e, pt=pt: e.matmul(pt[0:64, 0:nn], w2k[:, :], hidk[:, g, 0:nn], start=True, stop=True),
                         [w2kB, hidkB], [pB])
                    K.op("dve", lambda e, pt=pt: e.tensor_copy(out=kcmpT[:, g, n0:n0 + nn], in_=pt[0:64, 0:nn]), [pB], [kcmpTB])
                    pt, pB = psA.next()
                    K.op("pe", lambda e, pt=pt: e.matmul(pt[0:nk, 0:64], hidv[:, g, 0:nk], w2v[:, :], start=True, stop=True),
                         [w2vB, hidvB], [pB])
                    K.op("dve", lambda e, pt=pt: e.tensor_copy(out=vcmp[0:nk, g, 0:64], in_=pt[0:nk, 0:64]), [pB], [vcmpB])
                if STOP == "D":
                    st.close()
                    return
                for g in range(2):
                    im, imB = imp.next()
                    for r in range(4):
                        h = g * 4 + r
                        pt, pB = psA.next()
                        K.op("pe", lambda e, pt=pt, h=h: e.matmul(pt[0:nk, 0:512], kcmpT[:, g, 0:nk], qT[0:64, h, :],
                                                                  start=True, stop=True), [kcmpTB, qTB[h]], [pB])
                        pT, pTB = pTs.next()
                        K.op("act", lambda e, pt=pt, pT=pT: e.activation(out=pT[0:nk, :], in_=pt[0:nk, 0:512], func=AF.Exp),
                             [pB], [pTB])
                        if STOP == "E1":
                            st.close()
                            return
                        K.op("pool", lambda e, pT=pT: e.affine_select(out=pT[0:nk, :], in_=pT[0:nk, :], pattern=[[1, 512]],
                                                                      compare_op=ALU.is_ge, fill=0.0, base=t0 - 31,
                                                                      channel_multiplier=-16), [pTB], [pTB])
                        if STOP == "E2":
                            st.close()
                            return
                        po, poB = psB.next()
                        po3 = po[:, 0:388].rearrange("p (t c) -> p t c", t=4)
                        for tt in range(4):
                            K.op("pe", lambda e, tt=tt, po=po, pT=pT: e.matmul(po[:, tt * 97:(tt + 1) * 97],
                                                                                pT[0:nk, tt * 128:(tt + 1) * 128], vcmp[0:nk, g, :],
                                                                                start=True, stop=True),
                                 [pTB, vcmpB], [poB], signal=(tt == 3))
                        if STOP == "E3":
                            st.close()
                            return
                        rc, rcB = rcs.next()
                        K.op("dve", lambda e, rc=rc, po3=po3: e.tensor_scalar(out=rc[:, 0:4], in0=po3[:, :, 64], scalar1=1e-30,
                                                                              scalar2=None, op0=ALU.max), [poB], [rcB])
                        K.op("dve", lambda e, rc=rc: e.reciprocal(out=rc[:, 4:8], in_=rc[:, 0:4]), [rcB], [rcB])
                        K.op("dve", lambda e, rc=rc, h=h: e.tensor_tensor(out=rc[:, 8:12], in0=rc[:, 4:8], in1=gates[:, :, h],
                                                                          op=ALU.mult), [rcB] + gatesB, [rcB])
                        for tt in range(4):
                            K.op("dve", lambda e, tt=tt, po3=po3, rc=rc, h=h: e.tensor_scalar(
                                out=acc_o[:, tt, h * 64:(h + 1) * 64], in0=po3[:, tt, 0:64], scalar1=rc[:, 8 + tt:9 + tt],
                                scalar2=None, op0=ALU.mult), [poB, rcB], [acc_oB[h]])
                        for tt in range(4):
                            if r == 0:
                                K.op("dve", lambda e, tt=tt, po3=po3, rc=rc, im=im: e.tensor_scalar(
                                    out=im[:, tt, :], in0=po3[:, tt, 65:97], scalar1=rc[:, 4 + tt:5 + tt], scalar2=None,
                                    op0=ALU.mult), [poB, rcB], [imB])
                            else:
                                K.op("dve", lambda e, tt=tt, po3=po3, rc=rc, im=im: e.scalar_tensor_tensor(
                                    out=im[:, tt, :], in0=po3[:, tt, 65:97], scalar=rc[:, 4 + tt:5 + tt], in1=im[:, tt, :],
                                    op0=ALU.mult, op1=ALU.add), [poB, rcB, imB], [imB])
                    if STOP == "E4":
                        st.close()
                        return
                    K.op("dve", lambda e, im=im: e.tensor_tensor(out=im[:, :, :], in0=im[:, :, :], in1=ctab[:, 4 * b:4 * b + 4, :],
                                                                 op=ALU.add), [imB, ctabB], [imB])
                    t8, t8B = top8.next()
                    for tt in range(4):
                        K.op("dve", lambda e, tt=tt, t8=t8, im=im: e.max(out=t8[:, tt, :], in_=im[:, tt, :]), [imB], [t8B])
                    for tt in range(4):
                        K.op("dve", lambda e, tt=tt, t8=t8, im=im: e.tensor_scalar(
                            out=selpad[:, tt, 64:96], in0=im[:, tt, :], scalar1=t8[:, tt, 7:8], scalar2=-1.0,
                            op0=ALU.is_ge, op1=ALU.add), [imB, t8B], [selpadB])
                    if STOP == "E6":
                        st.close()
                        return
                    pt, pB = psA.next()
                    pb = pt[:].bitcast(BF16)
                    for tt in range(4):
                        K.op("pe", lambda e, tt=tt, pb=pb: e.transpose(pb[0:96, tt * 128:(tt + 1) * 128], selpad[:, tt, :],
                                                                       identb[:, :]), [selpadB, identbB], [pB], signal=(tt == 3))
                    for r in range(4):
                        h = g * 4 + r
                        K.op("dve", lambda e, h=h, pb=pb: e.tensor_copy(out=qT[64:96, h, :], in_=pb[64:96, 0:512]), [pB], [qSB[h]])
                if STOP == "E":
                    st.close()
                    return
                for h in range(8):
                    g = h // 4
                    for br in range(2):
                        acc, accB = psB.next()
                        acc3 = acc[:, 0:260].rearrange("p (t c) -> p t c", t=4)
                        kts = list(range(0, 4 * b + 4)) if br == 0 else list(range(max(0, 4 * b - 4), 4 * b + 4))
                        first = True
                        for kt in kts:
                            d = kt - 4 * b
                            lo = max(0, d)
                            hi = 3 if br == 0 else min(3, d + 4)
                            c0_, c1_ = lo * 128, (hi + 1) * 128
                            pt, pB = psA.next()
                            if br == 0:
                                K.op("pe", lambda e, pt=pt, kt=kt, c0_=c0_, c1_=c1_: e.matmul(
                                    pt[:, c0_:c1_], ksT[0:96, g, kt * 128:(kt + 1) * 128], qT[0:96, h, c0_:c1_],
                                    start=True, stop=True), [ksTB[g][kt // 4], ebB, qTB[h], qSB[h]], [pB])
                            else:
                                kr = kt % 8
                                K.op("pe", lambda e, pt=pt, kr=kr, c0_=c0_, c1_=c1_: e.matmul(
                                    pt[:, c0_:c1_], kwT[0:64, g, kr * 128:(kr + 1) * 128], qT[0:64, h, c0_:c1_],
                                    start=True, stop=True), [kwTB[g][kr // 4], qTB[h]], [pB])
                            pT, pTB = pTs.next()
                            K.op("act", lambda e, pt=pt, pT=pT, c0_=c0_, c1_=c1_: e.activation(
                                out=pT[:, c0_:c1_], in_=pt[:, c0_:c1_], func=AF.Exp), [pB], [pTB])
                            if d >= 0:
                                K.op("pool", lambda e, pT=pT, d=d: e.tensor_tensor(
                                    out=pT[:, d * 128:(d + 1) * 128], in0=pT[:, d * 128:(d + 1) * 128], in1=trilk[:, :],
                                    op=ALU.mult), [pTB, trilkB], [pTB])
                            if br == 1 and 0 <= d + 4 <= 3:
                                K.op("pool", lambda e, pT=pT, d=d: e.tensor_tensor(
                                    out=pT[:, (d + 4) * 128:(d + 5) * 128], in0=pT[:, (d + 4) * 128:(d + 5) * 128],
                                    in1=fark[:, :], op=ALU.mult), [pTB, farkB], [pTB])
                            for tt in range(lo, hi + 1):
                                last = (kt == kts[-1]) and (tt == hi)
                                if br == 0:
                                    vB_, rhs = vsAB[kt], vsA[:, kt, g, :]
                                else:
                                    vB_, rhs = vwAB[kt % 8], vwA[:, kt % 8, g, :]
                                K.op("pe", lambda e, tt=tt, acc=acc, pT=pT, rhs=rhs, first=first: e.matmul(
                                    acc[:, tt * 65:(tt + 1) * 65], pT[:, tt * 128:(tt + 1) * 128], rhs,
                                    start=first, stop=True, skip_group_check=True), [pTB, vB_], [accB], signal=(last or tt == hi))
                                first = False
                        rc, rcB = rcs.next()
                        K.op("dve", lambda e, rc=rc, acc3=acc3: e.tensor_scalar(out=rc[:, 0:4], in0=acc3[:, :, 64], scalar1=1e-30,
                                                                                scalar2=None, op0=ALU.max), [accB], [rcB])
                        K.op("dve", lambda e, rc=rc: e.reciprocal(out=rc[:, 4:8], in_=rc[:, 0:4]), [rcB], [rcB])
                        gi = (1 + br) * 8 + h
                        K.op("dve", lambda e, rc=rc, gi=gi: e.tensor_tensor(out=rc[:, 8:12], in0=rc[:, 4:8], in1=gates[:, :, gi],
                                                                            op=ALU.mult), [rcB] + gatesB, [rcB])
                        for tt in range(4):
                            K.op("dve", lambda e, tt=tt, acc3=acc3, rc=rc, h=h: e.scalar_tensor_tensor(
                                out=acc_o[:, tt, h * 64:(h + 1) * 64], in0=acc3[:, tt, 0:64], scalar=rc[:, 8 + tt:9 + tt],
                                in1=acc_o[:, tt, h * 64:(h + 1) * 64], op0=ALU.mult, op1=ALU.add),
                                 [accB, rcB, acc_oB[h]], [acc_oB[h]])
                for tt in range(4):
                    am, amB = amix.next()
                    K.op("pool", lambda e, tt=tt, am=am: e.tensor_tensor(out=am[:, :], in0=acc_o[:, tt, :], in1=sza[:, tt, :],
                                                                         op=ALU.mult), acc_oB + [szaB[tt]], [amB])
                    pt, pB = psA.next()
                    pb = pt[:].bitcast(BF16)
                    for fc in range(4):
                        K.op("pe", lambda e, fc=fc, pb=pb, am=am: e.transpose(pb[:, fc * 128:(fc + 1) * 128],
                                                                              am[:, fc * 128:(fc + 1) * 128], identb[:, :]),
                             [amB, identbB], [pB], signal=(fc == 3))
                    K.op("dve", lambda e, pb=pb, tt=tt: e.tensor_copy(out=mixT[:, 0:4, tt * 128:(tt + 1) * 128],
                                                                      in_=pb[:, 0:512].rearrange("p (k t) -> p k t", k=4)),
                         [pB], [mixTB[tt]])
                if STOP == "F":
                    st.close()
                    return
                phase_G(layer, s, b)
                if STOP == "G":
                    st.close()
                    return
        st.close()

    def odd_layer(layer, li):
        st = ExitStack()

        def sbl(name, shape, dt=F32):
            t = st.enter_context(nc.sbuf_tensor(f"{name}_{layer}", list(shape), dt))
            return t, Buf(name)

        def sbln(name, shape, dt, n):
            return Rot([sbl(f"{name}{i}", shape, dt) for i in range(n)])

        yc = st.enter_context(nc.sbuf_tensor(f"yc_{layer}", [128, 8, 544], BF16))
        ycB = [Buf(f"yc{c}") for c in range(8)]
        szz = st.enter_context(nc.sbuf_tensor(f"szz_{layer}", [128, 8, 512], BF16))
        szzB = [Buf(f"szz{c}") for c in range(8)]
        yv = st.enter_context(nc.sbuf_tensor(f"yv_{layer}", [128, 8, 512], F32))
        yvB = [Buf(f"yv{c}") for c in range(8)]
        diag = sbln("diag", [128, 31, 128], BF16, 2)
        sig = sbln("sig", [128, 512], F32, 2)
        ybf = sbln("ybf", [128, 512], BF16, 2)
        ysq = sbln("ysq", [128, 512], BF16, 2)
        onesb, onesbB = sbl("onesb", [128, 128], BF16)
        mean, meanB = sbl("mean", [128, 512], F32)
        rstdt, rstdtB = sbl("rstdt", [128, 512], F32)
        var, varB = sbl("var", [128, 512], F32)
        nhalf, nhalfB = sbl("nhalf", [128, 512], F32)
        t1 = sbln("t1", [128, 512], F32, 2)

        K.op("pool", lambda e: e.memset(onesb[:, :], 1.0 / D), [], [onesbB])
        K.op("pool", lambda e: e.memset(nhalf[:, :], -0.5), [], [nhalfB])

        for s in range(NSEQ):
            for c in range(8):
                K.op("pool", lambda e, c=c: e.memset(yc[:, c, 0:32], 0.0), [], [ycB[c]])
            for b in range(NBLK):
                phase_A(layer, s, b)
                for c in range(8):
                    if b > 0:
                        K.op("pool", lambda e, c=c: e.tensor_copy(out=yc[:, c, 0:32], in_=yc[:, c, 512:544]), [ycB[c]], [ycB[c]])
                    pg, pgB = fm_proj(1024 + c * 128, 128)
                    sg, sgB = sig.next()
                    K.op("act", lambda e, pg=pg, sg=sg: e.activation(out=sg[:, :], in_=pg[:, 0:512], func=AF.Sigmoid), [pgB], [sgB])
                    pa, paB = fm_proj(c * 128, 128)
                    K.op("dve", lambda e, pa=pa, sg=sg, c=c: e.tensor_tensor(out=yc[:, c, 32:544], in0=pa[:, 0:512], in1=sg[:, :],
                                                                             op=ALU.mult), [paB, sgB], [ycB[c]])
                    pz, pzB = fm_proj(2048 + c * 128, 128)
                    K.op("act", lambda e, pz=pz, c=c: e.activation(out=szz[:, c, :], in_=pz[:, 0:512], func=AF.Silu), [pzB], [szzB[c]])
                pm, pmB = psB.next()
                pq, pqB = psB.next()
                for c in range(8):
                    dg, dgB = diag.next()
                    K.op("pool", lambda e, dg=dg, c=c: e.tensor_tensor(
                        out=dg[:, :, :], in0=identb[:, :].unsqueeze(1).to_broadcast([128, 31, 128]),
                        in1=par[:, c, 1:32].unsqueeze(2).to_broadcast([128, 31, 128]), op=ALU.mult), [identbB, parB], [dgB])
                    pc, pcB = psA.next()
                    for k in range(31):
                        K.op("pe", lambda e, k=k, pc=pc, dg=dg, c=c: e.matmul(pc[:, 0:512], dg[:, k, :], yc[:, c, 2 + k:2 + k + 512],
                                                                              start=(k == 0), stop=(k == 30)),
                             [dgB, ycB[c]], [pcB], signal=(k == 30))
                    K.op("act", lambda e, pc=pc, c=c: e.activation(out=yv[:, c, :], in_=pc[:, 0:512], func=AF.Identity,
                                                                   bias=par[:, c, 32:33]), [pcB, parB], [yvB[c]])
                    yb, ybB = ybf.next()
                    yq, yqB = ysq.next()
                    K.op("dve", lambda e, yb=yb, c=c: e.tensor_copy(out=yb[:, :], in_=yv[:, c, :]), [yvB[c]], [ybB])
                    K.op("pool", lambda e, yq=yq, c=c: e.tensor_tensor(out=yq[:, :], in0=yv[:, c, :], in1=yv[:, c, :], op=ALU.mult),
                         [yvB[c]], [yqB])
                    K.op("pe", lambda e, yb=yb, c=c, pm=pm: e.matmul(pm[:, 0:512], onesb[:, :], yb[:, :], start=(c == 0), stop=(c == 7)),
                         [onesbB, ybB], [pmB], signal=(c == 7))
                    K.op("pe", lambda e, yq=yq, c=c, pq=pq: e.matmul(pq[:, 0:512], onesb[:, :], yq[:, :], start=(c == 0), stop=(c == 7)),
                         [onesbB, yqB], [pqB], signal=(c == 7))
                K.op("act", lambda e, pm=pm: e.copy(out=mean[:, :], in_=pm[:, 0:512]), [pmB], [meanB])
                K.op("dve", lambda e: e.tensor_tensor(out=var[:, :], in0=mean[:, :], in1=mean[:, :], op=ALU.mult), [meanB], [varB])
                K.op("dve", lambda e, pq=pq: e.scalar_tensor_tensor(out=var[:, :], in0=pq[:, 0:512], scalar=LN_EPS, in1=var[:, :],
                                                                    op0=ALU.add, op1=ALU.subtract), [pqB, varB], [varB])
                K.op("pool", lambda e: e.tensor_tensor(out=rstdt[:, :], in0=var[:, :], in1=nhalf[:, :], op=ALU.pow),
                     [varB, nhalfB], [rstdtB])
                for c in range(8):
                    ta, taB = t1.next()
                    K.op("dve", lambda e, ta=ta, c=c: e.tensor_tensor(out=ta[:, :], in0=yv[:, c, :], in1=mean[:, :], op=ALU.subtract),
                         [yvB[c], meanB], [taB])
                    K.op("pool", lambda e, ta=ta: e.tensor_tensor(out=ta[:, :], in0=ta[:, :], in1=rstdt[:, :], op=ALU.mult),
                         [taB, rstdtB], [taB])
                    K.op("act", lambda e, ta=ta, c=c: e.activation(out=ta[:, :], in_=ta[:, :], func=AF.Silu,
                                                                   scale=par[:, c, 33:34], bias=par[:, c, 34:35]),
                         [taB, parB], [taB])
                    K.op("dve", lambda e, ta=ta, c=c: e.tensor_tensor(out=mixT[:, c, :], in0=ta[:, :], in1=szz[:, c, :], op=ALU.mult),
                         [taB, szzB[c]], mixTB)
                phase_G(layer, s, b)
        st.close()

    for layer in range(n_layers):
        li = layer // 2
        if layer % 2 == 0:
            load_common(layer, e_w_in[li], EVEN_W, e_w_out[li], [])
        else:
            load_common(layer, o_w_in[li], ODD_W, o_w_out[li],
                        [(o_dw_w[li], 31), (o_dw_b[li:li + 1, :], 1), (o_ln_g[li:li + 1, :], 1), (o_ln_b[li:li + 1, :], 1)])
        if layer > 0:
            K.barrier()
        if layer % 2 == 0:
            even_layer(layer, li)
        else:
            odd_layer(layer, li)

    for s in range(NSEQ):
        for t in range(16):
            b = xd[s][t]
            if b.w is not None:
                sm, v = b.w
                nc.sync.wait_ge(sm.h, v)
    stack.close()
    return nc, K


def make_consts():
    p = np.arange(128)
    ident = np.eye(128, dtype=np.float32)
    tril = (p[:, None] <= p[None, :]).astype(np.float32)
    far = (p[:, None] > p[None, :]).astype(np.float32)
    ctab = np.zeros((128, 16, 32), np.float32)
    j = np.arange(32)
    for tile in range(16):
        t = tile * 128 + p
        cur = t // 64
        forced = (j[None, :] == 0) | (j[None, :] == cur[:, None]) | (j[None, :] == cur[:, None] - 1)
        causal = j[None, :] <= cur[:, None]
        ctab[:, tile, :] = np.where(causal, forced.astype(np.float32) * np.float32(1e4), np.float32(-1e30))
    ebig = ((np.arange(2048)[None, :] // 64) == j[:, None]).astype(np.float32) * np.float32(BIG)
    n_cmp = 127
    tok = np.arange(n_cmp)[:, None] * 16 + np.arange(32)[None, :]
    ovl = ((tok[:, :, None] // 64) == np.arange(32)[None, None, :]).sum(1).astype(np.float32) / np.float32(32)
    return {"c_ident": ident, "c_tril": tril, "c_far": far, "c_ctab": ctab, "c_ebig": ebig, "c_ovl": ovl}


_CACHE = {}


def kernel(**inputs):
    if "nc" not in _CACHE:
        _CACHE["nc"] = build_program(4)[0]
    nc = _CACHE["nc"]
    consts = make_consts()
    x = np.ascontiguousarray(inputs["x"], dtype=np.float32)
    shared = {k: np.ascontiguousarray(v, dtype=np.float32) for k, v in inputs.items() if k != "x"}
    shared.update(consts)
    in_maps = []
    for c in range(NCORES):
        m = dict(shared)
        m["x"] = x[c * NSEQ:(c + 1) * NSEQ]
        in_maps.append(m)
    res = run_bass_kernel_spmd(nc, in_maps, core_ids=list(range(NCORES)))
    return np.concatenate([r["out"] for r in res.results], axis=0).astype(np.float32)
```

```python
from contextlib import ExitStack

import numpy as np
import concourse.bass as bass
import concourse.mybir as mybir
from concourse.bass_utils import run_bass_kernel_spmd

F32 = mybir.dt.float32
BF16 = mybir.dt.bfloat16
AF = mybir.ActivationFunctionType
ALU = mybir.AluOpType

NCORES = 8
SEQ = 2048
D = 1024
NSEQ = 2
NBLK = 4
EVEN_W = 3352
ODD_W = 3072
RMS_EPS = 1e-6
LN_EPS = 1e-5
BIG = 30000.0
import os as _os
STOP = _os.environ.get("KSTOP", "")

C_Q, C_KC, C_VC, C_KS, C_VS, C_KW, C_VW, C_GATE, C_ZA, C_U, C_V, C_ZB = (
    0, 512, 640, 768, 896, 1024, 1152, 1280, 1304, 1816, 2328, 2840)


class Sem:
    __slots__ = ("h", "val", "name")

    def __init__(self, h, name=""):
        self.h = h
        self.val = 0
        self.name = name


class Buf:
    __slots__ = ("name", "w", "r", "dsem")

    def __init__(self, name):
        self.name = name
        self.w = None
        self.r = {}
        self.dsem = None


class Trk:
    def __init__(self, nc, stack):
        self.nc = nc
        self.stack = stack
        self.eng = {"pe": nc.tensor, "act": nc.scalar, "dve": nc.vector, "pool": nc.gpsimd, "sp": nc.sync}
        self.nsem = 0
        self.esem = {e: self.newsem("e_" + e) for e in self.eng}
        self.waited = {e: {} for e in self.eng}
        self.nops = 0
        self.dsems = []
        self.phase = ""
        self.waitlog = {e: [] for e in self.eng}

    def newsem(self, name):
        self.nsem += 1
        return Sem(self.stack.enter_context(self.nc.semaphore(f"{name}_{self.nsem}")), name)

    def _sync(self, E, reads, writes):
        deps = {}
        for b in reads:
            if b.w is not None:
                s, v = b.w
                if deps.get(s, 0) < v:
                    deps[s] = v
        for b in writes:
            if b.w is not None:
                s, v = b.w
                if deps.get(s, 0) < v:
                    deps[s] = v
            for s, v in b.r.items():
                if deps.get(s, 0) < v:
                    deps[s] = v
        eng = self.eng[E]
        w = self.waited[E]
        own = self.esem[E]
        for s, v in deps.items():
            if E == "pe" and s is own:
                continue
            if w.get(s, 0) < v:
                eng.wait_ge(s.h, v)
                w[s] = v
                self.waitlog[E].append((self.phase, s.name))

    def op(self, E, fn, reads=(), writes=(), signal=True):
        self._sync(E, reads, writes)
        ins = fn(self.eng[E])
        s = self.esem[E]
        if signal:
            s.val += 1
            ins.then_inc(s.h, 1)
            tag = (s, s.val)
        else:
            tag = (s, s.val + 1)
        for b in reads:
            if b.r.get(s, 0) < tag[1]:
                b.r[s] = tag[1]
        for b in writes:
            b.w = tag
            b.r = {}
        self.nops += 1
        return ins

    def barrier(self):
        sems = list(self.esem.values()) + self.dsems
        for E, eng in self.eng.items():
            w = self.waited[E]
            for s in sems:
                if s.val > 0 and w.get(s, 0) < s.val:
                    eng.wait_ge(s.h, s.val)
                    w[s] = s.val

    def dma(self, E, out, in_, reads, writes, dbuf, **kw):
        self._sync(E, reads, writes)
        if dbuf.dsem is None:
            dbuf.dsem = self.newsem("d_" + dbuf.name)
            self.dsems.append(dbuf.dsem)
        s = dbuf.dsem
        ins = self.eng[E].dma_start(out=out, in_=in_, **kw)
        s.val += 16
        ins.then_inc(s.h, 16)
        tag = (s, s.val)
        for b in reads:
            if b.r.get(s, 0) < tag[1]:
                b.r[s] = tag[1]
        for b in writes:
            b.w = tag
            b.r = {}
        self.nops += 1
        return ins


class Rot:
    def __init__(self, items):
        self.items = items
        self.i = 0

    def next(self):
        it = self.items[self.i % len(self.items)]
        self.i += 1
        return it


def build_program(n_layers=4):
    nc = bass.Bass("TRN2", target_bir_lowering=False, dynamic_dma_scratch_size=16384)
    stack = ExitStack()
    K = Trk(nc, stack)

    def dram(name, shape, dt=F32, kind="ExternalInput"):
        return nc.dram_tensor(name, list(shape), dt, kind=kind).ap()

    x_in = dram("x", [NSEQ, SEQ, D])
    out = dram("out", [NSEQ, SEQ, D], kind="ExternalOutput")
    norm_pre = dram("norm_pre", [4, D])
    norm_post = dram("norm_post", [4, D])
    e_w_in = dram("even_w_in", [2, D, EVEN_W])
    e_k_pe = dram("even_cmp_k_pe", [2, 32, 64])
    e_k_w1 = dram("even_cmp_k_w1", [2, 2048, 64])
    e_k_w2 = dram("even_cmp_k_w2", [2, 64, 64])
    e_v_pe = dram("even_cmp_v_pe", [2, 32, 64])
    e_v_w1 = dram("even_cmp_v_w1", [2, 2048, 64])
    e_v_w2 = dram("even_cmp_v_w2", [2, 64, 64])
    e_ln_g = dram("even_sgu_ln_g", [2, 512])
    e_ln_b = dram("even_sgu_ln_b", [2, 512])
    e_sgu_w = dram("even_sgu_w", [2, 8, 128, 128])
    e_sgu_b = dram("even_sgu_b", [2, 8, 128])
    e_w_out = dram("even_w_out", [2, D, D])
    o_w_in = dram("odd_w_in", [2, D, ODD_W])
    o_dw_w = dram("odd_dw_w", [2, 31, D])
    o_dw_b = dram("odd_dw_b", [2, D])
    o_ln_g = dram("odd_ln_g", [2, D])
    o_ln_b = dram("odd_ln_b", [2, D])
    o_w_out = dram("odd_w_out", [2, D, D])
    c_ident = dram("c_ident", [128, 128])
    c_tril = dram("c_tril", [128, 128])
    c_far = dram("c_far", [128, 128])
    c_ctab = dram("c_ctab", [128, 16, 32])
    c_ebig = dram("c_ebig", [32, 2048])
    c_ovl = dram("c_ovl", [127, 32])

    def sb(name, shape, dt=F32):
        t = stack.enter_context(nc.sbuf_tensor(name, list(shape), dt))
        return t, Buf(name)

    def sbn(name, shape, dt, n):
        return Rot([sb(f"{name}{i}", shape, dt) for i in range(n)])

    w_in = stack.enter_context(nc.sbuf_tensor("w_in", [128, 8, EVEN_W], BF16))
    w_inB = [Buf(f"w_in{k}") for k in range(8)]
    w_out = stack.enter_context(nc.sbuf_tensor("w_out", [128, 8, D], BF16))
    w_outB = [Buf(f"w_out{k}") for k in range(8)]
    identb, identbB = sb("identb", [128, 128], BF16)
    identf, identfB = sb("identf", [128, 128], F32)
    trilk, trilkB = sb("trilk", [128, 128], BF16)
    fark, farkB = sb("fark", [128, 128], BF16)
    neghalf, neghalfB = sb("neghalf", [128, 8], F32)
    gpost, gpostB = sb("gpost", [128, D], F32)
    xa = sbn("xa", [128, D], F32, 2)
    xr = sbn("xr", [128, D], F32, 2)
    hp = sbn("hp", [128, D], BF16, 2)
    hT = stack.enter_context(nc.sbuf_tensor("hT", [128, 8, 512], BF16))
    hTB = [Buf(f"hT{t}") for t in range(4)]
    mixT = stack.enter_context(nc.sbuf_tensor("mixT", [128, 8, 512], BF16))
    mixTB = [Buf(f"mixT{t}") for t in range(4)]
    junks = sbn("junk", [128, D], BF16, 2)
    ss, _ = sb("ss", [128, 8], F32)
    ssB = [Buf(f"ss{t}") for t in range(8)]
    ms, _ = sb("ms", [128, 8], F32)
    msB = [Buf(f"ms{t}") for t in range(8)]
    rstd, _ = sb("rstd", [128, 8], F32)
    rstdB = [Buf(f"rstd{t}") for t in range(8)]
    ssy = Rot([sb(f"ssy{i}", [128, 4], F32) + (Buf(f"ssy{i}a"), Buf(f"ssy{i}b")) for i in range(2)])
    rsy = sbn("rsy", [128, 4], F32, 2)
    par, parB = sb("par", [128, 8, 36], F32)
    tmpf = sbn("tmpf", [128, 512], F32, 2)

    PS = []
    for i in range(8):
        t = stack.enter_context(nc.psum_tensor(f"ps{i}", [128, 512], F32))
        PS.append((t, Buf(f"ps{i}")))
    psA = Rot(PS[0:4])
    psB = Rot(PS[4:8])

    xd = [[Buf(f"xd{s}_{t}") for t in range(16)] for s in range(NSEQ)]

    K.dma("pool", identb[:, :], c_ident[:, :], [], [identbB], identbB)
    K.dma("sp", identf[:, :], c_ident[:, :], [], [identfB], identfB)
    K.dma("pool", trilk[:, :], c_tril[:, :], [], [trilkB], trilkB)
    K.dma("pool", fark[:, :], c_far[:, :], [], [farkB], farkB)
    K.op("dve", lambda e: e.memset(neghalf[:, :], -0.5), [], [neghalfB])

    def load_common(layer, w_in_dram, w_in_cols, w_out_dram, rows):
        stg, stgB = xa.next()
        rows = [(norm_pre[layer:layer + 1, :], 1)] + rows
        if sum(r for _, r in rows) % 2:
            rows = rows + [(norm_post[layer:layer + 1, :], 1)]
        r0 = 0
        for ap, r in rows:
            K.dma("sp", stg[r0:r0 + r, :], ap, [], [stgB], stgB)
            r0 += r
        R = r0
        for c in range(8):
            pt, pB = psA.next()
            K.op("pe", lambda e, c=c, pt=pt: e.transpose(pt[:, 0:R], stg[0:R, c * 128:(c + 1) * 128], identf[0:R, 0:R]),
                 [stgB, identfB], [pB])
            K.op("dve", lambda e, c=c, pt=pt: e.tensor_copy(out=par[:, c, 0:R], in_=pt[:, 0:R]), [pB], [parB])
        for kc in range(8):
            K.dma("pool", w_in[:, kc, 0:w_in_cols], w_in_dram[kc * 128:(kc + 1) * 128, :], [], [w_inB[kc]], w_inB[kc],
                  max_dma_last_dim=4096)
        for kc in range(8):
            K.dma("pool", w_out[:, kc, :], w_out_dram[kc * 128:(kc + 1) * 128, :], [], [w_outB[kc]], w_outB[kc],
                  max_dma_last_dim=4096)
        K.dma("sp", gpost[:, :], norm_post[layer:layer + 1, :].to_broadcast([128, D]), [], [gpostB], gpostB)

    def phase_A(layer, s, b):
        src = x_in if layer == 0 else out
        xts = []
        for tt in range(4):
            xt, xB = xa.next()
            tile = b * 4 + tt
            K.dma("sp", xt[:, :], src[s, tile * 128:(tile + 1) * 128, :], [xd[s][tile]], [xB], xB)
            jk, jkB = junks.next()
            K.op("act", lambda e, xt=xt, tt=tt, jk=jk: e.activation(out=jk[:, :], in_=xt[:, :], func=AF.Square,
                                                                    accum_out=ss[:, tt:tt + 1]), [xB], [ssB[tt], jkB])
            K.op("dve", lambda e, tt=tt: e.tensor_scalar(out=ms[:, tt:tt + 1], in0=ss[:, tt:tt + 1], scalar1=1.0 / D,
                                                        scalar2=RMS_EPS, op0=ALU.mult, op1=ALU.add), [ssB[tt]], [msB[tt]])
            K.op("pool", lambda e, tt=tt: e.tensor_tensor(out=rstd[:, tt:tt + 1], in0=ms[:, tt:tt + 1],
                                                          in1=neghalf[:, 0:1], op=ALU.pow), [msB[tt], neghalfB], [rstdB[tt]])
            ht, hB = hp.next()
            K.op("dve", lambda e, ht=ht, xt=xt, tt=tt: e.tensor_scalar(out=ht[:, :], in0=xt[:, :], scalar1=rstd[:, tt:tt + 1],
                                                                       scalar2=None, op0=ALU.mult), [xB, rstdB[tt]], [hB])
            pt, pB = psA.next()
            pb = pt[:].bitcast(BF16)
            for kc in range(8):
                K.op("pe", lambda e, kc=kc, pb=pb, ht=ht: e.transpose(pb[:, kc * 128:(kc + 1) * 128],
                                                                      ht[:, kc * 128:(kc + 1) * 128], identb[:, :]),
                     [hB, identbB], [pB], signal=(kc == 7))
            K.op("dve", lambda e, pb=pb, tt=tt: e.tensor_tensor(
                out=hT[:, :, tt * 128:(tt + 1) * 128], in0=pb.rearrange("p (k t) -> p k t", k=8),
                in1=par[:, :, 0:1].to_broadcast([128, 8, 128]), op=ALU.mult), [pB, parB], [hTB[tt]])

    def fm_proj(col0, M):
        pt, pB = psA.next()
        for kc in range(8):
            K.op("pe", lambda e, kc=kc, pt=pt: e.matmul(pt[0:M, 0:512], w_in[:, kc, col0:col0 + M], hT[:, kc, :],
                                                        start=(kc == 0), stop=(kc == 7)),
                 [w_inB[kc]] + hTB, [pB], signal=(kc == 7))
        return pt, pB

    def tm_proj(tt, col0, N):
        pt, pB = psA.next()
        for kc in range(8):
            K.op("pe", lambda e, kc=kc, pt=pt: e.matmul(pt[:, 0:N], hT[:, kc, tt * 128:(tt + 1) * 128],
                                                        w_in[:, kc, col0:col0 + N], start=(kc == 0), stop=(kc == 7)),
                 [w_inB[kc], hTB[tt]], [pB], signal=(kc == 7))
        return pt, pB

    def phase_G(layer, s, b):
        src = x_in if layer == 0 else out
        for tt in range(4):
            tile = b * 4 + tt
            xt, xB = xr.next()
            K.dma("sp", xt[:, :], src[s, tile * 128:(tile + 1) * 128, :], [xd[s][tile]], [xB], xB)
            halves = []
            sy, syB, syB0, syB1 = ssy.next()
            ry, ryB = rsy.next()
            for hf in range(2):
                pt, pB = psB.next()
                for fc in range(8):
                    K.op("pe", lambda e, fc=fc, pt=pt, hf=hf: e.matmul(pt[:, 0:512], mixT[:, fc, tt * 128:(tt + 1) * 128],
                                                                        w_out[:, fc, hf * 512:(hf + 1) * 512],
                                                                        start=(fc == 0), stop=(fc == 7)),
                         [mixTB[tt], w_outB[fc]], [pB], signal=(fc == 7))
                halves.append((pt, pB))
            for hf, sB_ in ((0, syB0), (1, syB1)):
                pt, pB = halves[hf]
                jk, jkB = junks.next()
                K.op("act", lambda e, pt=pt, hf=hf, sy=sy, jk=jk: e.activation(out=jk[:, 0:512], in_=pt[:, 0:512], func=AF.Square,
                                                                               accum_out=sy[:, hf:hf + 1]), [pB], [sB_, jkB])
            K.op("dve", lambda e, sy=sy: e.tensor_tensor(out=sy[:, 2:3], in0=sy[:, 0:1], in1=sy[:, 1:2], op=ALU.add),
                 [syB0, syB1, syB], [syB])
            K.op("dve", lambda e, sy=sy: e.tensor_scalar(out=sy[:, 3:4], in0=sy[:, 2:3], scalar1=1.0 / D, scalar2=RMS_EPS,
                                                        op0=ALU.mult, op1=ALU.add), [syB], [syB])
            K.op("pool", lambda e, sy=sy, ry=ry: e.tensor_tensor(out=ry[:, 0:1], in0=sy[:, 3:4], in1=neghalf[:, 0:1],
                                                                 op=ALU.pow), [syB, neghalfB], [ryB])
            for hf in range(2):
                pt, pB = halves[hf]
                tf, tfB = tmpf.next()
                K.op("dve", lambda e, pt=pt, tf=tf, hf=hf, ry=ry: e.scalar_tensor_tensor(
                    out=tf[:, :], in0=pt[:, 0:512], scalar=ry[:, 0:1], in1=gpost[:, hf * 512:(hf + 1) * 512],
                    op0=ALU.mult, op1=ALU.mult), [pB, ryB, gpostB], [tfB])
                K.op("pool", lambda e, tf=tf, xt=xt, hf=hf: e.tensor_tensor(
                    out=xt[:, hf * 512:(hf + 1) * 512], in0=xt[:, hf * 512:(hf + 1) * 512], in1=tf[:, :], op=ALU.add),
                     [tfB, xB], [xB])
            K.dma("sp", out[s, tile * 128:(tile + 1) * 128, :], xt[:, :], [xB], [xd[s][tile]], xd[s][tile])

    def even_layer(layer, li):
        st = ExitStack()

        def sbl(name, shape, dt=F32):
            t = st.enter_context(nc.sbuf_tensor(f"{name}_{layer}", list(shape), dt))
            return t, Buf(name)

        def sbln(name, shape, dt, n):
            return Rot([sbl(f"{name}{i}", shape, dt) for i in range(n)])

        qT = st.enter_context(nc.sbuf_tensor(f"qT_{layer}", [128, 8, 512], BF16))
        qTB = [Buf(f"qT{h}") for h in range(8)]
        qSB = [Buf(f"qS{h}") for h in range(8)]
        ksT = st.enter_context(nc.sbuf_tensor(f"ksT_{layer}", [128, 2, 2048], BF16))
        ksTB = [[Buf(f"ksT{g}_{b}") for b in range(4)] for g in range(2)]
        ebB = Buf("ebig")
        kwT = st.enter_context(nc.sbuf_tensor(f"kwT_{layer}", [64, 2, 1024], BF16))
        kwTB = [[Buf(f"kwT{g}_{r}") for r in range(2)] for g in range(2)]
        kcT, kcTB = sbl("kcT", [128, 528], BF16)
        vcT, vcTB = sbl("vcT", [128, 528], BF16)
        vsA = st.enter_context(nc.sbuf_tensor(f"vsA_{layer}", [128, 16, 2, 65], BF16))
        vsAB = [Buf(f"vsA{k}") for k in range(16)]
        vwA = st.enter_context(nc.sbuf_tensor(f"vwA_{layer}", [128, 8, 2, 65], BF16))
        vwAB = [Buf(f"vwA{k}") for k in range(8)]
        w1k, w1kB = sbl("w1k", [128, 32, 64], BF16)
        w1v, w1vB = sbl("w1v", [128, 32, 64], BF16)
        w2k, w2kB = sbl("w2k", [64, 64], BF16)
        w2v, w2vB = sbl("w2v", [64, 64], BF16)
        pek, pekB = sbl("pek", [32, 64], BF16)
        pev, pevB = sbl("pev", [32, 64], BF16)
        peT, peTB = sbl("peT", [64, 2, 32], BF16)
        pebias, pebiasB = sbl("pebias", [64, 2], F32)
        hidk, hidkB = sbl("hidk", [64, 2, 32], BF16)
        hidv, hidvB = sbl("hidv", [64, 2, 128], BF16)
        kcmpT, kcmpTB = sbl("kcmpT", [64, 2, 128], BF16)
        vcmp, vcmpB = sbl("vcmp", [128, 2, 97], BF16)
        ctab, ctabB = sbl("ctab", [128, 16, 32], F32)
        WsT, WsTB = sbl("WsT", [128, 8, 128], BF16)
        bs, bsB = sbl("bs", [128, 8], F32)
        lng, lngB = sbl("lng", [128, 512], F32)
        lnb, lnbB = sbl("lnb", [128, 512], F32)
        gates = st.enter_context(nc.sbuf_tensor(f"gates_{layer}", [128, 4, 24], F32))
        gatesB = [Buf(f"gates{t}") for t in range(4)]
        sza = st.enter_context(nc.sbuf_tensor(f"sza_{layer}", [128, 4, 512], BF16))
        szaB = [Buf(f"sza{t}") for t in range(4)]
        ug = sbln("ug", [128, 512], BF16, 2)
        szb = sbln("szb", [128, 512], BF16, 2)
        vg = sbln("vg", [128, 512], F32, 2)
        vln = sbln("vln", [128, 512], BF16, 2)
        lnst = sbln("lnst", [128, 16], F32, 2)
        acc_o = st.enter_context(nc.sbuf_tensor(f"acc_o_{layer}", [128, 4, 512], F32))
        acc_oB = [Buf(f"acc_o{h}") for h in range(8)]
        imp = sbln("imp", [128, 4, 32], F32, 2)
        top8 = sbln("top8", [128, 4, 8], F32, 2)
        selpad, selpadB = sbl("selpad", [128, 4, 96], BF16)
        pTs = sbln("pT", [128, 512], BF16, 4)
        rcs = sbln("rc", [128, 16], F32, 4)
        amix = sbln("amix", [128, 512], BF16, 2)
        tmpb = sbln("tmpb", [128, 512], BF16, 2)

        for g in range(2):
            K.dma("pool", ksT[64:96, g, :], c_ebig[:, :], [], [ebB], ebB, max_dma_last_dim=4096)
        for (w1, w1B, src) in ((w1k, w1kB, e_k_w1), (w1v, w1vB, e_v_w1)):
            for half in range(2):
                K.dma("pool", w1[half * 64:(half + 1) * 64, :, :], src[li].rearrange("(l d) e -> d l e", d=64), [], [w1B], w1B)
        K.dma("pool", w2k[:, :], e_k_w2[li], [], [w2kB], w2kB)
        K.dma("pool", w2v[:, :], e_v_w2[li], [], [w2vB], w2vB)
        K.dma("pool", pek[:, :], e_k_pe[li], [], [pekB], pekB)
        K.dma("pool", pev[:, :], e_v_pe[li], [], [pevB], pevB)
        K.dma("sp", ctab[:, :, :], c_ctab[:, :, :], [], [ctabB], ctabB)
        wsst_t, wsstB = xa.next()
        wsst = wsst_t[:, :].rearrange("p (g j) -> p g j", g=8)
        K.dma("sp", wsst, e_sgu_w[li].rearrange("g i j -> i g j"), [], [wsstB], wsstB)
        bst, bstB = xa.next()
        K.dma("sp", bst[0:8, 0:128], e_sgu_b[li], [], [bstB], bstB)
        pt, pB = psA.next()
        K.op("pe", lambda e: e.transpose(pt[:, 0:8], bst[0:8, 0:128], identf[0:8, 0:8]), [bstB, identfB], [pB])
        K.op("dve", lambda e: e.tensor_copy(out=bs[:, :], in_=pt[:, 0:8]), [pB], [bsB])
        K.dma("sp", lng[:, :], e_ln_g[li:li + 1, :].to_broadcast([128, 512]), [], [lngB], lngB)
        K.dma("sp", lnb[:, :], e_ln_b[li:li + 1, :].to_broadcast([128, 512]), [], [lnbB], lnbB)
        K.op("pool", lambda e: e.memset(vsA[:, :, :, 64:65], 1.0), [], vsAB)
        K.op("pool", lambda e: e.memset(vwA[:, :, :, 64:65], 1.0), [], vwAB)
        K.op("pool", lambda e: e.memset(vcmp[:, :, :], 0.0), [], [vcmpB])
        K.op("pool", lambda e: e.memset(vcmp[:, :, 64:65], 1.0), [vcmpB], [vcmpB])
        for g in range(2):
            K.dma("pool", vcmp[0:127, g, 65:97], c_ovl[:, :], [], [vcmpB], vcmpB)
        K.op("pool", lambda e: e.memset(hidv[:, :, :], 0.0), [], [hidvB])
        K.op("pool", lambda e: e.memset(kcmpT[:, :, :], 0.0), [], [kcmpTB])
        K.op("pool", lambda e: e.memset(selpad[:, :, :], 0.0), [], [selpadB])
        K.op("dve", lambda e: e.memset(kcT[:, 0:16], 0.0), [], [kcTB])
        K.op("dve", lambda e: e.memset(vcT[:, 0:16], 0.0), [], [vcTB])
        for g in range(8):
            pt, pB = psA.next()
            K.op("pe", lambda e, g=g, pt=pt: e.transpose(pt[:, 0:128], wsst[:, g, :], identf[:, :]), [wsstB, identfB], [pB])
            K.op("dve", lambda e, g=g, pt=pt: e.tensor_tensor(out=WsT[:, g, :], in0=pt[:, 0:128], in1=trilk[:, :], op=ALU.mult),
                 [pB, trilkB], [WsTB])
        for xi, (pe_, peB_, w1, w1B) in enumerate(((pek, pekB, w1k, w1kB), (pev, pevB, w1v, w1vB))):
            pt, pB = psA.next()
            pb = pt[:].bitcast(BF16)
            K.op("pe", lambda e, pb=pb, pe_=pe_: e.transpose(pb[0:64, 0:32], pe_[:, :], identb[0:32, 0:32]), [peB_, identbB], [pB])
            K.op("dve", lambda e, pb=pb, xi=xi: e.tensor_copy(out=peT[:, xi, :], in_=pb[0:64, 0:32]), [pB], [peTB])
            pt2, pB2 = psA.next()
            for l in range(32):
                K.op("pe", lambda e, l=l, pt2=pt2, w1=w1, xi=xi: e.matmul(pt2[0:64, 0:1], w1[0:64, l, :], peT[:, xi, l:l + 1],
                                                                          start=(l == 0), stop=(l == 31)),
                     [w1B, peTB], [pB2], signal=(l == 31))
            K.op("dve", lambda e, pt2=pt2, xi=xi: e.tensor_copy(out=pebias[:, xi:xi + 1], in_=pt2[0:64, 0:1]), [pB2], [pebiasB])

        def evac_copy(i, out_ap, in_ap, reads, writes, scale=None):
            if i % 2 == 0:
                if scale is None:
                    K.op("act", lambda e: e.copy(out=out_ap, in_=in_ap), reads, writes)
                else:
                    K.op("act", lambda e: e.mul(out=out_ap, in_=in_ap, mul=scale), reads, writes)
            else:
                if scale is None:
                    K.op("dve", lambda e: e.tensor_copy(out=out_ap, in_=in_ap), reads, writes)
                else:
                    K.op("dve", lambda e: e.tensor_scalar(out=out_ap, in0=in_ap, scalar1=scale, scalar2=None, op0=ALU.mult),
                         reads, writes)

        if STOP == "params":
            st.close()
            return
        blocks = [(s_, b_) for s_ in range(NSEQ) for b_ in range(NBLK)]
        K.phase = "evA"
        phase_A(layer, 0, 0)
        for bi, (s, b) in enumerate(blocks):
            if True:
                t0 = b * 512
                K.phase = "evB"
                if STOP == "A":
                    st.close()
                    return
                for h in range(8):
                    pt, pB = fm_proj(C_Q + h * 64, 64)
                    evac_copy(h, qT[0:64, h, :], pt[0:64, 0:512], [pB], [qTB[h]], scale=0.125)
                for g in range(2):
                    pt, pB = fm_proj(C_KS + g * 64, 64)
                    evac_copy(g, ksT[0:64, g, t0:t0 + 512], pt[0:64, 0:512], [pB], [ksTB[g][b]])
                for g in range(2):
                    pt, pB = fm_proj(C_KW + g * 64, 64)
                    r = b % 2
                    evac_copy(g + 1, kwT[0:64, g, r * 512:(r + 1) * 512], pt[0:64, 0:512], [pB], [kwTB[g][r]])
                for i, (cX, XT, XTB) in enumerate(((C_KC, kcT, kcTB), (C_VC, vcT, vcTB))):
                    if b > 0:
                        K.op("pool", lambda e, XT=XT: e.tensor_copy(out=XT[:, 0:16], in_=XT[:, 512:528]), [XTB], [XTB])
                    pt, pB = fm_proj(cX, 128)
                    evac_copy(i, XT[:, 16:528], pt[:, 0:512], [pB], [XTB])
                if STOP == "B":
                    st.close()
                    return
                K.phase = "evC"
                for tt in range(4):
                    kt = b * 4 + tt
                    pt, pB = tm_proj(tt, C_VS, 408)
                    K.op("dve", lambda e, pt=pt, kt=kt: e.tensor_copy(out=vsA[:, kt, :, 0:64],
                                                                      in_=pt[:, 0:128].rearrange("p (g d) -> p g d", g=2)),
                         [pB], [vsAB[kt]])
                    K.op("dve", lambda e, pt=pt, kt=kt: e.tensor_copy(out=vwA[:, kt % 8, :, 0:64],
                                                                      in_=pt[:, 256:384].rearrange("p (g d) -> p g d", g=2)),
                         [pB], [vwAB[kt % 8]])
                    K.op("act", lambda e, pt=pt, tt=tt: e.activation(out=gates[:, tt, :], in_=pt[:, 384:408], func=AF.Sigmoid),
                         [pB], [gatesB[tt]])
                    pt, pB = tm_proj(tt, C_ZA, 512)
                    K.op("act", lambda e, pt=pt, tt=tt: e.activation(out=sza[:, tt, :], in_=pt[:, 0:512], func=AF.Silu),
                         [pB], [szaB[tt]])
                    pt, pB = tm_proj(tt, C_ZB, 512)
                    zt, ztB = szb.next()
                    K.op("act", lambda e, pt=pt, zt=zt: e.activation(out=zt[:, :], in_=pt[:, 0:512], func=AF.Silu), [pB], [ztB])
                    pt, pB = tm_proj(tt, C_U, 512)
                    ut, utB = ug.next()
                    K.op("act", lambda e, pt=pt, ut=ut: e.activation(out=ut[:, :], in_=pt[:, 0:512], func=AF.Gelu_apprx_tanh),
                         [pB], [utB])
                    pt, pB = tm_proj(tt, C_V, 512)
                    vt, vtB = vg.next()
                    K.op("act", lambda e, pt=pt, vt=vt: e.activation(out=vt[:, :], in_=pt[:, 0:512], func=AF.Gelu_apprx_tanh),
                         [pB], [vtB])
                    K.op("pool", lambda e, ut=ut, zt=zt: e.tensor_tensor(out=ut[:, :], in0=ut[:, :], in1=zt[:, :], op=ALU.mult),
                         [utB, ztB], [utB])
                    ls, lsB = lnst.next()
                    K.op("dve", lambda e, ls=ls, vt=vt: e.bn_stats(out=ls[:, 0:6], in_=vt[:, :]), [vtB], [lsB])
                    K.op("dve", lambda e, ls=ls: e.bn_aggr(out=ls[:, 6:8], in_=ls[:, 0:6]), [lsB], [lsB])
                    K.op("dve", lambda e, ls=ls: e.tensor_scalar(out=ls[:, 8:9], in0=ls[:, 7:8], scalar1=LN_EPS, scalar2=None,
                                                                op0=ALU.add), [lsB], [lsB])
                    K.op("pool", lambda e, ls=ls: e.tensor_tensor(out=ls[:, 9:10], in0=ls[:, 8:9], in1=neghalf[:, 0:1], op=ALU.pow),
                         [lsB, neghalfB], [lsB])
                    K.op("dve", lambda e, ls=ls, vt=vt: e.tensor_scalar(out=vt[:, :], in0=vt[:, :], scalar1=ls[:, 6:7],
                                                                        scalar2=ls[:, 9:10], op0=ALU.subtract, op1=ALU.mult),
                         [lsB, vtB], [vtB])
                    K.op("pool", lambda e, vt=vt: e.tensor_tensor(out=vt[:, :], in0=vt[:, :], in1=lng[:, :], op=ALU.mult),
                         [vtB, lngB], [vtB])
                    vl, vlB = vln.next()
                    K.op("pool", lambda e, vt=vt, vl=vl: e.tensor_tensor(out=vl[:, :], in0=vt[:, :], in1=lnb[:, :], op=ALU.add),
                         [vtB, lnbB], [vlB])
                    pt, pB = psB.next()
                    for g in range(8):
                        K.op("pe", lambda e, g=g, pt=pt, vl=vl: e.matmul(pt[:, g * 64:(g + 1) * 64], WsT[:, g, :],
                                                                          vl[:, g * 64:(g + 1) * 64], start=True, stop=True),
                             [WsTB, vlB], [pB], signal=(g == 7))
                    tb, tbB = tmpb.next()
                    K.op("dve", lambda e, pt=pt, tb=tb: e.tensor_tensor(
                        out=tb[:, :].rearrange("p (g d) -> p g d", g=8), in0=pt[:, 0:512].rearrange("p (g d) -> p g d", g=8),
                        in1=bs[:, :].unsqueeze(2).to_broadcast([128, 8, 64]), op=ALU.add), [pB, bsB], [tbB])
                    K.op("pool", lambda e, tb=tb, ut=ut: e.tensor_tensor(out=tb[:, :], in0=tb[:, :], in1=ut[:, :], op=ALU.mult),
                         [tbB, utB], [tbB])
                    pt, pB = psA.next()
                    pb = pt[:].bitcast(BF16)
                    for fc in range(4):
                        K.op("pe", lambda e, fc=fc, pb=pb, tb=tb: e.transpose(pb[:, fc * 128:(fc + 1) * 128],
                                                                              tb[:, fc * 128:(fc + 1) * 128], identb[:, :]),
                             [tbB, identbB], [pB], signal=(fc == 3))
                    K.op("dve", lambda e, pb=pb, tt=tt: e.tensor_copy(out=mixT[:, 4:8, tt * 128:(tt + 1) * 128],
                                                                      in_=pb[:, 0:512].rearrange("p (k t) -> p k t", k=4)),
                         [pB], [mixTB[tt]])
                if STOP == "C":
                    st.close()
                    return
                if bi + 1 < len(blocks):
                    K.phase = "evA"
                    phase_A(layer, blocks[bi + 1][0], blocks[bi + 1][1])
                K.phase = "evD"
                if b == 0:
                    n0, nn, c0 = 0, 31, 16
                else:
                    n0, nn, c0 = 32 * b - 1, 32, 0
                nk = 32 * (b + 1) - 1
                for g in range(2):
                    for xi, (XT, XTB, w1, w1B, hid, hidB, hcol) in enumerate((
                            (kcT, kcTB, w1k, w1kB, hidk, hidkB, 0), (vcT, vcTB, w1v, w1vB, hidv, hidvB, n0))):
                        pt, pB = psA.next()
                        for l in range(32):
                            K.op("pe", lambda e, l=l, pt=pt, XT=XT, w1=w1: e.matmul(
                                pt[0:64, 0:nn], w1[g * 64:(g + 1) * 64, l, :],
                                XT[g * 64:(g + 1) * 64, c0 + l:c0 + l + 16 * (nn - 1) + 1:16], start=(l == 0), stop=(l == 31)),
                                 [w1B, XTB], [pB], signal=(l == 31))
                        K.op("act", lambda e, pt=pt, hid=hid, hcol=hcol, xi=xi: e.activation(
                            out=hid[:, g, hcol:hcol + nn], in_=pt[0:64, 0:nn], func=AF.Silu, bias=pebias[:, xi:xi + 1]),
                             [pB, pebiasB], [hidB])
                    pt, pB = psA.next()
                    K.op("pe", lambda e, pt=pt: e.matmul(pt[0:64, 0:nn], w2k[:, :], hidk[:, g, 0:nn], start=True, stop=True),
                         [w2kB, hidkB], [pB])
                    K.op("dve", lambda e, pt=pt: e.tensor_copy(out=kcmpT[:, g, n0:n0 + nn], in_=pt[0:64, 0:nn]), [pB], [kcmpTB])
                    pt, pB = psA.next()
                    K.op("pe", lambda e, pt=pt: e.matmul(pt[0:nk, 0:64], hidv[:, g, 0:nk], w2v[:, :], start=True, stop=True),
                         [w2vB, hidvB], [pB])
                    K.op("dve", lambda e, pt=pt: e.tensor_copy(out=vcmp[0:nk, g, 0:64], in_=pt[0:nk, 0:64]), [pB], [vcmpB])
                if STOP == "D":
                    st.close()
                    return
                K.phase = "evE"
                for g in range(2):
                    im, imB = imp.next()
                    for r in range(4):
                        h = g * 4 + r
                        pt, pB = psA.next()
                        K.op("pe", lambda e, pt=pt, h=h: e.matmul(pt[0:nk, 0:512], kcmpT[:, g, 0:nk], qT[0:64, h, :],
                                                                  start=True, stop=True), [kcmpTB, qTB[h]], [pB])
                        pT, pTB = pTs.next()
                        K.op("act", lambda e, pt=pt, pT=pT: e.activation(out=pT[0:nk, :], in_=pt[0:nk, 0:512], func=AF.Exp),
                             [pB], [pTB])
                        if STOP == "E1":
                            st.close()
                            return
                        K.op("pool", lambda e, pT=pT: e.affine_select(out=pT[0:nk, :], in_=pT[0:nk, :], pattern=[[1, 512]],
                                                                      compare_op=ALU.is_ge, fill=0.0, base=t0 - 31,
                                                                      channel_multiplier=-16), [pTB], [pTB])
                        if STOP == "E2":
                            st.close()
                            return
                        po, poB = psB.next()
                        po3 = po[:, 0:388].rearrange("p (t c) -> p t c", t=4)
                        for tt in range(4):
                            K.op("pe", lambda e, tt=tt, po=po, pT=pT: e.matmul(po[:, tt * 97:(tt + 1) * 97],
                                                                                pT[0:nk, tt * 128:(tt + 1) * 128], vcmp[0:nk, g, :],
                                                                                start=True, stop=True),
                                 [pTB, vcmpB], [poB], signal=(tt == 3))
                        if STOP == "E3":
                            st.close()
                            return
                        rc, rcB = rcs.next()
                        K.op("dve", lambda e, rc=rc, po3=po3: e.tensor_scalar(out=rc[:, 0:4], in0=po3[:, :, 64], scalar1=1e-30,
                                                                              scalar2=None, op0=ALU.max), [poB], [rcB])
                        K.op("dve", lambda e, rc=rc: e.reciprocal(out=rc[:, 4:8], in_=rc[:, 0:4]), [rcB], [rcB])
                        K.op("dve", lambda e, rc=rc, h=h: e.tensor_tensor(out=rc[:, 8:12], in0=rc[:, 4:8], in1=gates[:, :, h],
                                                                          op=ALU.mult), [rcB] + gatesB, [rcB])
                        for tt in range(4):
                            K.op("dve", lambda e, tt=tt, po3=po3, rc=rc, h=h: e.tensor_scalar(
                                out=acc_o[:, tt, h * 64:(h + 1) * 64], in0=po3[:, tt, 0:64], scalar1=rc[:, 8 + tt:9 + tt],
                                scalar2=None, op0=ALU.mult), [poB, rcB], [acc_oB[h]])
                        for tt in range(4):
                            if r == 0:
                                K.op("dve", lambda e, tt=tt, po3=po3, rc=rc, im=im: e.tensor_scalar(
                                    out=im[:, tt, :], in0=po3[:, tt, 65:97], scalar1=rc[:, 4 + tt:5 + tt], scalar2=None,
                                    op0=ALU.mult), [poB, rcB], [imB])
                            else:
                                K.op("dve", lambda e, tt=tt, po3=po3, rc=rc, im=im: e.scalar_tensor_tensor(
                                    out=im[:, tt, :], in0=po3[:, tt, 65:97], scalar=rc[:, 4 + tt:5 + tt], in1=im[:, tt, :],
                                    op0=ALU.mult, op1=ALU.add), [poB, rcB, imB], [imB])
                    if STOP == "E4":
                        st.close()
                        return
                    K.op("dve", lambda e, im=im: e.tensor_tensor(out=im[:, :, :], in0=im[:, :, :], in1=ctab[:, 4 * b:4 * b + 4, :],
                                                                 op=ALU.add), [imB, ctabB], [imB])
                    t8, t8B = top8.next()
                    for tt in range(4):
                        K.op("dve", lambda e, tt=tt, t8=t8, im=im: e.max(out=t8[:, tt, :], in_=im[:, tt, :]), [imB], [t8B])
                    for tt in range(4):
                        K.op("dve", lambda e, tt=tt, t8=t8, im=im: e.tensor_scalar(
                            out=selpad[:, tt, 64:96], in0=im[:, tt, :], scalar1=t8[:, tt, 7:8], scalar2=-1.0,
                            op0=ALU.is_ge, op1=ALU.add), [imB, t8B], [selpadB])
                    if STOP == "E6":
                        st.close()
                        return
                    pt, pB = psA.next()
                    pb = pt[:].bitcast(BF16)
                    for tt in range(4):
                        K.op("pe", lambda e, tt=tt, pb=pb: e.transpose(pb[0:96, tt * 128:(tt + 1) * 128], selpad[:, tt, :],
                                                                       identb[:, :]), [selpadB, identbB], [pB], signal=(tt == 3))
                    for r in range(4):
                        h = g * 4 + r
                        K.op("dve", lambda e, h=h, pb=pb: e.tensor_copy(out=qT[64:96, h, :], in_=pb[64:96, 0:512]), [pB], [qSB[h]])
                if STOP == "E":
                    st.close()
                    return
                K.phase = "evF"
                items = []
                for h in range(8):
                    for br in range(2):
                        kts = list(range(0, 4 * b + 4)) if br == 0 else list(range(max(0, 4 * b - 4), 4 * b + 4))
                        for kt in kts:
                            items.append((h, br, kt, kt == kts[0], kt == kts[-1]))
                LA = 2
                stage = {}
                accs = {}

                def front(idx):
                    h, br, kt, isfirst, islast = items[idx]
                    g = h // 4
                    d = kt - 4 * b
                    lo = max(0, d)
                    hi = 3 if br == 0 else min(3, d + 4)
                    c0_, c1_ = lo * 128, (hi + 1) * 128
                    pt, pB = psA.next()
                    if br == 0:
                        K.op("pe", lambda e: e.matmul(
                            pt[:, c0_:c1_], ksT[0:96, g, kt * 128:(kt + 1) * 128], qT[0:96, h, c0_:c1_],
                            start=True, stop=True), [ksTB[g][kt // 4], ebB, qTB[h], qSB[h]], [pB])
                    else:
                        kr = kt % 8
                        K.op("pe", lambda e: e.matmul(
                            pt[:, c0_:c1_], kwT[0:64, g, kr * 128:(kr + 1) * 128], qT[0:64, h, c0_:c1_],
                            start=True, stop=True), [kwTB[g][kr // 4], qTB[h]], [pB])
                    pT, pTB = pTs.next()
                    K.op("act", lambda e: e.activation(out=pT[:, c0_:c1_], in_=pt[:, c0_:c1_], func=AF.Exp), [pB], [pTB])
                    if d >= 0:
                        K.op("pool", lambda e: e.tensor_tensor(
                            out=pT[:, d * 128:(d + 1) * 128], in0=pT[:, d * 128:(d + 1) * 128], in1=trilk[:, :],
                            op=ALU.mult), [pTB, trilkB], [pTB])
                    if br == 1 and 0 <= d + 4 <= 3:
                        K.op("pool", lambda e: e.tensor_tensor(
                            out=pT[:, (d + 4) * 128:(d + 5) * 128], in0=pT[:, (d + 4) * 128:(d + 5) * 128],
                            in1=fark[:, :], op=ALU.mult), [pTB, farkB], [pTB])
                    stage[idx] = (pT, pTB, lo, hi)

                def back(idx):
                    h, br, kt, isfirst, islast = items[idx]
                    g = h // 4
                    pT, pTB, lo, hi = stage.pop(idx)
                    if isfirst:
                        accs[(h, br)] = psB.next()
                    acc, accB = accs[(h, br)]
                    for tt in range(lo, hi + 1):
                        if br == 0:
                            vB_, rhs = vsAB[kt], vsA[:, kt, g, :]
                        else:
                            vB_, rhs = vwAB[kt % 8], vwA[:, kt % 8, g, :]
                        K.op("pe", lambda e, tt=tt, rhs=rhs: e.matmul(
                            acc[:, tt * 65:(tt + 1) * 65], pT[:, tt * 128:(tt + 1) * 128], rhs,
                            start=(isfirst and tt == lo), stop=True, skip_group_check=True), [pTB, vB_], [accB], signal=(tt == hi))
                    if islast:
                        acc3 = acc[:, 0:260].rearrange("p (t c) -> p t c", t=4)
                        rc, rcB = rcs.next()
                        K.op("dve", lambda e: e.tensor_scalar(out=rc[:, 0:4], in0=acc3[:, :, 64], scalar1=1e-30,
                                                              scalar2=None, op0=ALU.max), [accB], [rcB])
                        K.op("dve", lambda e: e.reciprocal(out=rc[:, 4:8], in_=rc[:, 0:4]), [rcB], [rcB])
                        gi = (1 + br) * 8 + h
                        K.op("dve", lambda e: e.tensor_tensor(out=rc[:, 8:12], in0=rc[:, 4:8], in1=gates[:, :, gi],
                                                              op=ALU.mult), [rcB] + gatesB, [rcB])
                        for tt in range(4):
                            K.op("dve", lambda e, tt=tt: e.scalar_tensor_tensor(
                                out=acc_o[:, tt, h * 64:(h + 1) * 64], in0=acc3[:, tt, 0:64], scalar=rc[:, 8 + tt:9 + tt],
                                in1=acc_o[:, tt, h * 64:(h + 1) * 64], op0=ALU.mult, op1=ALU.add),
                                 [accB, rcB, acc_oB[h]], [acc_oB[h]])
                        del accs[(h, br)]

                for idx in range(len(items) + LA):
                    if idx < len(items):
                        front(idx)
                    if idx - LA >= 0:
                        back(idx - LA)
                for tt in range(4):
                    am, amB = amix.next()
                    K.op("pool", lambda e, tt=tt, am=am: e.tensor_tensor(out=am[:, :], in0=acc_o[:, tt, :], in1=sza[:, tt, :],
                                                                         op=ALU.mult), acc_oB + [szaB[tt]], [amB])
                    pt, pB = psA.next()
                    pb = pt[:].bitcast(BF16)
                    for fc in range(4):
                        K.op("pe", lambda e, fc=fc, pb=pb, am=am: e.transpose(pb[:, fc * 128:(fc + 1) * 128],
                                                                              am[:, fc * 128:(fc + 1) * 128], identb[:, :]),
                             [amB, identbB], [pB], signal=(fc == 3))
                    K.op("dve", lambda e, pb=pb, tt=tt: e.tensor_copy(out=mixT[:, 0:4, tt * 128:(tt + 1) * 128],
                                                                      in_=pb[:, 0:512].rearrange("p (k t) -> p k t", k=4)),
                         [pB], [mixTB[tt]])
                if STOP == "F":
                    st.close()
                    return
                K.phase = "evG"
                phase_G(layer, s, b)
                if STOP == "G":
                    st.close()
                    return
        st.close()

    def odd_layer(layer, li):
        st = ExitStack()

        def sbl(name, shape, dt=F32):
            t = st.enter_context(nc.sbuf_tensor(f"{name}_{layer}", list(shape), dt))
            return t, Buf(name)

        def sbln(name, shape, dt, n):
            return Rot([sbl(f"{name}{i}", shape, dt) for i in range(n)])

        yc = st.enter_context(nc.sbuf_tensor(f"yc_{layer}", [128, 8, 544], BF16))
        ycB = [Buf(f"yc{c}") for c in range(8)]
        szz = st.enter_context(nc.sbuf_tensor(f"szz_{layer}", [128, 8, 512], BF16))
        szzB = [Buf(f"szz{c}") for c in range(8)]
        yv = st.enter_context(nc.sbuf_tensor(f"yv_{layer}", [128, 8, 512], F32))
        yvB = [Buf(f"yv{c}") for c in range(8)]
        diag = sbln("diag", [128, 31, 128], BF16, 2)
        sig = sbln("sig", [128, 512], F32, 2)
        ybf = sbln("ybf", [128, 512], BF16, 2)
        ysq = sbln("ysq", [128, 512], BF16, 2)
        onesb, onesbB = sbl("onesb", [128, 128], BF16)
        mean, meanB = sbl("mean", [128, 512], F32)
        rstdt, rstdtB = sbl("rstdt", [128, 512], F32)
        var, varB = sbl("var", [128, 512], F32)
        t1 = sbln("t1", [128, 512], F32, 2)

        K.op("pool", lambda e: e.memset(onesb[:, :], 1.0 / D), [], [onesbB])

        blocks = [(s_, b_) for s_ in range(NSEQ) for b_ in range(NBLK)]
        K.phase = "odA"
        phase_A(layer, 0, 0)
        for bi, (s, b) in enumerate(blocks):
            if b == 0:
                for c in range(8):
                    K.op("pool", lambda e, c=c: e.memset(yc[:, c, 0:32], 0.0), [], [ycB[c]])
            if True:
                K.phase = "odProj"
                for c in range(8):
                    if b > 0:
                        K.op("pool", lambda e, c=c: e.tensor_copy(out=yc[:, c, 0:32], in_=yc[:, c, 512:544]), [ycB[c]], [ycB[c]])
                    pg, pgB = fm_proj(1024 + c * 128, 128)
                    sg, sgB = sig.next()
                    K.op("act", lambda e, pg=pg, sg=sg: e.activation(out=sg[:, :], in_=pg[:, 0:512], func=AF.Sigmoid), [pgB], [sgB])
                    pa, paB = fm_proj(c * 128, 128)
                    K.op("dve", lambda e, pa=pa, sg=sg, c=c: e.tensor_tensor(out=yc[:, c, 32:544], in0=pa[:, 0:512], in1=sg[:, :],
                                                                             op=ALU.mult), [paB, sgB], [ycB[c]])
                    pz, pzB = fm_proj(2048 + c * 128, 128)
                    K.op("act", lambda e, pz=pz, c=c: e.activation(out=szz[:, c, :], in_=pz[:, 0:512], func=AF.Silu), [pzB], [szzB[c]])
                if bi + 1 < len(blocks):
                    K.phase = "odA"
                    phase_A(layer, blocks[bi + 1][0], blocks[bi + 1][1])
                K.phase = "odConv"
                pm, pmB = psB.next()
                pq, pqB = psB.next()
                for c in range(8):
                    dg, dgB = diag.next()
                    K.op("pool" if c % 2 == 0 else "dve", lambda e, dg=dg, c=c: e.tensor_tensor(
                        out=dg[:, :, :], in0=identb[:, :].unsqueeze(1).to_broadcast([128, 31, 128]),
                        in1=par[:, c, 1:32].unsqueeze(2).to_broadcast([128, 31, 128]), op=ALU.mult), [identbB, parB], [dgB])
                    pc, pcB = psA.next()
                    for k in range(31):
                        K.op("pe", lambda e, k=k, pc=pc, dg=dg, c=c: e.matmul(pc[:, 0:512], dg[:, k, :], yc[:, c, 2 + k:2 + k + 512],
                                                                              start=(k == 0), stop=(k == 30)),
                             [dgB, ycB[c]], [pcB], signal=(k == 30))
                    K.op("act", lambda e, pc=pc, c=c: e.activation(out=yv[:, c, :], in_=pc[:, 0:512], func=AF.Identity,
                                                                   bias=par[:, c, 32:33]), [pcB, parB], [yvB[c]])
                    yb, ybB = ybf.next()
                    yq, yqB = ysq.next()
                    K.op("dve", lambda e, yb=yb, c=c: e.tensor_copy(out=yb[:, :], in_=yv[:, c, :]), [yvB[c]], [ybB])
                    K.op("pool", lambda e, yq=yq, c=c: e.tensor_tensor(out=yq[:, :], in0=yv[:, c, :], in1=yv[:, c, :], op=ALU.mult),
                         [yvB[c]], [yqB])
                    K.op("pe", lambda e, yb=yb, c=c, pm=pm: e.matmul(pm[:, 0:512], onesb[:, :], yb[:, :], start=(c == 0), stop=(c == 7)),
                         [onesbB, ybB], [pmB], signal=(c == 7))
                    K.op("pe", lambda e, yq=yq, c=c, pq=pq: e.matmul(pq[:, 0:512], onesb[:, :], yq[:, :], start=(c == 0), stop=(c == 7)),
                         [onesbB, yqB], [pqB], signal=(c == 7))
                K.phase = "odLN"
                K.op("act", lambda e, pm=pm: e.copy(out=mean[:, :], in_=pm[:, 0:512]), [pmB], [meanB])
                K.op("dve", lambda e: e.tensor_tensor(out=var[:, :], in0=mean[:, :], in1=mean[:, :], op=ALU.mult), [meanB], [varB])
                K.op("dve", lambda e, pq=pq: e.scalar_tensor_tensor(out=var[:, :], in0=pq[:, 0:512], scalar=LN_EPS, in1=var[:, :],
                                                                    op0=ALU.add, op1=ALU.subtract), [pqB, varB], [varB])
                K.op("act", lambda e: e.activation(out=var[:, :], in_=var[:, :], func=AF.Sqrt), [varB], [varB])
                K.op("dve", lambda e: e.reciprocal(out=rstdt[:, :], in_=var[:, :]), [varB], [rstdtB])
                for c in range(8):
                    ta, taB = t1.next()
                    K.op("dve", lambda e, ta=ta, c=c: e.tensor_tensor(out=ta[:, :], in0=yv[:, c, :], in1=mean[:, :], op=ALU.subtract),
                         [yvB[c], meanB], [taB])
                    K.op("pool", lambda e, ta=ta: e.tensor_tensor(out=ta[:, :], in0=ta[:, :], in1=rstdt[:, :], op=ALU.mult),
                         [taB, rstdtB], [taB])
                    K.op("act", lambda e, ta=ta, c=c: e.activation(out=ta[:, :], in_=ta[:, :], func=AF.Silu,
                                                                   scale=par[:, c, 33:34], bias=par[:, c, 34:35]),
                         [taB, parB], [taB])
                    K.op("dve", lambda e, ta=ta, c=c: e.tensor_tensor(out=mixT[:, c, :], in0=ta[:, :], in1=szz[:, c, :], op=ALU.mult),
                         [taB, szzB[c]], mixTB)
                K.phase = "odG"
                phase_G(layer, s, b)
        st.close()

    for layer in range(n_layers):
        li = layer // 2
        if layer % 2 == 0:
            load_common(layer, e_w_in[li], EVEN_W, e_w_out[li], [])
        else:
            load_common(layer, o_w_in[li], ODD_W, o_w_out[li],
                        [(o_dw_w[li], 31), (o_dw_b[li:li + 1, :], 1), (o_ln_g[li:li + 1, :], 1), (o_ln_b[li:li + 1, :], 1)])
        if layer > 0:
            K.barrier()
        if layer % 2 == 0:
            even_layer(layer, li)
        else:
            odd_layer(layer, li)

    for s in range(NSEQ):
        for t in range(16):
            b = xd[s][t]
            if b.w is not None:
                sm, v = b.w
                nc.sync.wait_ge(sm.h, v)
    stack.close()
    return nc, K


def make_consts():
    p = np.arange(128)
    ident = np.eye(128, dtype=np.float32)
    tril = (p[:, None] <= p[None, :]).astype(np.float32)
    far = (p[:, None] > p[None, :]).astype(np.float32)
    ctab = np.zeros((128, 16, 32), np.float32)
    j = np.arange(32)
    for tile in range(16):
        t = tile * 128 + p
        cur = t // 64
        forced = (j[None, :] == 0) | (j[None, :] == cur[:, None]) | (j[None, :] == cur[:, None] - 1)
        causal = j[None, :] <= cur[:, None]
        ctab[:, tile, :] = np.where(causal, forced.astype(np.float32) * np.float32(1e4), np.float32(-1e30))
    ebig = ((np.arange(2048)[None, :] // 64) == j[:, None]).astype(np.float32) * np.float32(BIG)
    n_cmp = 127
    tok = np.arange(n_cmp)[:, None] * 16 + np.arange(32)[None, :]
    ovl = ((tok[:, :, None] // 64) == np.arange(32)[None, None, :]).sum(1).astype(np.float32) / np.float32(32)
    return {"c_ident": ident, "c_tril": tril, "c_far": far, "c_ctab": ctab, "c_ebig": ebig, "c_ovl": ovl}


_CACHE = {}


def kernel(**inputs):
    if "nc" not in _CACHE:
        _CACHE["nc"] = build_program(4)[0]
    nc = _CACHE["nc"]
    consts = make_consts()
    x = np.ascontiguousarray(inputs["x"], dtype=np.float32)
    shared = {k: np.ascontiguousarray(v, dtype=np.float32) for k, v in inputs.items() if k != "x"}
    shared.update(consts)
    in_maps = []
    for c in range(NCORES):
        m = dict(shared)
        m["x"] = x[c * NSEQ:(c + 1) * NSEQ]
        in_maps.append(m)
    res = run_bass_kernel_spmd(nc, in_maps, core_ids=list(range(NCORES)))
    return np.concatenate([r["out"] for r in res.results], axis=0).astype(np.float32)
```

```python
from contextlib import ExitStack

import numpy as np
import concourse.bass as bass
import concourse.mybir as mybir
from concourse.bass_utils import run_bass_kernel_spmd

F32 = mybir.dt.float32
BF16 = mybir.dt.bfloat16
AF = mybir.ActivationFunctionType
ALU = mybir.AluOpType

NCORES = 8
SEQ = 2048
D = 1024
NSEQ = 2
NBLK = 4
EVEN_W = 3352
ODD_W = 3072
RMS_EPS = 1e-6
LN_EPS = 1e-5
BIG = 30000.0
import os as _os
STOP = _os.environ.get("KSTOP", "")

C_Q, C_KC, C_VC, C_KS, C_VS, C_KW, C_VW, C_GATE, C_ZA, C_U, C_V, C_ZB = (
    0, 512, 640, 768, 896, 1024, 1152, 1280, 1304, 1816, 2328, 2840)


class Sem:
    __slots__ = ("h", "val", "name")

    def __init__(self, h, name=""):
        self.h = h
        self.val = 0
        self.name = name


class Buf:
    __slots__ = ("name", "w", "r", "dsem")

    def __init__(self, name):
        self.name = name
        self.w = None
        self.r = {}
        self.dsem = None


class Trk:
    def __init__(self, nc, stack):
        self.nc = nc
        self.stack = stack
        self.eng = {"pe": nc.tensor, "act": nc.scalar, "dve": nc.vector, "pool": nc.gpsimd, "sp": nc.sync}
        self.nsem = 0
        self.esem = {e: self.newsem("e_" + e) for e in self.eng}
        self.waited = {e: {} for e in self.eng}
        self.nops = 0
        self.dsems = []
        self.phase = ""
        self.waitlog = {e: [] for e in self.eng}

    def newsem(self, name):
        self.nsem += 1
        return Sem(self.stack.enter_context(self.nc.semaphore(f"{name}_{self.nsem}")), name)

    def _sync(self, E, reads, writes):
        deps = {}
        for b in reads:
            if b.w is not None:
                s, v = b.w
                if deps.get(s, 0) < v:
                    deps[s] = v
        for b in writes:
            if b.w is not None:
                s, v = b.w
                if deps.get(s, 0) < v:
                    deps[s] = v
            for s, v in b.r.items():
                if deps.get(s, 0) < v:
                    deps[s] = v
        eng = self.eng[E]
        w = self.waited[E]
        own = self.esem[E]
        for s, v in deps.items():
            if E == "pe" and s is own:
                continue
            if w.get(s, 0) < v:
                eng.wait_ge(s.h, v)
                w[s] = v
                self.waitlog[E].append((self.phase, s.name))

    def op(self, E, fn, reads=(), writes=(), signal=True):
        self._sync(E, reads, writes)
        ins = fn(self.eng[E])
        s = self.esem[E]
        if signal:
            s.val += 1
            ins.then_inc(s.h, 1)
            tag = (s, s.val)
        else:
            tag = (s, s.val + 1)
        for b in reads:
            if b.r.get(s, 0) < tag[1]:
                b.r[s] = tag[1]
        for b in writes:
            b.w = tag
            b.r = {}
        self.nops += 1
        return ins

    def barrier(self):
        sems = list(self.esem.values()) + self.dsems
        for E, eng in self.eng.items():
            w = self.waited[E]
            for s in sems:
                if s.val > 0 and w.get(s, 0) < s.val:
                    eng.wait_ge(s.h, s.val)
                    w[s] = s.val

    def dma(self, E, out, in_, reads, writes, dbuf, **kw):
        self._sync(E, reads, writes)
        if dbuf.dsem is None:
            dbuf.dsem = self.newsem("d_" + dbuf.name)
            self.dsems.append(dbuf.dsem)
        s = dbuf.dsem
        ins = self.eng[E].dma_start(out=out, in_=in_, **kw)
        s.val += 16
        ins.then_inc(s.h, 16)
        tag = (s, s.val)
        for b in reads:
            if b.r.get(s, 0) < tag[1]:
                b.r[s] = tag[1]
        for b in writes:
            b.w = tag
            b.r = {}
        self.nops += 1
        return ins


class Rot:
    def __init__(self, items):
        self.items = items
        self.i = 0

    def next(self):
        it = self.items[self.i % len(self.items)]
        self.i += 1
        return it


def build_program(n_layers=4):
    nc = bass.Bass("TRN2", target_bir_lowering=False, dynamic_dma_scratch_size=16384)
    stack = ExitStack()
    K = Trk(nc, stack)

    def dram(name, shape, dt=F32, kind="ExternalInput"):
        return nc.dram_tensor(name, list(shape), dt, kind=kind).ap()

    x_in = dram("x", [NSEQ, SEQ, D])
    out = dram("out", [NSEQ, SEQ, D], kind="ExternalOutput")
    norm_pre = dram("norm_pre", [4, D])
    norm_post = dram("norm_post", [4, D])
    e_w_in = dram("even_w_in", [2, D, EVEN_W])
    e_k_pe = dram("even_cmp_k_pe", [2, 32, 64])
    e_k_w1 = dram("even_cmp_k_w1", [2, 2048, 64])
    e_k_w2 = dram("even_cmp_k_w2", [2, 64, 64])
    e_v_pe = dram("even_cmp_v_pe", [2, 32, 64])
    e_v_w1 = dram("even_cmp_v_w1", [2, 2048, 64])
    e_v_w2 = dram("even_cmp_v_w2", [2, 64, 64])
    e_ln_g = dram("even_sgu_ln_g", [2, 512])
    e_ln_b = dram("even_sgu_ln_b", [2, 512])
    e_sgu_w = dram("even_sgu_w", [2, 8, 128, 128])
    e_sgu_b = dram("even_sgu_b", [2, 8, 128])
    e_w_out = dram("even_w_out", [2, D, D])
    o_w_in = dram("odd_w_in", [2, D, ODD_W])
    o_dw_w = dram("odd_dw_w", [2, 31, D])
    o_dw_b = dram("odd_dw_b", [2, D])
    o_ln_g = dram("odd_ln_g", [2, D])
    o_ln_b = dram("odd_ln_b", [2, D])
    o_w_out = dram("odd_w_out", [2, D, D])
    c_ident = dram("c_ident", [128, 128])
    c_tril = dram("c_tril", [128, 128])
    c_far = dram("c_far", [128, 128])
    c_ctab = dram("c_ctab", [128, 16, 32])
    c_ebig = dram("c_ebig", [32, 2048])
    c_ovl = dram("c_ovl", [127, 32])

    def sb(name, shape, dt=F32):
        t = stack.enter_context(nc.sbuf_tensor(name, list(shape), dt))
        return t, Buf(name)

    def sbn(name, shape, dt, n):
        return Rot([sb(f"{name}{i}", shape, dt) for i in range(n)])

    w_in = stack.enter_context(nc.sbuf_tensor("w_in", [128, 8, EVEN_W], BF16))
    w_inB = [Buf(f"w_in{k}") for k in range(8)]
    w_out = stack.enter_context(nc.sbuf_tensor("w_out", [128, 8, D], BF16))
    w_outB = [Buf(f"w_out{k}") for k in range(8)]
    identb, identbB = sb("identb", [128, 128], BF16)
    identf, identfB = sb("identf", [128, 128], F32)
    trilk, trilkB = sb("trilk", [128, 128], BF16)
    fark, farkB = sb("fark", [128, 128], BF16)
    neghalf, neghalfB = sb("neghalf", [128, 8], F32)
    gpost, gpostB = sb("gpost", [128, D], F32)
    xa = sbn("xa", [128, D], F32, 2)
    xr = sbn("xr", [128, D], F32, 2)
    hp = sbn("hp", [128, D], BF16, 2)
    hT = stack.enter_context(nc.sbuf_tensor("hT", [128, 8, 512], BF16))
    hTB = [Buf(f"hT{t}") for t in range(4)]
    mixT = stack.enter_context(nc.sbuf_tensor("mixT", [128, 8, 512], BF16))
    mixTB = [Buf(f"mixT{t}") for t in range(4)]
    junks = sbn("junk", [128, D], BF16, 2)
    ss, _ = sb("ss", [128, 8], F32)
    ssB = [Buf(f"ss{t}") for t in range(8)]
    ms, _ = sb("ms", [128, 8], F32)
    msB = [Buf(f"ms{t}") for t in range(8)]
    rstd, _ = sb("rstd", [128, 8], F32)
    rstdB = [Buf(f"rstd{t}") for t in range(8)]
    ssy = Rot([sb(f"ssy{i}", [128, 4], F32) + (Buf(f"ssy{i}a"), Buf(f"ssy{i}b")) for i in range(2)])
    rsy = sbn("rsy", [128, 4], F32, 2)
    par, parB = sb("par", [128, 8, 36], F32)
    tmpf = sbn("tmpf", [128, 512], F32, 2)

    PS = []
    for i in range(8):
        t = stack.enter_context(nc.psum_tensor(f"ps{i}", [128, 512], F32))
        PS.append((t, Buf(f"ps{i}")))
    psA = Rot(PS[0:4])
    psB = Rot(PS[4:8])

    xd = [[Buf(f"xd{s}_{t}") for t in range(16)] for s in range(NSEQ)]

    K.dma("pool", identb[:, :], c_ident[:, :], [], [identbB], identbB)
    K.dma("sp", identf[:, :], c_ident[:, :], [], [identfB], identfB)
    K.dma("pool", trilk[:, :], c_tril[:, :], [], [trilkB], trilkB)
    K.dma("pool", fark[:, :], c_far[:, :], [], [farkB], farkB)
    K.op("dve", lambda e: e.memset(neghalf[:, :], -0.5), [], [neghalfB])

    def layer_srcs(layer):
        li = layer // 2
        if layer % 2 == 0:
            return e_w_in[li], EVEN_W, e_w_out[li], []
        return (o_w_in[li], ODD_W, o_w_out[li],
                [(o_dw_w[li], 31), (o_dw_b[li:li + 1, :], 1), (o_ln_g[li:li + 1, :], 1), (o_ln_b[li:li + 1, :], 1)])

    def load_w_in(layer):
        w_in_dram, w_in_cols, _, _ = layer_srcs(layer)
        for kc in range(8):
            K.dma("pool", w_in[:, kc, 0:w_in_cols], w_in_dram[kc * 128:(kc + 1) * 128, :], [], [w_inB[kc]], w_inB[kc],
                  max_dma_last_dim=4096)

    def load_common(layer):
        _, _, w_out_dram, rows = layer_srcs(layer)
        stg, stgB = xa.next()
        rows = [(norm_pre[layer:layer + 1, :], 1)] + rows
        if sum(r for _, r in rows) % 2:
            rows = rows + [(norm_post[layer:layer + 1, :], 1)]
        r0 = 0
        for ap, r in rows:
            K.dma("sp", stg[r0:r0 + r, :], ap, [], [stgB], stgB)
            r0 += r
        R = r0
        for c in range(8):
            pt, pB = psA.next()
            K.op("pe", lambda e, c=c, pt=pt: e.transpose(pt[:, 0:R], stg[0:R, c * 128:(c + 1) * 128], identf[0:R, 0:R]),
                 [stgB, identfB], [pB])
            K.op("dve", lambda e, c=c, pt=pt: e.tensor_copy(out=par[:, c, 0:R], in_=pt[:, 0:R]), [pB], [parB])
        for kc in range(8):
            K.dma("pool", w_out[:, kc, :], w_out_dram[kc * 128:(kc + 1) * 128, :], [], [w_outB[kc]], w_outB[kc],
                  max_dma_last_dim=4096)
        K.dma("sp", gpost[:, :], norm_post[layer:layer + 1, :].to_broadcast([128, D]), [], [gpostB], gpostB)

    def phase_A(layer, s, b):
        src = x_in if layer == 0 else out
        xts = []
        for tt in range(4):
            xt, xB = xa.next()
            tile = b * 4 + tt
            K.dma("sp", xt[:, :], src[s, tile * 128:(tile + 1) * 128, :], [xd[s][tile]], [xB], xB)
            jk, jkB = junks.next()
            K.op("act", lambda e, xt=xt, tt=tt, jk=jk: e.activation(out=jk[:, :], in_=xt[:, :], func=AF.Square,
                                                                    accum_out=ss[:, tt:tt + 1]), [xB], [ssB[tt], jkB])
            K.op("dve", lambda e, tt=tt: e.tensor_scalar(out=ms[:, tt:tt + 1], in0=ss[:, tt:tt + 1], scalar1=1.0 / D,
                                                        scalar2=RMS_EPS, op0=ALU.mult, op1=ALU.add), [ssB[tt]], [msB[tt]])
            K.op("pool", lambda e, tt=tt: e.tensor_tensor(out=rstd[:, tt:tt + 1], in0=ms[:, tt:tt + 1],
                                                          in1=neghalf[:, 0:1], op=ALU.pow), [msB[tt], neghalfB], [rstdB[tt]])
            ht, hB = hp.next()
            K.op("dve", lambda e, ht=ht, xt=xt, tt=tt: e.tensor_scalar(out=ht[:, :], in0=xt[:, :], scalar1=rstd[:, tt:tt + 1],
                                                                       scalar2=None, op0=ALU.mult), [xB, rstdB[tt]], [hB])
            pt, pB = psA.next()
            pb = pt[:].bitcast(BF16)
            for kc in range(8):
                K.op("pe", lambda e, kc=kc, pb=pb, ht=ht: e.transpose(pb[:, kc * 128:(kc + 1) * 128],
                                                                      ht[:, kc * 128:(kc + 1) * 128], identb[:, :]),
                     [hB, identbB], [pB], signal=(kc == 7))
            K.op("dve", lambda e, pb=pb, tt=tt: e.tensor_tensor(
                out=hT[:, :, tt * 128:(tt + 1) * 128], in0=pb.rearrange("p (k t) -> p k t", k=8),
                in1=par[:, :, 0:1].to_broadcast([128, 8, 128]), op=ALU.mult), [pB, parB], [hTB[tt]])

    def fm_proj(col0, M):
        pt, pB = psA.next()
        for kc in range(8):
            K.op("pe", lambda e, kc=kc, pt=pt: e.matmul(pt[0:M, 0:512], w_in[:, kc, col0:col0 + M], hT[:, kc, :],
                                                        start=(kc == 0), stop=(kc == 7)),
                 [w_inB[kc]] + hTB, [pB], signal=(kc == 7))
        return pt, pB

    def tm_proj(tt, col0, N):
        pt, pB = psA.next()
        for kc in range(8):
            K.op("pe", lambda e, kc=kc, pt=pt: e.matmul(pt[:, 0:N], hT[:, kc, tt * 128:(tt + 1) * 128],
                                                        w_in[:, kc, col0:col0 + N], start=(kc == 0), stop=(kc == 7)),
                 [w_inB[kc], hTB[tt]], [pB], signal=(kc == 7))
        return pt, pB

    def phase_G(layer, s, b):
        src = x_in if layer == 0 else out
        for tt in range(4):
            tile = b * 4 + tt
            xt, xB = xr.next()
            K.dma("sp", xt[:, :], src[s, tile * 128:(tile + 1) * 128, :], [xd[s][tile]], [xB], xB)
            halves = []
            sy, syB, syB0, syB1 = ssy.next()
            ry, ryB = rsy.next()
            for hf in range(2):
                pt, pB = psB.next()
                for fc in range(8):
                    K.op("pe", lambda e, fc=fc, pt=pt, hf=hf: e.matmul(pt[:, 0:512], mixT[:, fc, tt * 128:(tt + 1) * 128],
                                                                        w_out[:, fc, hf * 512:(hf + 1) * 512],
                                                                        start=(fc == 0), stop=(fc == 7)),
                         [mixTB[tt], w_outB[fc]], [pB], signal=(fc == 7))
                halves.append((pt, pB))
            for hf, sB_ in ((0, syB0), (1, syB1)):
                pt, pB = halves[hf]
                jk, jkB = junks.next()
                K.op("act", lambda e, pt=pt, hf=hf, sy=sy, jk=jk: e.activation(out=jk[:, 0:512], in_=pt[:, 0:512], func=AF.Square,
                                                                               accum_out=sy[:, hf:hf + 1]), [pB], [sB_, jkB])
            K.op("dve", lambda e, sy=sy: e.tensor_tensor(out=sy[:, 2:3], in0=sy[:, 0:1], in1=sy[:, 1:2], op=ALU.add),
                 [syB0, syB1, syB], [syB])
            K.op("dve", lambda e, sy=sy: e.tensor_scalar(out=sy[:, 3:4], in0=sy[:, 2:3], scalar1=1.0 / D, scalar2=RMS_EPS,
                                                        op0=ALU.mult, op1=ALU.add), [syB], [syB])
            K.op("pool", lambda e, sy=sy, ry=ry: e.tensor_tensor(out=ry[:, 0:1], in0=sy[:, 3:4], in1=neghalf[:, 0:1],
                                                                 op=ALU.pow), [syB, neghalfB], [ryB])
            for hf in range(2):
                pt, pB = halves[hf]
                tf, tfB = tmpf.next()
                K.op("dve", lambda e, pt=pt, tf=tf, hf=hf, ry=ry: e.scalar_tensor_tensor(
                    out=tf[:, :], in0=pt[:, 0:512], scalar=ry[:, 0:1], in1=gpost[:, hf * 512:(hf + 1) * 512],
                    op0=ALU.mult, op1=ALU.mult), [pB, ryB, gpostB], [tfB])
                K.op("pool", lambda e, tf=tf, xt=xt, hf=hf: e.tensor_tensor(
                    out=xt[:, hf * 512:(hf + 1) * 512], in0=xt[:, hf * 512:(hf + 1) * 512], in1=tf[:, :], op=ALU.add),
                     [tfB, xB], [xB])
            K.dma("sp", out[s, tile * 128:(tile + 1) * 128, :], xt[:, :], [xB], [xd[s][tile]], xd[s][tile])

    def even_layer(layer, li):
        st = ExitStack()

        def sbl(name, shape, dt=F32):
            t = st.enter_context(nc.sbuf_tensor(f"{name}_{layer}", list(shape), dt))
            return t, Buf(name)

        def sbln(name, shape, dt, n):
            return Rot([sbl(f"{name}{i}", shape, dt) for i in range(n)])

        qT = st.enter_context(nc.sbuf_tensor(f"qT_{layer}", [128, 8, 512], BF16))
        qTB = [Buf(f"qT{h}") for h in range(8)]
        qSB = [Buf(f"qS{h}") for h in range(8)]
        ksT = st.enter_context(nc.sbuf_tensor(f"ksT_{layer}", [128, 2, 2048], BF16))
        ksTB = [[Buf(f"ksT{g}_{b}") for b in range(4)] for g in range(2)]
        ebB = Buf("ebig")
        kwT = st.enter_context(nc.sbuf_tensor(f"kwT_{layer}", [64, 2, 1024], BF16))
        kwTB = [[Buf(f"kwT{g}_{r}") for r in range(2)] for g in range(2)]
        kcT, kcTB = sbl("kcT", [128, 528], BF16)
        vcT, vcTB = sbl("vcT", [128, 528], BF16)
        vsA = st.enter_context(nc.sbuf_tensor(f"vsA_{layer}", [128, 16, 2, 65], BF16))
        vsAB = [Buf(f"vsA{k}") for k in range(16)]
        vwA = st.enter_context(nc.sbuf_tensor(f"vwA_{layer}", [128, 8, 2, 65], BF16))
        vwAB = [Buf(f"vwA{k}") for k in range(8)]
        w1k, w1kB = sbl("w1k", [128, 32, 64], BF16)
        w1v, w1vB = sbl("w1v", [128, 32, 64], BF16)
        w2k, w2kB = sbl("w2k", [64, 64], BF16)
        w2v, w2vB = sbl("w2v", [64, 64], BF16)
        pek, pekB = sbl("pek", [32, 64], BF16)
        pev, pevB = sbl("pev", [32, 64], BF16)
        peT, peTB = sbl("peT", [64, 2, 32], BF16)
        pebias, pebiasB = sbl("pebias", [64, 2], F32)
        hidk, hidkB = sbl("hidk", [64, 2, 32], BF16)
        hidv, hidvB = sbl("hidv", [64, 2, 128], BF16)
        kcmpT, kcmpTB = sbl("kcmpT", [64, 2, 128], BF16)
        vcmp, vcmpB = sbl("vcmp", [128, 2, 97], BF16)
        ctab, ctabB = sbl("ctab", [128, 16, 32], F32)
        WsT, WsTB = sbl("WsT", [128, 8, 128], BF16)
        bs, bsB = sbl("bs", [128, 8], F32)
        lng, lngB = sbl("lng", [128, 512], F32)
        lnb, lnbB = sbl("lnb", [128, 512], F32)
        gates = st.enter_context(nc.sbuf_tensor(f"gates_{layer}", [128, 4, 24], F32))
        gatesB = [Buf(f"gates{t}") for t in range(4)]
        sza = st.enter_context(nc.sbuf_tensor(f"sza_{layer}", [128, 4, 512], BF16))
        szaB = [Buf(f"sza{t}") for t in range(4)]
        ug = sbln("ug", [128, 512], BF16, 2)
        szb = sbln("szb", [128, 512], BF16, 2)
        vg = sbln("vg", [128, 512], F32, 2)
        vln = sbln("vln", [128, 512], BF16, 2)
        lnst = sbln("lnst", [128, 16], F32, 2)
        acc_o = st.enter_context(nc.sbuf_tensor(f"acc_o_{layer}", [128, 4, 512], F32))
        acc_oB = [Buf(f"acc_o{h}") for h in range(8)]
        imp = sbln("imp", [128, 4, 32], F32, 2)
        top8 = sbln("top8", [128, 4, 8], F32, 2)
        selpad, selpadB = sbl("selpad", [128, 4, 96], BF16)
        pTs = sbln("pT", [128, 512], BF16, 4)
        rcs = sbln("rc", [128, 16], F32, 4)
        amix = sbln("amix", [128, 512], BF16, 2)
        tmpb = sbln("tmpb", [128, 512], BF16, 2)

        for g in range(2):
            K.dma("pool", ksT[64:96, g, :], c_ebig[:, :], [], [ebB], ebB, max_dma_last_dim=4096)
        for (w1, w1B, src) in ((w1k, w1kB, e_k_w1), (w1v, w1vB, e_v_w1)):
            for half in range(2):
                K.dma("pool", w1[half * 64:(half + 1) * 64, :, :], src[li].rearrange("(l d) e -> d l e", d=64), [], [w1B], w1B)
        K.dma("pool", w2k[:, :], e_k_w2[li], [], [w2kB], w2kB)
        K.dma("pool", w2v[:, :], e_v_w2[li], [], [w2vB], w2vB)
        K.dma("pool", pek[:, :], e_k_pe[li], [], [pekB], pekB)
        K.dma("pool", pev[:, :], e_v_pe[li], [], [pevB], pevB)
        K.dma("sp", ctab[:, :, :], c_ctab[:, :, :], [], [ctabB], ctabB)
        wsst_t, wsstB = xa.next()
        wsst = wsst_t[:, :].rearrange("p (g j) -> p g j", g=8)
        K.dma("sp", wsst, e_sgu_w[li].rearrange("g i j -> i g j"), [], [wsstB], wsstB)
        bst, bstB = xa.next()
        K.dma("sp", bst[0:8, 0:128], e_sgu_b[li], [], [bstB], bstB)
        pt, pB = psA.next()
        K.op("pe", lambda e: e.transpose(pt[:, 0:8], bst[0:8, 0:128], identf[0:8, 0:8]), [bstB, identfB], [pB])
        K.op("dve", lambda e: e.tensor_copy(out=bs[:, :], in_=pt[:, 0:8]), [pB], [bsB])
        K.dma("sp", lng[:, :], e_ln_g[li:li + 1, :].to_broadcast([128, 512]), [], [lngB], lngB)
        K.dma("sp", lnb[:, :], e_ln_b[li:li + 1, :].to_broadcast([128, 512]), [], [lnbB], lnbB)
        K.op("pool", lambda e: e.memset(vsA[:, :, :, 64:65], 1.0), [], vsAB)
        K.op("pool", lambda e: e.memset(vwA[:, :, :, 64:65], 1.0), [], vwAB)
        K.op("pool", lambda e: e.memset(vcmp[:, :, :], 0.0), [], [vcmpB])
        K.op("pool", lambda e: e.memset(vcmp[:, :, 64:65], 1.0), [vcmpB], [vcmpB])
        for g in range(2):
            K.dma("pool", vcmp[0:127, g, 65:97], c_ovl[:, :], [], [vcmpB], vcmpB)
        K.op("pool", lambda e: e.memset(hidv[:, :, :], 0.0), [], [hidvB])
        K.op("pool", lambda e: e.memset(kcmpT[:, :, :], 0.0), [], [kcmpTB])
        K.op("pool", lambda e: e.memset(selpad[:, :, :], 0.0), [], [selpadB])
        K.op("dve", lambda e: e.memset(kcT[:, 0:16], 0.0), [], [kcTB])
        K.op("dve", lambda e: e.memset(vcT[:, 0:16], 0.0), [], [vcTB])
        for g in range(8):
            pt, pB = psA.next()
            K.op("pe", lambda e, g=g, pt=pt: e.transpose(pt[:, 0:128], wsst[:, g, :], identf[:, :]), [wsstB, identfB], [pB])
            K.op("dve", lambda e, g=g, pt=pt: e.tensor_tensor(out=WsT[:, g, :], in0=pt[:, 0:128], in1=trilk[:, :], op=ALU.mult),
                 [pB, trilkB], [WsTB])
        for xi, (pe_, peB_, w1, w1B) in enumerate(((pek, pekB, w1k, w1kB), (pev, pevB, w1v, w1vB))):
            pt, pB = psA.next()
            pb = pt[:].bitcast(BF16)
            K.op("pe", lambda e, pb=pb, pe_=pe_: e.transpose(pb[0:64, 0:32], pe_[:, :], identb[0:32, 0:32]), [peB_, identbB], [pB])
            K.op("dve", lambda e, pb=pb, xi=xi: e.tensor_copy(out=peT[:, xi, :], in_=pb[0:64, 0:32]), [pB], [peTB])
            pt2, pB2 = psA.next()
            for l in range(32):
                K.op("pe", lambda e, l=l, pt2=pt2, w1=w1, xi=xi: e.matmul(pt2[0:64, 0:1], w1[0:64, l, :], peT[:, xi, l:l + 1],
                                                                          start=(l == 0), stop=(l == 31)),
                     [w1B, peTB], [pB2], signal=(l == 31))
            K.op("dve", lambda e, pt2=pt2, xi=xi: e.tensor_copy(out=pebias[:, xi:xi + 1], in_=pt2[0:64, 0:1]), [pB2], [pebiasB])

        def evac_copy(i, out_ap, in_ap, reads, writes, scale=None):
            if i % 2 == 0:
                if scale is None:
                    K.op("act", lambda e: e.copy(out=out_ap, in_=in_ap), reads, writes)
                else:
                    K.op("act", lambda e: e.mul(out=out_ap, in_=in_ap, mul=scale), reads, writes)
            else:
                if scale is None:
                    K.op("dve", lambda e: e.tensor_copy(out=out_ap, in_=in_ap), reads, writes)
                else:
                    K.op("dve", lambda e: e.tensor_scalar(out=out_ap, in0=in_ap, scalar1=scale, scalar2=None, op0=ALU.mult),
                         reads, writes)

        if STOP == "params":
            st.close()
            return
        blocks = [(s_, b_) for s_ in range(NSEQ) for b_ in range(NBLK)]
        K.phase = "evA"
        phase_A(layer, 0, 0)
        for bi, (s, b) in enumerate(blocks):
            if True:
                t0 = b * 512
                K.phase = "evB"
                if STOP == "A":
                    st.close()
                    return
                for h in range(8):
                    pt, pB = fm_proj(C_Q + h * 64, 64)
                    evac_copy(h, qT[0:64, h, :], pt[0:64, 0:512], [pB], [qTB[h]], scale=0.125)
                for g in range(2):
                    pt, pB = fm_proj(C_KS + g * 64, 64)
                    evac_copy(g, ksT[0:64, g, t0:t0 + 512], pt[0:64, 0:512], [pB], [ksTB[g][b]])
                for g in range(2):
                    pt, pB = fm_proj(C_KW + g * 64, 64)
                    r = b % 2
                    evac_copy(g + 1, kwT[0:64, g, r * 512:(r + 1) * 512], pt[0:64, 0:512], [pB], [kwTB[g][r]])
                for i, (cX, XT, XTB) in enumerate(((C_KC, kcT, kcTB), (C_VC, vcT, vcTB))):
                    if b > 0:
                        K.op("pool", lambda e, XT=XT: e.tensor_copy(out=XT[:, 0:16], in_=XT[:, 512:528]), [XTB], [XTB])
                    pt, pB = fm_proj(cX, 128)
                    evac_copy(i, XT[:, 16:528], pt[:, 0:512], [pB], [XTB])
                if STOP == "B":
                    st.close()
                    return
                K.phase = "evC"
                for tt in range(4):
                    kt = b * 4 + tt
                    pt, pB = tm_proj(tt, C_VS, 408)
                    K.op("dve", lambda e, pt=pt, kt=kt: e.tensor_copy(out=vsA[:, kt, :, 0:64],
                                                                      in_=pt[:, 0:128].rearrange("p (g d) -> p g d", g=2)),
                         [pB], [vsAB[kt]])
                    K.op("dve", lambda e, pt=pt, kt=kt: e.tensor_copy(out=vwA[:, kt % 8, :, 0:64],
                                                                      in_=pt[:, 256:384].rearrange("p (g d) -> p g d", g=2)),
                         [pB], [vwAB[kt % 8]])
                    K.op("act", lambda e, pt=pt, tt=tt: e.activation(out=gates[:, tt, :], in_=pt[:, 384:408], func=AF.Sigmoid),
                         [pB], [gatesB[tt]])
                    pt, pB = tm_proj(tt, C_ZA, 512)
                    K.op("act", lambda e, pt=pt, tt=tt: e.activation(out=sza[:, tt, :], in_=pt[:, 0:512], func=AF.Silu),
                         [pB], [szaB[tt]])
                    pt, pB = tm_proj(tt, C_ZB, 512)
                    zt, ztB = szb.next()
                    K.op("act", lambda e, pt=pt, zt=zt: e.activation(out=zt[:, :], in_=pt[:, 0:512], func=AF.Silu), [pB], [ztB])
                    pt, pB = tm_proj(tt, C_U, 512)
                    ut, utB = ug.next()
                    K.op("act", lambda e, pt=pt, ut=ut: e.activation(out=ut[:, :], in_=pt[:, 0:512], func=AF.Gelu_apprx_tanh),
                         [pB], [utB])
                    pt, pB = tm_proj(tt, C_V, 512)
                    vt, vtB = vg.next()
                    K.op("act", lambda e, pt=pt, vt=vt: e.activation(out=vt[:, :], in_=pt[:, 0:512], func=AF.Gelu_apprx_tanh),
                         [pB], [vtB])
                    K.op("pool", lambda e, ut=ut, zt=zt: e.tensor_tensor(out=ut[:, :], in0=ut[:, :], in1=zt[:, :], op=ALU.mult),
                         [utB, ztB], [utB])
                    ls, lsB = lnst.next()
                    K.op("dve", lambda e, ls=ls, vt=vt: e.bn_stats(out=ls[:, 0:6], in_=vt[:, :]), [vtB], [lsB])
                    K.op("dve", lambda e, ls=ls: e.bn_aggr(out=ls[:, 6:8], in_=ls[:, 0:6]), [lsB], [lsB])
                    K.op("dve", lambda e, ls=ls: e.tensor_scalar(out=ls[:, 8:9], in0=ls[:, 7:8], scalar1=LN_EPS, scalar2=None,
                                                                op0=ALU.add), [lsB], [lsB])
                    K.op("pool", lambda e, ls=ls: e.tensor_tensor(out=ls[:, 9:10], in0=ls[:, 8:9], in1=neghalf[:, 0:1], op=ALU.pow),
                         [lsB, neghalfB], [lsB])
                    K.op("dve", lambda e, ls=ls, vt=vt: e.tensor_scalar(out=vt[:, :], in0=vt[:, :], scalar1=ls[:, 6:7],
                                                                        scalar2=ls[:, 9:10], op0=ALU.subtract, op1=ALU.mult),
                         [lsB, vtB], [vtB])
                    K.op("pool", lambda e, vt=vt: e.tensor_tensor(out=vt[:, :], in0=vt[:, :], in1=lng[:, :], op=ALU.mult),
                         [vtB, lngB], [vtB])
                    vl, vlB = vln.next()
                    K.op("pool", lambda e, vt=vt, vl=vl: e.tensor_tensor(out=vl[:, :], in0=vt[:, :], in1=lnb[:, :], op=ALU.add),
                         [vtB, lnbB], [vlB])
                    pt, pB = psB.next()
                    for g in range(8):
                        K.op("pe", lambda e, g=g, pt=pt, vl=vl: e.matmul(pt[:, g * 64:(g + 1) * 64], WsT[:, g, :],
                                                                          vl[:, g * 64:(g + 1) * 64], start=True, stop=True),
                             [WsTB, vlB], [pB], signal=(g == 7))
                    tb, tbB = tmpb.next()
                    K.op("dve", lambda e, pt=pt, tb=tb: e.tensor_tensor(
                        out=tb[:, :].rearrange("p (g d) -> p g d", g=8), in0=pt[:, 0:512].rearrange("p (g d) -> p g d", g=8),
                        in1=bs[:, :].unsqueeze(2).to_broadcast([128, 8, 64]), op=ALU.add), [pB, bsB], [tbB])
                    K.op("pool", lambda e, tb=tb, ut=ut: e.tensor_tensor(out=tb[:, :], in0=tb[:, :], in1=ut[:, :], op=ALU.mult),
                         [tbB, utB], [tbB])
                    pt, pB = psA.next()
                    pb = pt[:].bitcast(BF16)
                    for fc in range(4):
                        K.op("pe", lambda e, fc=fc, pb=pb, tb=tb: e.transpose(pb[:, fc * 128:(fc + 1) * 128],
                                                                              tb[:, fc * 128:(fc + 1) * 128], identb[:, :]),
                             [tbB, identbB], [pB], signal=(fc == 3))
                    K.op("dve", lambda e, pb=pb, tt=tt: e.tensor_copy(out=mixT[:, 4:8, tt * 128:(tt + 1) * 128],
                                                                      in_=pb[:, 0:512].rearrange("p (k t) -> p k t", k=4)),
                         [pB], [mixTB[tt]])
                if STOP == "C":
                    st.close()
                    return
                if bi + 1 < len(blocks):
                    K.phase = "evA"
                    phase_A(layer, blocks[bi + 1][0], blocks[bi + 1][1])
                elif layer + 1 < n_layers:
                    load_w_in(layer + 1)
                K.phase = "evD"
                if b == 0:
                    n0, nn, c0 = 0, 31, 16
                else:
                    n0, nn, c0 = 32 * b - 1, 32, 0
                nk = 32 * (b + 1) - 1
                for g in range(2):
                    for xi, (XT, XTB, w1, w1B, hid, hidB, hcol) in enumerate((
                            (kcT, kcTB, w1k, w1kB, hidk, hidkB, 0), (vcT, vcTB, w1v, w1vB, hidv, hidvB, n0))):
                        pt, pB = psA.next()
                        for l in range(32):
                            K.op("pe", lambda e, l=l, pt=pt, XT=XT, w1=w1: e.matmul(
                                pt[0:64, 0:nn], w1[g * 64:(g + 1) * 64, l, :],
                                XT[g * 64:(g + 1) * 64, c0 + l:c0 + l + 16 * (nn - 1) + 1:16], start=(l == 0), stop=(l == 31)),
                                 [w1B, XTB], [pB], signal=(l == 31))
                        K.op("act", lambda e, pt=pt, hid=hid, hcol=hcol, xi=xi: e.activation(
                            out=hid[:, g, hcol:hcol + nn], in_=pt[0:64, 0:nn], func=AF.Silu, bias=pebias[:, xi:xi + 1]),
                             [pB, pebiasB], [hidB])
                    pt, pB = psA.next()
                    K.op("pe", lambda e, pt=pt: e.matmul(pt[0:64, 0:nn], w2k[:, :], hidk[:, g, 0:nn], start=True, stop=True),
                         [w2kB, hidkB], [pB])
                    K.op("dve", lambda e, pt=pt: e.tensor_copy(out=kcmpT[:, g, n0:n0 + nn], in_=pt[0:64, 0:nn]), [pB], [kcmpTB])
                    pt, pB = psA.next()
                    K.op("pe", lambda e, pt=pt: e.matmul(pt[0:nk, 0:64], hidv[:, g, 0:nk], w2v[:, :], start=True, stop=True),
                         [w2vB, hidvB], [pB])
                    K.op("dve", lambda e, pt=pt: e.tensor_copy(out=vcmp[0:nk, g, 0:64], in_=pt[0:nk, 0:64]), [pB], [vcmpB])
                if STOP == "D":
                    st.close()
                    return
                K.phase = "evE"
                for g in range(2):
                    im, imB = imp.next()
                    for r in range(4):
                        h = g * 4 + r
                        pt, pB = psA.next()
                        K.op("pe", lambda e, pt=pt, h=h: e.matmul(pt[0:nk, 0:512], kcmpT[:, g, 0:nk], qT[0:64, h, :],
                                                                  start=True, stop=True), [kcmpTB, qTB[h]], [pB])
                        pT, pTB = pTs.next()
                        K.op("act", lambda e, pt=pt, pT=pT: e.activation(out=pT[0:nk, :], in_=pt[0:nk, 0:512], func=AF.Exp),
                             [pB], [pTB])
                        if STOP == "E1":
                            st.close()
                            return
                        K.op("pool", lambda e, pT=pT: e.affine_select(out=pT[0:nk, :], in_=pT[0:nk, :], pattern=[[1, 512]],
                                                                      compare_op=ALU.is_ge, fill=0.0, base=t0 - 31,
                                                                      channel_multiplier=-16), [pTB], [pTB])
                        if STOP == "E2":
                            st.close()
                            return
                        po, poB = psB.next()
                        po3 = po[:, 0:388].rearrange("p (t c) -> p t c", t=4)
                        for tt in range(4):
                            K.op("pe", lambda e, tt=tt, po=po, pT=pT: e.matmul(po[:, tt * 97:(tt + 1) * 97],
                                                                                pT[0:nk, tt * 128:(tt + 1) * 128], vcmp[0:nk, g, :],
                                                                                start=True, stop=True),
                                 [pTB, vcmpB], [poB], signal=(tt == 3))
                        if STOP == "E3":
                            st.close()
                            return
                        rc, rcB = rcs.next()
                        K.op("dve", lambda e, rc=rc, po3=po3: e.tensor_scalar(out=rc[:, 0:4], in0=po3[:, :, 64], scalar1=1e-30,
                                                                              scalar2=None, op0=ALU.max), [poB], [rcB])
                        K.op("dve", lambda e, rc=rc: e.reciprocal(out=rc[:, 4:8], in_=rc[:, 0:4]), [rcB], [rcB])
                        K.op("dve", lambda e, rc=rc, h=h: e.tensor_tensor(out=rc[:, 8:12], in0=rc[:, 4:8], in1=gates[:, :, h],
                                                                          op=ALU.mult), [rcB] + gatesB, [rcB])
                        for tt in range(4):
                            K.op("dve", lambda e, tt=tt, po3=po3, rc=rc, h=h: e.tensor_scalar(
                                out=acc_o[:, tt, h * 64:(h + 1) * 64], in0=po3[:, tt, 0:64], scalar1=rc[:, 8 + tt:9 + tt],
                                scalar2=None, op0=ALU.mult), [poB, rcB], [acc_oB[h]])
                        for tt in range(4):
                            if r == 0:
                                K.op("dve", lambda e, tt=tt, po3=po3, rc=rc, im=im: e.tensor_scalar(
                                    out=im[:, tt, :], in0=po3[:, tt, 65:97], scalar1=rc[:, 4 + tt:5 + tt], scalar2=None,
                                    op0=ALU.mult), [poB, rcB], [imB])
                            else:
                                K.op("dve", lambda e, tt=tt, po3=po3, rc=rc, im=im: e.scalar_tensor_tensor(
                                    out=im[:, tt, :], in0=po3[:, tt, 65:97], scalar=rc[:, 4 + tt:5 + tt], in1=im[:, tt, :],
                                    op0=ALU.mult, op1=ALU.add), [poB, rcB, imB], [imB])
                    if STOP == "E4":
                        st.close()
                        return
                    K.op("dve", lambda e, im=im: e.tensor_tensor(out=im[:, :, :], in0=im[:, :, :], in1=ctab[:, 4 * b:4 * b + 4, :],
                                                                 op=ALU.add), [imB, ctabB], [imB])
                    t8, t8B = top8.next()
                    for tt in range(4):
                        K.op("dve", lambda e, tt=tt, t8=t8, im=im: e.max(out=t8[:, tt, :], in_=im[:, tt, :]), [imB], [t8B])
                    for tt in range(4):
                        K.op("dve", lambda e, tt=tt, t8=t8, im=im: e.tensor_scalar(
                            out=selpad[:, tt, 64:96], in0=im[:, tt, :], scalar1=t8[:, tt, 7:8], scalar2=-1.0,
                            op0=ALU.is_ge, op1=ALU.add), [imB, t8B], [selpadB])
                    if STOP == "E6":
                        st.close()
                        return
                    pt, pB = psA.next()
                    pb = pt[:].bitcast(BF16)
                    for tt in range(4):
                        K.op("pe", lambda e, tt=tt, pb=pb: e.transpose(pb[0:96, tt * 128:(tt + 1) * 128], selpad[:, tt, :],
                                                                       identb[:, :]), [selpadB, identbB], [pB], signal=(tt == 3))
                    for r in range(4):
                        h = g * 4 + r
                        K.op("dve", lambda e, h=h, pb=pb: e.tensor_copy(out=qT[64:96, h, :], in_=pb[64:96, 0:512]), [pB], [qSB[h]])
                if STOP == "E":
                    st.close()
                    return
                K.phase = "evF"
                items = []
                for h in range(8):
                    for br in range(2):
                        kts = list(range(0, 4 * b + 4)) if br == 0 else list(range(max(0, 4 * b - 4), 4 * b + 4))
                        for kt in kts:
                            items.append((h, br, kt, kt == kts[0], kt == kts[-1]))
                LA = 2
                stage = {}
                accs = {}

                def front(idx):
                    h, br, kt, isfirst, islast = items[idx]
                    g = h // 4
                    d = kt - 4 * b
                    lo = max(0, d)
                    hi = 3 if br == 0 else min(3, d + 4)
                    c0_, c1_ = lo * 128, (hi + 1) * 128
                    pt, pB = psA.next()
                    if br == 0:
                        K.op("pe", lambda e: e.matmul(
                            pt[:, c0_:c1_], ksT[0:96, g, kt * 128:(kt + 1) * 128], qT[0:96, h, c0_:c1_],
                            start=True, stop=True), [ksTB[g][kt // 4], ebB, qTB[h], qSB[h]], [pB])
                    else:
                        kr = kt % 8
                        K.op("pe", lambda e: e.matmul(
                            pt[:, c0_:c1_], kwT[0:64, g, kr * 128:(kr + 1) * 128], qT[0:64, h, c0_:c1_],
                            start=True, stop=True), [kwTB[g][kr // 4], qTB[h]], [pB])
                    pT, pTB = pTs.next()
                    K.op("act", lambda e: e.activation(out=pT[:, c0_:c1_], in_=pt[:, c0_:c1_], func=AF.Exp), [pB], [pTB])
                    if d >= 0:
                        K.op("pool", lambda e: e.tensor_tensor(
                            out=pT[:, d * 128:(d + 1) * 128], in0=pT[:, d * 128:(d + 1) * 128], in1=trilk[:, :],
                            op=ALU.mult), [pTB, trilkB], [pTB])
                    if br == 1 and 0 <= d + 4 <= 3:
                        K.op("pool", lambda e: e.tensor_tensor(
                            out=pT[:, (d + 4) * 128:(d + 5) * 128], in0=pT[:, (d + 4) * 128:(d + 5) * 128],
                            in1=fark[:, :], op=ALU.mult), [pTB, farkB], [pTB])
                    stage[idx] = (pT, pTB, lo, hi)

                def back(idx):
                    h, br, kt, isfirst, islast = items[idx]
                    g = h // 4
                    pT, pTB, lo, hi = stage.pop(idx)
                    if isfirst:
                        accs[(h, br)] = psB.next()
                    acc, accB = accs[(h, br)]
                    for tt in range(lo, hi + 1):
                        if br == 0:
                            vB_, rhs = vsAB[kt], vsA[:, kt, g, :]
                        else:
                            vB_, rhs = vwAB[kt % 8], vwA[:, kt % 8, g, :]
                        K.op("pe", lambda e, tt=tt, rhs=rhs: e.matmul(
                            acc[:, tt * 65:(tt + 1) * 65], pT[:, tt * 128:(tt + 1) * 128], rhs,
                            start=(isfirst and tt == lo), stop=True, skip_group_check=True), [pTB, vB_], [accB], signal=(tt == hi))
                    if islast:
                        acc3 = acc[:, 0:260].rearrange("p (t c) -> p t c", t=4)
                        rc, rcB = rcs.next()
                        K.op("dve", lambda e: e.tensor_scalar(out=rc[:, 0:4], in0=acc3[:, :, 64], scalar1=1e-30,
                                                              scalar2=None, op0=ALU.max), [accB], [rcB])
                        K.op("dve", lambda e: e.reciprocal(out=rc[:, 4:8], in_=rc[:, 0:4]), [rcB], [rcB])
                        gi = (1 + br) * 8 + h
                        K.op("dve", lambda e: e.tensor_tensor(out=rc[:, 8:12], in0=rc[:, 4:8], in1=gates[:, :, gi],
                                                              op=ALU.mult), [rcB] + gatesB, [rcB])
                        for tt in range(4):
                            K.op("dve", lambda e, tt=tt: e.scalar_tensor_tensor(
                                out=acc_o[:, tt, h * 64:(h + 1) * 64], in0=acc3[:, tt, 0:64], scalar=rc[:, 8 + tt:9 + tt],
                                in1=acc_o[:, tt, h * 64:(h + 1) * 64], op0=ALU.mult, op1=ALU.add),
                                 [accB, rcB, acc_oB[h]], [acc_oB[h]])
                        del accs[(h, br)]

                for idx in range(len(items) + LA):
                    if idx < len(items):
                        front(idx)
                    if idx - LA >= 0:
                        back(idx - LA)
                for tt in range(4):
                    am, amB = amix.next()
                    K.op("pool", lambda e, tt=tt, am=am: e.tensor_tensor(out=am[:, :], in0=acc_o[:, tt, :], in1=sza[:, tt, :],
                                                                         op=ALU.mult), acc_oB + [szaB[tt]], [amB])
                    pt, pB = psA.next()
                    pb = pt[:].bitcast(BF16)
                    for fc in range(4):
                        K.op("pe", lambda e, fc=fc, pb=pb, am=am: e.transpose(pb[:, fc * 128:(fc + 1) * 128],
                                                                              am[:, fc * 128:(fc + 1) * 128], identb[:, :]),
                             [amB, identbB], [pB], signal=(fc == 3))
                    K.op("dve", lambda e, pb=pb, tt=tt: e.tensor_copy(out=mixT[:, 0:4, tt * 128:(tt + 1) * 128],
                                                                      in_=pb[:, 0:512].rearrange("p (k t) -> p k t", k=4)),
                         [pB], [mixTB[tt]])
                if STOP == "F":
                    st.close()
                    return
                K.phase = "evG"
                phase_G(layer, s, b)
                if STOP == "G":
                    st.close()
                    return
        st.close()

    def odd_layer(layer, li):
        st = ExitStack()

        def sbl(name, shape, dt=F32):
            t = st.enter_context(nc.sbuf_tensor(f"{name}_{layer}", list(shape), dt))
            return t, Buf(name)

        def sbln(name, shape, dt, n):
            return Rot([sbl(f"{name}{i}", shape, dt) for i in range(n)])

        yc = st.enter_context(nc.sbuf_tensor(f"yc_{layer}", [128, 8, 544], BF16))
        ycB = [Buf(f"yc{c}") for c in range(8)]
        szz = st.enter_context(nc.sbuf_tensor(f"szz_{layer}", [128, 8, 512], BF16))
        szzB = [Buf(f"szz{c}") for c in range(8)]
        yv = st.enter_context(nc.sbuf_tensor(f"yv_{layer}", [128, 8, 512], F32))
        yvB = [Buf(f"yv{c}") for c in range(8)]
        diag = sbln("diag", [128, 31, 128], BF16, 3)
        sig = sbln("sig", [128, 512], F32, 2)
        ybf = sbln("ybf", [128, 512], BF16, 2)
        ysq = sbln("ysq", [128, 512], BF16, 2)
        onesb, onesbB = sbl("onesb", [128, 128], BF16)
        mean, meanB = sbl("mean", [128, 512], F32)
        rstdt, rstdtB = sbl("rstdt", [128, 512], F32)
        var, varB = sbl("var", [128, 512], F32)
        t1 = sbln("t1", [128, 512], F32, 2)

        K.op("pool", lambda e: e.memset(onesb[:, :], 1.0 / D), [], [onesbB])

        blocks = [(s_, b_) for s_ in range(NSEQ) for b_ in range(NBLK)]
        K.phase = "odA"
        phase_A(layer, 0, 0)
        for bi, (s, b) in enumerate(blocks):
            if b == 0:
                for c in range(8):
                    K.op("pool", lambda e, c=c: e.memset(yc[:, c, 0:32], 0.0), [], [ycB[c]])
            if True:
                K.phase = "odProj"
                for c in range(8):
                    if b > 0:
                        K.op("pool", lambda e, c=c: e.tensor_copy(out=yc[:, c, 0:32], in_=yc[:, c, 512:544]), [ycB[c]], [ycB[c]])
                    pg, pgB = fm_proj(1024 + c * 128, 128)
                    sg, sgB = sig.next()
                    K.op("act", lambda e, pg=pg, sg=sg: e.activation(out=sg[:, :], in_=pg[:, 0:512], func=AF.Sigmoid), [pgB], [sgB])
                    pa, paB = fm_proj(c * 128, 128)
                    K.op("dve", lambda e, pa=pa, sg=sg, c=c: e.tensor_tensor(out=yc[:, c, 32:544], in0=pa[:, 0:512], in1=sg[:, :],
                                                                             op=ALU.mult), [paB, sgB], [ycB[c]])
                    pz, pzB = fm_proj(2048 + c * 128, 128)
                    K.op("act", lambda e, pz=pz, c=c: e.activation(out=szz[:, c, :], in_=pz[:, 0:512], func=AF.Silu), [pzB], [szzB[c]])
                if bi + 1 < len(blocks):
                    K.phase = "odA"
                    phase_A(layer, blocks[bi + 1][0], blocks[bi + 1][1])
                elif layer + 1 < n_layers:
                    load_w_in(layer + 1)
                K.phase = "odConv"
                pm, pmB = psB.next()
                pq, pqB = psB.next()
                dgs = {}

                def build_diag(c):
                    dg, dgB = diag.next()
                    K.op("pool" if c % 2 == 0 else "dve", lambda e: e.tensor_tensor(
                        out=dg[:, :, :], in0=identb[:, :].unsqueeze(1).to_broadcast([128, 31, 128]),
                        in1=par[:, c, 1:32].unsqueeze(2).to_broadcast([128, 31, 128]), op=ALU.mult), [identbB, parB], [dgB])
                    dgs[c] = (dg, dgB)

                for c in range(3):
                    build_diag(c)
                for c in range(8):
                    dg, dgB = dgs[c]
                    pc, pcB = psA.next()
                    for k in range(31):
                        K.op("pe", lambda e, k=k, pc=pc, dg=dg, c=c: e.matmul(pc[:, 0:512], dg[:, k, :], yc[:, c, 2 + k:2 + k + 512],
                                                                              start=(k == 0), stop=(k == 30)),
                             [dgB, ycB[c]], [pcB], signal=(k == 30))
                    if c + 3 < 8:
                        build_diag(c + 3)
                    K.op("act", lambda e, pc=pc, c=c: e.activation(out=yv[:, c, :], in_=pc[:, 0:512], func=AF.Identity,
                                                                   bias=par[:, c, 32:33]), [pcB, parB], [yvB[c]])
                    yb, ybB = ybf.next()
                    yq, yqB = ysq.next()
                    K.op("dve", lambda e, yb=yb, c=c: e.tensor_copy(out=yb[:, :], in_=yv[:, c, :]), [yvB[c]], [ybB])
                    K.op("pool", lambda e, yq=yq, c=c: e.tensor_tensor(out=yq[:, :], in0=yv[:, c, :], in1=yv[:, c, :], op=ALU.mult),
                         [yvB[c]], [yqB])
                    K.op("pe", lambda e, yb=yb, c=c, pm=pm: e.matmul(pm[:, 0:512], onesb[:, :], yb[:, :], start=(c == 0), stop=(c == 7)),
                         [onesbB, ybB], [pmB], signal=(c == 7))
                    K.op("pe", lambda e, yq=yq, c=c, pq=pq: e.matmul(pq[:, 0:512], onesb[:, :], yq[:, :], start=(c == 0), stop=(c == 7)),
                         [onesbB, yqB], [pqB], signal=(c == 7))
                K.phase = "odLN"
                K.op("act", lambda e, pm=pm: e.copy(out=mean[:, :], in_=pm[:, 0:512]), [pmB], [meanB])
                K.op("dve", lambda e: e.tensor_tensor(out=var[:, :], in0=mean[:, :], in1=mean[:, :], op=ALU.mult), [meanB], [varB])
                K.op("dve", lambda e, pq=pq: e.scalar_tensor_tensor(out=var[:, :], in0=pq[:, 0:512], scalar=LN_EPS, in1=var[:, :],
                                                                    op0=ALU.add, op1=ALU.subtract), [pqB, varB], [varB])
                K.op("act", lambda e: e.activation(out=var[:, :], in_=var[:, :], func=AF.Sqrt), [varB], [varB])
                K.op("dve", lambda e: e.reciprocal(out=rstdt[:, :], in_=var[:, :]), [varB], [rstdtB])
                for c in range(8):
                    ta, taB = t1.next()
                    K.op("dve", lambda e, ta=ta, c=c: e.tensor_tensor(out=ta[:, :], in0=yv[:, c, :], in1=mean[:, :], op=ALU.subtract),
                         [yvB[c], meanB], [taB])
                    K.op("pool", lambda e, ta=ta: e.tensor_tensor(out=ta[:, :], in0=ta[:, :], in1=rstdt[:, :], op=ALU.mult),
                         [taB, rstdtB], [taB])
                    K.op("act", lambda e, ta=ta, c=c: e.activation(out=ta[:, :], in_=ta[:, :], func=AF.Silu,
                                                                   scale=par[:, c, 33:34], bias=par[:, c, 34:35]),
                         [taB, parB], [taB])
                    K.op("dve", lambda e, ta=ta, c=c: e.tensor_tensor(out=mixT[:, c, :], in0=ta[:, :], in1=szz[:, c, :], op=ALU.mult),
                         [taB, szzB[c]], mixTB)
                K.phase = "odG"
                phase_G(layer, s, b)
        st.close()

    load_w_in(0)
    for layer in range(n_layers):
        li = layer // 2
        load_common(layer)
        if layer > 0:
            K.barrier()
        if layer % 2 == 0:
            even_layer(layer, li)
        else:
            odd_layer(layer, li)

    for s in range(NSEQ):
        for t in range(16):
            b = xd[s][t]
            if b.w is not None:
                sm, v = b.w
                nc.sync.wait_ge(sm.h, v)
    stack.close()
    return nc, K


def make_consts():
    p = np.arange(128)
    ident = np.eye(128, dtype=np.float32)
    tril = (p[:, None] <= p[None, :]).astype(np.float32)
    far = (p[:, None] > p[None, :]).astype(np.float32)
    ctab = np.zeros((128, 16, 32), np.float32)
    j = np.arange(32)
    for tile in range(16):
        t = tile * 128 + p
        cur = t // 64
        forced = (j[None, :] == 0) | (j[None, :] == cur[:, None]) | (j[None, :] == cur[:, None] - 1)
        causal = j[None, :] <= cur[:, None]
        ctab[:, tile, :] = np.where(causal, forced.astype(np.float32) * np.float32(1e4), np.float32(-1e30))
    ebig = ((np.arange(2048)[None, :] // 64) == j[:, None]).astype(np.float32) * np.float32(BIG)
    n_cmp = 127
    tok = np.arange(n_cmp)[:, None] * 16 + np.arange(32)[None, :]
    ovl = ((tok[:, :, None] // 64) == np.arange(32)[None, None, :]).sum(1).astype(np.float32) / np.float32(32)
    return {"c_ident": ident, "c_tril": tril, "c_far": far, "c_ctab": ctab, "c_ebig": ebig, "c_ovl": ovl}


_CACHE = {}


def kernel(**inputs):
    if "nc" not in _CACHE:
        _CACHE["nc"] = build_program(4)[0]
    nc = _CACHE["nc"]
    consts = make_consts()
    x = np.ascontiguousarray(inputs["x"], dtype=np.float32)
    shared = {k: np.ascontiguousarray(v, dtype=np.float32) for k, v in inputs.items() if k != "x"}
    shared.update(consts)
    in_maps = []
    for c in range(NCORES):
        m = dict(shared)
        m["x"] = x[c * NSEQ:(c + 1) * NSEQ]
        in_maps.append(m)
    res = run_bass_kernel_spmd(nc, in_maps, core_ids=list(range(NCORES)))
    return np.concatenate([r["out"] for r in res.results], axis=0).astype(np.float32)
```

```python
from contextlib import ExitStack

import numpy as np
import concourse.bass as bass
import concourse.mybir as mybir
from concourse.bass_utils import run_bass_kernel_spmd

F32 = mybir.dt.float32
BF16 = mybir.dt.bfloat16
AF = mybir.ActivationFunctionType
ALU = mybir.AluOpType

NCORES = 8
SEQ = 2048
D = 1024
NSEQ = 2
NBLK = 4
EVEN_W = 3352
ODD_W = 3072
RMS_EPS = 1e-6
LN_EPS = 1e-5
BIG = 30000.0
import os as _os
STOP = _os.environ.get("KSTOP", "")

C_Q, C_KC, C_VC, C_KS, C_VS, C_KW, C_VW, C_GATE, C_ZA, C_U, C_V, C_ZB = (
    0, 512, 640, 768, 896, 1024, 1152, 1280, 1304, 1816, 2328, 2840)


class Sem:
    __slots__ = ("h", "val", "name")

    def __init__(self, h, name=""):
        self.h = h
        self.val = 0
        self.name = name


class Buf:
    __slots__ = ("name", "w", "r", "dsem")

    def __init__(self, name):
        self.name = name
        self.w = None
        self.r = {}
        self.dsem = None


class Trk:
    def __init__(self, nc, stack):
        self.nc = nc
        self.stack = stack
        self.eng = {"pe": nc.tensor, "act": nc.scalar, "dve": nc.vector, "pool": nc.gpsimd, "sp": nc.sync}
        self.nsem = 0
        self.esem = {e: self.newsem("e_" + e) for e in self.eng}
        self.waited = {e: {} for e in self.eng}
        self.nops = 0
        self.dsems = []
        self.phase = ""
        self.waitlog = {e: [] for e in self.eng}

    def newsem(self, name):
        self.nsem += 1
        return Sem(self.stack.enter_context(self.nc.semaphore(f"{name}_{self.nsem}")), name)

    def _sync(self, E, reads, writes):
        deps = {}
        for b in reads:
            if b.w is not None:
                s, v = b.w
                if deps.get(s, 0) < v:
                    deps[s] = v
        for b in writes:
            if b.w is not None:
                s, v = b.w
                if deps.get(s, 0) < v:
                    deps[s] = v
            for s, v in b.r.items():
                if deps.get(s, 0) < v:
                    deps[s] = v
        eng = self.eng[E]
        w = self.waited[E]
        own = self.esem[E]
        for s, v in deps.items():
            if E == "pe" and s is own:
                continue
            if w.get(s, 0) < v:
                eng.wait_ge(s.h, v)
                w[s] = v
                self.waitlog[E].append((self.phase, s.name))

    def op(self, E, fn, reads=(), writes=(), signal=True):
        self._sync(E, reads, writes)
        ins = fn(self.eng[E])
        s = self.esem[E]
        if signal:
            s.val += 1
            ins.then_inc(s.h, 1)
            tag = (s, s.val)
        else:
            tag = (s, s.val + 1)
        for b in reads:
            if b.r.get(s, 0) < tag[1]:
                b.r[s] = tag[1]
        for b in writes:
            b.w = tag
            b.r = {}
        self.nops += 1
        return ins

    def barrier(self):
        sems = list(self.esem.values()) + self.dsems
        for E, eng in self.eng.items():
            w = self.waited[E]
            for s in sems:
                if s.val > 0 and w.get(s, 0) < s.val:
                    eng.wait_ge(s.h, s.val)
                    w[s] = s.val

    def dma(self, E, out, in_, reads, writes, dbuf, **kw):
        self._sync(E, reads, writes)
        if dbuf.dsem is None:
            dbuf.dsem = self.newsem("d_" + dbuf.name)
            self.dsems.append(dbuf.dsem)
        s = dbuf.dsem
        ins = self.eng[E].dma_start(out=out, in_=in_, **kw)
        s.val += 16
        ins.then_inc(s.h, 16)
        tag = (s, s.val)
        for b in reads:
            if b.r.get(s, 0) < tag[1]:
                b.r[s] = tag[1]
        for b in writes:
            b.w = tag
            b.r = {}
        self.nops += 1
        return ins


class Rot:
    def __init__(self, items):
        self.items = items
        self.i = 0

    def next(self):
        it = self.items[self.i % len(self.items)]
        self.i += 1
        return it


def build_program(n_layers=4):
    nc = bass.Bass("TRN2", target_bir_lowering=False, dynamic_dma_scratch_size=16384)
    stack = ExitStack()
    K = Trk(nc, stack)

    def dram(name, shape, dt=F32, kind="ExternalInput"):
        return nc.dram_tensor(name, list(shape), dt, kind=kind).ap()

    x_in = dram("x", [NSEQ, SEQ, D])
    out = dram("out", [NSEQ, SEQ, D], kind="ExternalOutput")
    norm_pre = dram("norm_pre", [4, D])
    norm_post = dram("norm_post", [4, D])
    e_w_in = dram("even_w_in", [2, D, EVEN_W])
    e_k_pe = dram("even_cmp_k_pe", [2, 32, 64])
    e_k_w1 = dram("even_cmp_k_w1", [2, 2048, 64])
    e_k_w2 = dram("even_cmp_k_w2", [2, 64, 64])
    e_v_pe = dram("even_cmp_v_pe", [2, 32, 64])
    e_v_w1 = dram("even_cmp_v_w1", [2, 2048, 64])
    e_v_w2 = dram("even_cmp_v_w2", [2, 64, 64])
    e_ln_g = dram("even_sgu_ln_g", [2, 512])
    e_ln_b = dram("even_sgu_ln_b", [2, 512])
    e_sgu_w = dram("even_sgu_w", [2, 8, 128, 128])
    e_sgu_b = dram("even_sgu_b", [2, 8, 128])
    e_w_out = dram("even_w_out", [2, D, D])
    o_w_in = dram("odd_w_in", [2, D, ODD_W])
    o_dw_w = dram("odd_dw_w", [2, 31, D])
    o_dw_b = dram("odd_dw_b", [2, D])
    o_ln_g = dram("odd_ln_g", [2, D])
    o_ln_b = dram("odd_ln_b", [2, D])
    o_w_out = dram("odd_w_out", [2, D, D])
    c_ident = dram("c_ident", [128, 128])
    c_tril = dram("c_tril", [128, 128])
    c_far = dram("c_far", [128, 128])
    c_ctab = dram("c_ctab", [128, 16, 32])
    c_ebig = dram("c_ebig", [32, 2048])
    c_ovl = dram("c_ovl", [127, 32])

    def sb(name, shape, dt=F32):
        t = stack.enter_context(nc.sbuf_tensor(name, list(shape), dt))
        return t, Buf(name)

    def sbn(name, shape, dt, n):
        return Rot([sb(f"{name}{i}", shape, dt) for i in range(n)])

    w_in = stack.enter_context(nc.sbuf_tensor("w_in", [128, 8, EVEN_W], BF16))
    w_inB = [Buf(f"w_in{k}") for k in range(8)]
    w_out = stack.enter_context(nc.sbuf_tensor("w_out", [128, 8, D], BF16))
    w_outB = [Buf(f"w_out{k}") for k in range(8)]
    identb, identbB = sb("identb", [128, 128], BF16)
    identf, identfB = sb("identf", [128, 128], F32)
    trilk, trilkB = sb("trilk", [128, 128], BF16)
    fark, farkB = sb("fark", [128, 128], BF16)
    neghalf, neghalfB = sb("neghalf", [128, 8], F32)
    gpost, gpostB = sb("gpost", [128, D], F32)
    xa = sbn("xa", [128, D], F32, 2)
    xr = sbn("xr", [128, D], F32, 2)
    hp = sbn("hp", [128, D], BF16, 2)
    hT = stack.enter_context(nc.sbuf_tensor("hT", [128, 8, 512], BF16))
    hTB = [Buf(f"hT{t}") for t in range(4)]
    mixT = stack.enter_context(nc.sbuf_tensor("mixT", [128, 8, 512], BF16))
    mixTB = [Buf(f"mixT{t}") for t in range(4)]
    junks = sbn("junk", [128, D], BF16, 2)
    ss, _ = sb("ss", [128, 8], F32)
    ssB = [Buf(f"ss{t}") for t in range(8)]
    ms, _ = sb("ms", [128, 8], F32)
    msB = [Buf(f"ms{t}") for t in range(8)]
    rstd, _ = sb("rstd", [128, 8], F32)
    rstdB = [Buf(f"rstd{t}") for t in range(8)]
    ssy = Rot([sb(f"ssy{i}", [128, 4], F32) + (Buf(f"ssy{i}a"), Buf(f"ssy{i}b")) for i in range(2)])
    rsy = sbn("rsy", [128, 4], F32, 2)
    par, parB = sb("par", [128, 8, 36], F32)
    tmpf = sbn("tmpf", [128, 512], F32, 4)

    PS = []
    for i in range(8):
        t = stack.enter_context(nc.psum_tensor(f"ps{i}", [128, 512], F32))
        PS.append((t, Buf(f"ps{i}")))
    psA = Rot(PS[0:4])
    psB = Rot(PS[4:8])

    xd = [[Buf(f"xd{s}_{t}") for t in range(16)] for s in range(NSEQ)]

    K.dma("pool", identb[:, :], c_ident[:, :], [], [identbB], identbB)
    K.dma("sp", identf[:, :], c_ident[:, :], [], [identfB], identfB)
    K.dma("pool", trilk[:, :], c_tril[:, :], [], [trilkB], trilkB)
    K.dma("pool", fark[:, :], c_far[:, :], [], [farkB], farkB)
    K.op("dve", lambda e: e.memset(neghalf[:, :], -0.5), [], [neghalfB])

    def layer_srcs(layer):
        li = layer // 2
        if layer % 2 == 0:
            return e_w_in[li], EVEN_W, e_w_out[li], []
        return (o_w_in[li], ODD_W, o_w_out[li],
                [(o_dw_w[li], 31), (o_dw_b[li:li + 1, :], 1), (o_ln_g[li:li + 1, :], 1), (o_ln_b[li:li + 1, :], 1)])

    def load_w_in(layer):
        w_in_dram, w_in_cols, _, _ = layer_srcs(layer)
        for kc in range(8):
            K.dma("pool", w_in[:, kc, 0:w_in_cols], w_in_dram[kc * 128:(kc + 1) * 128, :], [], [w_inB[kc]], w_inB[kc],
                  max_dma_last_dim=4096)

    def load_common(layer):
        _, _, w_out_dram, rows = layer_srcs(layer)
        stg, stgB = xa.next()
        rows = [(norm_pre[layer:layer + 1, :], 1)] + rows
        if sum(r for _, r in rows) % 2:
            rows = rows + [(norm_post[layer:layer + 1, :], 1)]
        r0 = 0
        for ap, r in rows:
            K.dma("sp", stg[r0:r0 + r, :], ap, [], [stgB], stgB)
            r0 += r
        R = r0
        for c in range(8):
            pt, pB = psA.next()
            K.op("pe", lambda e, c=c, pt=pt: e.transpose(pt[:, 0:R], stg[0:R, c * 128:(c + 1) * 128], identf[0:R, 0:R]),
                 [stgB, identfB], [pB])
            K.op("dve", lambda e, c=c, pt=pt: e.tensor_copy(out=par[:, c, 0:R], in_=pt[:, 0:R]), [pB], [parB])
        for kc in range(8):
            K.dma("pool", w_out[:, kc, :], w_out_dram[kc * 128:(kc + 1) * 128, :], [], [w_outB[kc]], w_outB[kc],
                  max_dma_last_dim=4096)
        K.dma("sp", gpost[:, :], norm_post[layer:layer + 1, :].to_broadcast([128, D]), [], [gpostB], gpostB)

    def phase_A(layer, s, b):
        src = x_in if layer == 0 else out
        xts = []
        for tt in range(4):
            xt, xB = xa.next()
            tile = b * 4 + tt
            K.dma("sp", xt[:, :], src[s, tile * 128:(tile + 1) * 128, :], [xd[s][tile]], [xB], xB)
            jk, jkB = junks.next()
            K.op("act", lambda e, xt=xt, tt=tt, jk=jk: e.activation(out=jk[:, :], in_=xt[:, :], func=AF.Square,
                                                                    accum_out=ss[:, tt:tt + 1]), [xB], [ssB[tt], jkB])
            K.op("dve", lambda e, tt=tt: e.tensor_scalar(out=ms[:, tt:tt + 1], in0=ss[:, tt:tt + 1], scalar1=1.0 / D,
                                                        scalar2=RMS_EPS, op0=ALU.mult, op1=ALU.add), [ssB[tt]], [msB[tt]])
            K.op("pool", lambda e, tt=tt: e.tensor_tensor(out=rstd[:, tt:tt + 1], in0=ms[:, tt:tt + 1],
                                                          in1=neghalf[:, 0:1], op=ALU.pow), [msB[tt], neghalfB], [rstdB[tt]])
            ht, hB = hp.next()
            K.op("dve", lambda e, ht=ht, xt=xt, tt=tt: e.tensor_scalar(out=ht[:, :], in0=xt[:, :], scalar1=rstd[:, tt:tt + 1],
                                                                       scalar2=None, op0=ALU.mult), [xB, rstdB[tt]], [hB])
            pt, pB = psA.next()
            pb = pt[:].bitcast(BF16)
            for kc in range(8):
                K.op("pe", lambda e, kc=kc, pb=pb, ht=ht: e.transpose(pb[:, kc * 128:(kc + 1) * 128],
                                                                      ht[:, kc * 128:(kc + 1) * 128], identb[:, :]),
                     [hB, identbB], [pB], signal=(kc == 7))
            K.op("dve", lambda e, pb=pb, tt=tt: e.tensor_tensor(
                out=hT[:, :, tt * 128:(tt + 1) * 128], in0=pb.rearrange("p (k t) -> p k t", k=8),
                in1=par[:, :, 0:1].to_broadcast([128, 8, 128]), op=ALU.mult), [pB, parB], [hTB[tt]])

    def fm_proj(col0, M):
        pt, pB = psA.next()
        for kc in range(8):
            K.op("pe", lambda e, kc=kc, pt=pt: e.matmul(pt[0:M, 0:512], w_in[:, kc, col0:col0 + M], hT[:, kc, :],
                                                        start=(kc == 0), stop=(kc == 7)),
                 [w_inB[kc]] + hTB, [pB], signal=(kc == 7))
        return pt, pB

    def tm_proj(tt, col0, N):
        pt, pB = psA.next()
        for kc in range(8):
            K.op("pe", lambda e, kc=kc, pt=pt: e.matmul(pt[:, 0:N], hT[:, kc, tt * 128:(tt + 1) * 128],
                                                        w_in[:, kc, col0:col0 + N], start=(kc == 0), stop=(kc == 7)),
                 [w_inB[kc], hTB[tt]], [pB], signal=(kc == 7))
        return pt, pB

    def phase_G(layer, s, b):
        src = x_in if layer == 0 else out
        for tt in range(4):
            tile = b * 4 + tt
            xt, xB = xr.next()
            K.dma("sp", xt[:, :], src[s, tile * 128:(tile + 1) * 128, :], [xd[s][tile]], [xB], xB)
            halves = []
            sy, syB, syB0, syB1 = ssy.next()
            ry, ryB = rsy.next()
            for hf in range(2):
                pt, pB = psB.next()
                for fc in range(8):
                    K.op("pe", lambda e, fc=fc, pt=pt, hf=hf: e.matmul(pt[:, 0:512], mixT[:, fc, tt * 128:(tt + 1) * 128],
                                                                        w_out[:, fc, hf * 512:(hf + 1) * 512],
                                                                        start=(fc == 0), stop=(fc == 7)),
                         [mixTB[tt], w_outB[fc]], [pB], signal=(fc == 7))
                halves.append((pt, pB))
            ysbs = []
            for hf, sB_ in ((0, syB0), (1, syB1)):
                pt, pB = halves[hf]
                tf, tfB = tmpf.next()
                if hf == 0:
                    K.op("act", lambda e, pt=pt, tf=tf: e.copy(out=tf[:, :], in_=pt[:, 0:512]), [pB], [tfB])
                else:
                    K.op("dve", lambda e, pt=pt, tf=tf: e.tensor_copy(out=tf[:, :], in_=pt[:, 0:512]), [pB], [tfB])
                jk, jkB = junks.next()
                K.op("act", lambda e, tf=tf, hf=hf, sy=sy, jk=jk: e.activation(out=jk[:, 0:512], in_=tf[:, :], func=AF.Square,
                                                                               accum_out=sy[:, hf:hf + 1]), [tfB], [sB_, jkB])
                ysbs.append((tf, tfB))
            K.op("dve", lambda e, sy=sy: e.tensor_tensor(out=sy[:, 2:3], in0=sy[:, 0:1], in1=sy[:, 1:2], op=ALU.add),
                 [syB0, syB1, syB], [syB])
            K.op("dve", lambda e, sy=sy: e.tensor_scalar(out=sy[:, 3:4], in0=sy[:, 2:3], scalar1=1.0 / D, scalar2=RMS_EPS,
                                                        op0=ALU.mult, op1=ALU.add), [syB], [syB])
            K.op("pool", lambda e, sy=sy, ry=ry: e.tensor_tensor(out=ry[:, 0:1], in0=sy[:, 3:4], in1=neghalf[:, 0:1],
                                                                 op=ALU.pow), [syB, neghalfB], [ryB])
            for hf in range(2):
                tf, tfB = ysbs[hf]
                K.op("dve", lambda e, tf=tf, hf=hf, ry=ry: e.scalar_tensor_tensor(
                    out=tf[:, :], in0=tf[:, :], scalar=ry[:, 0:1], in1=gpost[:, hf * 512:(hf + 1) * 512],
                    op0=ALU.mult, op1=ALU.mult), [tfB, ryB, gpostB], [tfB])
                K.op("pool", lambda e, tf=tf, xt=xt, hf=hf: e.tensor_tensor(
                    out=xt[:, hf * 512:(hf + 1) * 512], in0=xt[:, hf * 512:(hf + 1) * 512], in1=tf[:, :], op=ALU.add),
                     [tfB, xB], [xB])
            K.dma("sp", out[s, tile * 128:(tile + 1) * 128, :], xt[:, :], [xB], [xd[s][tile]], xd[s][tile])

    def even_layer(layer, li):
        st = ExitStack()

        def sbl(name, shape, dt=F32):
            t = st.enter_context(nc.sbuf_tensor(f"{name}_{layer}", list(shape), dt))
            return t, Buf(name)

        def sbln(name, shape, dt, n):
            return Rot([sbl(f"{name}{i}", shape, dt) for i in range(n)])

        qT = st.enter_context(nc.sbuf_tensor(f"qT_{layer}", [128, 8, 512], BF16))
        qTB = [Buf(f"qT{h}") for h in range(8)]
        qSB = [Buf(f"qS{h}") for h in range(8)]
        ksT = st.enter_context(nc.sbuf_tensor(f"ksT_{layer}", [128, 2, 2048], BF16))
        ksTB = [[Buf(f"ksT{g}_{b}") for b in range(4)] for g in range(2)]
        ebB = Buf("ebig")
        kwT = st.enter_context(nc.sbuf_tensor(f"kwT_{layer}", [64, 2, 1024], BF16))
        kwTB = [[Buf(f"kwT{g}_{r}") for r in range(2)] for g in range(2)]
        kcT, kcTB = sbl("kcT", [128, 528], BF16)
        vcT, vcTB = sbl("vcT", [128, 528], BF16)
        vsA = st.enter_context(nc.sbuf_tensor(f"vsA_{layer}", [128, 16, 2, 65], BF16))
        vsAB = [Buf(f"vsA{k}") for k in range(16)]
        vwA = st.enter_context(nc.sbuf_tensor(f"vwA_{layer}", [128, 8, 2, 65], BF16))
        vwAB = [Buf(f"vwA{k}") for k in range(8)]
        w1k, w1kB = sbl("w1k", [128, 32, 64], BF16)
        w1v, w1vB = sbl("w1v", [128, 32, 64], BF16)
        w2k, w2kB = sbl("w2k", [64, 64], BF16)
        w2v, w2vB = sbl("w2v", [64, 64], BF16)
        pek, pekB = sbl("pek", [32, 64], BF16)
        pev, pevB = sbl("pev", [32, 64], BF16)
        peT, peTB = sbl("peT", [64, 2, 32], BF16)
        pebias, pebiasB = sbl("pebias", [64, 2], F32)
        hidk, hidkB = sbl("hidk", [64, 2, 32], BF16)
        hidv, hidvB = sbl("hidv", [64, 2, 128], BF16)
        kcmpT, kcmpTB = sbl("kcmpT", [64, 2, 128], BF16)
        vcmp, vcmpB = sbl("vcmp", [128, 2, 97], BF16)
        ctab, ctabB = sbl("ctab", [128, 16, 32], F32)
        WsT, WsTB = sbl("WsT", [128, 8, 128], BF16)
        bs, bsB = sbl("bs", [128, 8], F32)
        lng, lngB = sbl("lng", [128, 512], F32)
        lnb, lnbB = sbl("lnb", [128, 512], F32)
        gates = st.enter_context(nc.sbuf_tensor(f"gates_{layer}", [128, 4, 24], F32))
        gatesB = [Buf(f"gates{t}") for t in range(4)]
        sza = st.enter_context(nc.sbuf_tensor(f"sza_{layer}", [128, 4, 512], BF16))
        szaB = [Buf(f"sza{t}") for t in range(4)]
        ug = sbln("ug", [128, 512], BF16, 2)
        szb = sbln("szb", [128, 512], BF16, 2)
        vg = sbln("vg", [128, 512], F32, 2)
        vln = sbln("vln", [128, 512], BF16, 2)
        lnst = sbln("lnst", [128, 16], F32, 2)
        acc_o = st.enter_context(nc.sbuf_tensor(f"acc_o_{layer}", [128, 4, 512], F32))
        acc_oB = [Buf(f"acc_o{h}") for h in range(8)]
        imp = sbln("imp", [128, 4, 32], F32, 2)
        top8 = sbln("top8", [128, 4, 8], F32, 2)
        selpad, selpadB = sbl("selpad", [128, 4, 96], BF16)
        pTs = sbln("pT", [128, 512], BF16, 4)
        rcs = sbln("rc", [128, 16], F32, 4)
        amix = sbln("amix", [128, 512], BF16, 2)
        tmpb = sbln("tmpb", [128, 512], BF16, 2)

        for g in range(2):
            K.dma("pool", ksT[64:96, g, :], c_ebig[:, :], [], [ebB], ebB, max_dma_last_dim=4096)
        for (w1, w1B, src) in ((w1k, w1kB, e_k_w1), (w1v, w1vB, e_v_w1)):
            for half in range(2):
                K.dma("pool", w1[half * 64:(half + 1) * 64, :, :], src[li].rearrange("(l d) e -> d l e", d=64), [], [w1B], w1B)
        K.dma("pool", w2k[:, :], e_k_w2[li], [], [w2kB], w2kB)
        K.dma("pool", w2v[:, :], e_v_w2[li], [], [w2vB], w2vB)
        K.dma("pool", pek[:, :], e_k_pe[li], [], [pekB], pekB)
        K.dma("pool", pev[:, :], e_v_pe[li], [], [pevB], pevB)
        K.dma("sp", ctab[:, :, :], c_ctab[:, :, :], [], [ctabB], ctabB)
        wsst_t, wsstB = xa.next()
        wsst = wsst_t[:, :].rearrange("p (g j) -> p g j", g=8)
        K.dma("sp", wsst, e_sgu_w[li].rearrange("g i j -> i g j"), [], [wsstB], wsstB)
        bst, bstB = xa.next()
        K.dma("sp", bst[0:8, 0:128], e_sgu_b[li], [], [bstB], bstB)
        pt, pB = psA.next()
        K.op("pe", lambda e: e.transpose(pt[:, 0:8], bst[0:8, 0:128], identf[0:8, 0:8]), [bstB, identfB], [pB])
        K.op("dve", lambda e: e.tensor_copy(out=bs[:, :], in_=pt[:, 0:8]), [pB], [bsB])
        K.dma("sp", lng[:, :], e_ln_g[li:li + 1, :].to_broadcast([128, 512]), [], [lngB], lngB)
        K.dma("sp", lnb[:, :], e_ln_b[li:li + 1, :].to_broadcast([128, 512]), [], [lnbB], lnbB)
        K.op("pool", lambda e: e.memset(vsA[:, :, :, 64:65], 1.0), [], vsAB)
        K.op("pool", lambda e: e.memset(vwA[:, :, :, 64:65], 1.0), [], vwAB)
        K.op("pool", lambda e: e.memset(vcmp[:, :, :], 0.0), [], [vcmpB])
        K.op("pool", lambda e: e.memset(vcmp[:, :, 64:65], 1.0), [vcmpB], [vcmpB])
        for g in range(2):
            K.dma("pool", vcmp[0:127, g, 65:97], c_ovl[:, :], [], [vcmpB], vcmpB)
        K.op("pool", lambda e: e.memset(hidv[:, :, :], 0.0), [], [hidvB])
        K.op("pool", lambda e: e.memset(kcmpT[:, :, :], 0.0), [], [kcmpTB])
        K.op("pool", lambda e: e.memset(selpad[:, :, :], 0.0), [], [selpadB])
        K.op("dve", lambda e: e.memset(kcT[:, 0:16], 0.0), [], [kcTB])
        K.op("dve", lambda e: e.memset(vcT[:, 0:16], 0.0), [], [vcTB])
        for g in range(8):
            pt, pB = psA.next()
            K.op("pe", lambda e, g=g, pt=pt: e.transpose(pt[:, 0:128], wsst[:, g, :], identf[:, :]), [wsstB, identfB], [pB])
            K.op("dve", lambda e, g=g, pt=pt: e.tensor_tensor(out=WsT[:, g, :], in0=pt[:, 0:128], in1=trilk[:, :], op=ALU.mult),
                 [pB, trilkB], [WsTB])
        for xi, (pe_, peB_, w1, w1B) in enumerate(((pek, pekB, w1k, w1kB), (pev, pevB, w1v, w1vB))):
            pt, pB = psA.next()
            pb = pt[:].bitcast(BF16)
            K.op("pe", lambda e, pb=pb, pe_=pe_: e.transpose(pb[0:64, 0:32], pe_[:, :], identb[0:32, 0:32]), [peB_, identbB], [pB])
            K.op("dve", lambda e, pb=pb, xi=xi: e.tensor_copy(out=peT[:, xi, :], in_=pb[0:64, 0:32]), [pB], [peTB])
            pt2, pB2 = psA.next()
            for l in range(32):
                K.op("pe", lambda e, l=l, pt2=pt2, w1=w1, xi=xi: e.matmul(pt2[0:64, 0:1], w1[0:64, l, :], peT[:, xi, l:l + 1],
                                                                          start=(l == 0), stop=(l == 31)),
                     [w1B, peTB], [pB2], signal=(l == 31))
            K.op("dve", lambda e, pt2=pt2, xi=xi: e.tensor_copy(out=pebias[:, xi:xi + 1], in_=pt2[0:64, 0:1]), [pB2], [pebiasB])

        def evac_copy(i, out_ap, in_ap, reads, writes, scale=None):
            if i % 2 == 0:
                if scale is None:
                    K.op("act", lambda e: e.copy(out=out_ap, in_=in_ap), reads, writes)
                else:
                    K.op("act", lambda e: e.mul(out=out_ap, in_=in_ap, mul=scale), reads, writes)
            else:
                if scale is None:
                    K.op("dve", lambda e: e.tensor_copy(out=out_ap, in_=in_ap), reads, writes)
                else:
                    K.op("dve", lambda e: e.tensor_scalar(out=out_ap, in0=in_ap, scalar1=scale, scalar2=None, op0=ALU.mult),
                         reads, writes)

        if STOP == "params":
            st.close()
            return
        blocks = [(s_, b_) for s_ in range(NSEQ) for b_ in range(NBLK)]
        K.phase = "evA"
        phase_A(layer, 0, 0)
        for bi, (s, b) in enumerate(blocks):
            if True:
                t0 = b * 512
                K.phase = "evB"
                if STOP == "A":
                    st.close()
                    return
                for h in range(8):
                    pt, pB = fm_proj(C_Q + h * 64, 64)
                    evac_copy(h, qT[0:64, h, :], pt[0:64, 0:512], [pB], [qTB[h]], scale=0.125)
                for g in range(2):
                    pt, pB = fm_proj(C_KS + g * 64, 64)
                    evac_copy(g, ksT[0:64, g, t0:t0 + 512], pt[0:64, 0:512], [pB], [ksTB[g][b]])
                for g in range(2):
                    pt, pB = fm_proj(C_KW + g * 64, 64)
                    r = b % 2
                    evac_copy(g + 1, kwT[0:64, g, r * 512:(r + 1) * 512], pt[0:64, 0:512], [pB], [kwTB[g][r]])
                for i, (cX, XT, XTB) in enumerate(((C_KC, kcT, kcTB), (C_VC, vcT, vcTB))):
                    if b > 0:
                        K.op("pool", lambda e, XT=XT: e.tensor_copy(out=XT[:, 0:16], in_=XT[:, 512:528]), [XTB], [XTB])
                    pt, pB = fm_proj(cX, 128)
                    evac_copy(i, XT[:, 16:528], pt[:, 0:512], [pB], [XTB])
                if STOP == "B":
                    st.close()
                    return
                K.phase = "evC"
                for tt in range(4):
                    kt = b * 4 + tt
                    pt, pB = tm_proj(tt, C_VS, 408)
                    K.op("dve", lambda e, pt=pt, kt=kt: e.tensor_copy(out=vsA[:, kt, :, 0:64],
                                                                      in_=pt[:, 0:128].rearrange("p (g d) -> p g d", g=2)),
                         [pB], [vsAB[kt]])
                    K.op("dve", lambda e, pt=pt, kt=kt: e.tensor_copy(out=vwA[:, kt % 8, :, 0:64],
                                                                      in_=pt[:, 256:384].rearrange("p (g d) -> p g d", g=2)),
                         [pB], [vwAB[kt % 8]])
                    K.op("act", lambda e, pt=pt, tt=tt: e.activation(out=gates[:, tt, :], in_=pt[:, 384:408], func=AF.Sigmoid),
                         [pB], [gatesB[tt]])
                    pt, pB = tm_proj(tt, C_ZA, 512)
                    K.op("act", lambda e, pt=pt, tt=tt: e.activation(out=sza[:, tt, :], in_=pt[:, 0:512], func=AF.Silu),
                         [pB], [szaB[tt]])
                    pt, pB = tm_proj(tt, C_ZB, 512)
                    zt, ztB = szb.next()
                    K.op("act", lambda e, pt=pt, zt=zt: e.activation(out=zt[:, :], in_=pt[:, 0:512], func=AF.Silu), [pB], [ztB])
                    pt, pB = tm_proj(tt, C_U, 512)
                    ut, utB = ug.next()
                    K.op("act", lambda e, pt=pt, ut=ut: e.activation(out=ut[:, :], in_=pt[:, 0:512], func=AF.Gelu_apprx_tanh),
                         [pB], [utB])
                    pt, pB = tm_proj(tt, C_V, 512)
                    vt, vtB = vg.next()
                    K.op("act", lambda e, pt=pt, vt=vt: e.activation(out=vt[:, :], in_=pt[:, 0:512], func=AF.Gelu_apprx_tanh),
                         [pB], [vtB])
                    K.op("pool", lambda e, ut=ut, zt=zt: e.tensor_tensor(out=ut[:, :], in0=ut[:, :], in1=zt[:, :], op=ALU.mult),
                         [utB, ztB], [utB])
                    ls, lsB = lnst.next()
                    K.op("dve", lambda e, ls=ls, vt=vt: e.bn_stats(out=ls[:, 0:6], in_=vt[:, :]), [vtB], [lsB])
                    K.op("dve", lambda e, ls=ls: e.bn_aggr(out=ls[:, 6:8], in_=ls[:, 0:6]), [lsB], [lsB])
                    K.op("dve", lambda e, ls=ls: e.tensor_scalar(out=ls[:, 8:9], in0=ls[:, 7:8], scalar1=LN_EPS, scalar2=None,
                                                                op0=ALU.add), [lsB], [lsB])
                    K.op("pool", lambda e, ls=ls: e.tensor_tensor(out=ls[:, 9:10], in0=ls[:, 8:9], in1=neghalf[:, 0:1], op=ALU.pow),
                         [lsB, neghalfB], [lsB])
                    K.op("dve", lambda e, ls=ls, vt=vt: e.tensor_scalar(out=vt[:, :], in0=vt[:, :], scalar1=ls[:, 6:7],
                                                                        scalar2=ls[:, 9:10], op0=ALU.subtract, op1=ALU.mult),
                         [lsB, vtB], [vtB])
                    K.op("pool", lambda e, vt=vt: e.tensor_tensor(out=vt[:, :], in0=vt[:, :], in1=lng[:, :], op=ALU.mult),
                         [vtB, lngB], [vtB])
                    vl, vlB = vln.next()
                    K.op("pool", lambda e, vt=vt, vl=vl: e.tensor_tensor(out=vl[:, :], in0=vt[:, :], in1=lnb[:, :], op=ALU.add),
                         [vtB, lnbB], [vlB])
                    pt, pB = psB.next()
                    for g in range(8):
                        K.op("pe", lambda e, g=g, pt=pt, vl=vl: e.matmul(pt[:, g * 64:(g + 1) * 64], WsT[:, g, :],
                                                                          vl[:, g * 64:(g + 1) * 64], start=True, stop=True),
                             [WsTB, vlB], [pB], signal=(g == 7))
                    tb, tbB = tmpb.next()
                    K.op("dve", lambda e, pt=pt, tb=tb: e.tensor_tensor(
                        out=tb[:, :].rearrange("p (g d) -> p g d", g=8), in0=pt[:, 0:512].rearrange("p (g d) -> p g d", g=8),
                        in1=bs[:, :].unsqueeze(2).to_broadcast([128, 8, 64]), op=ALU.add), [pB, bsB], [tbB])
                    K.op("pool", lambda e, tb=tb, ut=ut: e.tensor_tensor(out=tb[:, :], in0=tb[:, :], in1=ut[:, :], op=ALU.mult),
                         [tbB, utB], [tbB])
                    pt, pB = psA.next()
                    pb = pt[:].bitcast(BF16)
                    for fc in range(4):
                        K.op("pe", lambda e, fc=fc, pb=pb, tb=tb: e.transpose(pb[:, fc * 128:(fc + 1) * 128],
                                                                              tb[:, fc * 128:(fc + 1) * 128], identb[:, :]),
                             [tbB, identbB], [pB], signal=(fc == 3))
                    K.op("dve", lambda e, pb=pb, tt=tt: e.tensor_copy(out=mixT[:, 4:8, tt * 128:(tt + 1) * 128],
                                                                      in_=pb[:, 0:512].rearrange("p (k t) -> p k t", k=4)),
                         [pB], [mixTB[tt]])
                if STOP == "C":
                    st.close()
                    return
                if bi + 1 < len(blocks):
                    K.phase = "evA"
                    phase_A(layer, blocks[bi + 1][0], blocks[bi + 1][1])
                elif layer + 1 < n_layers:
                    load_w_in(layer + 1)
                K.phase = "evD"
                if b == 0:
                    n0, nn, c0 = 0, 31, 16
                else:
                    n0, nn, c0 = 32 * b - 1, 32, 0
                nk = 32 * (b + 1) - 1
                for g in range(2):
                    for xi, (XT, XTB, w1, w1B, hid, hidB, hcol) in enumerate((
                            (kcT, kcTB, w1k, w1kB, hidk, hidkB, 0), (vcT, vcTB, w1v, w1vB, hidv, hidvB, n0))):
                        pt, pB = psA.next()
                        for l in range(32):
                            K.op("pe", lambda e, l=l, pt=pt, XT=XT, w1=w1: e.matmul(
                                pt[0:64, 0:nn], w1[g * 64:(g + 1) * 64, l, :],
                                XT[g * 64:(g + 1) * 64, c0 + l:c0 + l + 16 * (nn - 1) + 1:16], start=(l == 0), stop=(l == 31)),
                                 [w1B, XTB], [pB], signal=(l == 31))
                        K.op("act", lambda e, pt=pt, hid=hid, hcol=hcol, xi=xi: e.activation(
                            out=hid[:, g, hcol:hcol + nn], in_=pt[0:64, 0:nn], func=AF.Silu, bias=pebias[:, xi:xi + 1]),
                             [pB, pebiasB], [hidB])
                    pt, pB = psA.next()
                    K.op("pe", lambda e, pt=pt: e.matmul(pt[0:64, 0:nn], w2k[:, :], hidk[:, g, 0:nn], start=True, stop=True),
                         [w2kB, hidkB], [pB])
                    K.op("dve", lambda e, pt=pt: e.tensor_copy(out=kcmpT[:, g, n0:n0 + nn], in_=pt[0:64, 0:nn]), [pB], [kcmpTB])
                    pt, pB = psA.next()
                    K.op("pe", lambda e, pt=pt: e.matmul(pt[0:nk, 0:64], hidv[:, g, 0:nk], w2v[:, :], start=True, stop=True),
                         [w2vB, hidvB], [pB])
                    K.op("dve", lambda e, pt=pt: e.tensor_copy(out=vcmp[0:nk, g, 0:64], in_=pt[0:nk, 0:64]), [pB], [vcmpB])
                if STOP == "D":
                    st.close()
                    return
                K.phase = "evE"
                for g in range(2):
                    im, imB = imp.next()
                    for r in range(4):
                        h = g * 4 + r
                        pt, pB = psA.next()
                        K.op("pe", lambda e, pt=pt, h=h: e.matmul(pt[0:nk, 0:512], kcmpT[:, g, 0:nk], qT[0:64, h, :],
                                                                  start=True, stop=True), [kcmpTB, qTB[h]], [pB])
                        pT, pTB = pTs.next()
                        K.op("act", lambda e, pt=pt, pT=pT: e.activation(out=pT[0:nk, :], in_=pt[0:nk, 0:512], func=AF.Exp),
                             [pB], [pTB])
                        if STOP == "E1":
                            st.close()
                            return
                        K.op("pool", lambda e, pT=pT: e.affine_select(out=pT[0:nk, :], in_=pT[0:nk, :], pattern=[[1, 512]],
                                                                      compare_op=ALU.is_ge, fill=0.0, base=t0 - 31,
                                                                      channel_multiplier=-16), [pTB], [pTB])
                        if STOP == "E2":
                            st.close()
                            return
                        po, poB = psB.next()
                        po3 = po[:, 0:388].rearrange("p (t c) -> p t c", t=4)
                        for tt in range(4):
                            K.op("pe", lambda e, tt=tt, po=po, pT=pT: e.matmul(po[:, tt * 97:(tt + 1) * 97],
                                                                                pT[0:nk, tt * 128:(tt + 1) * 128], vcmp[0:nk, g, :],
                                                                                start=True, stop=True),
                                 [pTB, vcmpB], [poB], signal=(tt == 3))
                        if STOP == "E3":
                            st.close()
                            return
                        rc, rcB = rcs.next()
                        K.op("dve", lambda e, rc=rc, po3=po3: e.tensor_scalar(out=rc[:, 0:4], in0=po3[:, :, 64], scalar1=1e-30,
                                                                              scalar2=None, op0=ALU.max), [poB], [rcB])
                        K.op("dve", lambda e, rc=rc: e.reciprocal(out=rc[:, 4:8], in_=rc[:, 0:4]), [rcB], [rcB])
                        K.op("dve", lambda e, rc=rc, h=h: e.tensor_tensor(out=rc[:, 8:12], in0=rc[:, 4:8], in1=gates[:, :, h],
                                                                          op=ALU.mult), [rcB] + gatesB, [rcB])
                        for tt in range(4):
                            K.op("dve", lambda e, tt=tt, po3=po3, rc=rc, h=h: e.tensor_scalar(
                                out=acc_o[:, tt, h * 64:(h + 1) * 64], in0=po3[:, tt, 0:64], scalar1=rc[:, 8 + tt:9 + tt],
                                scalar2=None, op0=ALU.mult), [poB, rcB], [acc_oB[h]])
                        for tt in range(4):
                            if r == 0:
                                K.op("dve", lambda e, tt=tt, po3=po3, rc=rc, im=im: e.tensor_scalar(
                                    out=im[:, tt, :], in0=po3[:, tt, 65:97], scalar1=rc[:, 4 + tt:5 + tt], scalar2=None,
                                    op0=ALU.mult), [poB, rcB], [imB])
                            else:
                                K.op("dve", lambda e, tt=tt, po3=po3, rc=rc, im=im: e.scalar_tensor_tensor(
                                    out=im[:, tt, :], in0=po3[:, tt, 65:97], scalar=rc[:, 4 + tt:5 + tt], in1=im[:, tt, :],
                                    op0=ALU.mult, op1=ALU.add), [poB, rcB, imB], [imB])
                    if STOP == "E4":
                        st.close()
                        return
                    K.op("dve", lambda e, im=im: e.tensor_tensor(out=im[:, :, :], in0=im[:, :, :], in1=ctab[:, 4 * b:4 * b + 4, :],
                                                                 op=ALU.add), [imB, ctabB], [imB])
                    t8, t8B = top8.next()
                    for tt in range(4):
                        K.op("dve", lambda e, tt=tt, t8=t8, im=im: e.max(out=t8[:, tt, :], in_=im[:, tt, :]), [imB], [t8B])
                    for tt in range(4):
                        K.op("dve", lambda e, tt=tt, t8=t8, im=im: e.tensor_scalar(
                            out=selpad[:, tt, 64:96], in0=im[:, tt, :], scalar1=t8[:, tt, 7:8], scalar2=-1.0,
                            op0=ALU.is_ge, op1=ALU.add), [imB, t8B], [selpadB])
                    if STOP == "E6":
                        st.close()
                        return
                    pt, pB = psA.next()
                    pb = pt[:].bitcast(BF16)
                    for tt in range(4):
                        K.op("pe", lambda e, tt=tt, pb=pb: e.transpose(pb[0:96, tt * 128:(tt + 1) * 128], selpad[:, tt, :],
                                                                       identb[:, :]), [selpadB, identbB], [pB], signal=(tt == 3))
                    for r in range(4):
                        h = g * 4 + r
                        K.op("dve", lambda e, h=h, pb=pb: e.tensor_copy(out=qT[64:96, h, :], in_=pb[64:96, 0:512]), [pB], [qSB[h]])
                if STOP == "E":
                    st.close()
                    return
                K.phase = "evF"
                items = []
                for h in range(8):
                    for br in range(2):
                        kts = list(range(0, 4 * b + 4)) if br == 0 else list(range(max(0, 4 * b - 4), 4 * b + 4))
                        for kt in kts:
                            items.append((h, br, kt, kt == kts[0], kt == kts[-1]))
                LA = 2
                stage = {}
                accs = {}

                def front(idx):
                    h, br, kt, isfirst, islast = items[idx]
                    g = h // 4
                    d = kt - 4 * b
                    lo = max(0, d)
                    hi = 3 if br == 0 else min(3, d + 4)
                    c0_, c1_ = lo * 128, (hi + 1) * 128
                    pt, pB = psA.next()
                    if br == 0:
                        K.op("pe", lambda e: e.matmul(
                            pt[:, c0_:c1_], ksT[0:96, g, kt * 128:(kt + 1) * 128], qT[0:96, h, c0_:c1_],
                            start=True, stop=True), [ksTB[g][kt // 4], ebB, qTB[h], qSB[h]], [pB])
                    else:
                        kr = kt % 8
                        K.op("pe", lambda e: e.matmul(
                            pt[:, c0_:c1_], kwT[0:64, g, kr * 128:(kr + 1) * 128], qT[0:64, h, c0_:c1_],
                            start=True, stop=True), [kwTB[g][kr // 4], qTB[h]], [pB])
                    pT, pTB = pTs.next()
                    K.op("act", lambda e: e.activation(out=pT[:, c0_:c1_], in_=pt[:, c0_:c1_], func=AF.Exp), [pB], [pTB])
                    if d >= 0:
                        K.op("pool", lambda e: e.tensor_tensor(
                            out=pT[:, d * 128:(d + 1) * 128], in0=pT[:, d * 128:(d + 1) * 128], in1=trilk[:, :],
                            op=ALU.mult), [pTB, trilkB], [pTB])
                    if br == 1 and 0 <= d + 4 <= 3:
                        K.op("pool", lambda e: e.tensor_tensor(
                            out=pT[:, (d + 4) * 128:(d + 5) * 128], in0=pT[:, (d + 4) * 128:(d + 5) * 128],
                            in1=fark[:, :], op=ALU.mult), [pTB, farkB], [pTB])
                    stage[idx] = (pT, pTB, lo, hi)

                def back(idx):
                    h, br, kt, isfirst, islast = items[idx]
                    g = h // 4
                    pT, pTB, lo, hi = stage.pop(idx)
                    if isfirst:
                        accs[(h, br)] = psB.next()
                    acc, accB = accs[(h, br)]
                    for tt in range(lo, hi + 1):
                        if br == 0:
                            vB_, rhs = vsAB[kt], vsA[:, kt, g, :]
                        else:
                            vB_, rhs = vwAB[kt % 8], vwA[:, kt % 8, g, :]
                        K.op("pe", lambda e, tt=tt, rhs=rhs: e.matmul(
                            acc[:, tt * 65:(tt + 1) * 65], pT[:, tt * 128:(tt + 1) * 128], rhs,
                            start=(isfirst and tt == lo), stop=True, skip_group_check=True), [pTB, vB_], [accB], signal=(tt == hi))
                    if islast:
                        acc3 = acc[:, 0:260].rearrange("p (t c) -> p t c", t=4)
                        rc, rcB = rcs.next()
                        K.op("dve", lambda e: e.tensor_scalar(out=rc[:, 0:4], in0=acc3[:, :, 64], scalar1=1e-30,
                                                              scalar2=None, op0=ALU.max), [accB], [rcB])
                        K.op("dve", lambda e: e.reciprocal(out=rc[:, 4:8], in_=rc[:, 0:4]), [rcB], [rcB])
                        gi = (1 + br) * 8 + h
                        K.op("dve", lambda e: e.tensor_tensor(out=rc[:, 8:12], in0=rc[:, 4:8], in1=gates[:, :, gi],
                                                              op=ALU.mult), [rcB] + gatesB, [rcB])
                        for tt in range(4):
                            K.op("dve", lambda e, tt=tt: e.scalar_tensor_tensor(
                                out=acc_o[:, tt, h * 64:(h + 1) * 64], in0=acc3[:, tt, 0:64], scalar=rc[:, 8 + tt:9 + tt],
                                in1=acc_o[:, tt, h * 64:(h + 1) * 64], op0=ALU.mult, op1=ALU.add),
                                 [accB, rcB, acc_oB[h]], [acc_oB[h]])
                        del accs[(h, br)]

                for idx in range(len(items) + LA):
                    if idx < len(items):
                        front(idx)
                    if idx - LA >= 0:
                        back(idx - LA)
                for tt in range(4):
                    am, amB = amix.next()
                    K.op("pool", lambda e, tt=tt, am=am: e.tensor_tensor(out=am[:, :], in0=acc_o[:, tt, :], in1=sza[:, tt, :],
                                                                         op=ALU.mult), acc_oB + [szaB[tt]], [amB])
                    pt, pB = psA.next()
                    pb = pt[:].bitcast(BF16)
                    for fc in range(4):
                        K.op("pe", lambda e, fc=fc, pb=pb, am=am: e.transpose(pb[:, fc * 128:(fc + 1) * 128],
                                                                              am[:, fc * 128:(fc + 1) * 128], identb[:, :]),
                             [amB, identbB], [pB], signal=(fc == 3))
                    K.op("dve", lambda e, pb=pb, tt=tt: e.tensor_copy(out=mixT[:, 0:4, tt * 128:(tt + 1) * 128],
                                                                      in_=pb[:, 0:512].rearrange("p (k t) -> p k t", k=4)),
                         [pB], [mixTB[tt]])
                if STOP == "F":
                    st.close()
                    return
                K.phase = "evG"
                phase_G(layer, s, b)
                if STOP == "G":
                    st.close()
                    return
        st.close()

    def odd_layer(layer, li):
        st = ExitStack()

        def sbl(name, shape, dt=F32):
            t = st.enter_context(nc.sbuf_tensor(f"{name}_{layer}", list(shape), dt))
            return t, Buf(name)

        def sbln(name, shape, dt, n):
            return Rot([sbl(f"{name}{i}", shape, dt) for i in range(n)])

        yc = st.enter_context(nc.sbuf_tensor(f"yc_{layer}", [128, 8, 544], BF16))
        ycB = [Buf(f"yc{c}") for c in range(8)]
        szz = st.enter_context(nc.sbuf_tensor(f"szz_{layer}", [128, 8, 512], BF16))
        szzB = [Buf(f"szz{c}") for c in range(8)]
        yv = st.enter_context(nc.sbuf_tensor(f"yv_{layer}", [128, 8, 512], F32))
        yvB = [Buf(f"yv{c}") for c in range(8)]
        diag = sbln("diag", [128, 31, 128], BF16, 3)
        sig = sbln("sig", [128, 512], F32, 2)
        ybf = sbln("ybf", [128, 512], BF16, 3)
        ysq = sbln("ysq", [128, 512], BF16, 3)
        onesb, onesbB = sbl("onesb", [128, 128], BF16)
        mean, meanB = sbl("mean", [128, 512], F32)
        rstdt, rstdtB = sbl("rstdt", [128, 512], F32)
        var, varB = sbl("var", [128, 512], F32)
        t1 = sbln("t1", [128, 512], F32, 2)

        K.op("pool", lambda e: e.memset(onesb[:, :], 1.0 / D), [], [onesbB])

        blocks = [(s_, b_) for s_ in range(NSEQ) for b_ in range(NBLK)]
        K.phase = "odA"
        phase_A(layer, 0, 0)
        for bi, (s, b) in enumerate(blocks):
            if b == 0:
                for c in range(8):
                    K.op("pool", lambda e, c=c: e.memset(yc[:, c, 0:32], 0.0), [], [ycB[c]])
            if True:
                K.phase = "odProj"
                for c in range(8):
                    if b > 0:
                        K.op("pool", lambda e, c=c: e.tensor_copy(out=yc[:, c, 0:32], in_=yc[:, c, 512:544]), [ycB[c]], [ycB[c]])
                    pg, pgB = fm_proj(1024 + c * 128, 128)
                    sg, sgB = sig.next()
                    K.op("act", lambda e, pg=pg, sg=sg: e.activation(out=sg[:, :], in_=pg[:, 0:512], func=AF.Sigmoid), [pgB], [sgB])
                    pa, paB = fm_proj(c * 128, 128)
                    K.op("dve", lambda e, pa=pa, sg=sg, c=c: e.tensor_tensor(out=yc[:, c, 32:544], in0=pa[:, 0:512], in1=sg[:, :],
                                                                             op=ALU.mult), [paB, sgB], [ycB[c]])
                    pz, pzB = fm_proj(2048 + c * 128, 128)
                    K.op("act", lambda e, pz=pz, c=c: e.activation(out=szz[:, c, :], in_=pz[:, 0:512], func=AF.Silu), [pzB], [szzB[c]])
                if bi + 1 < len(blocks):
                    K.phase = "odA"
                    phase_A(layer, blocks[bi + 1][0], blocks[bi + 1][1])
                elif layer + 1 < n_layers:
                    load_w_in(layer + 1)
                K.phase = "odConv"
                pm, pmB = psB.next()
                pq, pqB = psB.next()
                dgs = {}

                def build_diag(c):
                    dg, dgB = diag.next()
                    K.op("pool" if c % 2 == 0 else "dve", lambda e: e.tensor_tensor(
                        out=dg[:, :, :], in0=identb[:, :].unsqueeze(1).to_broadcast([128, 31, 128]),
                        in1=par[:, c, 1:32].unsqueeze(2).to_broadcast([128, 31, 128]), op=ALU.mult), [identbB, parB], [dgB])
                    dgs[c] = (dg, dgB)

                for c in range(3):
                    build_diag(c)
                pend_stats = []

                def emit_stats():
                    c_, yb_, ybB_, yq_, yqB_ = pend_stats.pop(0)
                    K.op("pe", lambda e: e.matmul(pm[:, 0:512], onesb[:, :], yb_[:, :], start=(c_ == 0), stop=(c_ == 7)),
                         [onesbB, ybB_], [pmB], signal=(c_ == 7))
                    K.op("pe", lambda e: e.matmul(pq[:, 0:512], onesb[:, :], yq_[:, :], start=(c_ == 0), stop=(c_ == 7)),
                         [onesbB, yqB_], [pqB], signal=(c_ == 7))

                for c in range(8):
                    dg, dgB = dgs[c]
                    pc, pcB = psA.next()
                    for k in range(31):
                        K.op("pe", lambda e, k=k, pc=pc, dg=dg, c=c: e.matmul(pc[:, 0:512], dg[:, k, :], yc[:, c, 2 + k:2 + k + 512],
                                                                              start=(k == 0), stop=(k == 30)),
                             [dgB, ycB[c]], [pcB], signal=(k == 30))
                    if c + 3 < 8:
                        build_diag(c + 3)
                    if pend_stats:
                        emit_stats()
                    K.op("act", lambda e, pc=pc, c=c: e.activation(out=yv[:, c, :], in_=pc[:, 0:512], func=AF.Identity,
                                                                   bias=par[:, c, 32:33]), [pcB, parB], [yvB[c]])
                    yb, ybB = ybf.next()
                    yq, yqB = ysq.next()
                    K.op("dve", lambda e, yb=yb, c=c: e.tensor_copy(out=yb[:, :], in_=yv[:, c, :]), [yvB[c]], [ybB])
                    K.op("pool", lambda e, yq=yq, c=c: e.tensor_tensor(out=yq[:, :], in0=yv[:, c, :], in1=yv[:, c, :], op=ALU.mult),
                         [yvB[c]], [yqB])
                    pend_stats.append((c, yb, ybB, yq, yqB))
                while pend_stats:
                    emit_stats()
                K.phase = "odLN"
                K.op("act", lambda e, pm=pm: e.copy(out=mean[:, :], in_=pm[:, 0:512]), [pmB], [meanB])
                K.op("dve", lambda e: e.tensor_tensor(out=var[:, :], in0=mean[:, :], in1=mean[:, :], op=ALU.mult), [meanB], [varB])
                K.op("dve", lambda e, pq=pq: e.scalar_tensor_tensor(out=var[:, :], in0=pq[:, 0:512], scalar=LN_EPS, in1=var[:, :],
                                                                    op0=ALU.add, op1=ALU.subtract), [pqB, varB], [varB])
                K.op("act", lambda e: e.activation(out=var[:, :], in_=var[:, :], func=AF.Sqrt), [varB], [varB])
                K.op("dve", lambda e: e.reciprocal(out=rstdt[:, :], in_=var[:, :]), [varB], [rstdtB])
                for c in range(8):
                    ta, taB = t1.next()
                    K.op("dve", lambda e, ta=ta, c=c: e.tensor_tensor(out=ta[:, :], in0=yv[:, c, :], in1=mean[:, :], op=ALU.subtract),
                         [yvB[c], meanB], [taB])
                    K.op("pool", lambda e, ta=ta: e.tensor_tensor(out=ta[:, :], in0=ta[:, :], in1=rstdt[:, :], op=ALU.mult),
                         [taB, rstdtB], [taB])
                    K.op("act", lambda e, ta=ta, c=c: e.activation(out=ta[:, :], in_=ta[:, :], func=AF.Silu,
                                                                   scale=par[:, c, 33:34], bias=par[:, c, 34:35]),
                         [taB, parB], [taB])
                    K.op("dve", lambda e, ta=ta, c=c: e.tensor_tensor(out=mixT[:, c, :], in0=ta[:, :], in1=szz[:, c, :], op=ALU.mult),
                         [taB, szzB[c]], mixTB)
                K.phase = "odG"
                phase_G(layer, s, b)
        st.close()

    load_w_in(0)
    for layer in range(n_layers):
        li = layer // 2
        load_common(layer)
        if layer > 0:
            K.barrier()
        if layer % 2 == 0:
            even_layer(layer, li)
        else:
            odd_layer(layer, li)

    for s in range(NSEQ):
        for t in range(16):
            b = xd[s][t]
            if b.w is not None:
                sm, v = b.w
                nc.sync.wait_ge(sm.h, v)
    stack.close()
    return nc, K


def make_consts():
    p = np.arange(128)
    ident = np.eye(128, dtype=np.float32)
    tril = (p[:, None] <= p[None, :]).astype(np.float32)
    far = (p[:, None] > p[None, :]).astype(np.float32)
    ctab = np.zeros((128, 16, 32), np.float32)
    j = np.arange(32)
    for tile in range(16):
        t = tile * 128 + p
        cur = t // 64
        forced = (j[None, :] == 0) | (j[None, :] == cur[:, None]) | (j[None, :] == cur[:, None] - 1)
        causal = j[None, :] <= cur[:, None]
        ctab[:, tile, :] = np.where(causal, forced.astype(np.float32) * np.float32(1e4), np.float32(-1e30))
    ebig = ((np.arange(2048)[None, :] // 64) == j[:, None]).astype(np.float32) * np.float32(BIG)
    n_cmp = 127
    tok = np.arange(n_cmp)[:, None] * 16 + np.arange(32)[None, :]
    ovl = ((tok[:, :, None] // 64) == np.arange(32)[None, None, :]).sum(1).astype(np.float32) / np.float32(32)
    return {"c_ident": ident, "c_tril": tril, "c_far": far, "c_ctab": ctab, "c_ebig": ebig, "c_ovl": ovl}


_CACHE = {}


def kernel(**inputs):
    if "nc" not in _CACHE:
        _CACHE["nc"] = build_program(4)[0]
    nc = _CACHE["nc"]
    consts = make_consts()
    x = np.ascontiguousarray(inputs["x"], dtype=np.float32)
    shared = {k: np.ascontiguousarray(v, dtype=np.float32) for k, v in inputs.items() if k != "x"}
    shared.update(consts)
    in_maps = []
    for c in range(NCORES):
        m = dict(shared)
        m["x"] = x[c * NSEQ:(c + 1) * NSEQ]
        in_maps.append(m)
    res = run_bass_kernel_spmd(nc, in_maps, core_ids=list(range(NCORES)))
    return np.concatenate([r["out"] for r in res.results], axis=0).astype(np.float32)
```

```python
from contextlib import ExitStack

import numpy as np
import concourse.bass as bass
import concourse.mybir as mybir
from concourse.bass_utils import run_bass_kernel_spmd

F32 = mybir.dt.float32
BF16 = mybir.dt.bfloat16
AF = mybir.ActivationFunctionType
ALU = mybir.AluOpType

NCORES = 8
SEQ = 2048
D = 1024
NSEQ = 2
NBLK = 4
EVEN_W = 3352
ODD_W = 3072
RMS_EPS = 1e-6
LN_EPS = 1e-5
BIG = 30000.0
import os as _os
STOP = _os.environ.get("KSTOP", "")

C_Q, C_KC, C_VC, C_KS, C_VS, C_KW, C_VW, C_GATE, C_ZA, C_U, C_V, C_ZB = (
    0, 512, 640, 768, 896, 1024, 1152, 1280, 1304, 1816, 2328, 2840)


class Sem:
    __slots__ = ("h", "val", "name")

    def __init__(self, h, name=""):
        self.h = h
        self.val = 0
        self.name = name


class Buf:
    __slots__ = ("name", "w", "r", "dsem")

    def __init__(self, name):
        self.name = name
        self.w = None
        self.r = {}
        self.dsem = None


class Trk:
    def __init__(self, nc, stack):
        self.nc = nc
        self.stack = stack
        self.eng = {"pe": nc.tensor, "act": nc.scalar, "dve": nc.vector, "pool": nc.gpsimd, "sp": nc.sync}
        self.nsem = 0
        self.esem = {e: self.newsem("e_" + e) for e in self.eng}
        self.waited = {e: {} for e in self.eng}
        self.nops = 0
        self.dsems = []
        self.phase = ""
        self.waitlog = {e: [] for e in self.eng}

    def newsem(self, name):
        self.nsem += 1
        return Sem(self.stack.enter_context(self.nc.semaphore(f"{name}_{self.nsem}")), name)

    def _sync(self, E, reads, writes):
        deps = {}
        for b in reads:
            if b.w is not None:
                s, v = b.w
                if deps.get(s, 0) < v:
                    deps[s] = v
        for b in writes:
            if b.w is not None:
                s, v = b.w
                if deps.get(s, 0) < v:
                    deps[s] = v
            for s, v in b.r.items():
                if deps.get(s, 0) < v:
                    deps[s] = v
        eng = self.eng[E]
        w = self.waited[E]
        own = self.esem[E]
        for s, v in deps.items():
            if E == "pe" and s is own:
                continue
            if w.get(s, 0) < v:
                eng.wait_ge(s.h, v)
                w[s] = v
                self.waitlog[E].append((self.phase, s.name))

    def op(self, E, fn, reads=(), writes=(), signal=True):
        self._sync(E, reads, writes)
        ins = fn(self.eng[E])
        s = self.esem[E]
        if signal:
            s.val += 1
            ins.then_inc(s.h, 1)
            tag = (s, s.val)
        else:
            tag = (s, s.val + 1)
        for b in reads:
            if b.r.get(s, 0) < tag[1]:
                b.r[s] = tag[1]
        for b in writes:
            b.w = tag
            b.r = {}
        self.nops += 1
        return ins

    def barrier(self):
        sems = list(self.esem.values()) + self.dsems
        for E, eng in self.eng.items():
            w = self.waited[E]
            for s in sems:
                if s.val > 0 and w.get(s, 0) < s.val:
                    eng.wait_ge(s.h, s.val)
                    w[s] = s.val

    def dma(self, E, out, in_, reads, writes, dbuf, **kw):
        self._sync(E, reads, writes)
        if dbuf.dsem is None:
            dbuf.dsem = self.newsem("d_" + dbuf.name)
            self.dsems.append(dbuf.dsem)
        s = dbuf.dsem
        ins = self.eng[E].dma_start(out=out, in_=in_, **kw)
        s.val += 16
        ins.then_inc(s.h, 16)
        tag = (s, s.val)
        for b in reads:
            if b.r.get(s, 0) < tag[1]:
                b.r[s] = tag[1]
        for b in writes:
            b.w = tag
            b.r = {}
        self.nops += 1
        return ins


class Rot:
    def __init__(self, items):
        self.items = items
        self.i = 0

    def next(self):
        it = self.items[self.i % len(self.items)]
        self.i += 1
        return it


def build_program(n_layers=4):
    nc = bass.Bass("TRN2", target_bir_lowering=False, dynamic_dma_scratch_size=16384)
    stack = ExitStack()
    K = Trk(nc, stack)

    def dram(name, shape, dt=F32, kind="ExternalInput"):
        return nc.dram_tensor(name, list(shape), dt, kind=kind).ap()

    x_in = dram("x", [NSEQ, SEQ, D])
    out = dram("out", [NSEQ, SEQ, D], kind="ExternalOutput")
    norm_pre = dram("norm_pre", [4, D])
    norm_post = dram("norm_post", [4, D])
    e_w_in = dram("even_w_in", [2, D, EVEN_W])
    e_k_pe = dram("even_cmp_k_pe", [2, 32, 64])
    e_k_w1 = dram("even_cmp_k_w1", [2, 2048, 64])
    e_k_w2 = dram("even_cmp_k_w2", [2, 64, 64])
    e_v_pe = dram("even_cmp_v_pe", [2, 32, 64])
    e_v_w1 = dram("even_cmp_v_w1", [2, 2048, 64])
    e_v_w2 = dram("even_cmp_v_w2", [2, 64, 64])
    e_ln_g = dram("even_sgu_ln_g", [2, 512])
    e_ln_b = dram("even_sgu_ln_b", [2, 512])
    e_sgu_w = dram("even_sgu_w", [2, 8, 128, 128])
    e_sgu_b = dram("even_sgu_b", [2, 8, 128])
    e_w_out = dram("even_w_out", [2, D, D])
    o_w_in = dram("odd_w_in", [2, D, ODD_W])
    o_dw_w = dram("odd_dw_w", [2, 31, D])
    o_dw_b = dram("odd_dw_b", [2, D])
    o_ln_g = dram("odd_ln_g", [2, D])
    o_ln_b = dram("odd_ln_b", [2, D])
    o_w_out = dram("odd_w_out", [2, D, D])
    c_ident = dram("c_ident", [128, 128])
    c_tril = dram("c_tril", [128, 128])
    c_far = dram("c_far", [128, 128])
    c_ctab = dram("c_ctab", [128, 16, 32])
    c_ebig = dram("c_ebig", [32, 2048])
    c_ovl = dram("c_ovl", [127, 32])

    def sb(name, shape, dt=F32):
        t = stack.enter_context(nc.sbuf_tensor(name, list(shape), dt))
        return t, Buf(name)

    def sbn(name, shape, dt, n):
        return Rot([sb(f"{name}{i}", shape, dt) for i in range(n)])

    w_in = stack.enter_context(nc.sbuf_tensor("w_in", [128, 8, EVEN_W], BF16))
    w_inB = [Buf(f"w_in{k}") for k in range(8)]
    w_out = stack.enter_context(nc.sbuf_tensor("w_out", [128, 8, D], BF16))
    w_outB = [Buf(f"w_out{k}") for k in range(8)]
    identb, identbB = sb("identb", [128, 128], BF16)
    identf, identfB = sb("identf", [128, 128], F32)
    trilk, trilkB = sb("trilk", [128, 128], BF16)
    fark, farkB = sb("fark", [128, 128], BF16)
    neghalf, neghalfB = sb("neghalf", [128, 8], F32)
    gpost, gpostB = sb("gpost", [128, D], F32)
    xa = sbn("xa", [128, D], F32, 2)
    xr = sbn("xr", [128, D], F32, 2)
    hp = sbn("hp", [128, D], BF16, 2)
    hT = stack.enter_context(nc.sbuf_tensor("hT", [128, 8, 512], BF16))
    hTB = [Buf(f"hT{t}") for t in range(4)]
    mixT = stack.enter_context(nc.sbuf_tensor("mixT", [128, 8, 512], BF16))
    mixTB = [Buf(f"mixT{t}") for t in range(4)]
    junks = sbn("junk", [128, D], BF16, 2)
    ss, _ = sb("ss", [128, 8], F32)
    ssB = [Buf(f"ss{t}") for t in range(8)]
    ms, _ = sb("ms", [128, 8], F32)
    msB = [Buf(f"ms{t}") for t in range(8)]
    rstd, _ = sb("rstd", [128, 8], F32)
    rstdB = [Buf(f"rstd{t}") for t in range(8)]
    ssy = Rot([sb(f"ssy{i}", [128, 4], F32) + (Buf(f"ssy{i}a"), Buf(f"ssy{i}b")) for i in range(2)])
    rsy = sbn("rsy", [128, 4], F32, 2)
    par, parB = sb("par", [128, 8, 36], F32)
    tmpf = sbn("tmpf", [128, 512], F32, 4)

    PS = []
    for i in range(8):
        t = stack.enter_context(nc.psum_tensor(f"ps{i}", [128, 512], F32))
        PS.append((t, Buf(f"ps{i}")))
    psA = Rot(PS[0:4])
    psB = Rot(PS[4:8])

    xd = [[Buf(f"xd{s}_{t}") for t in range(16)] for s in range(NSEQ)]

    K.dma("pool", identb[:, :], c_ident[:, :], [], [identbB], identbB)
    K.dma("sp", identf[:, :], c_ident[:, :], [], [identfB], identfB)
    K.dma("pool", trilk[:, :], c_tril[:, :], [], [trilkB], trilkB)
    K.dma("pool", fark[:, :], c_far[:, :], [], [farkB], farkB)
    K.op("dve", lambda e: e.memset(neghalf[:, :], -0.5), [], [neghalfB])

    def layer_srcs(layer):
        li = layer // 2
        if layer % 2 == 0:
            return e_w_in[li], EVEN_W, e_w_out[li], []
        return (o_w_in[li], ODD_W, o_w_out[li],
                [(o_dw_w[li], 31), (o_dw_b[li:li + 1, :], 1), (o_ln_g[li:li + 1, :], 1), (o_ln_b[li:li + 1, :], 1)])

    def load_w_in(layer):
        w_in_dram, w_in_cols, _, _ = layer_srcs(layer)
        for kc in range(8):
            K.dma("pool", w_in[:, kc, 0:w_in_cols], w_in_dram[kc * 128:(kc + 1) * 128, :], [], [w_inB[kc]], w_inB[kc],
                  max_dma_last_dim=4096)

    def load_common(layer):
        _, _, w_out_dram, rows = layer_srcs(layer)
        stg, stgB = xa.next()
        rows = [(norm_pre[layer:layer + 1, :], 1)] + rows
        if sum(r for _, r in rows) % 2:
            rows = rows + [(norm_post[layer:layer + 1, :], 1)]
        r0 = 0
        for ap, r in rows:
            K.dma("sp", stg[r0:r0 + r, :], ap, [], [stgB], stgB)
            r0 += r
        R = r0
        for c in range(8):
            pt, pB = psA.next()
            K.op("pe", lambda e, c=c, pt=pt: e.transpose(pt[:, 0:R], stg[0:R, c * 128:(c + 1) * 128], identf[0:R, 0:R]),
                 [stgB, identfB], [pB])
            K.op("dve", lambda e, c=c, pt=pt: e.tensor_copy(out=par[:, c, 0:R], in_=pt[:, 0:R]), [pB], [parB])
        for kc in range(8):
            K.dma("pool", w_out[:, kc, :], w_out_dram[kc * 128:(kc + 1) * 128, :], [], [w_outB[kc]], w_outB[kc],
                  max_dma_last_dim=4096)
        K.dma("sp", gpost[:, :], norm_post[layer:layer + 1, :].to_broadcast([128, D]), [], [gpostB], gpostB)

    def phase_A(layer, s, b):
        src = x_in if layer == 0 else out
        xts = []
        for tt in range(4):
            xt, xB = xa.next()
            tile = b * 4 + tt
            K.dma("sp", xt[:, :], src[s, tile * 128:(tile + 1) * 128, :], [xd[s][tile]], [xB], xB)
            jk, jkB = junks.next()
            K.op("act", lambda e, xt=xt, tt=tt, jk=jk: e.activation(out=jk[:, :], in_=xt[:, :], func=AF.Square,
                                                                    accum_out=ss[:, tt:tt + 1]), [xB], [ssB[tt], jkB])
            K.op("dve", lambda e, tt=tt: e.tensor_scalar(out=ms[:, tt:tt + 1], in0=ss[:, tt:tt + 1], scalar1=1.0 / D,
                                                        scalar2=RMS_EPS, op0=ALU.mult, op1=ALU.add), [ssB[tt]], [msB[tt]])
            K.op("pool", lambda e, tt=tt: e.tensor_tensor(out=rstd[:, tt:tt + 1], in0=ms[:, tt:tt + 1],
                                                          in1=neghalf[:, 0:1], op=ALU.pow), [msB[tt], neghalfB], [rstdB[tt]])
            ht, hB = hp.next()
            K.op("dve", lambda e, ht=ht, xt=xt, tt=tt: e.tensor_scalar(out=ht[:, :], in0=xt[:, :], scalar1=rstd[:, tt:tt + 1],
                                                                       scalar2=None, op0=ALU.mult), [xB, rstdB[tt]], [hB])
            pt, pB = psA.next()
            pb = pt[:].bitcast(BF16)
            for kc in range(8):
                K.op("pe", lambda e, kc=kc, pb=pb, ht=ht: e.transpose(pb[:, kc * 128:(kc + 1) * 128],
                                                                      ht[:, kc * 128:(kc + 1) * 128], identb[:, :]),
                     [hB, identbB], [pB], signal=(kc == 7))
            K.op("dve", lambda e, pb=pb, tt=tt: e.tensor_tensor(
                out=hT[:, :, tt * 128:(tt + 1) * 128], in0=pb.rearrange("p (k t) -> p k t", k=8),
                in1=par[:, :, 0:1].to_broadcast([128, 8, 128]), op=ALU.mult), [pB, parB], [hTB[tt]])

    def fm_proj(col0, M):
        pt, pB = psA.next()
        for kc in range(8):
            K.op("pe", lambda e, kc=kc, pt=pt: e.matmul(pt[0:M, 0:512], w_in[:, kc, col0:col0 + M], hT[:, kc, :],
                                                        start=(kc == 0), stop=(kc == 7)),
                 [w_inB[kc]] + hTB, [pB], signal=(kc == 7))
        return pt, pB

    def tm_proj(tt, col0, N):
        pt, pB = psA.next()
        for kc in range(8):
            K.op("pe", lambda e, kc=kc, pt=pt: e.matmul(pt[:, 0:N], hT[:, kc, tt * 128:(tt + 1) * 128],
                                                        w_in[:, kc, col0:col0 + N], start=(kc == 0), stop=(kc == 7)),
                 [w_inB[kc], hTB[tt]], [pB], signal=(kc == 7))
        return pt, pB

    def phase_G(layer, s, b):
        src = x_in if layer == 0 else out
        for tt in range(4):
            tile = b * 4 + tt
            xt, xB = xr.next()
            K.dma("sp", xt[:, :], src[s, tile * 128:(tile + 1) * 128, :], [xd[s][tile]], [xB], xB)
            halves = []
            sy, syB, syB0, syB1 = ssy.next()
            ry, ryB = rsy.next()
            for hf in range(2):
                pt, pB = psB.next()
                for fc in range(8):
                    K.op("pe", lambda e, fc=fc, pt=pt, hf=hf: e.matmul(pt[:, 0:512], mixT[:, fc, tt * 128:(tt + 1) * 128],
                                                                        w_out[:, fc, hf * 512:(hf + 1) * 512],
                                                                        start=(fc == 0), stop=(fc == 7)),
                         [mixTB[tt], w_outB[fc]], [pB], signal=(fc == 7))
                halves.append((pt, pB))
            ysbs = []
            for hf, sB_ in ((0, syB0), (1, syB1)):
                pt, pB = halves[hf]
                tf, tfB = tmpf.next()
                if hf == 0:
                    K.op("act", lambda e, pt=pt, tf=tf: e.copy(out=tf[:, :], in_=pt[:, 0:512]), [pB], [tfB])
                else:
                    K.op("dve", lambda e, pt=pt, tf=tf: e.tensor_copy(out=tf[:, :], in_=pt[:, 0:512]), [pB], [tfB])
                jk, jkB = junks.next()
                K.op("act", lambda e, tf=tf, hf=hf, sy=sy, jk=jk: e.activation(out=jk[:, 0:512], in_=tf[:, :], func=AF.Square,
                                                                               accum_out=sy[:, hf:hf + 1]), [tfB], [sB_, jkB])
                ysbs.append((tf, tfB))
            K.op("dve", lambda e, sy=sy: e.tensor_tensor(out=sy[:, 2:3], in0=sy[:, 0:1], in1=sy[:, 1:2], op=ALU.add),
                 [syB0, syB1, syB], [syB])
            K.op("dve", lambda e, sy=sy: e.tensor_scalar(out=sy[:, 3:4], in0=sy[:, 2:3], scalar1=1.0 / D, scalar2=RMS_EPS,
                                                        op0=ALU.mult, op1=ALU.add), [syB], [syB])
            K.op("pool", lambda e, sy=sy, ry=ry: e.tensor_tensor(out=ry[:, 0:1], in0=sy[:, 3:4], in1=neghalf[:, 0:1],
                                                                 op=ALU.pow), [syB, neghalfB], [ryB])
            for hf in range(2):
                tf, tfB = ysbs[hf]
                K.op("dve", lambda e, tf=tf, hf=hf, ry=ry: e.scalar_tensor_tensor(
                    out=tf[:, :], in0=tf[:, :], scalar=ry[:, 0:1], in1=gpost[:, hf * 512:(hf + 1) * 512],
                    op0=ALU.mult, op1=ALU.mult), [tfB, ryB, gpostB], [tfB])
                K.op("pool", lambda e, tf=tf, xt=xt, hf=hf: e.tensor_tensor(
                    out=xt[:, hf * 512:(hf + 1) * 512], in0=xt[:, hf * 512:(hf + 1) * 512], in1=tf[:, :], op=ALU.add),
                     [tfB, xB], [xB])
            K.dma("sp", out[s, tile * 128:(tile + 1) * 128, :], xt[:, :], [xB], [xd[s][tile]], xd[s][tile])

    def even_layer(layer, li):
        st = ExitStack()

        def sbl(name, shape, dt=F32):
            t = st.enter_context(nc.sbuf_tensor(f"{name}_{layer}", list(shape), dt))
            return t, Buf(name)

        def sbln(name, shape, dt, n):
            return Rot([sbl(f"{name}{i}", shape, dt) for i in range(n)])

        qT = st.enter_context(nc.sbuf_tensor(f"qT_{layer}", [128, 8, 512], BF16))
        qTB = [Buf(f"qT{h}") for h in range(8)]
        qSB = [Buf(f"qS{h}") for h in range(8)]
        ksT = st.enter_context(nc.sbuf_tensor(f"ksT_{layer}", [128, 2, 2048], BF16))
        ksTB = [[Buf(f"ksT{g}_{b}") for b in range(4)] for g in range(2)]
        ebB = Buf("ebig")
        kwT = st.enter_context(nc.sbuf_tensor(f"kwT_{layer}", [64, 2, 1024], BF16))
        kwTB = [[Buf(f"kwT{g}_{r}") for r in range(2)] for g in range(2)]
        kcT, kcTB = sbl("kcT", [128, 528], BF16)
        vcT, vcTB = sbl("vcT", [128, 528], BF16)
        vsA = st.enter_context(nc.sbuf_tensor(f"vsA_{layer}", [128, 16, 2, 65], BF16))
        vsAB = [Buf(f"vsA{k}") for k in range(16)]
        vwA = st.enter_context(nc.sbuf_tensor(f"vwA_{layer}", [128, 8, 2, 65], BF16))
        vwAB = [Buf(f"vwA{k}") for k in range(8)]
        w1k, w1kB = sbl("w1k", [128, 32, 64], BF16)
        w1v, w1vB = sbl("w1v", [128, 32, 64], BF16)
        w2k, w2kB = sbl("w2k", [64, 64], BF16)
        w2v, w2vB = sbl("w2v", [64, 64], BF16)
        pek, pekB = sbl("pek", [32, 64], BF16)
        pev, pevB = sbl("pev", [32, 64], BF16)
        peT, peTB = sbl("peT", [64, 2, 32], BF16)
        pebias, pebiasB = sbl("pebias", [64, 2], F32)
        hidk, hidkB = sbl("hidk", [64, 2, 32], BF16)
        hidv, hidvB = sbl("hidv", [64, 2, 128], BF16)
        kcmpT, kcmpTB = sbl("kcmpT", [64, 2, 128], BF16)
        vcmp, vcmpB = sbl("vcmp", [128, 2, 97], BF16)
        ctab, ctabB = sbl("ctab", [128, 16, 32], F32)
        WsT, WsTB = sbl("WsT", [128, 8, 128], BF16)
        bs, bsB = sbl("bs", [128, 8], F32)
        lng, lngB = sbl("lng", [128, 512], F32)
        lnb, lnbB = sbl("lnb", [128, 512], F32)
        gates = st.enter_context(nc.sbuf_tensor(f"gates_{layer}", [128, 4, 24], F32))
        gatesB = [Buf(f"gates{t}") for t in range(4)]
        sza = st.enter_context(nc.sbuf_tensor(f"sza_{layer}", [128, 4, 512], BF16))
        szaB = [Buf(f"sza{t}") for t in range(4)]
        ug = sbln("ug", [128, 512], BF16, 2)
        szb = sbln("szb", [128, 512], BF16, 2)
        vg = sbln("vg", [128, 512], F32, 2)
        vln = sbln("vln", [128, 512], BF16, 2)
        lnst = sbln("lnst", [128, 16], F32, 2)
        acc_o = st.enter_context(nc.sbuf_tensor(f"acc_o_{layer}", [128, 4, 512], F32))
        acc_oB = [Buf(f"acc_o{h}") for h in range(8)]
        imp = sbln("imp", [128, 4, 32], F32, 2)
        top8 = sbln("top8", [128, 4, 8], F32, 2)
        selpad, selpadB = sbl("selpad", [128, 4, 96], BF16)
        pTs = sbln("pT", [128, 512], BF16, 4)
        rcs = sbln("rc", [128, 16], F32, 4)
        amix = sbln("amix", [128, 512], BF16, 2)
        tmpb = sbln("tmpb", [128, 512], BF16, 2)

        for g in range(2):
            K.dma("pool", ksT[64:96, g, :], c_ebig[:, :], [], [ebB], ebB, max_dma_last_dim=4096)
        for (w1, w1B, src) in ((w1k, w1kB, e_k_w1), (w1v, w1vB, e_v_w1)):
            for half in range(2):
                K.dma("pool", w1[half * 64:(half + 1) * 64, :, :], src[li].rearrange("(l d) e -> d l e", d=64), [], [w1B], w1B)
        K.dma("pool", w2k[:, :], e_k_w2[li], [], [w2kB], w2kB)
        K.dma("pool", w2v[:, :], e_v_w2[li], [], [w2vB], w2vB)
        K.dma("pool", pek[:, :], e_k_pe[li], [], [pekB], pekB)
        K.dma("pool", pev[:, :], e_v_pe[li], [], [pevB], pevB)
        K.dma("sp", ctab[:, :, :], c_ctab[:, :, :], [], [ctabB], ctabB)
        wsst_t, wsstB = xa.next()
        wsst = wsst_t[:, :].rearrange("p (g j) -> p g j", g=8)
        K.dma("sp", wsst, e_sgu_w[li].rearrange("g i j -> i g j"), [], [wsstB], wsstB)
        bst, bstB = xa.next()
        K.dma("sp", bst[0:8, 0:128], e_sgu_b[li], [], [bstB], bstB)
        pt, pB = psA.next()
        K.op("pe", lambda e: e.transpose(pt[:, 0:8], bst[0:8, 0:128], identf[0:8, 0:8]), [bstB, identfB], [pB])
        K.op("dve", lambda e: e.tensor_copy(out=bs[:, :], in_=pt[:, 0:8]), [pB], [bsB])
        K.dma("sp", lng[:, :], e_ln_g[li:li + 1, :].to_broadcast([128, 512]), [], [lngB], lngB)
        K.dma("sp", lnb[:, :], e_ln_b[li:li + 1, :].to_broadcast([128, 512]), [], [lnbB], lnbB)
        K.op("pool", lambda e: e.memset(vsA[:, :, :, 64:65], 1.0), [], vsAB)
        K.op("pool", lambda e: e.memset(vwA[:, :, :, 64:65], 1.0), [], vwAB)
        K.op("pool", lambda e: e.memset(vcmp[:, :, :], 0.0), [], [vcmpB])
        K.op("pool", lambda e: e.memset(vcmp[:, :, 64:65], 1.0), [vcmpB], [vcmpB])
        for g in range(2):
            K.dma("pool", vcmp[0:127, g, 65:97], c_ovl[:, :], [], [vcmpB], vcmpB)
        K.op("pool", lambda e: e.memset(hidv[:, :, :], 0.0), [], [hidvB])
        K.op("pool", lambda e: e.memset(kcmpT[:, :, :], 0.0), [], [kcmpTB])
        K.op("pool", lambda e: e.memset(selpad[:, :, :], 0.0), [], [selpadB])
        K.op("dve", lambda e: e.memset(kcT[:, 0:16], 0.0), [], [kcTB])
        K.op("dve", lambda e: e.memset(vcT[:, 0:16], 0.0), [], [vcTB])
        for g in range(8):
            pt, pB = psA.next()
            K.op("pe", lambda e, g=g, pt=pt: e.transpose(pt[:, 0:128], wsst[:, g, :], identf[:, :]), [wsstB, identfB], [pB])
            K.op("dve", lambda e, g=g, pt=pt: e.tensor_tensor(out=WsT[:, g, :], in0=pt[:, 0:128], in1=trilk[:, :], op=ALU.mult),
                 [pB, trilkB], [WsTB])
        for xi, (pe_, peB_, w1, w1B) in enumerate(((pek, pekB, w1k, w1kB), (pev, pevB, w1v, w1vB))):
            pt, pB = psA.next()
            pb = pt[:].bitcast(BF16)
            K.op("pe", lambda e, pb=pb, pe_=pe_: e.transpose(pb[0:64, 0:32], pe_[:, :], identb[0:32, 0:32]), [peB_, identbB], [pB])
            K.op("dve", lambda e, pb=pb, xi=xi: e.tensor_copy(out=peT[:, xi, :], in_=pb[0:64, 0:32]), [pB], [peTB])
            pt2, pB2 = psA.next()
            for l in range(32):
                K.op("pe", lambda e, l=l, pt2=pt2, w1=w1, xi=xi: e.matmul(pt2[0:64, 0:1], w1[0:64, l, :], peT[:, xi, l:l + 1],
                                                                          start=(l == 0), stop=(l == 31)),
                     [w1B, peTB], [pB2], signal=(l == 31))
            K.op("dve", lambda e, pt2=pt2, xi=xi: e.tensor_copy(out=pebias[:, xi:xi + 1], in_=pt2[0:64, 0:1]), [pB2], [pebiasB])

        def evac_copy(i, out_ap, in_ap, reads, writes, scale=None):
            if i % 2 == 0:
                if scale is None:
                    K.op("act", lambda e: e.copy(out=out_ap, in_=in_ap), reads, writes)
                else:
                    K.op("act", lambda e: e.mul(out=out_ap, in_=in_ap, mul=scale), reads, writes)
            else:
                if scale is None:
                    K.op("dve", lambda e: e.tensor_copy(out=out_ap, in_=in_ap), reads, writes)
                else:
                    K.op("dve", lambda e: e.tensor_scalar(out=out_ap, in0=in_ap, scalar1=scale, scalar2=None, op0=ALU.mult),
                         reads, writes)

        if STOP == "params":
            st.close()
            return
        blocks = [(s_, b_) for s_ in range(NSEQ) for b_ in range(NBLK)]
        K.phase = "evA"
        phase_A(layer, 0, 0)
        for bi, (s, b) in enumerate(blocks):
            if True:
                t0 = b * 512
                K.phase = "evB"
                if STOP == "A":
                    st.close()
                    return
                for h in range(8):
                    pt, pB = fm_proj(C_Q + h * 64, 64)
                    evac_copy(h, qT[0:64, h, :], pt[0:64, 0:512], [pB], [qTB[h]], scale=0.125)
                for g in range(2):
                    pt, pB = fm_proj(C_KS + g * 64, 64)
                    evac_copy(g, ksT[0:64, g, t0:t0 + 512], pt[0:64, 0:512], [pB], [ksTB[g][b]])
                for g in range(2):
                    pt, pB = fm_proj(C_KW + g * 64, 64)
                    r = b % 2
                    evac_copy(g + 1, kwT[0:64, g, r * 512:(r + 1) * 512], pt[0:64, 0:512], [pB], [kwTB[g][r]])
                for i, (cX, XT, XTB) in enumerate(((C_KC, kcT, kcTB), (C_VC, vcT, vcTB))):
                    if b > 0:
                        K.op("pool", lambda e, XT=XT: e.tensor_copy(out=XT[:, 0:16], in_=XT[:, 512:528]), [XTB], [XTB])
                    pt, pB = fm_proj(cX, 128)
                    evac_copy(i, XT[:, 16:528], pt[:, 0:512], [pB], [XTB])
                if STOP == "B":
                    st.close()
                    return
                K.phase = "evC"
                cst = {}

                def c_front(tt):
                    kt = b * 4 + tt
                    pt, pB = tm_proj(tt, C_VS, 408)
                    K.op("dve", lambda e, pt=pt, kt=kt: e.tensor_copy(out=vsA[:, kt, :, 0:64],
                                                                      in_=pt[:, 0:128].rearrange("p (g d) -> p g d", g=2)),
                         [pB], [vsAB[kt]])
                    K.op("dve", lambda e, pt=pt, kt=kt: e.tensor_copy(out=vwA[:, kt % 8, :, 0:64],
                                                                      in_=pt[:, 256:384].rearrange("p (g d) -> p g d", g=2)),
                         [pB], [vwAB[kt % 8]])
                    K.op("act", lambda e, pt=pt, tt=tt: e.activation(out=gates[:, tt, :], in_=pt[:, 384:408], func=AF.Sigmoid),
                         [pB], [gatesB[tt]])
                    pt, pB = tm_proj(tt, C_ZA, 512)
                    K.op("act", lambda e, pt=pt, tt=tt: e.activation(out=sza[:, tt, :], in_=pt[:, 0:512], func=AF.Silu),
                         [pB], [szaB[tt]])
                    pt, pB = tm_proj(tt, C_ZB, 512)
                    zt, ztB = szb.next()
                    K.op("act", lambda e, pt=pt, zt=zt: e.activation(out=zt[:, :], in_=pt[:, 0:512], func=AF.Silu), [pB], [ztB])
                    pt, pB = tm_proj(tt, C_U, 512)
                    ut, utB = ug.next()
                    K.op("act", lambda e, pt=pt, ut=ut: e.activation(out=ut[:, :], in_=pt[:, 0:512], func=AF.Gelu_apprx_tanh),
                         [pB], [utB])
                    pt, pB = tm_proj(tt, C_V, 512)
                    vt, vtB = vg.next()
                    K.op("act", lambda e, pt=pt, vt=vt: e.activation(out=vt[:, :], in_=pt[:, 0:512], func=AF.Gelu_apprx_tanh),
                         [pB], [vtB])
                    K.op("pool", lambda e, ut=ut, zt=zt: e.tensor_tensor(out=ut[:, :], in0=ut[:, :], in1=zt[:, :], op=ALU.mult),
                         [utB, ztB], [utB])
                    ls, lsB = lnst.next()
                    K.op("dve", lambda e, ls=ls, vt=vt: e.bn_stats(out=ls[:, 0:6], in_=vt[:, :]), [vtB], [lsB])
                    K.op("dve", lambda e, ls=ls: e.bn_aggr(out=ls[:, 6:8], in_=ls[:, 0:6]), [lsB], [lsB])
                    K.op("dve", lambda e, ls=ls: e.tensor_scalar(out=ls[:, 8:9], in0=ls[:, 7:8], scalar1=LN_EPS, scalar2=None,
                                                                op0=ALU.add), [lsB], [lsB])
                    K.op("pool", lambda e, ls=ls: e.tensor_tensor(out=ls[:, 9:10], in0=ls[:, 8:9], in1=neghalf[:, 0:1], op=ALU.pow),
                         [lsB, neghalfB], [lsB])
                    K.op("dve", lambda e, ls=ls, vt=vt: e.tensor_scalar(out=vt[:, :], in0=vt[:, :], scalar1=ls[:, 6:7],
                                                                        scalar2=ls[:, 9:10], op0=ALU.subtract, op1=ALU.mult),
                         [lsB, vtB], [vtB])
                    K.op("pool", lambda e, vt=vt: e.tensor_tensor(out=vt[:, :], in0=vt[:, :], in1=lng[:, :], op=ALU.mult),
                         [vtB, lngB], [vtB])
                    vl, vlB = vln.next()
                    K.op("pool", lambda e, vt=vt, vl=vl: e.tensor_tensor(out=vl[:, :], in0=vt[:, :], in1=lnb[:, :], op=ALU.add),
                         [vtB, lnbB], [vlB])
                    cst[tt] = (vl, vlB, ut, utB)

                def c_back(tt):
                    vl, vlB, ut, utB = cst.pop(tt)
                    pt, pB = psB.next()
                    for g in range(8):
                        K.op("pe", lambda e, g=g, pt=pt, vl=vl: e.matmul(pt[:, g * 64:(g + 1) * 64], WsT[:, g, :],
                                                                          vl[:, g * 64:(g + 1) * 64], start=True, stop=True),
                             [WsTB, vlB], [pB], signal=(g == 7))
                    tb, tbB = tmpb.next()
                    K.op("dve", lambda e, pt=pt, tb=tb: e.tensor_tensor(
                        out=tb[:, :].rearrange("p (g d) -> p g d", g=8), in0=pt[:, 0:512].rearrange("p (g d) -> p g d", g=8),
                        in1=bs[:, :].unsqueeze(2).to_broadcast([128, 8, 64]), op=ALU.add), [pB, bsB], [tbB])
                    K.op("pool", lambda e, tb=tb, ut=ut: e.tensor_tensor(out=tb[:, :], in0=tb[:, :], in1=ut[:, :], op=ALU.mult),
                         [tbB, utB], [tbB])
                    pt, pB = psA.next()
                    pb = pt[:].bitcast(BF16)
                    for fc in range(4):
                        K.op("pe", lambda e, fc=fc, pb=pb, tb=tb: e.transpose(pb[:, fc * 128:(fc + 1) * 128],
                                                                              tb[:, fc * 128:(fc + 1) * 128], identb[:, :]),
                             [tbB, identbB], [pB], signal=(fc == 3))
                    K.op("dve", lambda e, pb=pb, tt=tt: e.tensor_copy(out=mixT[:, 4:8, tt * 128:(tt + 1) * 128],
                                                                      in_=pb[:, 0:512].rearrange("p (k t) -> p k t", k=4)),
                         [pB], [mixTB[tt]])

                for tt in range(5):
                    if tt < 4:
                        c_front(tt)
                    if tt >= 1:
                        c_back(tt - 1)
                if STOP == "C":
                    st.close()
                    return
                if bi + 1 < len(blocks):
                    K.phase = "evA"
                    phase_A(layer, blocks[bi + 1][0], blocks[bi + 1][1])
                elif layer + 1 < n_layers:
                    load_w_in(layer + 1)
                K.phase = "evD"
                if b == 0:
                    n0, nn, c0 = 0, 31, 16
                else:
                    n0, nn, c0 = 32 * b - 1, 32, 0
                nk = 32 * (b + 1) - 1
                for g in range(2):
                    for xi, (XT, XTB, w1, w1B, hid, hidB, hcol) in enumerate((
                            (kcT, kcTB, w1k, w1kB, hidk, hidkB, 0), (vcT, vcTB, w1v, w1vB, hidv, hidvB, n0))):
                        pt, pB = psA.next()
                        for l in range(32):
                            K.op("pe", lambda e, l=l, pt=pt, XT=XT, w1=w1: e.matmul(
                                pt[0:64, 0:nn], w1[g * 64:(g + 1) * 64, l, :],
                                XT[g * 64:(g + 1) * 64, c0 + l:c0 + l + 16 * (nn - 1) + 1:16], start=(l == 0), stop=(l == 31)),
                                 [w1B, XTB], [pB], signal=(l == 31))
                        K.op("act", lambda e, pt=pt, hid=hid, hcol=hcol, xi=xi: e.activation(
                            out=hid[:, g, hcol:hcol + nn], in_=pt[0:64, 0:nn], func=AF.Silu, bias=pebias[:, xi:xi + 1]),
                             [pB, pebiasB], [hidB])
                    pt, pB = psA.next()
                    K.op("pe", lambda e, pt=pt: e.matmul(pt[0:64, 0:nn], w2k[:, :], hidk[:, g, 0:nn], start=True, stop=True),
                         [w2kB, hidkB], [pB])
                    K.op("dve", lambda e, pt=pt: e.tensor_copy(out=kcmpT[:, g, n0:n0 + nn], in_=pt[0:64, 0:nn]), [pB], [kcmpTB])
                    pt, pB = psA.next()
                    K.op("pe", lambda e, pt=pt: e.matmul(pt[0:nk, 0:64], hidv[:, g, 0:nk], w2v[:, :], start=True, stop=True),
                         [w2vB, hidvB], [pB])
                    K.op("dve", lambda e, pt=pt: e.tensor_copy(out=vcmp[0:nk, g, 0:64], in_=pt[0:nk, 0:64]), [pB], [vcmpB])
                if STOP == "D":
                    st.close()
                    return
                K.phase = "evE"
                for g in range(2):
                    im, imB = imp.next()
                    for r in range(4):
                        h = g * 4 + r
                        pt, pB = psA.next()
                        K.op("pe", lambda e, pt=pt, h=h: e.matmul(pt[0:nk, 0:512], kcmpT[:, g, 0:nk], qT[0:64, h, :],
                                                                  start=True, stop=True), [kcmpTB, qTB[h]], [pB])
                        pT, pTB = pTs.next()
                        K.op("act", lambda e, pt=pt, pT=pT: e.activation(out=pT[0:nk, :], in_=pt[0:nk, 0:512], func=AF.Exp),
                             [pB], [pTB])
                        if STOP == "E1":
                            st.close()
                            return
                        K.op("pool", lambda e, pT=pT: e.affine_select(out=pT[0:nk, :], in_=pT[0:nk, :], pattern=[[1, 512]],
                                                                      compare_op=ALU.is_ge, fill=0.0, base=t0 - 31,
                                                                      channel_multiplier=-16), [pTB], [pTB])
                        if STOP == "E2":
                            st.close()
                            return
                        po, poB = psB.next()
                        po3 = po[:, 0:388].rearrange("p (t c) -> p t c", t=4)
                        for tt in range(4):
                            K.op("pe", lambda e, tt=tt, po=po, pT=pT: e.matmul(po[:, tt * 97:(tt + 1) * 97],
                                                                                pT[0:nk, tt * 128:(tt + 1) * 128], vcmp[0:nk, g, :],
                                                                                start=True, stop=True),
                                 [pTB, vcmpB], [poB], signal=(tt == 3))
                        if STOP == "E3":
                            st.close()
                            return
                        rc, rcB = rcs.next()
                        K.op("dve", lambda e, rc=rc, po3=po3: e.tensor_scalar(out=rc[:, 0:4], in0=po3[:, :, 64], scalar1=1e-30,
                                                                              scalar2=None, op0=ALU.max), [poB], [rcB])
                        K.op("dve", lambda e, rc=rc: e.reciprocal(out=rc[:, 4:8], in_=rc[:, 0:4]), [rcB], [rcB])
                        K.op("dve", lambda e, rc=rc, h=h: e.tensor_tensor(out=rc[:, 8:12], in0=rc[:, 4:8], in1=gates[:, :, h],
                                                                          op=ALU.mult), [rcB] + gatesB, [rcB])
                        for tt in range(4):
                            K.op("dve", lambda e, tt=tt, po3=po3, rc=rc, h=h: e.tensor_scalar(
                                out=acc_o[:, tt, h * 64:(h + 1) * 64], in0=po3[:, tt, 0:64], scalar1=rc[:, 8 + tt:9 + tt],
                                scalar2=None, op0=ALU.mult), [poB, rcB], [acc_oB[h]])
                        for tt in range(4):
                            if r == 0:
                                K.op("dve", lambda e, tt=tt, po3=po3, rc=rc, im=im: e.tensor_scalar(
                                    out=im[:, tt, :], in0=po3[:, tt, 65:97], scalar1=rc[:, 4 + tt:5 + tt], scalar2=None,
                                    op0=ALU.mult), [poB, rcB], [imB])
                            else:
                                K.op("dve", lambda e, tt=tt, po3=po3, rc=rc, im=im: e.scalar_tensor_tensor(
                                    out=im[:, tt, :], in0=po3[:, tt, 65:97], scalar=rc[:, 4 + tt:5 + tt], in1=im[:, tt, :],
                                    op0=ALU.mult, op1=ALU.add), [poB, rcB, imB], [imB])
                    if STOP == "E4":
                        st.close()
                        return
                    K.op("dve", lambda e, im=im: e.tensor_tensor(out=im[:, :, :], in0=im[:, :, :], in1=ctab[:, 4 * b:4 * b + 4, :],
                                                                 op=ALU.add), [imB, ctabB], [imB])
                    t8, t8B = top8.next()
                    for tt in range(4):
                        K.op("dve", lambda e, tt=tt, t8=t8, im=im: e.max(out=t8[:, tt, :], in_=im[:, tt, :]), [imB], [t8B])
                    for tt in range(4):
                        K.op("dve", lambda e, tt=tt, t8=t8, im=im: e.tensor_scalar(
                            out=selpad[:, tt, 64:96], in0=im[:, tt, :], scalar1=t8[:, tt, 7:8], scalar2=-1.0,
                            op0=ALU.is_ge, op1=ALU.add), [imB, t8B], [selpadB])
                    if STOP == "E6":
                        st.close()
                        return
                    pt, pB = psA.next()
                    pb = pt[:].bitcast(BF16)
                    for tt in range(4):
                        K.op("pe", lambda e, tt=tt, pb=pb: e.transpose(pb[0:96, tt * 128:(tt + 1) * 128], selpad[:, tt, :],
                                                                       identb[:, :]), [selpadB, identbB], [pB], signal=(tt == 3))
                    for r in range(4):
                        h = g * 4 + r
                        K.op("dve", lambda e, h=h, pb=pb: e.tensor_copy(out=qT[64:96, h, :], in_=pb[64:96, 0:512]), [pB], [qSB[h]])
                if STOP == "E":
                    st.close()
                    return
                K.phase = "evF"
                items = []
                for h in range(8):
                    for br in range(2):
                        kts = list(range(0, 4 * b + 4)) if br == 0 else list(range(max(0, 4 * b - 4), 4 * b + 4))
                        for kt in kts:
                            items.append((h, br, kt, kt == kts[0], kt == kts[-1]))
                LA = 2
                stage = {}
                accs = {}

                def front(idx):
                    h, br, kt, isfirst, islast = items[idx]
                    g = h // 4
                    d = kt - 4 * b
                    lo = max(0, d)
                    hi = 3 if br == 0 else min(3, d + 4)
                    c0_, c1_ = lo * 128, (hi + 1) * 128
                    pt, pB = psA.next()
                    if br == 0:
                        K.op("pe", lambda e: e.matmul(
                            pt[:, c0_:c1_], ksT[0:96, g, kt * 128:(kt + 1) * 128], qT[0:96, h, c0_:c1_],
                            start=True, stop=True), [ksTB[g][kt // 4], ebB, qTB[h], qSB[h]], [pB])
                    else:
                        kr = kt % 8
                        K.op("pe", lambda e: e.matmul(
                            pt[:, c0_:c1_], kwT[0:64, g, kr * 128:(kr + 1) * 128], qT[0:64, h, c0_:c1_],
                            start=True, stop=True), [kwTB[g][kr // 4], qTB[h]], [pB])
                    pT, pTB = pTs.next()
                    K.op("act", lambda e: e.activation(out=pT[:, c0_:c1_], in_=pt[:, c0_:c1_], func=AF.Exp), [pB], [pTB])
                    if d >= 0:
                        K.op("pool", lambda e: e.tensor_tensor(
                            out=pT[:, d * 128:(d + 1) * 128], in0=pT[:, d * 128:(d + 1) * 128], in1=trilk[:, :],
                            op=ALU.mult), [pTB, trilkB], [pTB])
                    if br == 1 and 0 <= d + 4 <= 3:
                        K.op("pool", lambda e: e.tensor_tensor(
                            out=pT[:, (d + 4) * 128:(d + 5) * 128], in0=pT[:, (d + 4) * 128:(d + 5) * 128],
                            in1=fark[:, :], op=ALU.mult), [pTB, farkB], [pTB])
                    stage[idx] = (pT, pTB, lo, hi)

                def back(idx):
                    h, br, kt, isfirst, islast = items[idx]
                    g = h // 4
                    pT, pTB, lo, hi = stage.pop(idx)
                    if isfirst:
                        accs[(h, br)] = psB.next()
                    acc, accB = accs[(h, br)]
                    for tt in range(lo, hi + 1):
                        if br == 0:
                            vB_, rhs = vsAB[kt], vsA[:, kt, g, :]
                        else:
                            vB_, rhs = vwAB[kt % 8], vwA[:, kt % 8, g, :]
                        K.op("pe", lambda e, tt=tt, rhs=rhs: e.matmul(
                            acc[:, tt * 65:(tt + 1) * 65], pT[:, tt * 128:(tt + 1) * 128], rhs,
                            start=(isfirst and tt == lo), stop=True, skip_group_check=True), [pTB, vB_], [accB], signal=(tt == hi))
                    if islast:
                        acc3 = acc[:, 0:260].rearrange("p (t c) -> p t c", t=4)
                        rc, rcB = rcs.next()
                        K.op("dve", lambda e: e.tensor_scalar(out=rc[:, 0:4], in0=acc3[:, :, 64], scalar1=1e-30,
                                                              scalar2=None, op0=ALU.max), [accB], [rcB])
                        K.op("dve", lambda e: e.reciprocal(out=rc[:, 4:8], in_=rc[:, 0:4]), [rcB], [rcB])
                        gi = (1 + br) * 8 + h
                        K.op("dve", lambda e: e.tensor_tensor(out=rc[:, 8:12], in0=rc[:, 4:8], in1=gates[:, :, gi],
                                                              op=ALU.mult), [rcB] + gatesB, [rcB])
                        for tt in range(4):
                            K.op("dve", lambda e, tt=tt: e.scalar_tensor_tensor(
                                out=acc_o[:, tt, h * 64:(h + 1) * 64], in0=acc3[:, tt, 0:64], scalar=rc[:, 8 + tt:9 + tt],
                                in1=acc_o[:, tt, h * 64:(h + 1) * 64], op0=ALU.mult, op1=ALU.add),
                                 [accB, rcB, acc_oB[h]], [acc_oB[h]])
                        del accs[(h, br)]

                for idx in range(len(items) + LA):
                    if idx < len(items):
                        front(idx)
                    if idx - LA >= 0:
                        back(idx - LA)
                for tt in range(4):
                    am, amB = amix.next()
                    K.op("pool", lambda e, tt=tt, am=am: e.tensor_tensor(out=am[:, :], in0=acc_o[:, tt, :], in1=sza[:, tt, :],
                                                                         op=ALU.mult), acc_oB + [szaB[tt]], [amB])
                    pt, pB = psA.next()
                    pb = pt[:].bitcast(BF16)
                    for fc in range(4):
                        K.op("pe", lambda e, fc=fc, pb=pb, am=am: e.transpose(pb[:, fc * 128:(fc + 1) * 128],
                                                                              am[:, fc * 128:(fc + 1) * 128], identb[:, :]),
                             [amB, identbB], [pB], signal=(fc == 3))
                    K.op("dve", lambda e, pb=pb, tt=tt: e.tensor_copy(out=mixT[:, 0:4, tt * 128:(tt + 1) * 128],
                                                                      in_=pb[:, 0:512].rearrange("p (k t) -> p k t", k=4)),
                         [pB], [mixTB[tt]])
                if STOP == "F":
                    st.close()
                    return
                K.phase = "evG"
                phase_G(layer, s, b)
                if STOP == "G":
                    st.close()
                    return
        st.close()

    def odd_layer(layer, li):
        st = ExitStack()

        def sbl(name, shape, dt=F32):
            t = st.enter_context(nc.sbuf_tensor(f"{name}_{layer}", list(shape), dt))
            return t, Buf(name)

        def sbln(name, shape, dt, n):
            return Rot([sbl(f"{name}{i}", shape, dt) for i in range(n)])

        yc = st.enter_context(nc.sbuf_tensor(f"yc_{layer}", [128, 8, 544], BF16))
        ycB = [Buf(f"yc{c}") for c in range(8)]
        szzs = [(st.enter_context(nc.sbuf_tensor(f"szz{i}_{layer}", [128, 8, 512], BF16)), [Buf(f"szz{i}_{c}") for c in range(8)])
                for i in range(2)]
        yv = st.enter_context(nc.sbuf_tensor(f"yv_{layer}", [128, 8, 512], F32))
        yvB = [Buf(f"yv{c}") for c in range(8)]
        diag = sbln("diag", [128, 31, 128], BF16, 3)
        sig = sbln("sig", [128, 512], F32, 2)
        ybf = sbln("ybf", [128, 512], BF16, 3)
        ysq = sbln("ysq", [128, 512], BF16, 3)
        onesb, onesbB = sbl("onesb", [128, 128], BF16)
        mean, meanB = sbl("mean", [128, 512], F32)
        rstdt, rstdtB = sbl("rstdt", [128, 512], F32)
        var, varB = sbl("var", [128, 512], F32)
        t1 = sbln("t1", [128, 512], F32, 2)

        K.op("pool", lambda e: e.memset(onesb[:, :], 1.0 / D), [], [onesbB])

        blocks = [(s_, b_) for s_ in range(NSEQ) for b_ in range(NBLK)]
        K.phase = "odA"
        phase_A(layer, 0, 0)
        def od_proj(bi):
            s, b = blocks[bi]
            szz, szzB = szzs[bi % 2]
            K.phase = "odProj"
            for c in range(8):
                if b == 0:
                    K.op("pool", lambda e: e.memset(yc[:, c, 0:32], 0.0), [], [ycB[c]])
                else:
                    K.op("pool", lambda e: e.tensor_copy(out=yc[:, c, 0:32], in_=yc[:, c, 512:544]), [ycB[c]], [ycB[c]])
                pg, pgB = fm_proj(1024 + c * 128, 128)
                sg, sgB = sig.next()
                K.op("act", lambda e: e.activation(out=sg[:, :], in_=pg[:, 0:512], func=AF.Sigmoid), [pgB], [sgB])
                pa, paB = fm_proj(c * 128, 128)
                K.op("dve", lambda e: e.tensor_tensor(out=yc[:, c, 32:544], in0=pa[:, 0:512], in1=sg[:, :], op=ALU.mult),
                     [paB, sgB], [ycB[c]])
            for c in range(8):
                pz, pzB = fm_proj(2048 + c * 128, 128)
                K.op("act", lambda e: e.activation(out=szz[:, c, :], in_=pz[:, 0:512], func=AF.Silu), [pzB], [szzB[c]])

        def od_conv(bi):
            K.phase = "odConv"
            pm, pmB = psB.next()
            pq, pqB = psB.next()
            dgs = {}

            def build_diag(c):
                dg, dgB = diag.next()
                K.op("pool" if c % 2 == 0 else "dve", lambda e: e.tensor_tensor(
                    out=dg[:, :, :], in0=identb[:, :].unsqueeze(1).to_broadcast([128, 31, 128]),
                    in1=par[:, c, 1:32].unsqueeze(2).to_broadcast([128, 31, 128]), op=ALU.mult), [identbB, parB], [dgB])
                dgs[c] = (dg, dgB)

            for c in range(3):
                build_diag(c)
            pend_stats = []

            def emit_stats():
                c_, yb_, ybB_, yq_, yqB_ = pend_stats.pop(0)
                K.op("pe", lambda e: e.matmul(pm[:, 0:512], onesb[:, :], yb_[:, :], start=(c_ == 0), stop=(c_ == 7)),
                     [onesbB, ybB_], [pmB], signal=(c_ == 7))
                K.op("pe", lambda e: e.matmul(pq[:, 0:512], onesb[:, :], yq_[:, :], start=(c_ == 0), stop=(c_ == 7)),
                     [onesbB, yqB_], [pqB], signal=(c_ == 7))

            for c in range(8):
                dg, dgB = dgs[c]
                pc, pcB = psA.next()
                for k in range(31):
                    K.op("pe", lambda e, k=k: e.matmul(pc[:, 0:512], dg[:, k, :], yc[:, c, 2 + k:2 + k + 512],
                                                       start=(k == 0), stop=(k == 30)),
                         [dgB, ycB[c]], [pcB], signal=(k == 30))
                if c + 3 < 8:
                    build_diag(c + 3)
                if pend_stats:
                    emit_stats()
                K.op("act", lambda e: e.activation(out=yv[:, c, :], in_=pc[:, 0:512], func=AF.Identity,
                                                   bias=par[:, c, 32:33]), [pcB, parB], [yvB[c]])
                yb, ybB = ybf.next()
                yq, yqB = ysq.next()
                K.op("dve", lambda e: e.tensor_copy(out=yb[:, :], in_=yv[:, c, :]), [yvB[c]], [ybB])
                K.op("pool", lambda e: e.tensor_tensor(out=yq[:, :], in0=yv[:, c, :], in1=yv[:, c, :], op=ALU.mult),
                     [yvB[c]], [yqB])
                pend_stats.append((c, yb, ybB, yq, yqB))
            while pend_stats:
                emit_stats()
            K.phase = "odLN"
            K.op("act", lambda e: e.copy(out=mean[:, :], in_=pm[:, 0:512]), [pmB], [meanB])
            K.op("dve", lambda e: e.tensor_tensor(out=var[:, :], in0=mean[:, :], in1=mean[:, :], op=ALU.mult), [meanB], [varB])
            K.op("dve", lambda e: e.scalar_tensor_tensor(out=var[:, :], in0=pq[:, 0:512], scalar=LN_EPS, in1=var[:, :],
                                                         op0=ALU.add, op1=ALU.subtract), [pqB, varB], [varB])
            K.op("act", lambda e: e.activation(out=var[:, :], in_=var[:, :], func=AF.Sqrt), [varB], [varB])
            K.op("dve", lambda e: e.reciprocal(out=rstdt[:, :], in_=var[:, :]), [varB], [rstdtB])
            for c in range(8):
                K.op("dve", lambda e: e.tensor_tensor(out=yv[:, c, :], in0=yv[:, c, :], in1=mean[:, :], op=ALU.subtract),
                     [yvB[c], meanB], [yvB[c]])
                K.op("pool", lambda e: e.tensor_tensor(out=yv[:, c, :], in0=yv[:, c, :], in1=rstdt[:, :], op=ALU.mult),
                     [yvB[c], rstdtB], [yvB[c]])

        def od_ln_fin(bi):
            szz, szzB = szzs[bi % 2]
            K.phase = "odLNf"
            for c in range(8):
                ta, taB = t1.next()
                K.op("act", lambda e: e.activation(out=ta[:, :], in_=yv[:, c, :], func=AF.Silu,
                                                   scale=par[:, c, 33:34], bias=par[:, c, 34:35]),
                     [yvB[c], parB], [taB])
                K.op("dve", lambda e: e.tensor_tensor(out=mixT[:, c, :], in0=ta[:, :], in1=szz[:, c, :], op=ALU.mult),
                     [taB, szzB[c]], mixTB)

        od_proj(0)
        for bi, (s, b) in enumerate(blocks):
            if bi + 1 < len(blocks):
                K.phase = "odA"
                phase_A(layer, blocks[bi + 1][0], blocks[bi + 1][1])
            elif layer + 1 < n_layers:
                load_w_in(layer + 1)
            od_conv(bi)
            if bi + 1 < len(blocks):
                od_proj(bi + 1)
            od_ln_fin(bi)
            K.phase = "odG"
            phase_G(layer, s, b)
        st.close()

    load_w_in(0)
    for layer in range(n_layers):
        li = layer // 2
        load_common(layer)
        if layer > 0:
            K.barrier()
        if layer % 2 == 0:
            even_layer(layer, li)
        else:
            odd_layer(layer, li)

    for s in range(NSEQ):
        for t in range(16):
            b = xd[s][t]
            if b.w is not None:
                sm, v = b.w
                nc.sync.wait_ge(sm.h, v)
    stack.close()
    return nc, K


def make_consts():
    p = np.arange(128)
    ident = np.eye(128, dtype=np.float32)
    tril = (p[:, None] <= p[None, :]).astype(np.float32)
    far = (p[:, None] > p[None, :]).astype(np.float32)
    ctab = np.zeros((128, 16, 32), np.float32)
    j = np.arange(32)
    for tile in range(16):
        t = tile * 128 + p
        cur = t // 64
        forced = (j[None, :] == 0) | (j[None, :] == cur[:, None]) | (j[None, :] == cur[:, None] - 1)
        causal = j[None, :] <= cur[:, None]
        ctab[:, tile, :] = np.where(causal, forced.astype(np.float32) * np.float32(1e4), np.float32(-1e30))
    ebig = ((np.arange(2048)[None, :] // 64) == j[:, None]).astype(np.float32) * np.float32(BIG)
    n_cmp = 127
    tok = np.arange(n_cmp)[:, None] * 16 + np.arange(32)[None, :]
    ovl = ((tok[:, :, None] // 64) == np.arange(32)[None, None, :]).sum(1).astype(np.float32) / np.float32(32)
    return {"c_ident": ident, "c_tril": tril, "c_far": far, "c_ctab": ctab, "c_ebig": ebig, "c_ovl": ovl}


_CACHE = {}


def kernel(**inputs):
    if "nc" not in _CACHE:
        _CACHE["nc"] = build_program(4)[0]
    nc = _CACHE["nc"]
    consts = make_consts()
    x = np.ascontiguousarray(inputs["x"], dtype=np.float32)
    shared = {k: np.ascontiguousarray(v, dtype=np.float32) for k, v in inputs.items() if k != "x"}
    shared.update(consts)
    in_maps = []
    for c in range(NCORES):
        m = dict(shared)
        m["x"] = x[c * NSEQ:(c + 1) * NSEQ]
        in_maps.append(m)
    res = run_bass_kernel_spmd(nc, in_maps, core_ids=list(range(NCORES)))
    return np.concatenate([r["out"] for r in res.results], axis=0).astype(np.float32)
```

```python
from contextlib import ExitStack

import numpy as np
import concourse.bass as bass
import concourse.mybir as mybir
from concourse.bass_utils import run_bass_kernel_spmd

F32 = mybir.dt.float32
BF16 = mybir.dt.bfloat16
AF = mybir.ActivationFunctionType
ALU = mybir.AluOpType

NCORES = 8
SEQ = 2048
D = 1024
NSEQ = 2
NBLK = 4
EVEN_W = 3352
ODD_W = 3072
RMS_EPS = 1e-6
LN_EPS = 1e-5
BIG = 30000.0
import os as _os
STOP = _os.environ.get("KSTOP", "")

C_Q, C_KC, C_VC, C_KS, C_VS, C_KW, C_VW, C_GATE, C_ZA, C_U, C_V, C_ZB = (
    0, 512, 640, 768, 896, 1024, 1152, 1280, 1304, 1816, 2328, 2840)


class Sem:
    __slots__ = ("h", "val", "name")

    def __init__(self, h, name=""):
        self.h = h
        self.val = 0
        self.name = name


class Buf:
    __slots__ = ("name", "w", "r", "dsem")

    def __init__(self, name):
        self.name = name
        self.w = None
        self.r = {}
        self.dsem = None


class Trk:
    def __init__(self, nc, stack):
        self.nc = nc
        self.stack = stack
        self.eng = {"pe": nc.tensor, "act": nc.scalar, "dve": nc.vector, "pool": nc.gpsimd, "sp": nc.sync}
        self.nsem = 0
        self.esem = {e: self.newsem("e_" + e) for e in self.eng}
        self.waited = {e: {} for e in self.eng}
        self.nops = 0
        self.dsems = []
        self.phase = ""
        self.waitlog = {e: [] for e in self.eng}

    def newsem(self, name):
        self.nsem += 1
        return Sem(self.stack.enter_context(self.nc.semaphore(f"{name}_{self.nsem}")), name)

    def _sync(self, E, reads, writes):
        deps = {}
        for b in reads:
            if b.w is not None:
                s, v = b.w
                if deps.get(s, 0) < v:
                    deps[s] = v
        for b in writes:
            if b.w is not None:
                s, v = b.w
                if deps.get(s, 0) < v:
                    deps[s] = v
            for s, v in b.r.items():
                if deps.get(s, 0) < v:
                    deps[s] = v
        eng = self.eng[E]
        w = self.waited[E]
        own = self.esem[E]
        for s, v in deps.items():
            if E == "pe" and s is own:
                continue
            if w.get(s, 0) < v:
                eng.wait_ge(s.h, v)
                w[s] = v
                self.waitlog[E].append((self.phase, s.name))

    def op(self, E, fn, reads=(), writes=(), signal=True):
        self._sync(E, reads, writes)
        ins = fn(self.eng[E])
        s = self.esem[E]
        if signal:
            s.val += 1
            ins.then_inc(s.h, 1)
            tag = (s, s.val)
        else:
            tag = (s, s.val + 1)
        for b in reads:
            if b.r.get(s, 0) < tag[1]:
                b.r[s] = tag[1]
        for b in writes:
            b.w = tag
            b.r = {}
        self.nops += 1
        return ins

    def barrier(self):
        sems = list(self.esem.values()) + self.dsems
        for E, eng in self.eng.items():
            w = self.waited[E]
            for s in sems:
                if s.val > 0 and w.get(s, 0) < s.val:
                    eng.wait_ge(s.h, s.val)
                    w[s] = s.val

    def dma(self, E, out, in_, reads, writes, dbuf, **kw):
        self._sync(E, reads, writes)
        if dbuf.dsem is None:
            dbuf.dsem = self.newsem("d_" + dbuf.name)
            self.dsems.append(dbuf.dsem)
        s = dbuf.dsem
        ins = self.eng[E].dma_start(out=out, in_=in_, **kw)
        s.val += 16
        ins.then_inc(s.h, 16)
        tag = (s, s.val)
        for b in reads:
            if b.r.get(s, 0) < tag[1]:
                b.r[s] = tag[1]
        for b in writes:
            b.w = tag
            b.r = {}
        self.nops += 1
        return ins


class Rot:
    def __init__(self, items):
        self.items = items
        self.i = 0

    def next(self):
        it = self.items[self.i % len(self.items)]
        self.i += 1
        return it


def build_program(n_layers=4):
    nc = bass.Bass("TRN2", target_bir_lowering=False, dynamic_dma_scratch_size=16384)
    stack = ExitStack()
    K = Trk(nc, stack)

    def dram(name, shape, dt=F32, kind="ExternalInput"):
        return nc.dram_tensor(name, list(shape), dt, kind=kind).ap()

    x_in = dram("x", [NSEQ, SEQ, D])
    out = dram("out", [NSEQ, SEQ, D], kind="ExternalOutput")
    norm_pre = dram("norm_pre", [4, D])
    norm_post = dram("norm_post", [4, D])
    e_w_in = dram("even_w_in", [2, D, EVEN_W])
    e_k_pe = dram("even_cmp_k_pe", [2, 32, 64])
    e_k_w1 = dram("even_cmp_k_w1", [2, 2048, 64])
    e_k_w2 = dram("even_cmp_k_w2", [2, 64, 64])
    e_v_pe = dram("even_cmp_v_pe", [2, 32, 64])
    e_v_w1 = dram("even_cmp_v_w1", [2, 2048, 64])
    e_v_w2 = dram("even_cmp_v_w2", [2, 64, 64])
    e_ln_g = dram("even_sgu_ln_g", [2, 512])
    e_ln_b = dram("even_sgu_ln_b", [2, 512])
    e_sgu_w = dram("even_sgu_w", [2, 8, 128, 128])
    e_sgu_b = dram("even_sgu_b", [2, 8, 128])
    e_w_out = dram("even_w_out", [2, D, D])
    o_w_in = dram("odd_w_in", [2, D, ODD_W])
    o_dw_w = dram("odd_dw_w", [2, 31, D])
    o_dw_b = dram("odd_dw_b", [2, D])
    o_ln_g = dram("odd_ln_g", [2, D])
    o_ln_b = dram("odd_ln_b", [2, D])
    o_w_out = dram("odd_w_out", [2, D, D])
    c_ident = dram("c_ident", [128, 128])
    c_tril = dram("c_tril", [128, 128])
    c_far = dram("c_far", [128, 128])
    c_ctab = dram("c_ctab", [128, 16, 32])
    c_ebig = dram("c_ebig", [32, 2048])
    c_ovl = dram("c_ovl", [127, 32])

    def sb(name, shape, dt=F32):
        t = stack.enter_context(nc.sbuf_tensor(name, list(shape), dt))
        return t, Buf(name)

    def sbn(name, shape, dt, n):
        return Rot([sb(f"{name}{i}", shape, dt) for i in range(n)])

    w_in = stack.enter_context(nc.sbuf_tensor("w_in", [128, 8, EVEN_W], BF16))
    w_inB = [Buf(f"w_in{k}") for k in range(8)]
    w_out = stack.enter_context(nc.sbuf_tensor("w_out", [128, 8, D], BF16))
    w_outB = [Buf(f"w_out{k}") for k in range(8)]
    identb, identbB = sb("identb", [128, 128], BF16)
    identf, identfB = sb("identf", [128, 128], F32)
    trilk, trilkB = sb("trilk", [128, 128], BF16)
    fark, farkB = sb("fark", [128, 128], BF16)
    neghalf, neghalfB = sb("neghalf", [128, 8], F32)
    gpost, gpostB = sb("gpost", [128, D], F32)
    xa = sbn("xa", [128, D], F32, 2)
    xr = sbn("xr", [128, D], F32, 2)
    hp = sbn("hp", [128, D], BF16, 2)
    hT = stack.enter_context(nc.sbuf_tensor("hT", [128, 8, 512], BF16))
    hTB = [Buf(f"hT{t}") for t in range(4)]
    mixT = stack.enter_context(nc.sbuf_tensor("mixT", [128, 8, 512], BF16))
    mixTB = [Buf(f"mixT{t}") for t in range(4)]
    junks = sbn("junk", [128, D], BF16, 2)
    ss, _ = sb("ss", [128, 8], F32)
    ssB = [Buf(f"ss{t}") for t in range(8)]
    ms, _ = sb("ms", [128, 8], F32)
    msB = [Buf(f"ms{t}") for t in range(8)]
    rstd, _ = sb("rstd", [128, 8], F32)
    rstdB = [Buf(f"rstd{t}") for t in range(8)]
    ssy = Rot([sb(f"ssy{i}", [128, 4], F32) + (Buf(f"ssy{i}a"), Buf(f"ssy{i}b")) for i in range(2)])
    rsy = sbn("rsy", [128, 4], F32, 2)
    par, parB = sb("par", [128, 8, 36], F32)
    tmpf = sbn("tmpf", [128, 512], F32, 4)

    PS = []
    for i in range(8):
        t = stack.enter_context(nc.psum_tensor(f"ps{i}", [128, 512], F32))
        PS.append((t, Buf(f"ps{i}")))
    psA = Rot(PS[0:4])
    psB = Rot(PS[4:8])

    xd = [[Buf(f"xd{s}_{t}") for t in range(16)] for s in range(NSEQ)]

    K.dma("pool", identb[:, :], c_ident[:, :], [], [identbB], identbB)
    K.dma("sp", identf[:, :], c_ident[:, :], [], [identfB], identfB)
    K.dma("pool", trilk[:, :], c_tril[:, :], [], [trilkB], trilkB)
    K.dma("pool", fark[:, :], c_far[:, :], [], [farkB], farkB)
    K.op("dve", lambda e: e.memset(neghalf[:, :], -0.5), [], [neghalfB])

    def layer_srcs(layer):
        li = layer // 2
        if layer % 2 == 0:
            return e_w_in[li], EVEN_W, e_w_out[li], []
        return (o_w_in[li], ODD_W, o_w_out[li],
                [(o_dw_w[li], 31), (o_dw_b[li:li + 1, :], 1), (o_ln_g[li:li + 1, :], 1), (o_ln_b[li:li + 1, :], 1)])

    def load_w_in(layer):
        w_in_dram, w_in_cols, _, _ = layer_srcs(layer)
        for kc in range(8):
            K.dma("pool", w_in[:, kc, 0:w_in_cols], w_in_dram[kc * 128:(kc + 1) * 128, :], [], [w_inB[kc]], w_inB[kc],
                  max_dma_last_dim=4096)

    def load_common(layer):
        _, _, w_out_dram, rows = layer_srcs(layer)
        stg, stgB = xa.next()
        rows = [(norm_pre[layer:layer + 1, :], 1)] + rows
        if sum(r for _, r in rows) % 2:
            rows = rows + [(norm_post[layer:layer + 1, :], 1)]
        r0 = 0
        for ap, r in rows:
            K.dma("sp", stg[r0:r0 + r, :], ap, [], [stgB], stgB)
            r0 += r
        R = r0
        for c in range(8):
            pt, pB = psA.next()
            K.op("pe", lambda e, c=c, pt=pt: e.transpose(pt[:, 0:R], stg[0:R, c * 128:(c + 1) * 128], identf[0:R, 0:R]),
                 [stgB, identfB], [pB])
            K.op("dve", lambda e, c=c, pt=pt: e.tensor_copy(out=par[:, c, 0:R], in_=pt[:, 0:R]), [pB], [parB])
        for kc in range(8):
            K.dma("pool", w_out[:, kc, :], w_out_dram[kc * 128:(kc + 1) * 128, :], [], [w_outB[kc]], w_outB[kc],
                  max_dma_last_dim=4096)
        K.dma("sp", gpost[:, :], norm_post[layer:layer + 1, :].to_broadcast([128, D]), [], [gpostB], gpostB)

    def phase_A(layer, s, b):
        src = x_in if layer == 0 else out
        xts = []
        for tt in range(4):
            xt, xB = xa.next()
            tile = b * 4 + tt
            K.dma("sp", xt[:, :], src[s, tile * 128:(tile + 1) * 128, :], [xd[s][tile]], [xB], xB)
            jk, jkB = junks.next()
            K.op("act", lambda e, xt=xt, tt=tt, jk=jk: e.activation(out=jk[:, :], in_=xt[:, :], func=AF.Square,
                                                                    accum_out=ss[:, tt:tt + 1]), [xB], [ssB[tt], jkB])
            K.op("dve", lambda e, tt=tt: e.tensor_scalar(out=ms[:, tt:tt + 1], in0=ss[:, tt:tt + 1], scalar1=1.0 / D,
                                                        scalar2=RMS_EPS, op0=ALU.mult, op1=ALU.add), [ssB[tt]], [msB[tt]])
            K.op("pool", lambda e, tt=tt: e.tensor_tensor(out=rstd[:, tt:tt + 1], in0=ms[:, tt:tt + 1],
                                                          in1=neghalf[:, 0:1], op=ALU.pow), [msB[tt], neghalfB], [rstdB[tt]])
            ht, hB = hp.next()
            K.op("dve", lambda e, ht=ht, xt=xt, tt=tt: e.tensor_scalar(out=ht[:, :], in0=xt[:, :], scalar1=rstd[:, tt:tt + 1],
                                                                       scalar2=None, op0=ALU.mult), [xB, rstdB[tt]], [hB])
            pt, pB = psA.next()
            pb = pt[:].bitcast(BF16)
            for kc in range(8):
                K.op("pe", lambda e, kc=kc, pb=pb, ht=ht: e.transpose(pb[:, kc * 128:(kc + 1) * 128],
                                                                      ht[:, kc * 128:(kc + 1) * 128], identb[:, :]),
                     [hB, identbB], [pB], signal=(kc == 7))
            K.op("dve", lambda e, pb=pb, tt=tt: e.tensor_tensor(
                out=hT[:, :, tt * 128:(tt + 1) * 128], in0=pb.rearrange("p (k t) -> p k t", k=8),
                in1=par[:, :, 0:1].to_broadcast([128, 8, 128]), op=ALU.mult), [pB, parB], [hTB[tt]])

    def fm_proj(col0, M):
        pt, pB = psA.next()
        for kc in range(8):
            K.op("pe", lambda e, kc=kc, pt=pt: e.matmul(pt[0:M, 0:512], w_in[:, kc, col0:col0 + M], hT[:, kc, :],
                                                        start=(kc == 0), stop=(kc == 7)),
                 [w_inB[kc]] + hTB, [pB], signal=(kc == 7))
        return pt, pB

    def tm_proj(tt, col0, N):
        pt, pB = psA.next()
        for kc in range(8):
            K.op("pe", lambda e, kc=kc, pt=pt: e.matmul(pt[:, 0:N], hT[:, kc, tt * 128:(tt + 1) * 128],
                                                        w_in[:, kc, col0:col0 + N], start=(kc == 0), stop=(kc == 7)),
                 [w_inB[kc], hTB[tt]], [pB], signal=(kc == 7))
        return pt, pB

    def phase_G(layer, s, b):
        src = x_in if layer == 0 else out
        for tt in range(4):
            tile = b * 4 + tt
            xt, xB = xr.next()
            K.dma("sp", xt[:, :], src[s, tile * 128:(tile + 1) * 128, :], [xd[s][tile]], [xB], xB)
            halves = []
            sy, syB, syB0, syB1 = ssy.next()
            ry, ryB = rsy.next()
            for hf in range(2):
                pt, pB = psB.next()
                for fc in range(8):
                    K.op("pe", lambda e, fc=fc, pt=pt, hf=hf: e.matmul(pt[:, 0:512], mixT[:, fc, tt * 128:(tt + 1) * 128],
                                                                        w_out[:, fc, hf * 512:(hf + 1) * 512],
                                                                        start=(fc == 0), stop=(fc == 7)),
                         [mixTB[tt], w_outB[fc]], [pB], signal=(fc == 7))
                halves.append((pt, pB))
            ysbs = []
            for hf, sB_ in ((0, syB0), (1, syB1)):
                pt, pB = halves[hf]
                tf, tfB = tmpf.next()
                if hf == 0:
                    K.op("act", lambda e, pt=pt, tf=tf: e.copy(out=tf[:, :], in_=pt[:, 0:512]), [pB], [tfB])
                else:
                    K.op("dve", lambda e, pt=pt, tf=tf: e.tensor_copy(out=tf[:, :], in_=pt[:, 0:512]), [pB], [tfB])
                jk, jkB = junks.next()
                K.op("act", lambda e, tf=tf, hf=hf, sy=sy, jk=jk: e.activation(out=jk[:, 0:512], in_=tf[:, :], func=AF.Square,
                                                                               accum_out=sy[:, hf:hf + 1]), [tfB], [sB_, jkB])
                ysbs.append((tf, tfB))
            K.op("dve", lambda e, sy=sy: e.tensor_tensor(out=sy[:, 2:3], in0=sy[:, 0:1], in1=sy[:, 1:2], op=ALU.add),
                 [syB0, syB1, syB], [syB])
            K.op("dve", lambda e, sy=sy: e.tensor_scalar(out=sy[:, 3:4], in0=sy[:, 2:3], scalar1=1.0 / D, scalar2=RMS_EPS,
                                                        op0=ALU.mult, op1=ALU.add), [syB], [syB])
            K.op("pool", lambda e, sy=sy, ry=ry: e.tensor_tensor(out=ry[:, 0:1], in0=sy[:, 3:4], in1=neghalf[:, 0:1],
                                                                 op=ALU.pow), [syB, neghalfB], [ryB])
            for hf in range(2):
                tf, tfB = ysbs[hf]
                K.op("dve", lambda e, tf=tf, hf=hf, ry=ry: e.scalar_tensor_tensor(
                    out=tf[:, :], in0=tf[:, :], scalar=ry[:, 0:1], in1=gpost[:, hf * 512:(hf + 1) * 512],
                    op0=ALU.mult, op1=ALU.mult), [tfB, ryB, gpostB], [tfB])
                K.op("pool", lambda e, tf=tf, xt=xt, hf=hf: e.tensor_tensor(
                    out=xt[:, hf * 512:(hf + 1) * 512], in0=xt[:, hf * 512:(hf + 1) * 512], in1=tf[:, :], op=ALU.add),
                     [tfB, xB], [xB])
            K.dma("sp", out[s, tile * 128:(tile + 1) * 128, :], xt[:, :], [xB], [xd[s][tile]], xd[s][tile])

    def even_layer(layer, li):
        st = ExitStack()

        def sbl(name, shape, dt=F32):
            t = st.enter_context(nc.sbuf_tensor(f"{name}_{layer}", list(shape), dt))
            return t, Buf(name)

        def sbln(name, shape, dt, n):
            return Rot([sbl(f"{name}{i}", shape, dt) for i in range(n)])

        qT = st.enter_context(nc.sbuf_tensor(f"qT_{layer}", [128, 8, 512], BF16))
        qTB = [Buf(f"qT{h}") for h in range(8)]
        qSB = [Buf(f"qS{h}") for h in range(8)]
        ksT = st.enter_context(nc.sbuf_tensor(f"ksT_{layer}", [128, 2, 2048], BF16))
        ksTB = [[Buf(f"ksT{g}_{b}") for b in range(4)] for g in range(2)]
        ebB = Buf("ebig")
        kwT = st.enter_context(nc.sbuf_tensor(f"kwT_{layer}", [64, 2, 1024], BF16))
        kwTB = [[Buf(f"kwT{g}_{r}") for r in range(2)] for g in range(2)]
        kcT, kcTB = sbl("kcT", [128, 528], BF16)
        vcT, vcTB = sbl("vcT", [128, 528], BF16)
        vsA = st.enter_context(nc.sbuf_tensor(f"vsA_{layer}", [128, 16, 2, 65], BF16))
        vsAB = [Buf(f"vsA{k}") for k in range(16)]
        vwA = st.enter_context(nc.sbuf_tensor(f"vwA_{layer}", [128, 8, 2, 65], BF16))
        vwAB = [Buf(f"vwA{k}") for k in range(8)]
        w1k, w1kB = sbl("w1k", [128, 32, 64], BF16)
        w1v, w1vB = sbl("w1v", [128, 32, 64], BF16)
        w2k, w2kB = sbl("w2k", [64, 64], BF16)
        w2v, w2vB = sbl("w2v", [64, 64], BF16)
        pek, pekB = sbl("pek", [32, 64], BF16)
        pev, pevB = sbl("pev", [32, 64], BF16)
        peT, peTB = sbl("peT", [64, 2, 32], BF16)
        pebias, pebiasB = sbl("pebias", [64, 2], F32)
        hidk, hidkB = sbl("hidk", [64, 2, 32], BF16)
        hidv, hidvB = sbl("hidv", [64, 2, 128], BF16)
        kcmpT, kcmpTB = sbl("kcmpT", [64, 2, 128], BF16)
        vcmp, vcmpB = sbl("vcmp", [128, 2, 97], BF16)
        ctab, ctabB = sbl("ctab", [128, 16, 32], F32)
        WsT, WsTB = sbl("WsT", [128, 8, 128], BF16)
        bs, bsB = sbl("bs", [128, 8], F32)
        lng, lngB = sbl("lng", [128, 512], F32)
        lnb, lnbB = sbl("lnb", [128, 512], F32)
        gates = st.enter_context(nc.sbuf_tensor(f"gates_{layer}", [128, 4, 24], F32))
        gatesB = [Buf(f"gates{t}") for t in range(4)]
        sza = st.enter_context(nc.sbuf_tensor(f"sza_{layer}", [128, 4, 512], BF16))
        szaB = [Buf(f"sza{t}") for t in range(4)]
        ug = sbln("ug", [128, 512], BF16, 2)
        szb = sbln("szb", [128, 512], BF16, 2)
        vg = sbln("vg", [128, 512], F32, 2)
        vln = sbln("vln", [128, 512], BF16, 2)
        lnst = sbln("lnst", [128, 16], F32, 2)
        acc_o = st.enter_context(nc.sbuf_tensor(f"acc_o_{layer}", [128, 4, 512], F32))
        acc_oB = [Buf(f"acc_o{h}") for h in range(8)]
        imp = sbln("imp", [128, 4, 32], F32, 2)
        top8 = sbln("top8", [128, 4, 8], F32, 2)
        selpads = [sbl(f"selpad{g}", [128, 4, 96], BF16) for g in range(2)]
        pTs = sbln("pT", [128, 512], BF16, 4)
        rcs = sbln("rc", [128, 16], F32, 4)
        amix = sbln("amix", [128, 512], BF16, 2)
        tmpb = sbln("tmpb", [128, 512], BF16, 3)

        for g in range(2):
            K.dma("pool", ksT[64:96, g, :], c_ebig[:, :], [], [ebB], ebB, max_dma_last_dim=4096)
        for (w1, w1B, src) in ((w1k, w1kB, e_k_w1), (w1v, w1vB, e_v_w1)):
            for half in range(2):
                K.dma("pool", w1[half * 64:(half + 1) * 64, :, :], src[li].rearrange("(l d) e -> d l e", d=64), [], [w1B], w1B)
        K.dma("pool", w2k[:, :], e_k_w2[li], [], [w2kB], w2kB)
        K.dma("pool", w2v[:, :], e_v_w2[li], [], [w2vB], w2vB)
        K.dma("pool", pek[:, :], e_k_pe[li], [], [pekB], pekB)
        K.dma("pool", pev[:, :], e_v_pe[li], [], [pevB], pevB)
        K.dma("sp", ctab[:, :, :], c_ctab[:, :, :], [], [ctabB], ctabB)
        wsst_t, wsstB = xa.next()
        wsst = wsst_t[:, :].rearrange("p (g j) -> p g j", g=8)
        K.dma("sp", wsst, e_sgu_w[li].rearrange("g i j -> i g j"), [], [wsstB], wsstB)
        bst, bstB = xa.next()
        K.dma("sp", bst[0:8, 0:128], e_sgu_b[li], [], [bstB], bstB)
        pt, pB = psA.next()
        K.op("pe", lambda e: e.transpose(pt[:, 0:8], bst[0:8, 0:128], identf[0:8, 0:8]), [bstB, identfB], [pB])
        K.op("dve", lambda e: e.tensor_copy(out=bs[:, :], in_=pt[:, 0:8]), [pB], [bsB])
        K.dma("sp", lng[:, :], e_ln_g[li:li + 1, :].to_broadcast([128, 512]), [], [lngB], lngB)
        K.dma("sp", lnb[:, :], e_ln_b[li:li + 1, :].to_broadcast([128, 512]), [], [lnbB], lnbB)
        K.op("pool", lambda e: e.memset(vsA[:, :, :, 64:65], 1.0), [], vsAB)
        K.op("pool", lambda e: e.memset(vwA[:, :, :, 64:65], 1.0), [], vwAB)
        K.op("pool", lambda e: e.memset(vcmp[:, :, :], 0.0), [], [vcmpB])
        K.op("pool", lambda e: e.memset(vcmp[:, :, 64:65], 1.0), [vcmpB], [vcmpB])
        for g in range(2):
            K.dma("pool", vcmp[0:127, g, 65:97], c_ovl[:, :], [], [vcmpB], vcmpB)
        K.op("pool", lambda e: e.memset(hidv[:, :, :], 0.0), [], [hidvB])
        K.op("pool", lambda e: e.memset(kcmpT[:, :, :], 0.0), [], [kcmpTB])
        for selpad, selpadB in selpads:
            K.op("pool", lambda e, selpad=selpad: e.memset(selpad[:, :, :], 0.0), [], [selpadB])
        K.op("dve", lambda e: e.memset(kcT[:, 0:16], 0.0), [], [kcTB])
        K.op("dve", lambda e: e.memset(vcT[:, 0:16], 0.0), [], [vcTB])
        for g in range(8):
            pt, pB = psA.next()
            K.op("pe", lambda e, g=g, pt=pt: e.transpose(pt[:, 0:128], wsst[:, g, :], identf[:, :]), [wsstB, identfB], [pB])
            K.op("dve", lambda e, g=g, pt=pt: e.tensor_tensor(out=WsT[:, g, :], in0=pt[:, 0:128], in1=trilk[:, :], op=ALU.mult),
                 [pB, trilkB], [WsTB])
        for xi, (pe_, peB_, w1, w1B) in enumerate(((pek, pekB, w1k, w1kB), (pev, pevB, w1v, w1vB))):
            pt, pB = psA.next()
            pb = pt[:].bitcast(BF16)
            K.op("pe", lambda e, pb=pb, pe_=pe_: e.transpose(pb[0:64, 0:32], pe_[:, :], identb[0:32, 0:32]), [peB_, identbB], [pB])
            K.op("dve", lambda e, pb=pb, xi=xi: e.tensor_copy(out=peT[:, xi, :], in_=pb[0:64, 0:32]), [pB], [peTB])
            pt2, pB2 = psA.next()
            for l in range(32):
                K.op("pe", lambda e, l=l, pt2=pt2, w1=w1, xi=xi: e.matmul(pt2[0:64, 0:1], w1[0:64, l, :], peT[:, xi, l:l + 1],
                                                                          start=(l == 0), stop=(l == 31)),
                     [w1B, peTB], [pB2], signal=(l == 31))
            K.op("dve", lambda e, pt2=pt2, xi=xi: e.tensor_copy(out=pebias[:, xi:xi + 1], in_=pt2[0:64, 0:1]), [pB2], [pebiasB])

        def evac_copy(i, out_ap, in_ap, reads, writes, scale=None):
            if i % 2 == 0:
                if scale is None:
                    K.op("act", lambda e: e.copy(out=out_ap, in_=in_ap), reads, writes)
                else:
                    K.op("act", lambda e: e.mul(out=out_ap, in_=in_ap, mul=scale), reads, writes)
            else:
                if scale is None:
                    K.op("dve", lambda e: e.tensor_copy(out=out_ap, in_=in_ap), reads, writes)
                else:
                    K.op("dve", lambda e: e.tensor_scalar(out=out_ap, in0=in_ap, scalar1=scale, scalar2=None, op0=ALU.mult),
                         reads, writes)

        if STOP == "params":
            st.close()
            return
        blocks = [(s_, b_) for s_ in range(NSEQ) for b_ in range(NBLK)]
        K.phase = "evA"
        phase_A(layer, 0, 0)
        for bi, (s, b) in enumerate(blocks):
            if True:
                t0 = b * 512
                K.phase = "evB"
                if STOP == "A":
                    st.close()
                    return
                for h in range(8):
                    pt, pB = fm_proj(C_Q + h * 64, 64)
                    evac_copy(h, qT[0:64, h, :], pt[0:64, 0:512], [pB], [qTB[h]], scale=0.125)
                for g in range(2):
                    pt, pB = fm_proj(C_KS + g * 64, 64)
                    evac_copy(g, ksT[0:64, g, t0:t0 + 512], pt[0:64, 0:512], [pB], [ksTB[g][b]])
                for g in range(2):
                    pt, pB = fm_proj(C_KW + g * 64, 64)
                    r = b % 2
                    evac_copy(g + 1, kwT[0:64, g, r * 512:(r + 1) * 512], pt[0:64, 0:512], [pB], [kwTB[g][r]])
                for i, (cX, XT, XTB) in enumerate(((C_KC, kcT, kcTB), (C_VC, vcT, vcTB))):
                    if b > 0:
                        K.op("pool", lambda e, XT=XT: e.tensor_copy(out=XT[:, 0:16], in_=XT[:, 512:528]), [XTB], [XTB])
                    pt, pB = fm_proj(cX, 128)
                    evac_copy(i, XT[:, 16:528], pt[:, 0:512], [pB], [XTB])
                if STOP == "B":
                    st.close()
                    return
                K.phase = "evC"
                cst = {}
                cst2 = {}

                def c_front(tt):
                    kt = b * 4 + tt
                    pt, pB = tm_proj(tt, C_VS, 408)
                    K.op("dve", lambda e, pt=pt, kt=kt: e.tensor_copy(out=vsA[:, kt, :, 0:64],
                                                                      in_=pt[:, 0:128].rearrange("p (g d) -> p g d", g=2)),
                         [pB], [vsAB[kt]])
                    K.op("dve", lambda e, pt=pt, kt=kt: e.tensor_copy(out=vwA[:, kt % 8, :, 0:64],
                                                                      in_=pt[:, 256:384].rearrange("p (g d) -> p g d", g=2)),
                         [pB], [vwAB[kt % 8]])
                    K.op("act", lambda e, pt=pt, tt=tt: e.activation(out=gates[:, tt, :], in_=pt[:, 384:408], func=AF.Sigmoid),
                         [pB], [gatesB[tt]])
                    pt, pB = tm_proj(tt, C_ZA, 512)
                    K.op("act", lambda e, pt=pt, tt=tt: e.activation(out=sza[:, tt, :], in_=pt[:, 0:512], func=AF.Silu),
                         [pB], [szaB[tt]])
                    pt, pB = tm_proj(tt, C_ZB, 512)
                    zt, ztB = szb.next()
                    K.op("act", lambda e, pt=pt, zt=zt: e.activation(out=zt[:, :], in_=pt[:, 0:512], func=AF.Silu), [pB], [ztB])
                    pt, pB = tm_proj(tt, C_U, 512)
                    ut, utB = ug.next()
                    K.op("act", lambda e, pt=pt, ut=ut: e.activation(out=ut[:, :], in_=pt[:, 0:512], func=AF.Gelu_apprx_tanh),
                         [pB], [utB])
                    pt, pB = tm_proj(tt, C_V, 512)
                    vt, vtB = vg.next()
                    K.op("act", lambda e, pt=pt, vt=vt: e.activation(out=vt[:, :], in_=pt[:, 0:512], func=AF.Gelu_apprx_tanh),
                         [pB], [vtB])
                    K.op("pool", lambda e, ut=ut, zt=zt: e.tensor_tensor(out=ut[:, :], in0=ut[:, :], in1=zt[:, :], op=ALU.mult),
                         [utB, ztB], [utB])
                    ls, lsB = lnst.next()
                    K.op("dve", lambda e, ls=ls, vt=vt: e.bn_stats(out=ls[:, 0:6], in_=vt[:, :]), [vtB], [lsB])
                    K.op("dve", lambda e, ls=ls: e.bn_aggr(out=ls[:, 6:8], in_=ls[:, 0:6]), [lsB], [lsB])
                    K.op("dve", lambda e, ls=ls: e.tensor_scalar(out=ls[:, 8:9], in0=ls[:, 7:8], scalar1=LN_EPS, scalar2=None,
                                                                op0=ALU.add), [lsB], [lsB])
                    K.op("pool", lambda e, ls=ls: e.tensor_tensor(out=ls[:, 9:10], in0=ls[:, 8:9], in1=neghalf[:, 0:1], op=ALU.pow),
                         [lsB, neghalfB], [lsB])
                    K.op("dve", lambda e, ls=ls, vt=vt: e.tensor_scalar(out=vt[:, :], in0=vt[:, :], scalar1=ls[:, 6:7],
                                                                        scalar2=ls[:, 9:10], op0=ALU.subtract, op1=ALU.mult),
                         [lsB, vtB], [vtB])
                    K.op("pool", lambda e, vt=vt: e.tensor_tensor(out=vt[:, :], in0=vt[:, :], in1=lng[:, :], op=ALU.mult),
                         [vtB, lngB], [vtB])
                    vl, vlB = vln.next()
                    K.op("pool", lambda e, vt=vt, vl=vl: e.tensor_tensor(out=vl[:, :], in0=vt[:, :], in1=lnb[:, :], op=ALU.add),
                         [vtB, lnbB], [vlB])
                    cst[tt] = (vl, vlB, ut, utB)

                def c_back(tt):
                    vl, vlB, ut, utB = cst.pop(tt)
                    pt, pB = psB.next()
                    for g in range(8):
                        K.op("pe", lambda e, g=g, pt=pt, vl=vl: e.matmul(pt[:, g * 64:(g + 1) * 64], WsT[:, g, :],
                                                                          vl[:, g * 64:(g + 1) * 64], start=True, stop=True),
                             [WsTB, vlB], [pB], signal=(g == 7))
                    tb, tbB = tmpb.next()
                    K.op("dve", lambda e, pt=pt, tb=tb: e.tensor_tensor(
                        out=tb[:, :].rearrange("p (g d) -> p g d", g=8), in0=pt[:, 0:512].rearrange("p (g d) -> p g d", g=8),
                        in1=bs[:, :].unsqueeze(2).to_broadcast([128, 8, 64]), op=ALU.add), [pB, bsB], [tbB])
                    K.op("pool", lambda e, tb=tb, ut=ut: e.tensor_tensor(out=tb[:, :], in0=tb[:, :], in1=ut[:, :], op=ALU.mult),
                         [tbB, utB], [tbB])
                    cst2[tt] = (tb, tbB)

                def c_back2(tt):
                    tb, tbB = cst2.pop(tt)
                    pt, pB = psA.next()
                    pb = pt[:].bitcast(BF16)
                    for fc in range(4):
                        K.op("pe", lambda e, fc=fc, pb=pb, tb=tb: e.transpose(pb[:, fc * 128:(fc + 1) * 128],
                                                                              tb[:, fc * 128:(fc + 1) * 128], identb[:, :]),
                             [tbB, identbB], [pB], signal=(fc == 3))
                    K.op("dve", lambda e, pb=pb, tt=tt: e.tensor_copy(out=mixT[:, 4:8, tt * 128:(tt + 1) * 128],
                                                                      in_=pb[:, 0:512].rearrange("p (k t) -> p k t", k=4)),
                         [pB], [mixTB[tt]])

                for tt in range(6):
                    if tt < 4:
                        c_front(tt)
                    if 1 <= tt <= 4:
                        c_back(tt - 1)
                    if tt >= 2:
                        c_back2(tt - 2)
                if STOP == "C":
                    st.close()
                    return
                if bi + 1 < len(blocks):
                    K.phase = "evA"
                    phase_A(layer, blocks[bi + 1][0], blocks[bi + 1][1])
                elif layer + 1 < n_layers:
                    load_w_in(layer + 1)
                K.phase = "evD"
                if b == 0:
                    n0, nn, c0 = 0, 31, 16
                else:
                    n0, nn, c0 = 32 * b - 1, 32, 0
                nk = 32 * (b + 1) - 1
                for g in range(2):
                    for xi, (XT, XTB, w1, w1B, hid, hidB, hcol) in enumerate((
                            (kcT, kcTB, w1k, w1kB, hidk, hidkB, 0), (vcT, vcTB, w1v, w1vB, hidv, hidvB, n0))):
                        pt, pB = psA.next()
                        for l in range(32):
                            K.op("pe", lambda e, l=l, pt=pt, XT=XT, w1=w1: e.matmul(
                                pt[0:64, 0:nn], w1[g * 64:(g + 1) * 64, l, :],
                                XT[g * 64:(g + 1) * 64, c0 + l:c0 + l + 16 * (nn - 1) + 1:16], start=(l == 0), stop=(l == 31)),
                                 [w1B, XTB], [pB], signal=(l == 31))
                        K.op("act", lambda e, pt=pt, hid=hid, hcol=hcol, xi=xi: e.activation(
                            out=hid[:, g, hcol:hcol + nn], in_=pt[0:64, 0:nn], func=AF.Silu, bias=pebias[:, xi:xi + 1]),
                             [pB, pebiasB], [hidB])
                    pt, pB = psA.next()
                    K.op("pe", lambda e, pt=pt: e.matmul(pt[0:64, 0:nn], w2k[:, :], hidk[:, g, 0:nn], start=True, stop=True),
                         [w2kB, hidkB], [pB])
                    K.op("dve", lambda e, pt=pt: e.tensor_copy(out=kcmpT[:, g, n0:n0 + nn], in_=pt[0:64, 0:nn]), [pB], [kcmpTB])
                    pt, pB = psA.next()
                    K.op("pe", lambda e, pt=pt: e.matmul(pt[0:nk, 0:64], hidv[:, g, 0:nk], w2v[:, :], start=True, stop=True),
                         [w2vB, hidvB], [pB])
                    K.op("dve", lambda e, pt=pt: e.tensor_copy(out=vcmp[0:nk, g, 0:64], in_=pt[0:nk, 0:64]), [pB], [vcmpB])
                if STOP == "D":
                    st.close()
                    return
                K.phase = "evE"
                for g in range(2):
                    selpad, selpadB = selpads[g]
                    im, imB = imp.next()
                    for r in range(4):
                        h = g * 4 + r
                        pt, pB = psA.next()
                        K.op("pe", lambda e, pt=pt, h=h: e.matmul(pt[0:nk, 0:512], kcmpT[:, g, 0:nk], qT[0:64, h, :],
                                                                  start=True, stop=True), [kcmpTB, qTB[h]], [pB])
                        pT, pTB = pTs.next()
                        K.op("act", lambda e, pt=pt, pT=pT: e.activation(out=pT[0:nk, :], in_=pt[0:nk, 0:512], func=AF.Exp),
                             [pB], [pTB])
                        if STOP == "E1":
                            st.close()
                            return
                        K.op("pool", lambda e, pT=pT: e.affine_select(out=pT[0:nk, :], in_=pT[0:nk, :], pattern=[[1, 512]],
                                                                      compare_op=ALU.is_ge, fill=0.0, base=t0 - 31,
                                                                      channel_multiplier=-16), [pTB], [pTB])
                        if STOP == "E2":
                            st.close()
                            return
                        po, poB = psB.next()
                        po3 = po[:, 0:388].rearrange("p (t c) -> p t c", t=4)
                        for tt in range(4):
                            K.op("pe", lambda e, tt=tt, po=po, pT=pT: e.matmul(po[:, tt * 97:(tt + 1) * 97],
                                                                                pT[0:nk, tt * 128:(tt + 1) * 128], vcmp[0:nk, g, :],
                                                                                start=True, stop=True),
                                 [pTB, vcmpB], [poB], signal=(tt == 3))
                        if STOP == "E3":
                            st.close()
                            return
                        rc, rcB = rcs.next()
                        K.op("dve", lambda e, rc=rc, po3=po3: e.tensor_scalar(out=rc[:, 0:4], in0=po3[:, :, 64], scalar1=1e-30,
                                                                              scalar2=None, op0=ALU.max), [poB], [rcB])
                        K.op("dve", lambda e, rc=rc: e.reciprocal(out=rc[:, 4:8], in_=rc[:, 0:4]), [rcB], [rcB])
                        K.op("dve", lambda e, rc=rc, h=h: e.tensor_tensor(out=rc[:, 8:12], in0=rc[:, 4:8], in1=gates[:, :, h],
                                                                          op=ALU.mult), [rcB] + gatesB, [rcB])
                        for tt in range(4):
                            K.op("dve", lambda e, tt=tt, po3=po3, rc=rc, h=h: e.tensor_scalar(
                                out=acc_o[:, tt, h * 64:(h + 1) * 64], in0=po3[:, tt, 0:64], scalar1=rc[:, 8 + tt:9 + tt],
                                scalar2=None, op0=ALU.mult), [poB, rcB], [acc_oB[h]])
                        for tt in range(4):
                            if r == 0:
                                K.op("dve", lambda e, tt=tt, po3=po3, rc=rc, im=im: e.tensor_scalar(
                                    out=im[:, tt, :], in0=po3[:, tt, 65:97], scalar1=rc[:, 4 + tt:5 + tt], scalar2=None,
                                    op0=ALU.mult), [poB, rcB], [imB])
                            else:
                                K.op("dve", lambda e, tt=tt, po3=po3, rc=rc, im=im: e.scalar_tensor_tensor(
                                    out=im[:, tt, :], in0=po3[:, tt, 65:97], scalar=rc[:, 4 + tt:5 + tt], in1=im[:, tt, :],
                                    op0=ALU.mult, op1=ALU.add), [poB, rcB, imB], [imB])
                    if STOP == "E4":
                        st.close()
                        return
                    K.op("dve", lambda e, im=im: e.tensor_tensor(out=im[:, :, :], in0=im[:, :, :], in1=ctab[:, 4 * b:4 * b + 4, :],
                                                                 op=ALU.add), [imB, ctabB], [imB])
                    t8, t8B = top8.next()
                    for tt in range(4):
                        K.op("dve", lambda e, tt=tt, t8=t8, im=im: e.max(out=t8[:, tt, :], in_=im[:, tt, :]), [imB], [t8B])
                    for tt in range(4):
                        K.op("dve", lambda e, tt=tt, t8=t8, im=im: e.tensor_scalar(
                            out=selpad[:, tt, 64:96], in0=im[:, tt, :], scalar1=t8[:, tt, 7:8], scalar2=-1.0,
                            op0=ALU.is_ge, op1=ALU.add), [imB, t8B], [selpadB])
                    if STOP == "E6":
                        st.close()
                        return
                for g in range(2):
                    selpad, selpadB = selpads[g]
                    pt, pB = psA.next()
                    pb = pt[:].bitcast(BF16)
                    for tt in range(4):
                        K.op("pe", lambda e, tt=tt, pb=pb: e.transpose(pb[0:96, tt * 128:(tt + 1) * 128], selpad[:, tt, :],
                                                                       identb[:, :]), [selpadB, identbB], [pB], signal=(tt == 3))
                    for r in range(4):
                        h = g * 4 + r
                        K.op("dve", lambda e, h=h, pb=pb: e.tensor_copy(out=qT[64:96, h, :], in_=pb[64:96, 0:512]), [pB], [qSB[h]])
                if STOP == "E":
                    st.close()
                    return
                K.phase = "evF"
                items = []
                for h in range(8):
                    for br in (1, 0):
                        kts = list(range(0, 4 * b + 4)) if br == 0 else list(range(max(0, 4 * b - 4), 4 * b + 4))
                        for kt in kts:
                            items.append((h, br, kt, kt == kts[0], kt == kts[-1]))
                LA = 2
                stage = {}
                accs = {}

                def front(idx):
                    h, br, kt, isfirst, islast = items[idx]
                    g = h // 4
                    d = kt - 4 * b
                    lo = max(0, d)
                    hi = 3 if br == 0 else min(3, d + 4)
                    c0_, c1_ = lo * 128, (hi + 1) * 128
                    pt, pB = psA.next()
                    if br == 0:
                        K.op("pe", lambda e: e.matmul(
                            pt[:, c0_:c1_], ksT[0:96, g, kt * 128:(kt + 1) * 128], qT[0:96, h, c0_:c1_],
                            start=True, stop=True), [ksTB[g][kt // 4], ebB, qTB[h], qSB[h]], [pB])
                    else:
                        kr = kt % 8
                        K.op("pe", lambda e: e.matmul(
                            pt[:, c0_:c1_], kwT[0:64, g, kr * 128:(kr + 1) * 128], qT[0:64, h, c0_:c1_],
                            start=True, stop=True), [kwTB[g][kr // 4], qTB[h]], [pB])
                    pT, pTB = pTs.next()
                    K.op("act", lambda e: e.activation(out=pT[:, c0_:c1_], in_=pt[:, c0_:c1_], func=AF.Exp), [pB], [pTB])
                    if d >= 0:
                        K.op("pool", lambda e: e.tensor_tensor(
                            out=pT[:, d * 128:(d + 1) * 128], in0=pT[:, d * 128:(d + 1) * 128], in1=trilk[:, :],
                            op=ALU.mult), [pTB, trilkB], [pTB])
                    if br == 1 and 0 <= d + 4 <= 3:
                        K.op("pool", lambda e: e.tensor_tensor(
                            out=pT[:, (d + 4) * 128:(d + 5) * 128], in0=pT[:, (d + 4) * 128:(d + 5) * 128],
                            in1=fark[:, :], op=ALU.mult), [pTB, farkB], [pTB])
                    stage[idx] = (pT, pTB, lo, hi)

                def back(idx):
                    h, br, kt, isfirst, islast = items[idx]
                    g = h // 4
                    pT, pTB, lo, hi = stage.pop(idx)
                    if isfirst:
                        accs[(h, br)] = psB.next()
                    acc, accB = accs[(h, br)]
                    for tt in range(lo, hi + 1):
                        if br == 0:
                            vB_, rhs = vsAB[kt], vsA[:, kt, g, :]
                        else:
                            vB_, rhs = vwAB[kt % 8], vwA[:, kt % 8, g, :]
                        K.op("pe", lambda e, tt=tt, rhs=rhs: e.matmul(
                            acc[:, tt * 65:(tt + 1) * 65], pT[:, tt * 128:(tt + 1) * 128], rhs,
                            start=(isfirst and tt == lo), stop=True, skip_group_check=True), [pTB, vB_], [accB], signal=(tt == hi))
                    if islast:
                        acc3 = acc[:, 0:260].rearrange("p (t c) -> p t c", t=4)
                        rc, rcB = rcs.next()
                        K.op("dve", lambda e: e.tensor_scalar(out=rc[:, 0:4], in0=acc3[:, :, 64], scalar1=1e-30,
                                                              scalar2=None, op0=ALU.max), [accB], [rcB])
                        K.op("dve", lambda e: e.reciprocal(out=rc[:, 4:8], in_=rc[:, 0:4]), [rcB], [rcB])
                        gi = (1 + br) * 8 + h
                        K.op("dve", lambda e: e.tensor_tensor(out=rc[:, 8:12], in0=rc[:, 4:8], in1=gates[:, :, gi],
                                                              op=ALU.mult), [rcB] + gatesB, [rcB])
                        for tt in range(4):
                            K.op("dve", lambda e, tt=tt: e.scalar_tensor_tensor(
                                out=acc_o[:, tt, h * 64:(h + 1) * 64], in0=acc3[:, tt, 0:64], scalar=rc[:, 8 + tt:9 + tt],
                                in1=acc_o[:, tt, h * 64:(h + 1) * 64], op0=ALU.mult, op1=ALU.add),
                                 [accB, rcB, acc_oB[h]], [acc_oB[h]])
                        del accs[(h, br)]

                for idx in range(len(items) + LA):
                    if idx < len(items):
                        front(idx)
                    if idx - LA >= 0:
                        back(idx - LA)
                for tt in range(4):
                    am, amB = amix.next()
                    K.op("pool", lambda e, tt=tt, am=am: e.tensor_tensor(out=am[:, :], in0=acc_o[:, tt, :], in1=sza[:, tt, :],
                                                                         op=ALU.mult), acc_oB + [szaB[tt]], [amB])
                    pt, pB = psA.next()
                    pb = pt[:].bitcast(BF16)
                    for fc in range(4):
                        K.op("pe", lambda e, fc=fc, pb=pb, am=am: e.transpose(pb[:, fc * 128:(fc + 1) * 128],
                                                                              am[:, fc * 128:(fc + 1) * 128], identb[:, :]),
                             [amB, identbB], [pB], signal=(fc == 3))
                    K.op("dve", lambda e, pb=pb, tt=tt: e.tensor_copy(out=mixT[:, 0:4, tt * 128:(tt + 1) * 128],
                                                                      in_=pb[:, 0:512].rearrange("p (k t) -> p k t", k=4)),
                         [pB], [mixTB[tt]])
                if STOP == "F":
                    st.close()
                    return
                K.phase = "evG"
                phase_G(layer, s, b)
                if STOP == "G":
                    st.close()
                    return
        st.close()

    def odd_layer(layer, li):
        st = ExitStack()

        def sbl(name, shape, dt=F32):
            t = st.enter_context(nc.sbuf_tensor(f"{name}_{layer}", list(shape), dt))
            return t, Buf(name)

        def sbln(name, shape, dt, n):
            return Rot([sbl(f"{name}{i}", shape, dt) for i in range(n)])

        yc = st.enter_context(nc.sbuf_tensor(f"yc_{layer}", [128, 8, 544], BF16))
        ycB = [Buf(f"yc{c}") for c in range(8)]
        szzs = [(st.enter_context(nc.sbuf_tensor(f"szz{i}_{layer}", [128, 8, 512], BF16)), [Buf(f"szz{i}_{c}") for c in range(8)])
                for i in range(2)]
        yv = st.enter_context(nc.sbuf_tensor(f"yv_{layer}", [128, 8, 512], F32))
        yvB = [Buf(f"yv{c}") for c in range(8)]
        diag = sbln("diag", [128, 31, 128], BF16, 3)
        sig = sbln("sig", [128, 512], F32, 2)
        ybf = sbln("ybf", [128, 512], BF16, 3)
        ysq = sbln("ysq", [128, 512], BF16, 3)
        onesb, onesbB = sbl("onesb", [128, 128], BF16)
        mean, meanB = sbl("mean", [128, 512], F32)
        rstdt, rstdtB = sbl("rstdt", [128, 512], F32)
        var, varB = sbl("var", [128, 512], F32)
        t1 = sbln("t1", [128, 512], F32, 2)

        K.op("pool", lambda e: e.memset(onesb[:, :], 1.0 / D), [], [onesbB])

        blocks = [(s_, b_) for s_ in range(NSEQ) for b_ in range(NBLK)]
        K.phase = "odA"
        phase_A(layer, 0, 0)
        def od_proj(bi, mid=None):
            s, b = blocks[bi]
            szz, szzB = szzs[bi % 2]
            K.phase = "odProj"
            for c in range(8):
                if b == 0:
                    K.op("pool", lambda e: e.memset(yc[:, c, 0:32], 0.0), [], [ycB[c]])
                else:
                    K.op("pool", lambda e: e.tensor_copy(out=yc[:, c, 0:32], in_=yc[:, c, 512:544]), [ycB[c]], [ycB[c]])
                pg, pgB = fm_proj(1024 + c * 128, 128)
                sg, sgB = sig.next()
                K.op("act", lambda e: e.activation(out=sg[:, :], in_=pg[:, 0:512], func=AF.Sigmoid), [pgB], [sgB])
                pa, paB = fm_proj(c * 128, 128)
                K.op("dve", lambda e: e.tensor_tensor(out=yc[:, c, 32:544], in0=pa[:, 0:512], in1=sg[:, :], op=ALU.mult),
                     [paB, sgB], [ycB[c]])
            if mid is not None:
                mid()
                K.phase = "odProj"
            for c in range(8):
                pz, pzB = fm_proj(2048 + c * 128, 128)
                K.op("act", lambda e: e.activation(out=szz[:, c, :], in_=pz[:, 0:512], func=AF.Silu), [pzB], [szzB[c]])

        def od_conv(bi):
            K.phase = "odConv"
            pm, pmB = psB.next()
            pq, pqB = psB.next()
            dgs = {}

            def build_diag(c):
                dg, dgB = diag.next()
                K.op("pool" if c % 2 == 0 else "dve", lambda e: e.tensor_tensor(
                    out=dg[:, :, :], in0=identb[:, :].unsqueeze(1).to_broadcast([128, 31, 128]),
                    in1=par[:, c, 1:32].unsqueeze(2).to_broadcast([128, 31, 128]), op=ALU.mult), [identbB, parB], [dgB])
                dgs[c] = (dg, dgB)

            for c in range(3):
                build_diag(c)
            pend_stats = []

            def emit_stats():
                c_, yb_, ybB_, yq_, yqB_ = pend_stats.pop(0)
                K.op("pe", lambda e: e.matmul(pm[:, 0:512], onesb[:, :], yb_[:, :], start=(c_ == 0), stop=(c_ == 7)),
                     [onesbB, ybB_], [pmB], signal=(c_ == 7))
                K.op("pe", lambda e: e.matmul(pq[:, 0:512], onesb[:, :], yq_[:, :], start=(c_ == 0), stop=(c_ == 7)),
                     [onesbB, yqB_], [pqB], signal=(c_ == 7))

            for c in range(8):
                dg, dgB = dgs[c]
                pc, pcB = psA.next()
                for k in range(31):
                    K.op("pe", lambda e, k=k: e.matmul(pc[:, 0:512], dg[:, k, :], yc[:, c, 2 + k:2 + k + 512],
                                                       start=(k == 0), stop=(k == 30)),
                         [dgB, ycB[c]], [pcB], signal=(k == 30))
                if c + 3 < 8:
                    build_diag(c + 3)
                if pend_stats:
                    emit_stats()
                K.op("act", lambda e: e.activation(out=yv[:, c, :], in_=pc[:, 0:512], func=AF.Identity,
                                                   bias=par[:, c, 32:33]), [pcB, parB], [yvB[c]])
                yb, ybB = ybf.next()
                yq, yqB = ysq.next()
                K.op("dve", lambda e: e.tensor_copy(out=yb[:, :], in_=yv[:, c, :]), [yvB[c]], [ybB])
                K.op("pool", lambda e: e.tensor_tensor(out=yq[:, :], in0=yv[:, c, :], in1=yv[:, c, :], op=ALU.mult),
                     [yvB[c]], [yqB])
                pend_stats.append((c, yb, ybB, yq, yqB))
            while pend_stats:
                emit_stats()
            K.phase = "odLN"
            K.op("dve", lambda e: e.tensor_copy(out=mean[:, :], in_=pm[:, 0:512]), [pmB], [meanB])
            K.op("dve", lambda e: e.tensor_tensor(out=var[:, :], in0=mean[:, :], in1=mean[:, :], op=ALU.mult), [meanB], [varB])
            K.op("dve", lambda e: e.scalar_tensor_tensor(out=var[:, :], in0=pq[:, 0:512], scalar=LN_EPS, in1=var[:, :],
                                                         op0=ALU.add, op1=ALU.subtract), [pqB, varB], [varB])

        def od_ln_prep2():
            K.phase = "odLN"
            K.op("act", lambda e: e.activation(out=var[:, :], in_=var[:, :], func=AF.Sqrt), [varB], [varB])
            K.op("dve", lambda e: e.reciprocal(out=rstdt[:, :], in_=var[:, :]), [varB], [rstdtB])
            for c in range(8):
                K.op("dve", lambda e: e.tensor_tensor(out=yv[:, c, :], in0=yv[:, c, :], in1=mean[:, :], op=ALU.subtract),
                     [yvB[c], meanB], [yvB[c]])
                K.op("pool", lambda e: e.tensor_tensor(out=yv[:, c, :], in0=yv[:, c, :], in1=rstdt[:, :], op=ALU.mult),
                     [yvB[c], rstdtB], [yvB[c]])

        def od_ln_fin(bi):
            szz, szzB = szzs[bi % 2]
            K.phase = "odLNf"
            for c in range(8):
                ta, taB = t1.next()
                K.op("act", lambda e: e.activation(out=ta[:, :], in_=yv[:, c, :], func=AF.Silu,
                                                   scale=par[:, c, 33:34], bias=par[:, c, 34:35]),
                     [yvB[c], parB], [taB])
                K.op("dve", lambda e: e.tensor_tensor(out=mixT[:, c, :], in0=ta[:, :], in1=szz[:, c, :], op=ALU.mult),
                     [taB, szzB[c]], mixTB)

        od_proj(0)
        for bi, (s, b) in enumerate(blocks):
            if bi + 1 < len(blocks):
                K.phase = "odA"
                phase_A(layer, blocks[bi + 1][0], blocks[bi + 1][1])
            elif layer + 1 < n_layers:
                load_w_in(layer + 1)
            od_conv(bi)
            if bi + 1 < len(blocks):
                od_proj(bi + 1, mid=od_ln_prep2)
            else:
                od_ln_prep2()
            od_ln_fin(bi)
            K.phase = "odG"
            phase_G(layer, s, b)
        st.close()

    load_w_in(0)
    for layer in range(n_layers):
        li = layer // 2
        load_common(layer)
        if layer > 0:
            K.barrier()
        if layer % 2 == 0:
            even_layer(layer, li)
        else:
            odd_layer(layer, li)

    for s in range(NSEQ):
        for t in range(16):
            b = xd[s][t]
            if b.w is not None:
                sm, v = b.w
                nc.sync.wait_ge(sm.h, v)
    stack.close()
    return nc, K


def make_consts():
    p = np.arange(128)
    ident = np.eye(128, dtype=np.float32)
    tril = (p[:, None] <= p[None, :]).astype(np.float32)
    far = (p[:, None] > p[None, :]).astype(np.float32)
    ctab = np.zeros((128, 16, 32), np.float32)
    j = np.arange(32)
    for tile in range(16):
        t = tile * 128 + p
        cur = t // 64
        forced = (j[None, :] == 0) | (j[None, :] == cur[:, None]) | (j[None, :] == cur[:, None] - 1)
        causal = j[None, :] <= cur[:, None]
        ctab[:, tile, :] = np.where(causal, forced.astype(np.float32) * np.float32(1e4), np.float32(-1e30))
    ebig = ((np.arange(2048)[None, :] // 64) == j[:, None]).astype(np.float32) * np.float32(BIG)
    n_cmp = 127
    tok = np.arange(n_cmp)[:, None] * 16 + np.arange(32)[None, :]
    ovl = ((tok[:, :, None] // 64) == np.arange(32)[None, None, :]).sum(1).astype(np.float32) / np.float32(32)
    return {"c_ident": ident, "c_tril": tril, "c_far": far, "c_ctab": ctab, "c_ebig": ebig, "c_ovl": ovl}


_CACHE = {}


def kernel(**inputs):
    if "nc" not in _CACHE:
        _CACHE["nc"] = build_program(4)[0]
    nc = _CACHE["nc"]
    consts = make_consts()
    x = np.ascontiguousarray(inputs["x"], dtype=np.float32)
    shared = {k: np.ascontiguousarray(v, dtype=np.float32) for k, v in inputs.items() if k != "x"}
    shared.update(consts)
    in_maps = []
    for c in range(NCORES):
        m = dict(shared)
        m["x"] = x[c * NSEQ:(c + 1) * NSEQ]
        in_maps.append(m)
    res = run_bass_kernel_spmd(nc, in_maps, core_ids=list(range(NCORES)))
    return np.concatenate([r["out"] for r in res.results], axis=0).astype(np.float32)
```

```python
from contextlib import ExitStack

import numpy as np
import concourse.bass as bass
import concourse.mybir as mybir
from concourse.bass_utils import run_bass_kernel_spmd

F32 = mybir.dt.float32
BF16 = mybir.dt.bfloat16
AF = mybir.ActivationFunctionType
ALU = mybir.AluOpType

NCORES = 8
SEQ = 2048
D = 1024
NSEQ = 2
NBLK = 4
EVEN_W = 3352
ODD_W = 3072
RMS_EPS = 1e-6
LN_EPS = 1e-5
BIG = 30000.0
import os as _os
STOP = _os.environ.get("KSTOP", "")

C_Q, C_KC, C_VC, C_KS, C_VS, C_KW, C_VW, C_GATE, C_ZA, C_U, C_V, C_ZB = (
    0, 512, 640, 768, 896, 1024, 1152, 1280, 1304, 1816, 2328, 2840)


class Sem:
    __slots__ = ("h", "val", "name")

    def __init__(self, h, name=""):
        self.h = h
        self.val = 0
        self.name = name


class Buf:
    __slots__ = ("name", "w", "r", "dsem")

    def __init__(self, name):
        self.name = name
        self.w = None
        self.r = {}
        self.dsem = None


class Trk:
    def __init__(self, nc, stack):
        self.nc = nc
        self.stack = stack
        self.eng = {"pe": nc.tensor, "act": nc.scalar, "dve": nc.vector, "pool": nc.gpsimd, "sp": nc.sync}
        self.nsem = 0
        self.esem = {e: self.newsem("e_" + e) for e in self.eng}
        self.waited = {e: {} for e in self.eng}
        self.nops = 0
        self.dsems = []
        self.phase = ""
        self.waitlog = {e: [] for e in self.eng}

    def newsem(self, name):
        self.nsem += 1
        return Sem(self.stack.enter_context(self.nc.semaphore(f"{name}_{self.nsem}")), name)

    def _sync(self, E, reads, writes):
        deps = {}
        for b in reads:
            if b.w is not None:
                s, v = b.w
                if deps.get(s, 0) < v:
                    deps[s] = v
        for b in writes:
            if b.w is not None:
                s, v = b.w
                if deps.get(s, 0) < v:
                    deps[s] = v
            for s, v in b.r.items():
                if deps.get(s, 0) < v:
                    deps[s] = v
        eng = self.eng[E]
        w = self.waited[E]
        own = self.esem[E]
        for s, v in deps.items():
            if E == "pe" and s is own:
                continue
            if w.get(s, 0) < v:
                eng.wait_ge(s.h, v)
                w[s] = v
                self.waitlog[E].append((self.phase, s.name))

    def op(self, E, fn, reads=(), writes=(), signal=True):
        self._sync(E, reads, writes)
        ins = fn(self.eng[E])
        s = self.esem[E]
        if signal:
            s.val += 1
            ins.then_inc(s.h, 1)
            tag = (s, s.val)
        else:
            tag = (s, s.val + 1)
        for b in reads:
            if b.r.get(s, 0) < tag[1]:
                b.r[s] = tag[1]
        for b in writes:
            b.w = tag
            b.r = {}
        self.nops += 1
        return ins

    def barrier(self):
        sems = list(self.esem.values()) + self.dsems
        for E, eng in self.eng.items():
            w = self.waited[E]
            for s in sems:
                if s.val > 0 and w.get(s, 0) < s.val:
                    eng.wait_ge(s.h, s.val)
                    w[s] = s.val

    def dma(self, E, out, in_, reads, writes, dbuf, **kw):
        self._sync(E, reads, writes)
        if dbuf.dsem is None:
            dbuf.dsem = self.newsem("d_" + dbuf.name)
            self.dsems.append(dbuf.dsem)
        s = dbuf.dsem
        ins = self.eng[E].dma_start(out=out, in_=in_, **kw)
        s.val += 16
        ins.then_inc(s.h, 16)
        tag = (s, s.val)
        for b in reads:
            if b.r.get(s, 0) < tag[1]:
                b.r[s] = tag[1]
        for b in writes:
            b.w = tag
            b.r = {}
        self.nops += 1
        return ins


class Rot:
    def __init__(self, items):
        self.items = items
        self.i = 0

    def next(self):
        it = self.items[self.i % len(self.items)]
        self.i += 1
        return it


def build_program(n_layers=4):
    nc = bass.Bass("TRN2", target_bir_lowering=False, dynamic_dma_scratch_size=16384)
    stack = ExitStack()
    K = Trk(nc, stack)

    def dram(name, shape, dt=F32, kind="ExternalInput"):
        return nc.dram_tensor(name, list(shape), dt, kind=kind).ap()

    x_in = dram("x", [NSEQ, SEQ, D])
    out = dram("out", [NSEQ, SEQ, D], kind="ExternalOutput")
    norm_pre = dram("norm_pre", [4, D])
    norm_post = dram("norm_post", [4, D])
    e_w_in = dram("even_w_in", [2, D, EVEN_W])
    e_k_pe = dram("even_cmp_k_pe", [2, 32, 64])
    e_k_w1 = dram("even_cmp_k_w1", [2, 2048, 64])
    e_k_w2 = dram("even_cmp_k_w2", [2, 64, 64])
    e_v_pe = dram("even_cmp_v_pe", [2, 32, 64])
    e_v_w1 = dram("even_cmp_v_w1", [2, 2048, 64])
    e_v_w2 = dram("even_cmp_v_w2", [2, 64, 64])
    e_ln_g = dram("even_sgu_ln_g", [2, 512])
    e_ln_b = dram("even_sgu_ln_b", [2, 512])
    e_sgu_w = dram("even_sgu_w", [2, 8, 128, 128])
    e_sgu_b = dram("even_sgu_b", [2, 8, 128])
    e_w_out = dram("even_w_out", [2, D, D])
    o_w_in = dram("odd_w_in", [2, D, ODD_W])
    o_dw_w = dram("odd_dw_w", [2, 31, D])
    o_dw_b = dram("odd_dw_b", [2, D])
    o_ln_g = dram("odd_ln_g", [2, D])
    o_ln_b = dram("odd_ln_b", [2, D])
    o_w_out = dram("odd_w_out", [2, D, D])
    c_ident = dram("c_ident", [128, 128])
    c_tril = dram("c_tril", [128, 128])
    c_far = dram("c_far", [128, 128])
    c_ctab = dram("c_ctab", [128, 16, 32])
    c_ebig = dram("c_ebig", [32, 2048])
    c_ovl = dram("c_ovl", [127, 32])

    def sb(name, shape, dt=F32):
        t = stack.enter_context(nc.sbuf_tensor(name, list(shape), dt))
        return t, Buf(name)

    def sbn(name, shape, dt, n):
        return Rot([sb(f"{name}{i}", shape, dt) for i in range(n)])

    w_in = stack.enter_context(nc.sbuf_tensor("w_in", [128, 8, EVEN_W], BF16))
    w_inB = [Buf(f"w_in{k}") for k in range(8)]
    w_out = stack.enter_context(nc.sbuf_tensor("w_out", [128, 8, D], BF16))
    w_outB = [Buf(f"w_out{k}") for k in range(8)]
    identb, identbB = sb("identb", [128, 128], BF16)
    identf, identfB = sb("identf", [128, 128], F32)
    trilk, trilkB = sb("trilk", [128, 128], BF16)
    fark, farkB = sb("fark", [128, 128], BF16)
    neghalf, neghalfB = sb("neghalf", [128, 8], F32)
    gpost, gpostB = sb("gpost", [128, D], F32)
    xa = sbn("xa", [128, D], F32, 2)
    xr = sbn("xr", [128, D], F32, 2)
    hp = sbn("hp", [128, D], BF16, 2)
    hT = stack.enter_context(nc.sbuf_tensor("hT", [128, 8, 512], BF16))
    hTB = [Buf(f"hT{t}") for t in range(4)]
    mixT = stack.enter_context(nc.sbuf_tensor("mixT", [128, 8, 512], BF16))
    mixTB = [Buf(f"mixT{t}") for t in range(4)]
    junks = sbn("junk", [128, D], BF16, 2)
    ss, _ = sb("ss", [128, 8], F32)
    ssB = [Buf(f"ss{t}") for t in range(8)]
    ms, _ = sb("ms", [128, 8], F32)
    msB = [Buf(f"ms{t}") for t in range(8)]
    rstd, _ = sb("rstd", [128, 8], F32)
    rstdB = [Buf(f"rstd{t}") for t in range(8)]
    ssy = Rot([sb(f"ssy{i}", [128, 4], F32) + (Buf(f"ssy{i}a"), Buf(f"ssy{i}b")) for i in range(2)])
    rsy = sbn("rsy", [128, 4], F32, 2)
    par, parB = sb("par", [128, 8, 36], F32)
    tmpf = sbn("tmpf", [128, 512], F32, 4)

    PS = []
    for i in range(8):
        t = stack.enter_context(nc.psum_tensor(f"ps{i}", [128, 512], F32))
        PS.append((t, Buf(f"ps{i}")))
    psA = Rot(PS[0:4])
    psB = Rot(PS[4:8])

    xd = [[Buf(f"xd{s}_{t}") for t in range(16)] for s in range(NSEQ)]

    K.dma("pool", identb[:, :], c_ident[:, :], [], [identbB], identbB)
    K.dma("sp", identf[:, :], c_ident[:, :], [], [identfB], identfB)
    K.dma("pool", trilk[:, :], c_tril[:, :], [], [trilkB], trilkB)
    K.dma("pool", fark[:, :], c_far[:, :], [], [farkB], farkB)
    K.op("dve", lambda e: e.memset(neghalf[:, :], -0.5), [], [neghalfB])

    def layer_srcs(layer):
        li = layer // 2
        if layer % 2 == 0:
            return e_w_in[li], EVEN_W, e_w_out[li], []
        return (o_w_in[li], ODD_W, o_w_out[li],
                [(o_dw_w[li], 31), (o_dw_b[li:li + 1, :], 1), (o_ln_g[li:li + 1, :], 1), (o_ln_b[li:li + 1, :], 1)])

    def load_w_in(layer):
        w_in_dram, w_in_cols, _, _ = layer_srcs(layer)
        for kc in range(8):
            K.dma("pool", w_in[:, kc, 0:w_in_cols], w_in_dram[kc * 128:(kc + 1) * 128, :], [], [w_inB[kc]], w_inB[kc],
                  max_dma_last_dim=8192)

    def load_common(layer):
        _, _, w_out_dram, rows = layer_srcs(layer)
        stg, stgB = xa.next()
        rows = [(norm_pre[layer:layer + 1, :], 1)] + rows
        if sum(r for _, r in rows) % 2:
            rows = rows + [(norm_post[layer:layer + 1, :], 1)]
        r0 = 0
        for ap, r in rows:
            K.dma("sp", stg[r0:r0 + r, :], ap, [], [stgB], stgB)
            r0 += r
        R = r0
        for c in range(8):
            pt, pB = psA.next()
            K.op("pe", lambda e, c=c, pt=pt: e.transpose(pt[:, 0:R], stg[0:R, c * 128:(c + 1) * 128], identf[0:R, 0:R]),
                 [stgB, identfB], [pB])
            K.op("dve", lambda e, c=c, pt=pt: e.tensor_copy(out=par[:, c, 0:R], in_=pt[:, 0:R]), [pB], [parB])
        for kc in range(8):
            K.dma("pool", w_out[:, kc, :], w_out_dram[kc * 128:(kc + 1) * 128, :], [], [w_outB[kc]], w_outB[kc],
                  max_dma_last_dim=4096)
        K.dma("sp", gpost[:, :], norm_post[layer:layer + 1, :].to_broadcast([128, D]), [], [gpostB], gpostB)

    def phase_A(layer, s, b):
        src = x_in if layer == 0 else out
        xts = []
        for tt in range(4):
            xt, xB = xa.next()
            tile = b * 4 + tt
            K.dma("sp", xt[:, :], src[s, tile * 128:(tile + 1) * 128, :], [xd[s][tile]], [xB], xB)
            jk, jkB = junks.next()
            K.op("act", lambda e, xt=xt, tt=tt, jk=jk: e.activation(out=jk[:, :], in_=xt[:, :], func=AF.Square,
                                                                    accum_out=ss[:, tt:tt + 1]), [xB], [ssB[tt], jkB])
            K.op("dve", lambda e, tt=tt: e.tensor_scalar(out=ms[:, tt:tt + 1], in0=ss[:, tt:tt + 1], scalar1=1.0 / D,
                                                        scalar2=RMS_EPS, op0=ALU.mult, op1=ALU.add), [ssB[tt]], [msB[tt]])
            K.op("pool", lambda e, tt=tt: e.tensor_tensor(out=rstd[:, tt:tt + 1], in0=ms[:, tt:tt + 1],
                                                          in1=neghalf[:, 0:1], op=ALU.pow), [msB[tt], neghalfB], [rstdB[tt]])
            ht, hB = hp.next()
            K.op("dve", lambda e, ht=ht, xt=xt, tt=tt: e.tensor_scalar(out=ht[:, :], in0=xt[:, :], scalar1=rstd[:, tt:tt + 1],
                                                                       scalar2=None, op0=ALU.mult), [xB, rstdB[tt]], [hB])
            pt, pB = psA.next()
            pb = pt[:].bitcast(BF16)
            for kc in range(8):
                K.op("pe", lambda e, kc=kc, pb=pb, ht=ht: e.transpose(pb[:, kc * 128:(kc + 1) * 128],
                                                                      ht[:, kc * 128:(kc + 1) * 128], identb[:, :]),
                     [hB, identbB], [pB], signal=(kc == 7))
            K.op("dve", lambda e, pb=pb, tt=tt: e.tensor_tensor(
                out=hT[:, :, tt * 128:(tt + 1) * 128], in0=pb.rearrange("p (k t) -> p k t", k=8),
                in1=par[:, :, 0:1].to_broadcast([128, 8, 128]), op=ALU.mult), [pB, parB], [hTB[tt]])

    def fm_proj(col0, M):
        pt, pB = psA.next()
        for kc in range(8):
            K.op("pe", lambda e, kc=kc, pt=pt: e.matmul(pt[0:M, 0:512], w_in[:, kc, col0:col0 + M], hT[:, kc, :],
                                                        start=(kc == 0), stop=(kc == 7)),
                 [w_inB[kc]] + hTB, [pB], signal=(kc == 7))
        return pt, pB

    def tm_proj(tt, col0, N):
        pt, pB = psA.next()
        for kc in range(8):
            K.op("pe", lambda e, kc=kc, pt=pt: e.matmul(pt[:, 0:N], hT[:, kc, tt * 128:(tt + 1) * 128],
                                                        w_in[:, kc, col0:col0 + N], start=(kc == 0), stop=(kc == 7)),
                 [w_inB[kc], hTB[tt]], [pB], signal=(kc == 7))
        return pt, pB

    def phase_G(layer, s, b):
        src = x_in if layer == 0 else out
        for tt in range(4):
            tile = b * 4 + tt
            xt, xB = xr.next()
            K.dma("sp", xt[:, :], src[s, tile * 128:(tile + 1) * 128, :], [xd[s][tile]], [xB], xB)
            halves = []
            sy, syB, syB0, syB1 = ssy.next()
            ry, ryB = rsy.next()
            for hf in range(2):
                pt, pB = psB.next()
                for fc in range(8):
                    K.op("pe", lambda e, fc=fc, pt=pt, hf=hf: e.matmul(pt[:, 0:512], mixT[:, fc, tt * 128:(tt + 1) * 128],
                                                                        w_out[:, fc, hf * 512:(hf + 1) * 512],
                                                                        start=(fc == 0), stop=(fc == 7)),
                         [mixTB[tt], w_outB[fc]], [pB], signal=(fc == 7))
                halves.append((pt, pB))
            ysbs = []
            for hf, sB_ in ((0, syB0), (1, syB1)):
                pt, pB = halves[hf]
                tf, tfB = tmpf.next()
                if hf == 0:
                    K.op("act", lambda e, pt=pt, tf=tf: e.copy(out=tf[:, :], in_=pt[:, 0:512]), [pB], [tfB])
                else:
                    K.op("dve", lambda e, pt=pt, tf=tf: e.tensor_copy(out=tf[:, :], in_=pt[:, 0:512]), [pB], [tfB])
                jk, jkB = junks.next()
                K.op("act", lambda e, tf=tf, hf=hf, sy=sy, jk=jk: e.activation(out=jk[:, 0:512], in_=tf[:, :], func=AF.Square,
                                                                               accum_out=sy[:, hf:hf + 1]), [tfB], [sB_, jkB])
                ysbs.append((tf, tfB))
            K.op("dve", lambda e, sy=sy: e.tensor_tensor(out=sy[:, 2:3], in0=sy[:, 0:1], in1=sy[:, 1:2], op=ALU.add),
                 [syB0, syB1, syB], [syB])
            K.op("dve", lambda e, sy=sy: e.tensor_scalar(out=sy[:, 3:4], in0=sy[:, 2:3], scalar1=1.0 / D, scalar2=RMS_EPS,
                                                        op0=ALU.mult, op1=ALU.add), [syB], [syB])
            K.op("pool", lambda e, sy=sy, ry=ry: e.tensor_tensor(out=ry[:, 0:1], in0=sy[:, 3:4], in1=neghalf[:, 0:1],
                                                                 op=ALU.pow), [syB, neghalfB], [ryB])
            for hf in range(2):
                tf, tfB = ysbs[hf]
                K.op("dve", lambda e, tf=tf, hf=hf, ry=ry: e.scalar_tensor_tensor(
                    out=tf[:, :], in0=tf[:, :], scalar=ry[:, 0:1], in1=gpost[:, hf * 512:(hf + 1) * 512],
                    op0=ALU.mult, op1=ALU.mult), [tfB, ryB, gpostB], [tfB])
                K.op("pool", lambda e, tf=tf, xt=xt, hf=hf: e.tensor_tensor(
                    out=xt[:, hf * 512:(hf + 1) * 512], in0=xt[:, hf * 512:(hf + 1) * 512], in1=tf[:, :], op=ALU.add),
                     [tfB, xB], [xB])
            K.dma("sp", out[s, tile * 128:(tile + 1) * 128, :], xt[:, :], [xB], [xd[s][tile]], xd[s][tile])

    def even_layer(layer, li):
        st = ExitStack()

        def sbl(name, shape, dt=F32):
            t = st.enter_context(nc.sbuf_tensor(f"{name}_{layer}", list(shape), dt))
            return t, Buf(name)

        def sbln(name, shape, dt, n):
            return Rot([sbl(f"{name}{i}", shape, dt) for i in range(n)])

        qT = st.enter_context(nc.sbuf_tensor(f"qT_{layer}", [128, 8, 512], BF16))
        qTB = [Buf(f"qT{h}") for h in range(8)]
        qSB = [Buf(f"qS{h}") for h in range(8)]
        ksT = st.enter_context(nc.sbuf_tensor(f"ksT_{layer}", [128, 2, 2048], BF16))
        ksTB = [[Buf(f"ksT{g}_{b}") for b in range(4)] for g in range(2)]
        ebB = Buf("ebig")
        kwT = st.enter_context(nc.sbuf_tensor(f"kwT_{layer}", [64, 2, 1024], BF16))
        kwTB = [[Buf(f"kwT{g}_{r}") for r in range(2)] for g in range(2)]
        kcT, kcTB = sbl("kcT", [128, 528], BF16)
        vcT, vcTB = sbl("vcT", [128, 528], BF16)
        vsA = st.enter_context(nc.sbuf_tensor(f"vsA_{layer}", [128, 16, 2, 65], BF16))
        vsAB = [Buf(f"vsA{k}") for k in range(16)]
        vwA = st.enter_context(nc.sbuf_tensor(f"vwA_{layer}", [128, 8, 2, 65], BF16))
        vwAB = [Buf(f"vwA{k}") for k in range(8)]
        w1k, w1kB = sbl("w1k", [128, 32, 64], BF16)
        w1v, w1vB = sbl("w1v", [128, 32, 64], BF16)
        w2k, w2kB = sbl("w2k", [64, 64], BF16)
        w2v, w2vB = sbl("w2v", [64, 64], BF16)
        pek, pekB = sbl("pek", [32, 64], BF16)
        pev, pevB = sbl("pev", [32, 64], BF16)
        peT, peTB = sbl("peT", [64, 2, 32], BF16)
        pebias, pebiasB = sbl("pebias", [64, 2], F32)
        hidk, hidkB = sbl("hidk", [64, 2, 32], BF16)
        hidv, hidvB = sbl("hidv", [64, 2, 128], BF16)
        kcmpT, kcmpTB = sbl("kcmpT", [64, 2, 128], BF16)
        vcmp, vcmpB = sbl("vcmp", [128, 2, 97], BF16)
        ctab, ctabB = sbl("ctab", [128, 16, 32], F32)
        WsT, WsTB = sbl("WsT", [128, 8, 128], BF16)
        bs, bsB = sbl("bs", [128, 8], F32)
        lng, lngB = sbl("lng", [128, 512], F32)
        lnb, lnbB = sbl("lnb", [128, 512], F32)
        gates = st.enter_context(nc.sbuf_tensor(f"gates_{layer}", [128, 4, 24], F32))
        gatesB = [Buf(f"gates{t}") for t in range(4)]
        sza = st.enter_context(nc.sbuf_tensor(f"sza_{layer}", [128, 4, 512], BF16))
        szaB = [Buf(f"sza{t}") for t in range(4)]
        ug = sbln("ug", [128, 512], BF16, 2)
        szb = sbln("szb", [128, 512], BF16, 2)
        vg = sbln("vg", [128, 512], F32, 2)
        vln = sbln("vln", [128, 512], BF16, 2)
        lnst = sbln("lnst", [128, 16], F32, 2)
        acc_o = st.enter_context(nc.sbuf_tensor(f"acc_o_{layer}", [128, 4, 512], F32))
        acc_oB = [Buf(f"acc_o{h}") for h in range(8)]
        imp = sbln("imp", [128, 4, 32], F32, 2)
        top8 = sbln("top8", [128, 4, 8], F32, 2)
        selpads = [sbl(f"selpad{g}", [128, 4, 96], BF16) for g in range(2)]
        pTs = sbln("pT", [128, 512], BF16, 4)
        rcs = sbln("rc", [128, 16], F32, 4)
        amix = sbln("amix", [128, 512], BF16, 2)
        tmpb = sbln("tmpb", [128, 512], BF16, 3)

        for g in range(2):
            K.dma("pool", ksT[64:96, g, :], c_ebig[:, :], [], [ebB], ebB, max_dma_last_dim=4096)
        for wi, (w1, w1B, src) in enumerate(((w1k, w1kB, e_k_w1), (w1v, w1vB, e_v_w1))):
            srcv = src[li].rearrange("(l d) e -> d l e", d=64)
            for lh in range(2):
                stg, stgB = xa.next()
                stgv = stg[:, :].rearrange("p (l e) -> p l e", l=16)
                for half in range(2):
                    K.dma("sp", stgv[half * 64:(half + 1) * 64, :, :], srcv[:, lh * 16:(lh + 1) * 16, :], [], [stgB], stgB)
                if (wi + lh) % 2 == 0:
                    K.op("dve", lambda e, w1=w1, lh=lh, stgv=stgv: e.tensor_copy(out=w1[:, lh * 16:(lh + 1) * 16, :], in_=stgv),
                         [stgB], [w1B])
                else:
                    K.op("act", lambda e, w1=w1, lh=lh, stgv=stgv: e.copy(out=w1[:, lh * 16:(lh + 1) * 16, :], in_=stgv),
                         [stgB], [w1B])
        K.dma("pool", w2k[:, :], e_k_w2[li], [], [w2kB], w2kB)
        K.dma("pool", w2v[:, :], e_v_w2[li], [], [w2vB], w2vB)
        K.dma("pool", pek[:, :], e_k_pe[li], [], [pekB], pekB)
        K.dma("pool", pev[:, :], e_v_pe[li], [], [pevB], pevB)
        K.dma("sp", ctab[:, :, :], c_ctab[:, :, :], [], [ctabB], ctabB)
        wsst_t, wsstB = xa.next()
        wsst = wsst_t[:, :].rearrange("p (g j) -> p g j", g=8)
        K.dma("sp", wsst, e_sgu_w[li].rearrange("g i j -> i g j"), [], [wsstB], wsstB)
        bst, bstB = xa.next()
        K.dma("sp", bst[0:8, 0:128], e_sgu_b[li], [], [bstB], bstB)
        pt, pB = psA.next()
        K.op("pe", lambda e: e.transpose(pt[:, 0:8], bst[0:8, 0:128], identf[0:8, 0:8]), [bstB, identfB], [pB])
        K.op("dve", lambda e: e.tensor_copy(out=bs[:, :], in_=pt[:, 0:8]), [pB], [bsB])
        K.dma("sp", lng[:, :], e_ln_g[li:li + 1, :].to_broadcast([128, 512]), [], [lngB], lngB)
        K.dma("sp", lnb[:, :], e_ln_b[li:li + 1, :].to_broadcast([128, 512]), [], [lnbB], lnbB)
        K.op("pool", lambda e: e.memset(vsA[:, :, :, 64:65], 1.0), [], vsAB)
        K.op("pool", lambda e: e.memset(vwA[:, :, :, 64:65], 1.0), [], vwAB)
        K.op("pool", lambda e: e.memset(vcmp[:, :, :], 0.0), [], [vcmpB])
        K.op("pool", lambda e: e.memset(vcmp[:, :, 64:65], 1.0), [vcmpB], [vcmpB])
        for g in range(2):
            K.dma("pool", vcmp[0:127, g, 65:97], c_ovl[:, :], [], [vcmpB], vcmpB)
        K.op("pool", lambda e: e.memset(hidv[:, :, :], 0.0), [], [hidvB])
        K.op("pool", lambda e: e.memset(kcmpT[:, :, :], 0.0), [], [kcmpTB])
        for selpad, selpadB in selpads:
            K.op("pool", lambda e, selpad=selpad: e.memset(selpad[:, :, :], 0.0), [], [selpadB])
        K.op("dve", lambda e: e.memset(kcT[:, 0:16], 0.0), [], [kcTB])
        K.op("dve", lambda e: e.memset(vcT[:, 0:16], 0.0), [], [vcTB])
        for g in range(8):
            pt, pB = psA.next()
            K.op("pe", lambda e, g=g, pt=pt: e.transpose(pt[:, 0:128], wsst[:, g, :], identf[:, :]), [wsstB, identfB], [pB])
            K.op("dve", lambda e, g=g, pt=pt: e.tensor_tensor(out=WsT[:, g, :], in0=pt[:, 0:128], in1=trilk[:, :], op=ALU.mult),
                 [pB, trilkB], [WsTB])
        for xi, (pe_, peB_, w1, w1B) in enumerate(((pek, pekB, w1k, w1kB), (pev, pevB, w1v, w1vB))):
            pt, pB = psA.next()
            pb = pt[:].bitcast(BF16)
            K.op("pe", lambda e, pb=pb, pe_=pe_: e.transpose(pb[0:64, 0:32], pe_[:, :], identb[0:32, 0:32]), [peB_, identbB], [pB])
            K.op("dve", lambda e, pb=pb, xi=xi: e.tensor_copy(out=peT[:, xi, :], in_=pb[0:64, 0:32]), [pB], [peTB])
            pt2, pB2 = psA.next()
            for l in range(32):
                K.op("pe", lambda e, l=l, pt2=pt2, w1=w1, xi=xi: e.matmul(pt2[0:64, 0:1], w1[0:64, l, :], peT[:, xi, l:l + 1],
                                                                          start=(l == 0), stop=(l == 31)),
                     [w1B, peTB], [pB2], signal=(l == 31))
            K.op("dve", lambda e, pt2=pt2, xi=xi: e.tensor_copy(out=pebias[:, xi:xi + 1], in_=pt2[0:64, 0:1]), [pB2], [pebiasB])

        def evac_copy(i, out_ap, in_ap, reads, writes, scale=None):
            if i % 2 == 0:
                if scale is None:
                    K.op("act", lambda e: e.copy(out=out_ap, in_=in_ap), reads, writes)
                else:
                    K.op("act", lambda e: e.mul(out=out_ap, in_=in_ap, mul=scale), reads, writes)
            else:
                if scale is None:
                    K.op("dve", lambda e: e.tensor_copy(out=out_ap, in_=in_ap), reads, writes)
                else:
                    K.op("dve", lambda e: e.tensor_scalar(out=out_ap, in0=in_ap, scalar1=scale, scalar2=None, op0=ALU.mult),
                         reads, writes)

        if STOP == "params":
            st.close()
            return
        blocks = [(s_, b_) for s_ in range(NSEQ) for b_ in range(NBLK)]
        K.phase = "evA"
        phase_A(layer, 0, 0)
        for bi, (s, b) in enumerate(blocks):
            if True:
                t0 = b * 512
                K.phase = "evB"
                if STOP == "A":
                    st.close()
                    return
                for h in range(8):
                    pt, pB = fm_proj(C_Q + h * 64, 64)
                    evac_copy(h, qT[0:64, h, :], pt[0:64, 0:512], [pB], [qTB[h]], scale=0.125)
                for g in range(2):
                    pt, pB = fm_proj(C_KS + g * 64, 64)
                    evac_copy(g, ksT[0:64, g, t0:t0 + 512], pt[0:64, 0:512], [pB], [ksTB[g][b]])
                for g in range(2):
                    pt, pB = fm_proj(C_KW + g * 64, 64)
                    r = b % 2
                    evac_copy(g + 1, kwT[0:64, g, r * 512:(r + 1) * 512], pt[0:64, 0:512], [pB], [kwTB[g][r]])
                for i, (cX, XT, XTB) in enumerate(((C_KC, kcT, kcTB), (C_VC, vcT, vcTB))):
                    if b > 0:
                        K.op("pool", lambda e, XT=XT: e.tensor_copy(out=XT[:, 0:16], in_=XT[:, 512:528]), [XTB], [XTB])
                    pt, pB = fm_proj(cX, 128)
                    evac_copy(i, XT[:, 16:528], pt[:, 0:512], [pB], [XTB])
                if STOP == "B":
                    st.close()
                    return
                K.phase = "evC"
                cst = {}
                cst2 = {}

                def c_front(tt):
                    kt = b * 4 + tt
                    pt, pB = tm_proj(tt, C_VS, 408)
                    K.op("dve", lambda e, pt=pt, kt=kt: e.tensor_copy(out=vsA[:, kt, :, 0:64],
                                                                      in_=pt[:, 0:128].rearrange("p (g d) -> p g d", g=2)),
                         [pB], [vsAB[kt]])
                    K.op("dve", lambda e, pt=pt, kt=kt: e.tensor_copy(out=vwA[:, kt % 8, :, 0:64],
                                                                      in_=pt[:, 256:384].rearrange("p (g d) -> p g d", g=2)),
                         [pB], [vwAB[kt % 8]])
                    K.op("act", lambda e, pt=pt, tt=tt: e.activation(out=gates[:, tt, :], in_=pt[:, 384:408], func=AF.Sigmoid),
                         [pB], [gatesB[tt]])
                    pt, pB = tm_proj(tt, C_ZA, 512)
                    K.op("act", lambda e, pt=pt, tt=tt: e.activation(out=sza[:, tt, :], in_=pt[:, 0:512], func=AF.Silu),
                         [pB], [szaB[tt]])
                    pt, pB = tm_proj(tt, C_ZB, 512)
                    zt, ztB = szb.next()
                    K.op("act", lambda e, pt=pt, zt=zt: e.activation(out=zt[:, :], in_=pt[:, 0:512], func=AF.Silu), [pB], [ztB])
                    pt, pB = tm_proj(tt, C_U, 512)
                    ut, utB = ug.next()
                    K.op("act", lambda e, pt=pt, ut=ut: e.activation(out=ut[:, :], in_=pt[:, 0:512], func=AF.Gelu_apprx_tanh),
                         [pB], [utB])
                    pt, pB = tm_proj(tt, C_V, 512)
                    vt, vtB = vg.next()
                    K.op("act", lambda e, pt=pt, vt=vt: e.activation(out=vt[:, :], in_=pt[:, 0:512], func=AF.Gelu_apprx_tanh),
                         [pB], [vtB])
                    K.op("pool", lambda e, ut=ut, zt=zt: e.tensor_tensor(out=ut[:, :], in0=ut[:, :], in1=zt[:, :], op=ALU.mult),
                         [utB, ztB], [utB])
                    ls, lsB = lnst.next()
                    K.op("dve", lambda e, ls=ls, vt=vt: e.bn_stats(out=ls[:, 0:6], in_=vt[:, :]), [vtB], [lsB])
                    K.op("dve", lambda e, ls=ls: e.bn_aggr(out=ls[:, 6:8], in_=ls[:, 0:6]), [lsB], [lsB])
                    K.op("dve", lambda e, ls=ls: e.tensor_scalar(out=ls[:, 8:9], in0=ls[:, 7:8], scalar1=LN_EPS, scalar2=None,
                                                                op0=ALU.add), [lsB], [lsB])
                    K.op("pool", lambda e, ls=ls: e.tensor_tensor(out=ls[:, 9:10], in0=ls[:, 8:9], in1=neghalf[:, 0:1], op=ALU.pow),
                         [lsB, neghalfB], [lsB])
                    K.op("dve", lambda e, ls=ls, vt=vt: e.tensor_scalar(out=vt[:, :], in0=vt[:, :], scalar1=ls[:, 6:7],
                                                                        scalar2=ls[:, 9:10], op0=ALU.subtract, op1=ALU.mult),
                         [lsB, vtB], [vtB])
                    K.op("pool", lambda e, vt=vt: e.tensor_tensor(out=vt[:, :], in0=vt[:, :], in1=lng[:, :], op=ALU.mult),
                         [vtB, lngB], [vtB])
                    vl, vlB = vln.next()
                    K.op("pool", lambda e, vt=vt, vl=vl: e.tensor_tensor(out=vl[:, :], in0=vt[:, :], in1=lnb[:, :], op=ALU.add),
                         [vtB, lnbB], [vlB])
                    cst[tt] = (vl, vlB, ut, utB)

                def c_back(tt):
                    vl, vlB, ut, utB = cst.pop(tt)
                    pt, pB = psB.next()
                    for g in range(8):
                        K.op("pe", lambda e, g=g, pt=pt, vl=vl: e.matmul(pt[:, g * 64:(g + 1) * 64], WsT[:, g, :],
                                                                          vl[:, g * 64:(g + 1) * 64], start=True, stop=True),
                             [WsTB, vlB], [pB], signal=(g == 7))
                    tb, tbB = tmpb.next()
                    K.op("dve", lambda e, pt=pt, tb=tb: e.tensor_tensor(
                        out=tb[:, :].rearrange("p (g d) -> p g d", g=8), in0=pt[:, 0:512].rearrange("p (g d) -> p g d", g=8),
                        in1=bs[:, :].unsqueeze(2).to_broadcast([128, 8, 64]), op=ALU.add), [pB, bsB], [tbB])
                    K.op("pool", lambda e, tb=tb, ut=ut: e.tensor_tensor(out=tb[:, :], in0=tb[:, :], in1=ut[:, :], op=ALU.mult),
                         [tbB, utB], [tbB])
                    cst2[tt] = (tb, tbB)

                def c_back2(tt):
                    tb, tbB = cst2.pop(tt)
                    pt, pB = psA.next()
                    pb = pt[:].bitcast(BF16)
                    for fc in range(4):
                        K.op("pe", lambda e, fc=fc, pb=pb, tb=tb: e.transpose(pb[:, fc * 128:(fc + 1) * 128],
                                                                              tb[:, fc * 128:(fc + 1) * 128], identb[:, :]),
                             [tbB, identbB], [pB], signal=(fc == 3))
                    K.op("dve", lambda e, pb=pb, tt=tt: e.tensor_copy(out=mixT[:, 4:8, tt * 128:(tt + 1) * 128],
                                                                      in_=pb[:, 0:512].rearrange("p (k t) -> p k t", k=4)),
                         [pB], [mixTB[tt]])

                for tt in range(6):
                    if tt < 4:
                        c_front(tt)
                    if 1 <= tt <= 4:
                        c_back(tt - 1)
                    if tt >= 2:
                        c_back2(tt - 2)
                if STOP == "C":
                    st.close()
                    return
                if bi + 1 < len(blocks):
                    K.phase = "evA"
                    phase_A(layer, blocks[bi + 1][0], blocks[bi + 1][1])
                elif layer + 1 < n_layers:
                    load_w_in(layer + 1)
                K.phase = "evD"
                if b == 0:
                    n0, nn, c0 = 0, 31, 16
                else:
                    n0, nn, c0 = 32 * b - 1, 32, 0
                nk = 32 * (b + 1) - 1
                for g in range(2):
                    for xi, (XT, XTB, w1, w1B, hid, hidB, hcol) in enumerate((
                            (kcT, kcTB, w1k, w1kB, hidk, hidkB, 0), (vcT, vcTB, w1v, w1vB, hidv, hidvB, n0))):
                        pt, pB = psA.next()
                        for l in range(32):
                            K.op("pe", lambda e, l=l, pt=pt, XT=XT, w1=w1: e.matmul(
                                pt[0:64, 0:nn], w1[g * 64:(g + 1) * 64, l, :],
                                XT[g * 64:(g + 1) * 64, c0 + l:c0 + l + 16 * (nn - 1) + 1:16], start=(l == 0), stop=(l == 31)),
                                 [w1B, XTB], [pB], signal=(l == 31))
                        K.op("act", lambda e, pt=pt, hid=hid, hcol=hcol, xi=xi: e.activation(
                            out=hid[:, g, hcol:hcol + nn], in_=pt[0:64, 0:nn], func=AF.Silu, bias=pebias[:, xi:xi + 1]),
                             [pB, pebiasB], [hidB])
                    pt, pB = psA.next()
                    K.op("pe", lambda e, pt=pt: e.matmul(pt[0:64, 0:nn], w2k[:, :], hidk[:, g, 0:nn], start=True, stop=True),
                         [w2kB, hidkB], [pB])
                    K.op("dve", lambda e, pt=pt: e.tensor_copy(out=kcmpT[:, g, n0:n0 + nn], in_=pt[0:64, 0:nn]), [pB], [kcmpTB])
                    pt, pB = psA.next()
                    K.op("pe", lambda e, pt=pt: e.matmul(pt[0:nk, 0:64], hidv[:, g, 0:nk], w2v[:, :], start=True, stop=True),
                         [w2vB, hidvB], [pB])
                    K.op("dve", lambda e, pt=pt: e.tensor_copy(out=vcmp[0:nk, g, 0:64], in_=pt[0:nk, 0:64]), [pB], [vcmpB])
                if STOP == "D":
                    st.close()
                    return
                K.phase = "evE"
                for g in range(2):
                    selpad, selpadB = selpads[g]
                    im, imB = imp.next()
                    for r in range(4):
                        h = g * 4 + r
                        pt, pB = psA.next()
                        K.op("pe", lambda e, pt=pt, h=h: e.matmul(pt[0:nk, 0:512], kcmpT[:, g, 0:nk], qT[0:64, h, :],
                                                                  start=True, stop=True), [kcmpTB, qTB[h]], [pB])
                        pT, pTB = pTs.next()
                        K.op("act", lambda e, pt=pt, pT=pT: e.activation(out=pT[0:nk, :], in_=pt[0:nk, 0:512], func=AF.Exp),
                             [pB], [pTB])
                        if STOP == "E1":
                            st.close()
                            return
                        K.op("pool", lambda e, pT=pT: e.affine_select(out=pT[0:nk, :], in_=pT[0:nk, :], pattern=[[1, 512]],
                                                                      compare_op=ALU.is_ge, fill=0.0, base=t0 - 31,
                                                                      channel_multiplier=-16), [pTB], [pTB])
                        if STOP == "E2":
                            st.close()
                            return
                        po, poB = psB.next()
                        po3 = po[:, 0:388].rearrange("p (t c) -> p t c", t=4)
                        for tt in range(4):
                            K.op("pe", lambda e, tt=tt, po=po, pT=pT: e.matmul(po[:, tt * 97:(tt + 1) * 97],
                                                                                pT[0:nk, tt * 128:(tt + 1) * 128], vcmp[0:nk, g, :],
                                                                                start=True, stop=True),
                                 [pTB, vcmpB], [poB], signal=(tt == 3))
                        if STOP == "E3":
                            st.close()
                            return
                        rc, rcB = rcs.next()
                        K.op("dve", lambda e, rc=rc, po3=po3: e.tensor_scalar(out=rc[:, 0:4], in0=po3[:, :, 64], scalar1=1e-30,
                                                                              scalar2=None, op0=ALU.max), [poB], [rcB])
                        K.op("dve", lambda e, rc=rc: e.reciprocal(out=rc[:, 4:8], in_=rc[:, 0:4]), [rcB], [rcB])
                        K.op("dve", lambda e, rc=rc, h=h: e.tensor_tensor(out=rc[:, 8:12], in0=rc[:, 4:8], in1=gates[:, :, h],
                                                                          op=ALU.mult), [rcB] + gatesB, [rcB])
                        for tt in range(4):
                            K.op("dve", lambda e, tt=tt, po3=po3, rc=rc, h=h: e.tensor_scalar(
                                out=acc_o[:, tt, h * 64:(h + 1) * 64], in0=po3[:, tt, 0:64], scalar1=rc[:, 8 + tt:9 + tt],
                                scalar2=None, op0=ALU.mult), [poB, rcB], [acc_oB[h]])
                        for tt in range(4):
                            if r == 0:
                                K.op("dve", lambda e, tt=tt, po3=po3, rc=rc, im=im: e.tensor_scalar(
                                    out=im[:, tt, :], in0=po3[:, tt, 65:97], scalar1=rc[:, 4 + tt:5 + tt], scalar2=None,
                                    op0=ALU.mult), [poB, rcB], [imB])
                            else:
                                K.op("dve", lambda e, tt=tt, po3=po3, rc=rc, im=im: e.scalar_tensor_tensor(
                                    out=im[:, tt, :], in0=po3[:, tt, 65:97], scalar=rc[:, 4 + tt:5 + tt], in1=im[:, tt, :],
                                    op0=ALU.mult, op1=ALU.add), [poB, rcB, imB], [imB])
                    if STOP == "E4":
                        st.close()
                        return
                    K.op("dve", lambda e, im=im: e.tensor_tensor(out=im[:, :, :], in0=im[:, :, :], in1=ctab[:, 4 * b:4 * b + 4, :],
                                                                 op=ALU.add), [imB, ctabB], [imB])
                    t8, t8B = top8.next()
                    for tt in range(4):
                        K.op("dve", lambda e, tt=tt, t8=t8, im=im: e.max(out=t8[:, tt, :], in_=im[:, tt, :]), [imB], [t8B])
                    for tt in range(4):
                        K.op("dve", lambda e, tt=tt, t8=t8, im=im: e.tensor_scalar(
                            out=selpad[:, tt, 64:96], in0=im[:, tt, :], scalar1=t8[:, tt, 7:8], scalar2=-1.0,
                            op0=ALU.is_ge, op1=ALU.add), [imB, t8B], [selpadB])
                    if STOP == "E6":
                        st.close()
                        return
                for g in range(2):
                    selpad, selpadB = selpads[g]
                    pt, pB = psA.next()
                    pb = pt[:].bitcast(BF16)
                    for tt in range(4):
                        K.op("pe", lambda e, tt=tt, pb=pb: e.transpose(pb[0:96, tt * 128:(tt + 1) * 128], selpad[:, tt, :],
                                                                       identb[:, :]), [selpadB, identbB], [pB], signal=(tt == 3))
                    for r in range(4):
                        h = g * 4 + r
                        K.op("dve", lambda e, h=h, pb=pb: e.tensor_copy(out=qT[64:96, h, :], in_=pb[64:96, 0:512]), [pB], [qSB[h]])
                if STOP == "E":
                    st.close()
                    return
                K.phase = "evF"
                items = []
                for h in range(8):
                    for br in (1, 0):
                        kts = list(range(0, 4 * b + 4)) if br == 0 else list(range(max(0, 4 * b - 4), 4 * b + 4))
                        for kt in kts:
                            items.append((h, br, kt, kt == kts[0], kt == kts[-1]))
                LA = 2
                stage = {}
                accs = {}

                def front(idx):
                    h, br, kt, isfirst, islast = items[idx]
                    g = h // 4
                    d = kt - 4 * b
                    lo = max(0, d)
                    hi = 3 if br == 0 else min(3, d + 4)
                    c0_, c1_ = lo * 128, (hi + 1) * 128
                    pt, pB = psA.next()
                    if br == 0:
                        K.op("pe", lambda e: e.matmul(
                            pt[:, c0_:c1_], ksT[0:96, g, kt * 128:(kt + 1) * 128], qT[0:96, h, c0_:c1_],
                            start=True, stop=True), [ksTB[g][kt // 4], ebB, qTB[h], qSB[h]], [pB])
                    else:
                        kr = kt % 8
                        K.op("pe", lambda e: e.matmul(
                            pt[:, c0_:c1_], kwT[0:64, g, kr * 128:(kr + 1) * 128], qT[0:64, h, c0_:c1_],
                            start=True, stop=True), [kwTB[g][kr // 4], qTB[h]], [pB])
                    pT, pTB = pTs.next()
                    K.op("act", lambda e: e.activation(out=pT[:, c0_:c1_], in_=pt[:, c0_:c1_], func=AF.Exp), [pB], [pTB])
                    if d >= 0:
                        K.op("pool", lambda e: e.tensor_tensor(
                            out=pT[:, d * 128:(d + 1) * 128], in0=pT[:, d * 128:(d + 1) * 128], in1=trilk[:, :],
                            op=ALU.mult), [pTB, trilkB], [pTB])
                    if br == 1 and 0 <= d + 4 <= 3:
                        K.op("pool", lambda e: e.tensor_tensor(
                            out=pT[:, (d + 4) * 128:(d + 5) * 128], in0=pT[:, (d + 4) * 128:(d + 5) * 128],
                            in1=fark[:, :], op=ALU.mult), [pTB, farkB], [pTB])
                    stage[idx] = (pT, pTB, lo, hi)

                def back(idx):
                    h, br, kt, isfirst, islast = items[idx]
                    g = h // 4
                    pT, pTB, lo, hi = stage.pop(idx)
                    if isfirst:
                        accs[(h, br)] = psB.next()
                    acc, accB = accs[(h, br)]
                    for tt in range(lo, hi + 1):
                        if br == 0:
                            vB_, rhs = vsAB[kt], vsA[:, kt, g, :]
                        else:
                            vB_, rhs = vwAB[kt % 8], vwA[:, kt % 8, g, :]
                        K.op("pe", lambda e, tt=tt, rhs=rhs: e.matmul(
                            acc[:, tt * 65:(tt + 1) * 65], pT[:, tt * 128:(tt + 1) * 128], rhs,
                            start=(isfirst and tt == lo), stop=True, skip_group_check=True), [pTB, vB_], [accB], signal=(tt == hi))
                    if islast:
                        acc3 = acc[:, 0:260].rearrange("p (t c) -> p t c", t=4)
                        rc, rcB = rcs.next()
                        K.op("dve", lambda e: e.tensor_scalar(out=rc[:, 0:4], in0=acc3[:, :, 64], scalar1=1e-30,
                                                              scalar2=None, op0=ALU.max), [accB], [rcB])
                        K.op("dve", lambda e: e.reciprocal(out=rc[:, 4:8], in_=rc[:, 0:4]), [rcB], [rcB])
                        gi = (1 + br) * 8 + h
                        K.op("dve", lambda e: e.tensor_tensor(out=rc[:, 8:12], in0=rc[:, 4:8], in1=gates[:, :, gi],
                                                              op=ALU.mult), [rcB] + gatesB, [rcB])
                        for tt in range(4):
                            K.op("dve", lambda e, tt=tt: e.scalar_tensor_tensor(
                                out=acc_o[:, tt, h * 64:(h + 1) * 64], in0=acc3[:, tt, 0:64], scalar=rc[:, 8 + tt:9 + tt],
                                in1=acc_o[:, tt, h * 64:(h + 1) * 64], op0=ALU.mult, op1=ALU.add),
                                 [accB, rcB, acc_oB[h]], [acc_oB[h]])
                        del accs[(h, br)]

                for idx in range(len(items) + LA):
                    if idx < len(items):
                        front(idx)
                    if idx - LA >= 0:
                        back(idx - LA)
                for tt in range(4):
                    am, amB = amix.next()
                    K.op("pool", lambda e, tt=tt, am=am: e.tensor_tensor(out=am[:, :], in0=acc_o[:, tt, :], in1=sza[:, tt, :],
                                                                         op=ALU.mult), acc_oB + [szaB[tt]], [amB])
                    pt, pB = psA.next()
                    pb = pt[:].bitcast(BF16)
                    for fc in range(4):
                        K.op("pe", lambda e, fc=fc, pb=pb, am=am: e.transpose(pb[:, fc * 128:(fc + 1) * 128],
                                                                              am[:, fc * 128:(fc + 1) * 128], identb[:, :]),
                             [amB, identbB], [pB], signal=(fc == 3))
                    K.op("dve", lambda e, pb=pb, tt=tt: e.tensor_copy(out=mixT[:, 0:4, tt * 128:(tt + 1) * 128],
                                                                      in_=pb[:, 0:512].rearrange("p (k t) -> p k t", k=4)),
                         [pB], [mixTB[tt]])
                if STOP == "F":
                    st.close()
                    return
                K.phase = "evG"
                phase_G(layer, s, b)
                if STOP == "G":
                    st.close()
                    return
        st.close()

    def odd_layer(layer, li):
        st = ExitStack()

        def sbl(name, shape, dt=F32):
            t = st.enter_context(nc.sbuf_tensor(f"{name}_{layer}", list(shape), dt))
            return t, Buf(name)

        def sbln(name, shape, dt, n):
            return Rot([sbl(f"{name}{i}", shape, dt) for i in range(n)])

        yc = st.enter_context(nc.sbuf_tensor(f"yc_{layer}", [128, 8, 544], BF16))
        ycB = [Buf(f"yc{c}") for c in range(8)]
        szzs = [(st.enter_context(nc.sbuf_tensor(f"szz{i}_{layer}", [128, 8, 512], BF16)), [Buf(f"szz{i}_{c}") for c in range(8)])
                for i in range(2)]
        yv = st.enter_context(nc.sbuf_tensor(f"yv_{layer}", [128, 8, 512], F32))
        yvB = [Buf(f"yv{c}") for c in range(8)]
        diag = sbln("diag", [128, 31, 128], BF16, 3)
        sig = sbln("sig", [128, 512], F32, 2)
        ybf = sbln("ybf", [128, 512], BF16, 3)
        ysq = sbln("ysq", [128, 512], BF16, 3)
        onesb, onesbB = sbl("onesb", [128, 128], BF16)
        mean, meanB = sbl("mean", [128, 512], F32)
        rstdt, rstdtB = sbl("rstdt", [128, 512], F32)
        var, varB = sbl("var", [128, 512], F32)
        t1 = sbln("t1", [128, 512], F32, 2)

        K.op("pool", lambda e: e.memset(onesb[:, :], 1.0 / D), [], [onesbB])

        blocks = [(s_, b_) for s_ in range(NSEQ) for b_ in range(NBLK)]
        K.phase = "odA"
        phase_A(layer, 0, 0)
        def od_proj(bi, mid=None):
            s, b = blocks[bi]
            szz, szzB = szzs[bi % 2]
            K.phase = "odProj"
            for c in range(8):
                if b == 0:
                    K.op("pool", lambda e: e.memset(yc[:, c, 0:32], 0.0), [], [ycB[c]])
                else:
                    K.op("pool", lambda e: e.tensor_copy(out=yc[:, c, 0:32], in_=yc[:, c, 512:544]), [ycB[c]], [ycB[c]])
                pg, pgB = fm_proj(1024 + c * 128, 128)
                sg, sgB = sig.next()
                K.op("act", lambda e: e.activation(out=sg[:, :], in_=pg[:, 0:512], func=AF.Sigmoid), [pgB], [sgB])
                pa, paB = fm_proj(c * 128, 128)
                K.op("dve", lambda e: e.tensor_tensor(out=yc[:, c, 32:544], in0=pa[:, 0:512], in1=sg[:, :], op=ALU.mult),
                     [paB, sgB], [ycB[c]])
            if mid is not None:
                mid()
                K.phase = "odProj"
            for c in range(8):
                pz, pzB = fm_proj(2048 + c * 128, 128)
                K.op("act", lambda e: e.activation(out=szz[:, c, :], in_=pz[:, 0:512], func=AF.Silu), [pzB], [szzB[c]])

        def od_conv(bi):
            K.phase = "odConv"
            pm, pmB = psB.next()
            pq, pqB = psB.next()
            dgs = {}

            def build_diag(c):
                dg, dgB = diag.next()
                K.op("pool" if c % 2 == 0 else "dve", lambda e: e.tensor_tensor(
                    out=dg[:, :, :], in0=identb[:, :].unsqueeze(1).to_broadcast([128, 31, 128]),
                    in1=par[:, c, 1:32].unsqueeze(2).to_broadcast([128, 31, 128]), op=ALU.mult), [identbB, parB], [dgB])
                dgs[c] = (dg, dgB)

            for c in range(3):
                build_diag(c)
            pend_stats = []

            def emit_stats():
                c_, yb_, ybB_, yq_, yqB_ = pend_stats.pop(0)
                K.op("pe", lambda e: e.matmul(pm[:, 0:512], onesb[:, :], yb_[:, :], start=(c_ == 0), stop=(c_ == 7)),
                     [onesbB, ybB_], [pmB], signal=(c_ == 7))
                K.op("pe", lambda e: e.matmul(pq[:, 0:512], onesb[:, :], yq_[:, :], start=(c_ == 0), stop=(c_ == 7)),
                     [onesbB, yqB_], [pqB], signal=(c_ == 7))

            for c in range(8):
                dg, dgB = dgs[c]
                pc, pcB = psA.next()
                for k in range(31):
                    K.op("pe", lambda e, k=k: e.matmul(pc[:, 0:512], dg[:, k, :], yc[:, c, 2 + k:2 + k + 512],
                                                       start=(k == 0), stop=(k == 30)),
                         [dgB, ycB[c]], [pcB], signal=(k == 30))
                if c + 3 < 8:
                    build_diag(c + 3)
                if pend_stats:
                    emit_stats()
                K.op("act", lambda e: e.activation(out=yv[:, c, :], in_=pc[:, 0:512], func=AF.Identity,
                                                   bias=par[:, c, 32:33]), [pcB, parB], [yvB[c]])
                yb, ybB = ybf.next()
                yq, yqB = ysq.next()
                K.op("dve", lambda e: e.tensor_copy(out=yb[:, :], in_=yv[:, c, :]), [yvB[c]], [ybB])
                K.op("pool", lambda e: e.tensor_tensor(out=yq[:, :], in0=yv[:, c, :], in1=yv[:, c, :], op=ALU.mult),
                     [yvB[c]], [yqB])
                pend_stats.append((c, yb, ybB, yq, yqB))
            while pend_stats:
                emit_stats()
            K.phase = "odLN"
            K.op("dve", lambda e: e.tensor_copy(out=mean[:, :], in_=pm[:, 0:512]), [pmB], [meanB])
            K.op("dve", lambda e: e.tensor_tensor(out=var[:, :], in0=mean[:, :], in1=mean[:, :], op=ALU.mult), [meanB], [varB])
            K.op("dve", lambda e: e.scalar_tensor_tensor(out=var[:, :], in0=pq[:, 0:512], scalar=LN_EPS, in1=var[:, :],
                                                         op0=ALU.add, op1=ALU.subtract), [pqB, varB], [varB])

        def od_ln_prep2():
            K.phase = "odLN"
            K.op("act", lambda e: e.activation(out=var[:, :], in_=var[:, :], func=AF.Sqrt), [varB], [varB])
            K.op("dve", lambda e: e.reciprocal(out=rstdt[:, :], in_=var[:, :]), [varB], [rstdtB])
            for c in range(8):
                K.op("dve", lambda e: e.tensor_tensor(out=yv[:, c, :], in0=yv[:, c, :], in1=mean[:, :], op=ALU.subtract),
                     [yvB[c], meanB], [yvB[c]])
                K.op("pool", lambda e: e.tensor_tensor(out=yv[:, c, :], in0=yv[:, c, :], in1=rstdt[:, :], op=ALU.mult),
                     [yvB[c], rstdtB], [yvB[c]])

        def od_ln_fin(bi):
            szz, szzB = szzs[bi % 2]
            K.phase = "odLNf"
            for c in range(8):
                ta, taB = t1.next()
                K.op("act", lambda e: e.activation(out=ta[:, :], in_=yv[:, c, :], func=AF.Silu,
                                                   scale=par[:, c, 33:34], bias=par[:, c, 34:35]),
                     [yvB[c], parB], [taB])
                K.op("dve", lambda e: e.tensor_tensor(out=mixT[:, c, :], in0=ta[:, :], in1=szz[:, c, :], op=ALU.mult),
                     [taB, szzB[c]], mixTB)

        od_proj(0)
        for bi, (s, b) in enumerate(blocks):
            if bi + 1 < len(blocks):
                K.phase = "odA"
                phase_A(layer, blocks[bi + 1][0], blocks[bi + 1][1])
            elif layer + 1 < n_layers:
                load_w_in(layer + 1)
            od_conv(bi)
            if bi + 1 < len(blocks):
                od_proj(bi + 1, mid=od_ln_prep2)
            else:
                od_ln_prep2()
            od_ln_fin(bi)
            K.phase = "odG"
            phase_G(layer, s, b)
        st.close()

    load_w_in(0)
    for layer in range(n_layers):
        li = layer // 2
        load_common(layer)
        if layer > 0:
            K.barrier()
        if layer % 2 == 0:
            even_layer(layer, li)
        else:
            odd_layer(layer, li)

    for s in range(NSEQ):
        for t in range(16):
            b = xd[s][t]
            if b.w is not None:
                sm, v = b.w
                nc.sync.wait_ge(sm.h, v)
    stack.close()
    return nc, K


def make_consts():
    p = np.arange(128)
    ident = np.eye(128, dtype=np.float32)
    tril = (p[:, None] <= p[None, :]).astype(np.float32)
    far = (p[:, None] > p[None, :]).astype(np.float32)
    ctab = np.zeros((128, 16, 32), np.float32)
    j = np.arange(32)
    for tile in range(16):
        t = tile * 128 + p
        cur = t // 64
        forced = (j[None, :] == 0) | (j[None, :] == cur[:, None]) | (j[None, :] == cur[:, None] - 1)
        causal = j[None, :] <= cur[:, None]
        ctab[:, tile, :] = np.where(causal, forced.astype(np.float32) * np.float32(1e4), np.float32(-1e30))
    ebig = ((np.arange(2048)[None, :] // 64) == j[:, None]).astype(np.float32) * np.float32(BIG)
    n_cmp = 127
    tok = np.arange(n_cmp)[:, None] * 16 + np.arange(32)[None, :]
    ovl = ((tok[:, :, None] // 64) == np.arange(32)[None, None, :]).sum(1).astype(np.float32) / np.float32(32)
    return {"c_ident": ident, "c_tril": tril, "c_far": far, "c_ctab": ctab, "c_ebig": ebig, "c_ovl": ovl}


_CACHE = {}


def kernel(**inputs):
    if "nc" not in _CACHE:
        _CACHE["nc"] = build_program(4)[0]
    nc = _CACHE["nc"]
    consts = make_consts()
    x = np.ascontiguousarray(inputs["x"], dtype=np.float32)
    shared = {k: np.ascontiguousarray(v, dtype=np.float32) for k, v in inputs.items() if k != "x"}
    shared.update(consts)
    in_maps = []
    for c in range(NCORES):
        m = dict(shared)
        m["x"] = x[c * NSEQ:(c + 1) * NSEQ]
        in_maps.append(m)
    res = run_bass_kernel_spmd(nc, in_maps, core_ids=list(range(NCORES)))
    return np.concatenate([r["out"] for r in res.results], axis=0).astype(np.float32)
```

```python
from contextlib import ExitStack

import numpy as np
import concourse.bass as bass
import concourse.mybir as mybir
from concourse.bass_utils import run_bass_kernel_spmd

F32 = mybir.dt.float32
BF16 = mybir.dt.bfloat16
AF = mybir.ActivationFunctionType
ALU = mybir.AluOpType

NCORES = 8
SEQ = 2048
D = 1024
NSEQ = 2
NBLK = 4
EVEN_W = 3352
ODD_W = 3072
RMS_EPS = 1e-6
LN_EPS = 1e-5
BIG = 30000.0
import os as _os
STOP = _os.environ.get("KSTOP", "")

C_Q, C_KC, C_VC, C_KS, C_VS, C_KW, C_VW, C_GATE, C_ZA, C_U, C_V, C_ZB = (
    0, 512, 640, 768, 896, 1024, 1152, 1280, 1304, 1816, 2328, 2840)


class Sem:
    __slots__ = ("h", "val", "name")

    def __init__(self, h, name=""):
        self.h = h
        self.val = 0
        self.name = name


class Buf:
    __slots__ = ("name", "w", "r", "dsem")

    def __init__(self, name):
        self.name = name
        self.w = None
        self.r = {}
        self.dsem = None


class Trk:
    def __init__(self, nc, stack):
        self.nc = nc
        self.stack = stack
        self.eng = {"pe": nc.tensor, "act": nc.scalar, "dve": nc.vector, "pool": nc.gpsimd, "sp": nc.sync}
        self.nsem = 0
        self.esem = {e: self.newsem("e_" + e) for e in self.eng}
        self.waited = {e: {} for e in self.eng}
        self.nops = 0
        self.dsems = []
        self.phase = ""
        self.waitlog = {e: [] for e in self.eng}

    def newsem(self, name):
        self.nsem += 1
        return Sem(self.stack.enter_context(self.nc.semaphore(f"{name}_{self.nsem}")), name)

    def _sync(self, E, reads, writes):
        deps = {}
        for b in reads:
            if b.w is not None:
                s, v = b.w
                if deps.get(s, 0) < v:
                    deps[s] = v
        for b in writes:
            if b.w is not None:
                s, v = b.w
                if deps.get(s, 0) < v:
                    deps[s] = v
            for s, v in b.r.items():
                if deps.get(s, 0) < v:
                    deps[s] = v
        eng = self.eng[E]
        w = self.waited[E]
        own = self.esem[E]
        for s, v in deps.items():
            if E == "pe" and s is own:
                continue
            if w.get(s, 0) < v:
                eng.wait_ge(s.h, v)
                w[s] = v
                self.waitlog[E].append((self.phase, s.name))

    def op(self, E, fn, reads=(), writes=(), signal=True):
        self._sync(E, reads, writes)
        ins = fn(self.eng[E])
        s = self.esem[E]
        if signal:
            s.val += 1
            ins.then_inc(s.h, 1)
            tag = (s, s.val)
        else:
            tag = (s, s.val + 1)
        for b in reads:
            if b.r.get(s, 0) < tag[1]:
                b.r[s] = tag[1]
        for b in writes:
            b.w = tag
            b.r = {}
        self.nops += 1
        return ins

    def barrier(self):
        sems = list(self.esem.values()) + self.dsems
        for E, eng in self.eng.items():
            w = self.waited[E]
            for s in sems:
                if s.val > 0 and w.get(s, 0) < s.val:
                    eng.wait_ge(s.h, s.val)
                    w[s] = s.val

    def dma(self, E, out, in_, reads, writes, dbuf, **kw):
        self._sync(E, reads, writes)
        if dbuf.dsem is None:
            dbuf.dsem = self.newsem("d_" + dbuf.name)
            self.dsems.append(dbuf.dsem)
        s = dbuf.dsem
        ins = self.eng[E].dma_start(out=out, in_=in_, **kw)
        s.val += 16
        ins.then_inc(s.h, 16)
        tag = (s, s.val)
        for b in reads:
            if b.r.get(s, 0) < tag[1]:
                b.r[s] = tag[1]
        for b in writes:
            b.w = tag
            b.r = {}
        self.nops += 1
        return ins


class Rot:
    def __init__(self, items):
        self.items = items
        self.i = 0

    def next(self):
        it = self.items[self.i % len(self.items)]
        self.i += 1
        return it


def build_program(n_layers=4):
    nc = bass.Bass("TRN2", target_bir_lowering=False, dynamic_dma_scratch_size=16384)
    stack = ExitStack()
    K = Trk(nc, stack)

    def dram(name, shape, dt=F32, kind="ExternalInput"):
        return nc.dram_tensor(name, list(shape), dt, kind=kind).ap()

    x_in = dram("x", [NSEQ, SEQ, D])
    out = dram("out", [NSEQ, SEQ, D], kind="ExternalOutput")
    norm_pre = dram("norm_pre", [4, D])
    norm_post = dram("norm_post", [4, D])
    e_w_in = dram("even_w_in", [2, D, EVEN_W])
    e_k_pe = dram("even_cmp_k_pe", [2, 32, 64])
    e_k_w1 = dram("even_cmp_k_w1", [2, 2048, 64])
    e_k_w2 = dram("even_cmp_k_w2", [2, 64, 64])
    e_v_pe = dram("even_cmp_v_pe", [2, 32, 64])
    e_v_w1 = dram("even_cmp_v_w1", [2, 2048, 64])
    e_v_w2 = dram("even_cmp_v_w2", [2, 64, 64])
    e_ln_g = dram("even_sgu_ln_g", [2, 512])
    e_ln_b = dram("even_sgu_ln_b", [2, 512])
    e_sgu_w = dram("even_sgu_w", [2, 8, 128, 128])
    e_sgu_b = dram("even_sgu_b", [2, 8, 128])
    e_w_out = dram("even_w_out", [2, D, D])
    o_w_in = dram("odd_w_in", [2, D, ODD_W])
    o_dw_w = dram("odd_dw_w", [2, 31, D])
    o_dw_b = dram("odd_dw_b", [2, D])
    o_ln_g = dram("odd_ln_g", [2, D])
    o_ln_b = dram("odd_ln_b", [2, D])
    o_w_out = dram("odd_w_out", [2, D, D])
    c_ident = dram("c_ident", [128, 128])
    c_tril = dram("c_tril", [128, 128])
    c_far = dram("c_far", [128, 128])
    c_ctab = dram("c_ctab", [128, 16, 32])
    c_ebig = dram("c_ebig", [32, 2048])
    c_ovl = dram("c_ovl", [127, 32])

    def sb(name, shape, dt=F32):
        t = stack.enter_context(nc.sbuf_tensor(name, list(shape), dt))
        return t, Buf(name)

    def sbn(name, shape, dt, n):
        return Rot([sb(f"{name}{i}", shape, dt) for i in range(n)])

    w_in = stack.enter_context(nc.sbuf_tensor("w_in", [128, 8, EVEN_W], BF16))
    w_inB = [Buf(f"w_in{k}") for k in range(8)]
    w_out = stack.enter_context(nc.sbuf_tensor("w_out", [128, 8, D], BF16))
    w_outB = [Buf(f"w_out{k}") for k in range(8)]
    identb, identbB = sb("identb", [128, 128], BF16)
    identf, identfB = sb("identf", [128, 128], F32)
    trilk, trilkB = sb("trilk", [128, 128], BF16)
    fark, farkB = sb("fark", [128, 128], BF16)
    neghalf, neghalfB = sb("neghalf", [128, 8], F32)
    gpost, gpostB = sb("gpost", [128, D], F32)
    xa = sbn("xa", [128, D], F32, 2)
    xr = sbn("xr", [128, D], F32, 2)
    hp = sbn("hp", [128, D], BF16, 2)
    hT = stack.enter_context(nc.sbuf_tensor("hT", [128, 8, 512], BF16))
    hTB = [Buf(f"hT{t}") for t in range(4)]
    mixT = stack.enter_context(nc.sbuf_tensor("mixT", [128, 8, 512], BF16))
    mixTB = [Buf(f"mixT{t}") for t in range(4)]
    junks = sbn("junk", [128, D], BF16, 2)
    ss, _ = sb("ss", [128, 8], F32)
    ssB = [Buf(f"ss{t}") for t in range(8)]
    ms, _ = sb("ms", [128, 8], F32)
    msB = [Buf(f"ms{t}") for t in range(8)]
    rstd, _ = sb("rstd", [128, 8], F32)
    rstdB = [Buf(f"rstd{t}") for t in range(8)]
    ssy = Rot([sb(f"ssy{i}", [128, 4], F32) + (Buf(f"ssy{i}a"), Buf(f"ssy{i}b")) for i in range(2)])
    rsy = sbn("rsy", [128, 4], F32, 2)
    par, parB = sb("par", [128, 8, 36], F32)
    tmpf = sbn("tmpf", [128, 512], F32, 4)

    PS = []
    for i in range(8):
        t = stack.enter_context(nc.psum_tensor(f"ps{i}", [128, 512], F32))
        PS.append((t, Buf(f"ps{i}")))
    psA = Rot(PS[0:4])
    psB = Rot(PS[4:8])

    xd = [[Buf(f"xd{s}_{t}") for t in range(16)] for s in range(NSEQ)]

    K.dma("pool", identb[:, :], c_ident[:, :], [], [identbB], identbB)
    K.dma("sp", identf[:, :], c_ident[:, :], [], [identfB], identfB)
    K.dma("pool", trilk[:, :], c_tril[:, :], [], [trilkB], trilkB)
    K.dma("pool", fark[:, :], c_far[:, :], [], [farkB], farkB)
    K.op("dve", lambda e: e.memset(neghalf[:, :], -0.5), [], [neghalfB])

    def layer_srcs(layer):
        li = layer // 2
        if layer % 2 == 0:
            return e_w_in[li], EVEN_W, e_w_out[li], []
        return (o_w_in[li], ODD_W, o_w_out[li],
                [(o_dw_w[li], 31), (o_dw_b[li:li + 1, :], 1), (o_ln_g[li:li + 1, :], 1), (o_ln_b[li:li + 1, :], 1)])

    def load_w_in(layer):
        w_in_dram, w_in_cols, _, _ = layer_srcs(layer)
        for kc in range(8):
            K.dma("pool", w_in[:, kc, 0:w_in_cols], w_in_dram[kc * 128:(kc + 1) * 128, :], [], [w_inB[kc]], w_inB[kc],
                  max_dma_last_dim=8192)

    def load_common(layer):
        _, _, w_out_dram, rows = layer_srcs(layer)
        stg, stgB = xa.next()
        rows = [(norm_pre[layer:layer + 1, :], 1)] + rows
        if sum(r for _, r in rows) % 2:
            rows = rows + [(norm_post[layer:layer + 1, :], 1)]
        r0 = 0
        for ap, r in rows:
            K.dma("sp", stg[r0:r0 + r, :], ap, [], [stgB], stgB)
            r0 += r
        R = r0
        for c in range(8):
            pt, pB = psA.next()
            K.op("pe", lambda e, c=c, pt=pt: e.transpose(pt[:, 0:R], stg[0:R, c * 128:(c + 1) * 128], identf[0:R, 0:R]),
                 [stgB, identfB], [pB])
            K.op("dve", lambda e, c=c, pt=pt: e.tensor_copy(out=par[:, c, 0:R], in_=pt[:, 0:R]), [pB], [parB])
        for kc in range(8):
            K.dma("pool", w_out[:, kc, :], w_out_dram[kc * 128:(kc + 1) * 128, :], [], [w_outB[kc]], w_outB[kc],
                  max_dma_last_dim=4096)
        K.dma("sp", gpost[:, :], norm_post[layer:layer + 1, :].to_broadcast([128, D]), [], [gpostB], gpostB)

    def phase_A(layer, s, b):
        src = x_in if layer == 0 else out
        xts = []
        for tt in range(4):
            xt, xB = xa.next()
            tile = b * 4 + tt
            K.dma("sp", xt[:, :], src[s, tile * 128:(tile + 1) * 128, :], [xd[s][tile]], [xB], xB)
            jk, jkB = junks.next()
            K.op("act", lambda e, xt=xt, tt=tt, jk=jk: e.activation(out=jk[:, :], in_=xt[:, :], func=AF.Square,
                                                                    accum_out=ss[:, tt:tt + 1]), [xB], [ssB[tt], jkB])
            K.op("dve", lambda e, tt=tt: e.tensor_scalar(out=ms[:, tt:tt + 1], in0=ss[:, tt:tt + 1], scalar1=1.0 / D,
                                                        scalar2=RMS_EPS, op0=ALU.mult, op1=ALU.add), [ssB[tt]], [msB[tt]])
            K.op("pool", lambda e, tt=tt: e.tensor_tensor(out=rstd[:, tt:tt + 1], in0=ms[:, tt:tt + 1],
                                                          in1=neghalf[:, 0:1], op=ALU.pow), [msB[tt], neghalfB], [rstdB[tt]])
            ht, hB = hp.next()
            K.op("dve", lambda e, ht=ht, xt=xt, tt=tt: e.tensor_scalar(out=ht[:, :], in0=xt[:, :], scalar1=rstd[:, tt:tt + 1],
                                                                       scalar2=None, op0=ALU.mult), [xB, rstdB[tt]], [hB])
            pt, pB = psA.next()
            pb = pt[:].bitcast(BF16)
            for kc in range(8):
                K.op("pe", lambda e, kc=kc, pb=pb, ht=ht: e.transpose(pb[:, kc * 128:(kc + 1) * 128],
                                                                      ht[:, kc * 128:(kc + 1) * 128], identb[:, :]),
                     [hB, identbB], [pB], signal=(kc == 7))
            K.op("dve", lambda e, pb=pb, tt=tt: e.tensor_tensor(
                out=hT[:, :, tt * 128:(tt + 1) * 128], in0=pb.rearrange("p (k t) -> p k t", k=8),
                in1=par[:, :, 0:1].to_broadcast([128, 8, 128]), op=ALU.mult), [pB, parB], [hTB[tt]])

    def fm_proj(col0, M):
        pt, pB = psA.next()
        for kc in range(8):
            K.op("pe", lambda e, kc=kc, pt=pt: e.matmul(pt[0:M, 0:512], w_in[:, kc, col0:col0 + M], hT[:, kc, :],
                                                        start=(kc == 0), stop=(kc == 7)),
                 [w_inB[kc]] + hTB, [pB], signal=(kc == 7))
        return pt, pB

    def tm_proj(tt, col0, N):
        pt, pB = psA.next()
        for kc in range(8):
            K.op("pe", lambda e, kc=kc, pt=pt: e.matmul(pt[:, 0:N], hT[:, kc, tt * 128:(tt + 1) * 128],
                                                        w_in[:, kc, col0:col0 + N], start=(kc == 0), stop=(kc == 7)),
                 [w_inB[kc], hTB[tt]], [pB], signal=(kc == 7))
        return pt, pB

    def phase_G(layer, s, b):
        src = x_in if layer == 0 else out
        for tt in range(4):
            tile = b * 4 + tt
            xt, xB = xr.next()
            K.dma("sp", xt[:, :], src[s, tile * 128:(tile + 1) * 128, :], [xd[s][tile]], [xB], xB)
            halves = []
            sy, syB, syB0, syB1 = ssy.next()
            ry, ryB = rsy.next()
            for hf in range(2):
                pt, pB = psB.next()
                for fc in range(8):
                    K.op("pe", lambda e, fc=fc, pt=pt, hf=hf: e.matmul(pt[:, 0:512], mixT[:, fc, tt * 128:(tt + 1) * 128],
                                                                        w_out[:, fc, hf * 512:(hf + 1) * 512],
                                                                        start=(fc == 0), stop=(fc == 7)),
                         [mixTB[tt], w_outB[fc]], [pB], signal=(fc == 7))
                halves.append((pt, pB))
            ysbs = []
            for hf, sB_ in ((0, syB0), (1, syB1)):
                pt, pB = halves[hf]
                tf, tfB = tmpf.next()
                if hf == 0:
                    K.op("act", lambda e, pt=pt, tf=tf: e.copy(out=tf[:, :], in_=pt[:, 0:512]), [pB], [tfB])
                else:
                    K.op("dve", lambda e, pt=pt, tf=tf: e.tensor_copy(out=tf[:, :], in_=pt[:, 0:512]), [pB], [tfB])
                jk, jkB = junks.next()
                K.op("act", lambda e, tf=tf, hf=hf, sy=sy, jk=jk: e.activation(out=jk[:, 0:512], in_=tf[:, :], func=AF.Square,
                                                                               accum_out=sy[:, hf:hf + 1]), [tfB], [sB_, jkB])
                ysbs.append((tf, tfB))
            K.op("dve", lambda e, sy=sy: e.tensor_tensor(out=sy[:, 2:3], in0=sy[:, 0:1], in1=sy[:, 1:2], op=ALU.add),
                 [syB0, syB1, syB], [syB])
            K.op("dve", lambda e, sy=sy: e.tensor_scalar(out=sy[:, 3:4], in0=sy[:, 2:3], scalar1=1.0 / D, scalar2=RMS_EPS,
                                                        op0=ALU.mult, op1=ALU.add), [syB], [syB])
            K.op("pool", lambda e, sy=sy, ry=ry: e.tensor_tensor(out=ry[:, 0:1], in0=sy[:, 3:4], in1=neghalf[:, 0:1],
                                                                 op=ALU.pow), [syB, neghalfB], [ryB])
            for hf in range(2):
                tf, tfB = ysbs[hf]
                K.op("dve", lambda e, tf=tf, hf=hf, ry=ry: e.scalar_tensor_tensor(
                    out=tf[:, :], in0=tf[:, :], scalar=ry[:, 0:1], in1=gpost[:, hf * 512:(hf + 1) * 512],
                    op0=ALU.mult, op1=ALU.mult), [tfB, ryB, gpostB], [tfB])
                K.op("pool", lambda e, tf=tf, xt=xt, hf=hf: e.tensor_tensor(
                    out=xt[:, hf * 512:(hf + 1) * 512], in0=xt[:, hf * 512:(hf + 1) * 512], in1=tf[:, :], op=ALU.add),
                     [tfB, xB], [xB])
            K.dma("sp", out[s, tile * 128:(tile + 1) * 128, :], xt[:, :], [xB], [xd[s][tile]], xd[s][tile])

    def even_layer(layer, li):
        st = ExitStack()

        def sbl(name, shape, dt=F32):
            t = st.enter_context(nc.sbuf_tensor(f"{name}_{layer}", list(shape), dt))
            return t, Buf(name)

        def sbln(name, shape, dt, n):
            return Rot([sbl(f"{name}{i}", shape, dt) for i in range(n)])

        qT = st.enter_context(nc.sbuf_tensor(f"qT_{layer}", [128, 8, 512], BF16))
        qTB = [Buf(f"qT{h}") for h in range(8)]
        qSB = [Buf(f"qS{h}") for h in range(8)]
        ksT = st.enter_context(nc.sbuf_tensor(f"ksT_{layer}", [128, 2, 2048], BF16))
        ksTB = [[Buf(f"ksT{g}_{b}") for b in range(4)] for g in range(2)]
        ebB = Buf("ebig")
        kwT = st.enter_context(nc.sbuf_tensor(f"kwT_{layer}", [64, 2, 1024], BF16))
        kwTB = [[Buf(f"kwT{g}_{r}") for r in range(2)] for g in range(2)]
        kcT, kcTB = sbl("kcT", [128, 528], BF16)
        vcT, vcTB = sbl("vcT", [128, 528], BF16)
        vsA = st.enter_context(nc.sbuf_tensor(f"vsA_{layer}", [128, 16, 2, 65], BF16))
        vsAB = [Buf(f"vsA{k}") for k in range(16)]
        vwA = st.enter_context(nc.sbuf_tensor(f"vwA_{layer}", [128, 8, 2, 65], BF16))
        vwAB = [Buf(f"vwA{k}") for k in range(8)]
        w1k, w1kB = sbl("w1k", [128, 32, 64], BF16)
        w1v, w1vB = sbl("w1v", [128, 32, 64], BF16)
        w2k, w2kB = sbl("w2k", [64, 64], BF16)
        w2v, w2vB = sbl("w2v", [64, 64], BF16)
        pek, pekB = sbl("pek", [32, 64], BF16)
        pev, pevB = sbl("pev", [32, 64], BF16)
        peT, peTB = sbl("peT", [64, 2, 32], BF16)
        pebias, pebiasB = sbl("pebias", [64, 2], F32)
        hidk, hidkB = sbl("hidk", [64, 2, 32], BF16)
        hidv, hidvB = sbl("hidv", [64, 2, 128], BF16)
        kcmpT, kcmpTB = sbl("kcmpT", [64, 2, 128], BF16)
        vcmp, vcmpB = sbl("vcmp", [128, 2, 97], BF16)
        ctab, ctabB = sbl("ctab", [128, 16, 32], F32)
        WsT, WsTB = sbl("WsT", [128, 8, 128], BF16)
        bs, bsB = sbl("bs", [128, 8], F32)
        lng, lngB = sbl("lng", [128, 512], F32)
        lnb, lnbB = sbl("lnb", [128, 512], F32)
        gates = st.enter_context(nc.sbuf_tensor(f"gates_{layer}", [128, 4, 24], F32))
        gatesB = [Buf(f"gates{t}") for t in range(4)]
        sza = st.enter_context(nc.sbuf_tensor(f"sza_{layer}", [128, 4, 512], BF16))
        szaB = [Buf(f"sza{t}") for t in range(4)]
        ug = sbln("ug", [128, 512], BF16, 2)
        szb = sbln("szb", [128, 512], BF16, 2)
        vg = sbln("vg", [128, 512], F32, 2)
        vln = sbln("vln", [128, 512], BF16, 2)
        lnst = sbln("lnst", [128, 16], F32, 2)
        acc_o = st.enter_context(nc.sbuf_tensor(f"acc_o_{layer}", [128, 4, 512], F32))
        acc_oB = [Buf(f"acc_o{h}") for h in range(8)]
        imp = sbln("imp", [128, 4, 32], F32, 2)
        top8 = sbln("top8", [128, 4, 8], F32, 2)
        selpads = [sbl(f"selpad{g}", [128, 4, 96], BF16) for g in range(2)]
        pTs = sbln("pT", [128, 512], BF16, 4)
        rcs = sbln("rc", [128, 16], F32, 4)
        amix = sbln("amix", [128, 512], BF16, 2)
        tmpb = sbln("tmpb", [128, 512], BF16, 3)

        for g in range(2):
            K.dma("pool", ksT[64:96, g, :], c_ebig[:, :], [], [ebB], ebB, max_dma_last_dim=4096)
        for wi, (w1, w1B, src) in enumerate(((w1k, w1kB, e_k_w1), (w1v, w1vB, e_v_w1))):
            srcv = src[li].rearrange("(l d) e -> d l e", d=64)
            for lh in range(2):
                stg, stgB = xa.next()
                stgv = stg[:, :].rearrange("p (l e) -> p l e", l=16)
                for half in range(2):
                    K.dma("sp", stgv[half * 64:(half + 1) * 64, :, :], srcv[:, lh * 16:(lh + 1) * 16, :], [], [stgB], stgB)
                if (wi + lh) % 2 == 0:
                    K.op("dve", lambda e, w1=w1, lh=lh, stgv=stgv: e.tensor_copy(out=w1[:, lh * 16:(lh + 1) * 16, :], in_=stgv),
                         [stgB], [w1B])
                else:
                    K.op("act", lambda e, w1=w1, lh=lh, stgv=stgv: e.copy(out=w1[:, lh * 16:(lh + 1) * 16, :], in_=stgv),
                         [stgB], [w1B])
        K.dma("pool", w2k[:, :], e_k_w2[li], [], [w2kB], w2kB)
        K.dma("pool", w2v[:, :], e_v_w2[li], [], [w2vB], w2vB)
        K.dma("pool", pek[:, :], e_k_pe[li], [], [pekB], pekB)
        K.dma("pool", pev[:, :], e_v_pe[li], [], [pevB], pevB)
        K.dma("sp", ctab[:, :, :], c_ctab[:, :, :], [], [ctabB], ctabB)
        wsst_t, wsstB = xa.next()
        wsst = wsst_t[:, :].rearrange("p (g j) -> p g j", g=8)
        K.dma("sp", wsst, e_sgu_w[li].rearrange("g i j -> i g j"), [], [wsstB], wsstB)
        bst, bstB = xa.next()
        K.dma("sp", bst[0:8, 0:128], e_sgu_b[li], [], [bstB], bstB)
        pt, pB = psA.next()
        K.op("pe", lambda e: e.transpose(pt[:, 0:8], bst[0:8, 0:128], identf[0:8, 0:8]), [bstB, identfB], [pB])
        K.op("dve", lambda e: e.tensor_copy(out=bs[:, :], in_=pt[:, 0:8]), [pB], [bsB])
        K.dma("sp", lng[:, :], e_ln_g[li:li + 1, :].to_broadcast([128, 512]), [], [lngB], lngB)
        K.dma("sp", lnb[:, :], e_ln_b[li:li + 1, :].to_broadcast([128, 512]), [], [lnbB], lnbB)
        K.op("pool", lambda e: e.memset(vsA[:, :, :, 64:65], 1.0), [], vsAB)
        K.op("pool", lambda e: e.memset(vwA[:, :, :, 64:65], 1.0), [], vwAB)
        K.op("pool", lambda e: e.memset(vcmp[:, :, :], 0.0), [], [vcmpB])
        K.op("pool", lambda e: e.memset(vcmp[:, :, 64:65], 1.0), [vcmpB], [vcmpB])
        for g in range(2):
            K.dma("pool", vcmp[0:127, g, 65:97], c_ovl[:, :], [], [vcmpB], vcmpB)
        K.op("pool", lambda e: e.memset(hidv[:, :, :], 0.0), [], [hidvB])
        K.op("pool", lambda e: e.memset(kcmpT[:, :, :], 0.0), [], [kcmpTB])
        for selpad, selpadB in selpads:
            K.op("pool", lambda e, selpad=selpad: e.memset(selpad[:, :, :], 0.0), [], [selpadB])
        K.op("dve", lambda e: e.memset(kcT[:, 0:16], 0.0), [], [kcTB])
        K.op("dve", lambda e: e.memset(vcT[:, 0:16], 0.0), [], [vcTB])
        for g in range(8):
            pt, pB = psA.next()
            K.op("pe", lambda e, g=g, pt=pt: e.transpose(pt[:, 0:128], wsst[:, g, :], identf[:, :]), [wsstB, identfB], [pB])
            K.op("dve", lambda e, g=g, pt=pt: e.tensor_tensor(out=WsT[:, g, :], in0=pt[:, 0:128], in1=trilk[:, :], op=ALU.mult),
                 [pB, trilkB], [WsTB])
        for xi, (pe_, peB_, w1, w1B) in enumerate(((pek, pekB, w1k, w1kB), (pev, pevB, w1v, w1vB))):
            pt, pB = psA.next()
            pb = pt[:].bitcast(BF16)
            K.op("pe", lambda e, pb=pb, pe_=pe_: e.transpose(pb[0:64, 0:32], pe_[:, :], identb[0:32, 0:32]), [peB_, identbB], [pB])
            K.op("dve", lambda e, pb=pb, xi=xi: e.tensor_copy(out=peT[:, xi, :], in_=pb[0:64, 0:32]), [pB], [peTB])
            pt2, pB2 = psA.next()
            for l in range(32):
                K.op("pe", lambda e, l=l, pt2=pt2, w1=w1, xi=xi: e.matmul(pt2[0:64, 0:1], w1[0:64, l, :], peT[:, xi, l:l + 1],
                                                                          start=(l == 0), stop=(l == 31)),
                     [w1B, peTB], [pB2], signal=(l == 31))
            K.op("dve", lambda e, pt2=pt2, xi=xi: e.tensor_copy(out=pebias[:, xi:xi + 1], in_=pt2[0:64, 0:1]), [pB2], [pebiasB])

        def evac_copy(i, out_ap, in_ap, reads, writes, scale=None):
            if i % 2 == 0:
                if scale is None:
                    K.op("act", lambda e: e.copy(out=out_ap, in_=in_ap), reads, writes)
                else:
                    K.op("act", lambda e: e.mul(out=out_ap, in_=in_ap, mul=scale), reads, writes)
            else:
                if scale is None:
                    K.op("dve", lambda e: e.tensor_copy(out=out_ap, in_=in_ap), reads, writes)
                else:
                    K.op("dve", lambda e: e.tensor_scalar(out=out_ap, in0=in_ap, scalar1=scale, scalar2=None, op0=ALU.mult),
                         reads, writes)

        if STOP == "params":
            st.close()
            return
        blocks = [(s_, b_) for s_ in range(NSEQ) for b_ in range(NBLK)]
        K.phase = "evA"
        phase_A(layer, 0, 0)
        for bi, (s, b) in enumerate(blocks):
            if True:
                t0 = b * 512
                K.phase = "evB"
                if STOP == "A":
                    st.close()
                    return
                for h in range(8):
                    pt, pB = fm_proj(C_Q + h * 64, 64)
                    evac_copy(h, qT[0:64, h, :], pt[0:64, 0:512], [pB], [qTB[h]], scale=0.125)
                for g in range(2):
                    pt, pB = fm_proj(C_KS + g * 64, 64)
                    evac_copy(g, ksT[0:64, g, t0:t0 + 512], pt[0:64, 0:512], [pB], [ksTB[g][b]])
                for g in range(2):
                    pt, pB = fm_proj(C_KW + g * 64, 64)
                    r = b % 2
                    evac_copy(g + 1, kwT[0:64, g, r * 512:(r + 1) * 512], pt[0:64, 0:512], [pB], [kwTB[g][r]])
                for i, (cX, XT, XTB) in enumerate(((C_KC, kcT, kcTB), (C_VC, vcT, vcTB))):
                    if b > 0:
                        K.op("pool", lambda e, XT=XT: e.tensor_copy(out=XT[:, 0:16], in_=XT[:, 512:528]), [XTB], [XTB])
                    pt, pB = fm_proj(cX, 128)
                    evac_copy(i, XT[:, 16:528], pt[:, 0:512], [pB], [XTB])
                if STOP == "B":
                    st.close()
                    return
                K.phase = "evC"
                cst = {}
                cst2 = {}

                def c_front(tt):
                    kt = b * 4 + tt
                    pt, pB = tm_proj(tt, C_VS, 408)
                    K.op("dve", lambda e, pt=pt, kt=kt: e.tensor_copy(out=vsA[:, kt, :, 0:64],
                                                                      in_=pt[:, 0:128].rearrange("p (g d) -> p g d", g=2)),
                         [pB], [vsAB[kt]])
                    K.op("dve", lambda e, pt=pt, kt=kt: e.tensor_copy(out=vwA[:, kt % 8, :, 0:64],
                                                                      in_=pt[:, 256:384].rearrange("p (g d) -> p g d", g=2)),
                         [pB], [vwAB[kt % 8]])
                    K.op("act", lambda e, pt=pt, tt=tt: e.activation(out=gates[:, tt, :], in_=pt[:, 384:408], func=AF.Sigmoid),
                         [pB], [gatesB[tt]])
                    pt, pB = tm_proj(tt, C_ZA, 512)
                    K.op("act", lambda e, pt=pt, tt=tt: e.activation(out=sza[:, tt, :], in_=pt[:, 0:512], func=AF.Silu),
                         [pB], [szaB[tt]])
                    pt, pB = tm_proj(tt, C_ZB, 512)
                    zt, ztB = szb.next()
                    K.op("act", lambda e, pt=pt, zt=zt: e.activation(out=zt[:, :], in_=pt[:, 0:512], func=AF.Silu), [pB], [ztB])
                    pt, pB = tm_proj(tt, C_U, 512)
                    ut, utB = ug.next()
                    K.op("act", lambda e, pt=pt, ut=ut: e.activation(out=ut[:, :], in_=pt[:, 0:512], func=AF.Gelu_apprx_tanh),
                         [pB], [utB])
                    pt, pB = tm_proj(tt, C_V, 512)
                    vt, vtB = vg.next()
                    K.op("act", lambda e, pt=pt, vt=vt: e.activation(out=vt[:, :], in_=pt[:, 0:512], func=AF.Gelu_apprx_tanh),
                         [pB], [vtB])
                    K.op("pool", lambda e, ut=ut, zt=zt: e.tensor_tensor(out=ut[:, :], in0=ut[:, :], in1=zt[:, :], op=ALU.mult),
                         [utB, ztB], [utB])
                    ls, lsB = lnst.next()
                    K.op("dve", lambda e, ls=ls, vt=vt: e.bn_stats(out=ls[:, 0:6], in_=vt[:, :]), [vtB], [lsB])
                    K.op("dve", lambda e, ls=ls: e.bn_aggr(out=ls[:, 6:8], in_=ls[:, 0:6]), [lsB], [lsB])
                    K.op("dve", lambda e, ls=ls: e.tensor_scalar(out=ls[:, 8:9], in0=ls[:, 7:8], scalar1=LN_EPS, scalar2=None,
                                                                op0=ALU.add), [lsB], [lsB])
                    K.op("pool", lambda e, ls=ls: e.tensor_tensor(out=ls[:, 9:10], in0=ls[:, 8:9], in1=neghalf[:, 0:1], op=ALU.pow),
                         [lsB, neghalfB], [lsB])
                    K.op("dve", lambda e, ls=ls, vt=vt: e.tensor_scalar(out=vt[:, :], in0=vt[:, :], scalar1=ls[:, 6:7],
                                                                        scalar2=ls[:, 9:10], op0=ALU.subtract, op1=ALU.mult),
                         [lsB, vtB], [vtB])
                    K.op("pool", lambda e, vt=vt: e.tensor_tensor(out=vt[:, :], in0=vt[:, :], in1=lng[:, :], op=ALU.mult),
                         [vtB, lngB], [vtB])
                    vl, vlB = vln.next()
                    K.op("pool", lambda e, vt=vt, vl=vl: e.tensor_tensor(out=vl[:, :], in0=vt[:, :], in1=lnb[:, :], op=ALU.add),
                         [vtB, lnbB], [vlB])
                    cst[tt] = (vl, vlB, ut, utB)

                def c_back(tt):
                    vl, vlB, ut, utB = cst.pop(tt)
                    pt, pB = psB.next()
                    for g in range(8):
                        K.op("pe", lambda e, g=g, pt=pt, vl=vl: e.matmul(pt[:, g * 64:(g + 1) * 64], WsT[:, g, :],
                                                                          vl[:, g * 64:(g + 1) * 64], start=True, stop=True),
                             [WsTB, vlB], [pB], signal=(g == 7))
                    tb, tbB = tmpb.next()
                    K.op("dve", lambda e, pt=pt, tb=tb: e.tensor_tensor(
                        out=tb[:, :].rearrange("p (g d) -> p g d", g=8), in0=pt[:, 0:512].rearrange("p (g d) -> p g d", g=8),
                        in1=bs[:, :].unsqueeze(2).to_broadcast([128, 8, 64]), op=ALU.add), [pB, bsB], [tbB])
                    K.op("pool", lambda e, tb=tb, ut=ut: e.tensor_tensor(out=tb[:, :], in0=tb[:, :], in1=ut[:, :], op=ALU.mult),
                         [tbB, utB], [tbB])
                    cst2[tt] = (tb, tbB)

                def c_back2(tt):
                    tb, tbB = cst2.pop(tt)
                    pt, pB = psA.next()
                    pb = pt[:].bitcast(BF16)
                    for fc in range(4):
                        K.op("pe", lambda e, fc=fc, pb=pb, tb=tb: e.transpose(pb[:, fc * 128:(fc + 1) * 128],
                                                                              tb[:, fc * 128:(fc + 1) * 128], identb[:, :]),
                             [tbB, identbB], [pB], signal=(fc == 3))
                    K.op("dve", lambda e, pb=pb, tt=tt: e.tensor_copy(out=mixT[:, 4:8, tt * 128:(tt + 1) * 128],
                                                                      in_=pb[:, 0:512].rearrange("p (k t) -> p k t", k=4)),
                         [pB], [mixTB[tt]])

                for tt in range(6):
                    if tt < 4:
                        c_front(tt)
                    if 1 <= tt <= 4:
                        c_back(tt - 1)
                    if tt >= 2:
                        c_back2(tt - 2)
                if STOP == "C":
                    st.close()
                    return
                if bi + 1 < len(blocks):
                    K.phase = "evA"
                    phase_A(layer, blocks[bi + 1][0], blocks[bi + 1][1])
                elif layer + 1 < n_layers:
                    load_w_in(layer + 1)
                K.phase = "evD"
                if b == 0:
                    n0, nn, c0 = 0, 31, 16
                else:
                    n0, nn, c0 = 32 * b - 1, 32, 0
                nk = 32 * (b + 1) - 1
                for g in range(2):
                    for xi, (XT, XTB, w1, w1B, hid, hidB, hcol) in enumerate((
                            (kcT, kcTB, w1k, w1kB, hidk, hidkB, 0), (vcT, vcTB, w1v, w1vB, hidv, hidvB, n0))):
                        pt, pB = psA.next()
                        for l in range(32):
                            K.op("pe", lambda e, l=l, pt=pt, XT=XT, w1=w1: e.matmul(
                                pt[0:64, 0:nn], w1[g * 64:(g + 1) * 64, l, :],
                                XT[g * 64:(g + 1) * 64, c0 + l:c0 + l + 16 * (nn - 1) + 1:16], start=(l == 0), stop=(l == 31)),
                                 [w1B, XTB], [pB], signal=(l == 31))
                        K.op("act", lambda e, pt=pt, hid=hid, hcol=hcol, xi=xi: e.activation(
                            out=hid[:, g, hcol:hcol + nn], in_=pt[0:64, 0:nn], func=AF.Silu, bias=pebias[:, xi:xi + 1]),
                             [pB, pebiasB], [hidB])
                    pt, pB = psA.next()
                    K.op("pe", lambda e, pt=pt: e.matmul(pt[0:64, 0:nn], w2k[:, :], hidk[:, g, 0:nn], start=True, stop=True),
                         [w2kB, hidkB], [pB])
                    K.op("dve", lambda e, pt=pt: e.tensor_copy(out=kcmpT[:, g, n0:n0 + nn], in_=pt[0:64, 0:nn]), [pB], [kcmpTB])
                    pt, pB = psA.next()
                    K.op("pe", lambda e, pt=pt: e.matmul(pt[0:nk, 0:64], hidv[:, g, 0:nk], w2v[:, :], start=True, stop=True),
                         [w2vB, hidvB], [pB])
                    K.op("dve", lambda e, pt=pt: e.tensor_copy(out=vcmp[0:nk, g, 0:64], in_=pt[0:nk, 0:64]), [pB], [vcmpB])
                if STOP == "D":
                    st.close()
                    return
                K.phase = "evE"
                for g in range(2):
                    selpad, selpadB = selpads[g]
                    im, imB = imp.next()
                    for r in range(4):
                        h = g * 4 + r
                        pt, pB = psA.next()
                        K.op("pe", lambda e, pt=pt, h=h: e.matmul(pt[0:nk, 0:512], kcmpT[:, g, 0:nk], qT[0:64, h, :],
                                                                  start=True, stop=True), [kcmpTB, qTB[h]], [pB])
                        pT, pTB = pTs.next()
                        K.op("act", lambda e, pt=pt, pT=pT: e.activation(out=pT[0:nk, :], in_=pt[0:nk, 0:512], func=AF.Exp),
                             [pB], [pTB])
                        if STOP == "E1":
                            st.close()
                            return
                        K.op("pool", lambda e, pT=pT: e.affine_select(out=pT[0:nk, :], in_=pT[0:nk, :], pattern=[[1, 512]],
                                                                      compare_op=ALU.is_ge, fill=0.0, base=t0 - 31,
                                                                      channel_multiplier=-16), [pTB], [pTB])
                        if STOP == "E2":
                            st.close()
                            return
                        po, poB = psB.next()
                        po3 = po[:, 0:388].rearrange("p (t c) -> p t c", t=4)
                        for tt in range(4):
                            K.op("pe", lambda e, tt=tt, po=po, pT=pT: e.matmul(po[:, tt * 97:(tt + 1) * 97],
                                                                                pT[0:nk, tt * 128:(tt + 1) * 128], vcmp[0:nk, g, :],
                                                                                start=True, stop=True),
                                 [pTB, vcmpB], [poB], signal=(tt == 3))
                        if STOP == "E3":
                            st.close()
                            return
                        rc, rcB = rcs.next()
                        K.op("dve", lambda e, rc=rc, po3=po3: e.tensor_scalar(out=rc[:, 0:4], in0=po3[:, :, 64], scalar1=1e-30,
                                                                              scalar2=None, op0=ALU.max), [poB], [rcB])
                        K.op("dve", lambda e, rc=rc: e.reciprocal(out=rc[:, 4:8], in_=rc[:, 0:4]), [rcB], [rcB])
                        K.op("dve", lambda e, rc=rc, h=h: e.tensor_tensor(out=rc[:, 8:12], in0=rc[:, 4:8], in1=gates[:, :, h],
                                                                          op=ALU.mult), [rcB] + gatesB, [rcB])
                        for tt in range(4):
                            K.op("dve", lambda e, tt=tt, po3=po3, rc=rc, h=h: e.tensor_scalar(
                                out=acc_o[:, tt, h * 64:(h + 1) * 64], in0=po3[:, tt, 0:64], scalar1=rc[:, 8 + tt:9 + tt],
                                scalar2=None, op0=ALU.mult), [poB, rcB], [acc_oB[h]])
                        for tt in range(4):
                            if r == 0:
                                K.op("dve", lambda e, tt=tt, po3=po3, rc=rc, im=im: e.tensor_scalar(
                                    out=im[:, tt, :], in0=po3[:, tt, 65:97], scalar1=rc[:, 4 + tt:5 + tt], scalar2=None,
                                    op0=ALU.mult), [poB, rcB], [imB])
                            else:
                                K.op("dve", lambda e, tt=tt, po3=po3, rc=rc, im=im: e.scalar_tensor_tensor(
                                    out=im[:, tt, :], in0=po3[:, tt, 65:97], scalar=rc[:, 4 + tt:5 + tt], in1=im[:, tt, :],
                                    op0=ALU.mult, op1=ALU.add), [poB, rcB, imB], [imB])
                    if STOP == "E4":
                        st.close()
                        return
                    K.op("dve", lambda e, im=im: e.tensor_tensor(out=im[:, :, :], in0=im[:, :, :], in1=ctab[:, 4 * b:4 * b + 4, :],
                                                                 op=ALU.add), [imB, ctabB], [imB])
                    t8, t8B = top8.next()
                    for tt in range(4):
                        K.op("dve", lambda e, tt=tt, t8=t8, im=im: e.max(out=t8[:, tt, :], in_=im[:, tt, :]), [imB], [t8B])
                    for tt in range(4):
                        K.op("dve", lambda e, tt=tt, t8=t8, im=im: e.tensor_scalar(
                            out=selpad[:, tt, 64:96], in0=im[:, tt, :], scalar1=t8[:, tt, 7:8], scalar2=-1.0,
                            op0=ALU.is_ge, op1=ALU.add), [imB, t8B], [selpadB])
                    if STOP == "E6":
                        st.close()
                        return
                for g in range(2):
                    selpad, selpadB = selpads[g]
                    pt, pB = psA.next()
                    pb = pt[:].bitcast(BF16)
                    for tt in range(4):
                        K.op("pe", lambda e, tt=tt, pb=pb: e.transpose(pb[0:96, tt * 128:(tt + 1) * 128], selpad[:, tt, :],
                                                                       identb[:, :]), [selpadB, identbB], [pB], signal=(tt == 3))
                    for r in range(4):
                        h = g * 4 + r
                        K.op("dve", lambda e, h=h, pb=pb: e.tensor_copy(out=qT[64:96, h, :], in_=pb[64:96, 0:512]), [pB], [qSB[h]])
                if STOP == "E":
                    st.close()
                    return
                K.phase = "evF"
                items = []
                for h in range(8):
                    for br in (1, 0):
                        kts = list(range(0, 4 * b + 4)) if br == 0 else list(range(max(0, 4 * b - 4), 4 * b + 4))
                        for kt in kts:
                            items.append((h, br, kt, kt == kts[0], kt == kts[-1]))
                LA = 2
                stage = {}
                accs = {}

                def front(idx):
                    h, br, kt, isfirst, islast = items[idx]
                    g = h // 4
                    d = kt - 4 * b
                    lo = max(0, d)
                    hi = 3 if br == 0 else min(3, d + 4)
                    c0_, c1_ = lo * 128, (hi + 1) * 128
                    pt, pB = psA.next()
                    if br == 0:
                        K.op("pe", lambda e: e.matmul(
                            pt[:, c0_:c1_], ksT[0:96, g, kt * 128:(kt + 1) * 128], qT[0:96, h, c0_:c1_],
                            start=True, stop=True), [ksTB[g][kt // 4], ebB, qTB[h], qSB[h]], [pB])
                    else:
                        kr = kt % 8
                        K.op("pe", lambda e: e.matmul(
                            pt[:, c0_:c1_], kwT[0:64, g, kr * 128:(kr + 1) * 128], qT[0:64, h, c0_:c1_],
                            start=True, stop=True), [kwTB[g][kr // 4], qTB[h]], [pB])
                    pT, pTB = pTs.next()
                    K.op("act", lambda e: e.activation(out=pT[:, c0_:c1_], in_=pt[:, c0_:c1_], func=AF.Exp), [pB], [pTB])
                    if d >= 0:
                        K.op("pool", lambda e: e.tensor_tensor(
                            out=pT[:, d * 128:(d + 1) * 128], in0=pT[:, d * 128:(d + 1) * 128], in1=trilk[:, :],
                            op=ALU.mult), [pTB, trilkB], [pTB])
                    if br == 1 and 0 <= d + 4 <= 3:
                        K.op("pool", lambda e: e.tensor_tensor(
                            out=pT[:, (d + 4) * 128:(d + 5) * 128], in0=pT[:, (d + 4) * 128:(d + 5) * 128],
                            in1=fark[:, :], op=ALU.mult), [pTB, farkB], [pTB])
                    stage[idx] = (pT, pTB, lo, hi)

                def back(idx):
                    h, br, kt, isfirst, islast = items[idx]
                    g = h // 4
                    pT, pTB, lo, hi = stage.pop(idx)
                    if isfirst:
                        accs[(h, br)] = psB.next()
                    acc, accB = accs[(h, br)]
                    for tt in range(lo, hi + 1):
                        if br == 0:
                            vB_, rhs = vsAB[kt], vsA[:, kt, g, :]
                        else:
                            vB_, rhs = vwAB[kt % 8], vwA[:, kt % 8, g, :]
                        K.op("pe", lambda e, tt=tt, rhs=rhs: e.matmul(
                            acc[:, tt * 65:(tt + 1) * 65], pT[:, tt * 128:(tt + 1) * 128], rhs,
                            start=(isfirst and tt == lo), stop=True, skip_group_check=True), [pTB, vB_], [accB], signal=(tt == hi))
                    if islast:
                        acc3 = acc[:, 0:260].rearrange("p (t c) -> p t c", t=4)
                        rc, rcB = rcs.next()
                        K.op("dve", lambda e: e.tensor_scalar(out=rc[:, 0:4], in0=acc3[:, :, 64], scalar1=1e-30,
                                                              scalar2=None, op0=ALU.max), [accB], [rcB])
                        K.op("dve", lambda e: e.reciprocal(out=rc[:, 4:8], in_=rc[:, 0:4]), [rcB], [rcB])
                        gi = (1 + br) * 8 + h
                        K.op("dve", lambda e: e.tensor_tensor(out=rc[:, 8:12], in0=rc[:, 4:8], in1=gates[:, :, gi],
                                                              op=ALU.mult), [rcB] + gatesB, [rcB])
                        for tt in range(4):
                            K.op("dve", lambda e, tt=tt: e.scalar_tensor_tensor(
                                out=acc_o[:, tt, h * 64:(h + 1) * 64], in0=acc3[:, tt, 0:64], scalar=rc[:, 8 + tt:9 + tt],
                                in1=acc_o[:, tt, h * 64:(h + 1) * 64], op0=ALU.mult, op1=ALU.add),
                                 [accB, rcB, acc_oB[h]], [acc_oB[h]])
                        del accs[(h, br)]

                for idx in range(len(items) + LA):
                    if idx < len(items):
                        front(idx)
                    if idx - LA >= 0:
                        back(idx - LA)
                for tt in range(4):
                    am, amB = amix.next()
                    K.op("pool", lambda e, tt=tt, am=am: e.tensor_tensor(out=am[:, :], in0=acc_o[:, tt, :], in1=sza[:, tt, :],
                                                                         op=ALU.mult), acc_oB + [szaB[tt]], [amB])
                    pt, pB = psA.next()
                    pb = pt[:].bitcast(BF16)
                    for fc in range(4):
                        K.op("pe", lambda e, fc=fc, pb=pb, am=am: e.transpose(pb[:, fc * 128:(fc + 1) * 128],
                                                                              am[:, fc * 128:(fc + 1) * 128], identb[:, :]),
                             [amB, identbB], [pB], signal=(fc == 3))
                    K.op("dve", lambda e, pb=pb, tt=tt: e.tensor_copy(out=mixT[:, 0:4, tt * 128:(tt + 1) * 128],
                                                                      in_=pb[:, 0:512].rearrange("p (k t) -> p k t", k=4)),
                         [pB], [mixTB[tt]])
                if STOP == "F":
                    st.close()
                    return
                K.phase = "evG"
                phase_G(layer, s, b)
                if STOP == "G":
                    st.close()
                    return
        st.close()

    def odd_layer(layer, li):
        st = ExitStack()

        def sbl(name, shape, dt=F32):
            t = st.enter_context(nc.sbuf_tensor(f"{name}_{layer}", list(shape), dt))
            return t, Buf(name)

        def sbln(name, shape, dt, n):
            return Rot([sbl(f"{name}{i}", shape, dt) for i in range(n)])

        yc = st.enter_context(nc.sbuf_tensor(f"yc_{layer}", [128, 8, 544], BF16))
        ycB = [Buf(f"yc{c}") for c in range(8)]
        szzs = [(st.enter_context(nc.sbuf_tensor(f"szz{i}_{layer}", [128, 8, 512], BF16)), [Buf(f"szz{i}_{c}") for c in range(8)])
                for i in range(2)]
        yv = st.enter_context(nc.sbuf_tensor(f"yv_{layer}", [128, 8, 512], F32))
        yvB = [Buf(f"yv{c}") for c in range(8)]
        diag = sbln("diag", [128, 31, 128], BF16, 3)
        sig = sbln("sig", [128, 512], F32, 2)
        ybf = sbln("ybf", [128, 512], BF16, 3)
        ysq = sbln("ysq", [128, 512], BF16, 3)
        onesb, onesbB = sbl("onesb", [128, 128], BF16)
        mean, meanB = sbl("mean", [128, 512], F32)
        rstdt, rstdtB = sbl("rstdt", [128, 512], F32)
        var, varB = sbl("var", [128, 512], F32)
        t1 = sbln("t1", [128, 512], F32, 2)

        K.op("pool", lambda e: e.memset(onesb[:, :], 1.0 / D), [], [onesbB])

        blocks = [(s_, b_) for s_ in range(NSEQ) for b_ in range(NBLK)]
        K.phase = "odA"
        phase_A(layer, 0, 0)
        def od_proj(bi, mid=None):
            s, b = blocks[bi]
            szz, szzB = szzs[bi % 2]
            K.phase = "odProj"
            for c in range(8):
                if b == 0:
                    K.op("pool", lambda e: e.memset(yc[:, c, 0:32], 0.0), [], [ycB[c]])
                else:
                    K.op("pool", lambda e: e.tensor_copy(out=yc[:, c, 0:32], in_=yc[:, c, 512:544]), [ycB[c]], [ycB[c]])
                pg, pgB = fm_proj(1024 + c * 128, 128)
                sg, sgB = sig.next()
                K.op("act", lambda e: e.activation(out=sg[:, :], in_=pg[:, 0:512], func=AF.Sigmoid), [pgB], [sgB])
                pa, paB = fm_proj(c * 128, 128)
                K.op("dve", lambda e: e.tensor_tensor(out=yc[:, c, 32:544], in0=pa[:, 0:512], in1=sg[:, :], op=ALU.mult),
                     [paB, sgB], [ycB[c]])
            if mid is not None:
                mid()
                K.phase = "odProj"
            for c in range(8):
                pz, pzB = fm_proj(2048 + c * 128, 128)
                K.op("act", lambda e: e.activation(out=szz[:, c, :], in_=pz[:, 0:512], func=AF.Silu), [pzB], [szzB[c]])

        def od_conv(bi):
            K.phase = "odConv"
            pm, pmB = psB.next()
            pq, pqB = psB.next()
            dgs = {}

            def build_diag(c):
                dg, dgB = diag.next()
                K.op("pool" if c % 2 == 0 else "dve", lambda e: e.tensor_tensor(
                    out=dg[:, :, :], in0=identb[:, :].unsqueeze(1).to_broadcast([128, 31, 128]),
                    in1=par[:, c, 1:32].unsqueeze(2).to_broadcast([128, 31, 128]), op=ALU.mult), [identbB, parB], [dgB])
                dgs[c] = (dg, dgB)

            for c in range(3):
                build_diag(c)
            pend_stats = []

            def emit_stats():
                c_, yb_, ybB_, yq_, yqB_ = pend_stats.pop(0)
                K.op("pe", lambda e: e.matmul(pm[:, 0:512], onesb[:, :], yb_[:, :], start=(c_ == 0), stop=(c_ == 7)),
                     [onesbB, ybB_], [pmB], signal=(c_ == 7))
                K.op("pe", lambda e: e.matmul(pq[:, 0:512], onesb[:, :], yq_[:, :], start=(c_ == 0), stop=(c_ == 7)),
                     [onesbB, yqB_], [pqB], signal=(c_ == 7))

            for c in range(8):
                dg, dgB = dgs[c]
                pc, pcB = psA.next()
                for k in range(31):
                    K.op("pe", lambda e, k=k: e.matmul(pc[:, 0:512], dg[:, k, :], yc[:, c, 2 + k:2 + k + 512],
                                                       start=(k == 0), stop=(k == 30)),
                         [dgB, ycB[c]], [pcB], signal=(k == 30))
                if c + 3 < 8:
                    build_diag(c + 3)
                if pend_stats:
                    emit_stats()
                K.op("act", lambda e: e.activation(out=yv[:, c, :], in_=pc[:, 0:512], func=AF.Identity,
                                                   bias=par[:, c, 32:33]), [pcB, parB], [yvB[c]])
                yb, ybB = ybf.next()
                yq, yqB = ysq.next()
                K.op("dve", lambda e: e.tensor_copy(out=yb[:, :], in_=yv[:, c, :]), [yvB[c]], [ybB])
                K.op("pool", lambda e: e.tensor_tensor(out=yq[:, :], in0=yv[:, c, :], in1=yv[:, c, :], op=ALU.mult),
                     [yvB[c]], [yqB])
                pend_stats.append((c, yb, ybB, yq, yqB))
            while pend_stats:
                emit_stats()
            K.phase = "odLN"
            K.op("dve", lambda e: e.tensor_copy(out=mean[:, :], in_=pm[:, 0:512]), [pmB], [meanB])
            K.op("dve", lambda e: e.tensor_tensor(out=var[:, :], in0=mean[:, :], in1=mean[:, :], op=ALU.mult), [meanB], [varB])
            K.op("dve", lambda e: e.scalar_tensor_tensor(out=var[:, :], in0=pq[:, 0:512], scalar=LN_EPS, in1=var[:, :],
                                                         op0=ALU.add, op1=ALU.subtract), [pqB, varB], [varB])

        def od_ln_prep2():
            K.phase = "odLN"
            K.op("act", lambda e: e.activation(out=var[:, :], in_=var[:, :], func=AF.Sqrt), [varB], [varB])
            K.op("dve", lambda e: e.reciprocal(out=rstdt[:, :], in_=var[:, :]), [varB], [rstdtB])
            for c in range(8):
                K.op("dve", lambda e: e.tensor_tensor(out=yv[:, c, :], in0=yv[:, c, :], in1=mean[:, :], op=ALU.subtract),
                     [yvB[c], meanB], [yvB[c]])
                K.op("pool", lambda e: e.tensor_tensor(out=yv[:, c, :], in0=yv[:, c, :], in1=rstdt[:, :], op=ALU.mult),
                     [yvB[c], rstdtB], [yvB[c]])

        def od_ln_fin(bi):
            szz, szzB = szzs[bi % 2]
            K.phase = "odLNf"
            for c in range(8):
                ta, taB = t1.next()
                K.op("act", lambda e: e.activation(out=ta[:, :], in_=yv[:, c, :], func=AF.Silu,
                                                   scale=par[:, c, 33:34], bias=par[:, c, 34:35]),
                     [yvB[c], parB], [taB])
                K.op("dve", lambda e: e.tensor_tensor(out=mixT[:, c, :], in0=ta[:, :], in1=szz[:, c, :], op=ALU.mult),
                     [taB, szzB[c]], mixTB)

        od_proj(0)
        for bi, (s, b) in enumerate(blocks):
            if bi + 1 >= len(blocks) and layer + 1 < n_layers:
                load_w_in(layer + 1)
            od_conv(bi)
            if bi + 1 < len(blocks):
                K.phase = "odA"
                phase_A(layer, blocks[bi + 1][0], blocks[bi + 1][1])
                od_proj(bi + 1, mid=od_ln_prep2)
            else:
                od_ln_prep2()
            od_ln_fin(bi)
            K.phase = "odG"
            phase_G(layer, s, b)
        st.close()

    load_w_in(0)
    for layer in range(n_layers):
        li = layer // 2
        load_common(layer)
        if layer > 0:
            K.barrier()
        if layer % 2 == 0:
            even_layer(layer, li)
        else:
            odd_layer(layer, li)

    for s in range(NSEQ):
        for t in range(16):
            b = xd[s][t]
            if b.w is not None:
                sm, v = b.w
                nc.sync.wait_ge(sm.h, v)
    stack.close()
    return nc, K


def make_consts():
    p = np.arange(128)
    ident = np.eye(128, dtype=np.float32)
    tril = (p[:, None] <= p[None, :]).astype(np.float32)
    far = (p[:, None] > p[None, :]).astype(np.float32)
    ctab = np.zeros((128, 16, 32), np.float32)
    j = np.arange(32)
    for tile in range(16):
        t = tile * 128 + p
        cur = t // 64
        forced = (j[None, :] == 0) | (j[None, :] == cur[:, None]) | (j[None, :] == cur[:, None] - 1)
        causal = j[None, :] <= cur[:, None]
        ctab[:, tile, :] = np.where(causal, forced.astype(np.float32) * np.float32(1e4), np.float32(-1e30))
    ebig = ((np.arange(2048)[None, :] // 64) == j[:, None]).astype(np.float32) * np.float32(BIG)
    n_cmp = 127
    tok = np.arange(n_cmp)[:, None] * 16 + np.arange(32)[None, :]
    ovl = ((tok[:, :, None] // 64) == np.arange(32)[None, None, :]).sum(1).astype(np.float32) / np.float32(32)
    return {"c_ident": ident, "c_tril": tril, "c_far": far, "c_ctab": ctab, "c_ebig": ebig, "c_ovl": ovl}


_CACHE = {}


def kernel(**inputs):
    if "nc" not in _CACHE:
        _CACHE["nc"] = build_program(4)[0]
    nc = _CACHE["nc"]
    consts = make_consts()
    x = np.ascontiguousarray(inputs["x"], dtype=np.float32)
    shared = {k: np.ascontiguousarray(v, dtype=np.float32) for k, v in inputs.items() if k != "x"}
    shared.update(consts)
    in_maps = []
    for c in range(NCORES):
        m = dict(shared)
        m["x"] = x[c * NSEQ:(c + 1) * NSEQ]
        in_maps.append(m)
    res = run_bass_kernel_spmd(nc, in_maps, core_ids=list(range(NCORES)))
    return np.concatenate([r["out"] for r in res.results], axis=0).astype(np.float32)
```

```python
from contextlib import ExitStack

import numpy as np
import concourse.bass as bass
import concourse.mybir as mybir
from concourse.bass_utils import run_bass_kernel_spmd

F32 = mybir.dt.float32
BF16 = mybir.dt.bfloat16
AF = mybir.ActivationFunctionType
ALU = mybir.AluOpType

NCORES = 8
SEQ = 2048
D = 1024
NSEQ = 2
NBLK = 4
EVEN_W = 3352
ODD_W = 3072
RMS_EPS = 1e-6
LN_EPS = 1e-5
BIG = 30000.0
import os as _os
STOP = _os.environ.get("KSTOP", "")

C_Q, C_KC, C_VC, C_KS, C_VS, C_KW, C_VW, C_GATE, C_ZA, C_U, C_V, C_ZB = (
    0, 512, 640, 768, 896, 1024, 1152, 1280, 1304, 1816, 2328, 2840)


class Sem:
    __slots__ = ("h", "val", "name")

    def __init__(self, h, name=""):
        self.h = h
        self.val = 0
        self.name = name


class Buf:
    __slots__ = ("name", "w", "r", "dsem")

    def __init__(self, name):
        self.name = name
        self.w = None
        self.r = {}
        self.dsem = None


class Trk:
    def __init__(self, nc, stack):
        self.nc = nc
        self.stack = stack
        self.eng = {"pe": nc.tensor, "act": nc.scalar, "dve": nc.vector, "pool": nc.gpsimd, "sp": nc.sync}
        self.nsem = 0
        self.esem = {e: self.newsem("e_" + e) for e in self.eng}
        self.waited = {e: {} for e in self.eng}
        self.nops = 0
        self.dsems = []
        self.phase = ""
        self.waitlog = {e: [] for e in self.eng}

    def newsem(self, name):
        self.nsem += 1
        return Sem(self.stack.enter_context(self.nc.semaphore(f"{name}_{self.nsem}")), name)

    def _sync(self, E, reads, writes):
        deps = {}
        for b in reads:
            if b.w is not None:
                s, v = b.w
                if deps.get(s, 0) < v:
                    deps[s] = v
        for b in writes:
            if b.w is not None:
                s, v = b.w
                if deps.get(s, 0) < v:
                    deps[s] = v
            for s, v in b.r.items():
                if deps.get(s, 0) < v:
                    deps[s] = v
        eng = self.eng[E]
        w = self.waited[E]
        own = self.esem[E]
        for s, v in deps.items():
            if E == "pe" and s is own:
                continue
            if w.get(s, 0) < v:
                eng.wait_ge(s.h, v)
                w[s] = v
                self.waitlog[E].append((self.phase, s.name))

    def op(self, E, fn, reads=(), writes=(), signal=True):
        self._sync(E, reads, writes)
        ins = fn(self.eng[E])
        s = self.esem[E]
        if signal:
            s.val += 1
            ins.then_inc(s.h, 1)
            tag = (s, s.val)
        else:
            tag = (s, s.val + 1)
        for b in reads:
            if b.r.get(s, 0) < tag[1]:
                b.r[s] = tag[1]
        for b in writes:
            b.w = tag
            b.r = {}
        self.nops += 1
        return ins

    def barrier(self):
        sems = list(self.esem.values()) + self.dsems
        for E, eng in self.eng.items():
            w = self.waited[E]
            for s in sems:
                if s.val > 0 and w.get(s, 0) < s.val:
                    eng.wait_ge(s.h, s.val)
                    w[s] = s.val

    def dma(self, E, out, in_, reads, writes, dbuf, **kw):
        self._sync(E, reads, writes)
        if dbuf.dsem is None:
            dbuf.dsem = self.newsem("d_" + dbuf.name)
            self.dsems.append(dbuf.dsem)
        s = dbuf.dsem
        ins = self.eng[E].dma_start(out=out, in_=in_, **kw)
        s.val += 16
        ins.then_inc(s.h, 16)
        tag = (s, s.val)
        for b in reads:
            if b.r.get(s, 0) < tag[1]:
                b.r[s] = tag[1]
        for b in writes:
            b.w = tag
            b.r = {}
        self.nops += 1
        return ins


class Rot:
    def __init__(self, items):
        self.items = items
        self.i = 0

    def next(self):
        it = self.items[self.i % len(self.items)]
        self.i += 1
        return it


def build_program(n_layers=4):
    nc = bass.Bass("TRN2", target_bir_lowering=False, dynamic_dma_scratch_size=16384)
    stack = ExitStack()
    K = Trk(nc, stack)

    def dram(name, shape, dt=F32, kind="ExternalInput"):
        return nc.dram_tensor(name, list(shape), dt, kind=kind).ap()

    x_in = dram("x", [NSEQ, SEQ, D])
    out = dram("out", [NSEQ, SEQ, D], kind="ExternalOutput")
    norm_pre = dram("norm_pre", [4, D])
    norm_post = dram("norm_post", [4, D])
    e_w_in = dram("even_w_in", [2, D, EVEN_W])
    e_k_pe = dram("even_cmp_k_pe", [2, 32, 64])
    e_k_w1 = dram("even_cmp_k_w1", [2, 2048, 64])
    e_k_w2 = dram("even_cmp_k_w2", [2, 64, 64])
    e_v_pe = dram("even_cmp_v_pe", [2, 32, 64])
    e_v_w1 = dram("even_cmp_v_w1", [2, 2048, 64])
    e_v_w2 = dram("even_cmp_v_w2", [2, 64, 64])
    e_ln_g = dram("even_sgu_ln_g", [2, 512])
    e_ln_b = dram("even_sgu_ln_b", [2, 512])
    e_sgu_w = dram("even_sgu_w", [2, 8, 128, 128])
    e_sgu_b = dram("even_sgu_b", [2, 8, 128])
    e_w_out = dram("even_w_out", [2, D, D])
    o_w_in = dram("odd_w_in", [2, D, ODD_W])
    o_dw_w = dram("odd_dw_w", [2, 31, D])
    o_dw_b = dram("odd_dw_b", [2, D])
    o_ln_g = dram("odd_ln_g", [2, D])
    o_ln_b = dram("odd_ln_b", [2, D])
    o_w_out = dram("odd_w_out", [2, D, D])
    c_ident = dram("c_ident", [128, 128])
    c_tril = dram("c_tril", [128, 128])
    c_far = dram("c_far", [128, 128])
    c_ctab = dram("c_ctab", [128, 16, 32])
    c_ebig = dram("c_ebig", [32, 2048])
    c_ovl = dram("c_ovl", [127, 32])

    def sb(name, shape, dt=F32):
        t = stack.enter_context(nc.sbuf_tensor(name, list(shape), dt))
        return t, Buf(name)

    def sbn(name, shape, dt, n):
        return Rot([sb(f"{name}{i}", shape, dt) for i in range(n)])

    w_in = stack.enter_context(nc.sbuf_tensor("w_in", [128, 8, EVEN_W], BF16))
    w_inB = [Buf(f"w_in{k}") for k in range(8)]
    w_out = stack.enter_context(nc.sbuf_tensor("w_out", [128, 8, D], BF16))
    w_outB = [Buf(f"w_out{k}") for k in range(8)]
    identb, identbB = sb("identb", [128, 128], BF16)
    identf, identfB = sb("identf", [128, 128], F32)
    trilk, trilkB = sb("trilk", [128, 128], BF16)
    fark, farkB = sb("fark", [128, 128], BF16)
    neghalf, neghalfB = sb("neghalf", [128, 8], F32)
    gpost, gpostB = sb("gpost", [128, D], F32)
    xa = sbn("xa", [128, D], F32, 2)
    xr = sbn("xr", [128, D], F32, 2)
    hp = sbn("hp", [128, D], BF16, 2)
    hT = stack.enter_context(nc.sbuf_tensor("hT", [128, 8, 512], BF16))
    hTB = [Buf(f"hT{t}") for t in range(4)]
    mixT = stack.enter_context(nc.sbuf_tensor("mixT", [128, 8, 512], BF16))
    mixTB = [Buf(f"mixT{t}") for t in range(4)]
    junks = sbn("junk", [128, D], BF16, 2)
    ss, _ = sb("ss", [128, 8], F32)
    ssB = [Buf(f"ss{t}") for t in range(8)]
    ms, _ = sb("ms", [128, 8], F32)
    msB = [Buf(f"ms{t}") for t in range(8)]
    rstd, _ = sb("rstd", [128, 8], F32)
    rstdB = [Buf(f"rstd{t}") for t in range(8)]
    ssy = Rot([sb(f"ssy{i}", [128, 4], F32) + (Buf(f"ssy{i}a"), Buf(f"ssy{i}b")) for i in range(2)])
    rsy = sbn("rsy", [128, 4], F32, 2)
    par, parB = sb("par", [128, 8, 36], F32)
    tmpf = sbn("tmpf", [128, 512], F32, 4)

    PS = []
    for i in range(8):
        t = stack.enter_context(nc.psum_tensor(f"ps{i}", [128, 512], F32))
        PS.append((t, Buf(f"ps{i}")))
    psA = Rot(PS[0:4])
    psB = Rot(PS[4:8])

    xd = [[Buf(f"xd{s}_{t}") for t in range(16)] for s in range(NSEQ)]

    K.dma("pool", identb[:, :], c_ident[:, :], [], [identbB], identbB)
    K.dma("sp", identf[:, :], c_ident[:, :], [], [identfB], identfB)
    K.dma("pool", trilk[:, :], c_tril[:, :], [], [trilkB], trilkB)
    K.dma("pool", fark[:, :], c_far[:, :], [], [farkB], farkB)
    K.op("dve", lambda e: e.memset(neghalf[:, :], -0.5), [], [neghalfB])

    def layer_srcs(layer):
        li = layer // 2
        if layer % 2 == 0:
            return e_w_in[li], EVEN_W, e_w_out[li], []
        return (o_w_in[li], ODD_W, o_w_out[li],
                [(o_dw_w[li], 31), (o_dw_b[li:li + 1, :], 1), (o_ln_g[li:li + 1, :], 1), (o_ln_b[li:li + 1, :], 1)])

    def load_w_in(layer):
        w_in_dram, w_in_cols, _, _ = layer_srcs(layer)
        for kc in range(8):
            K.dma("pool", w_in[:, kc, 0:w_in_cols], w_in_dram[kc * 128:(kc + 1) * 128, :], [], [w_inB[kc]], w_inB[kc],
                  max_dma_last_dim=8192)

    def load_common(layer):
        _, _, w_out_dram, rows = layer_srcs(layer)
        stg, stgB = xa.next()
        rows = [(norm_pre[layer:layer + 1, :], 1)] + rows
        if sum(r for _, r in rows) % 2:
            rows = rows + [(norm_post[layer:layer + 1, :], 1)]
        r0 = 0
        for ap, r in rows:
            K.dma("sp", stg[r0:r0 + r, :], ap, [], [stgB], stgB)
            r0 += r
        R = r0
        for c in range(8):
            pt, pB = psA.next()
            K.op("pe", lambda e, c=c, pt=pt: e.transpose(pt[:, 0:R], stg[0:R, c * 128:(c + 1) * 128], identf[0:R, 0:R]),
                 [stgB, identfB], [pB])
            K.op("dve", lambda e, c=c, pt=pt: e.tensor_copy(out=par[:, c, 0:R], in_=pt[:, 0:R]), [pB], [parB])
        for kc in range(8):
            K.dma("pool", w_out[:, kc, :], w_out_dram[kc * 128:(kc + 1) * 128, :], [], [w_outB[kc]], w_outB[kc],
                  max_dma_last_dim=4096)
        K.dma("sp", gpost[:, :], norm_post[layer:layer + 1, :].to_broadcast([128, D]), [], [gpostB], gpostB)

    def phase_A(layer, s, b):
        src = x_in if layer == 0 else out
        xts = []
        for tt in range(4):
            xt, xB = xa.next()
            tile = b * 4 + tt
            K.dma("sp", xt[:, :], src[s, tile * 128:(tile + 1) * 128, :], [xd[s][tile]], [xB], xB)
            jk, jkB = junks.next()
            K.op("act", lambda e, xt=xt, tt=tt, jk=jk: e.activation(out=jk[:, :], in_=xt[:, :], func=AF.Square,
                                                                    accum_out=ss[:, tt:tt + 1]), [xB], [ssB[tt], jkB])
            K.op("dve", lambda e, tt=tt: e.tensor_scalar(out=ms[:, tt:tt + 1], in0=ss[:, tt:tt + 1], scalar1=1.0 / D,
                                                        scalar2=RMS_EPS, op0=ALU.mult, op1=ALU.add), [ssB[tt]], [msB[tt]])
            K.op("pool", lambda e, tt=tt: e.tensor_tensor(out=rstd[:, tt:tt + 1], in0=ms[:, tt:tt + 1],
                                                          in1=neghalf[:, 0:1], op=ALU.pow), [msB[tt], neghalfB], [rstdB[tt]])
            ht, hB = hp.next()
            K.op("dve", lambda e, ht=ht, xt=xt, tt=tt: e.tensor_scalar(out=ht[:, :], in0=xt[:, :], scalar1=rstd[:, tt:tt + 1],
                                                                       scalar2=None, op0=ALU.mult), [xB, rstdB[tt]], [hB])
            pt, pB = psA.next()
            pb = pt[:].bitcast(BF16)
            for kc in range(8):
                K.op("pe", lambda e, kc=kc, pb=pb, ht=ht: e.transpose(pb[:, kc * 128:(kc + 1) * 128],
                                                                      ht[:, kc * 128:(kc + 1) * 128], identb[:, :]),
                     [hB, identbB], [pB], signal=(kc == 7))
            K.op("dve", lambda e, pb=pb, tt=tt: e.tensor_tensor(
                out=hT[:, :, tt * 128:(tt + 1) * 128], in0=pb.rearrange("p (k t) -> p k t", k=8),
                in1=par[:, :, 0:1].to_broadcast([128, 8, 128]), op=ALU.mult), [pB, parB], [hTB[tt]])

    def fm_proj(col0, M):
        pt, pB = psA.next()
        for kc in range(8):
            K.op("pe", lambda e, kc=kc, pt=pt: e.matmul(pt[0:M, 0:512], w_in[:, kc, col0:col0 + M], hT[:, kc, :],
                                                        start=(kc == 0), stop=(kc == 7)),
                 [w_inB[kc]] + hTB, [pB], signal=(kc == 7))
        return pt, pB

    def tm_proj(tt, col0, N):
        pt, pB = psA.next()
        for kc in range(8):
            K.op("pe", lambda e, kc=kc, pt=pt: e.matmul(pt[:, 0:N], hT[:, kc, tt * 128:(tt + 1) * 128],
                                                        w_in[:, kc, col0:col0 + N], start=(kc == 0), stop=(kc == 7)),
                 [w_inB[kc], hTB[tt]], [pB], signal=(kc == 7))
        return pt, pB

    def phase_G(layer, s, b):
        src = x_in if layer == 0 else out
        for tt in range(4):
            tile = b * 4 + tt
            xt, xB = xr.next()
            K.dma("sp", xt[:, :], src[s, tile * 128:(tile + 1) * 128, :], [xd[s][tile]], [xB], xB)
            halves = []
            sy, syB, syB0, syB1 = ssy.next()
            ry, ryB = rsy.next()
            for hf in range(2):
                pt, pB = psB.next()
                for fc in range(8):
                    K.op("pe", lambda e, fc=fc, pt=pt, hf=hf: e.matmul(pt[:, 0:512], mixT[:, fc, tt * 128:(tt + 1) * 128],
                                                                        w_out[:, fc, hf * 512:(hf + 1) * 512],
                                                                        start=(fc == 0), stop=(fc == 7)),
                         [mixTB[tt], w_outB[fc]], [pB], signal=(fc == 7))
                halves.append((pt, pB))
            ysbs = []
            for hf, sB_ in ((0, syB0), (1, syB1)):
                pt, pB = halves[hf]
                tf, tfB = tmpf.next()
                if hf == 0:
                    K.op("act", lambda e, pt=pt, tf=tf: e.copy(out=tf[:, :], in_=pt[:, 0:512]), [pB], [tfB])
                else:
                    K.op("dve", lambda e, pt=pt, tf=tf: e.tensor_copy(out=tf[:, :], in_=pt[:, 0:512]), [pB], [tfB])
                jk, jkB = junks.next()
                K.op("act", lambda e, tf=tf, hf=hf, sy=sy, jk=jk: e.activation(out=jk[:, 0:512], in_=tf[:, :], func=AF.Square,
                                                                               accum_out=sy[:, hf:hf + 1]), [tfB], [sB_, jkB])
                ysbs.append((tf, tfB))
            K.op("dve", lambda e, sy=sy: e.tensor_tensor(out=sy[:, 2:3], in0=sy[:, 0:1], in1=sy[:, 1:2], op=ALU.add),
                 [syB0, syB1, syB], [syB])
            K.op("dve", lambda e, sy=sy: e.tensor_scalar(out=sy[:, 3:4], in0=sy[:, 2:3], scalar1=1.0 / D, scalar2=RMS_EPS,
                                                        op0=ALU.mult, op1=ALU.add), [syB], [syB])
            K.op("pool", lambda e, sy=sy, ry=ry: e.tensor_tensor(out=ry[:, 0:1], in0=sy[:, 3:4], in1=neghalf[:, 0:1],
                                                                 op=ALU.pow), [syB, neghalfB], [ryB])
            for hf in range(2):
                tf, tfB = ysbs[hf]
                K.op("dve", lambda e, tf=tf, hf=hf, ry=ry: e.scalar_tensor_tensor(
                    out=tf[:, :], in0=tf[:, :], scalar=ry[:, 0:1], in1=gpost[:, hf * 512:(hf + 1) * 512],
                    op0=ALU.mult, op1=ALU.mult), [tfB, ryB, gpostB], [tfB])
                K.op("pool", lambda e, tf=tf, xt=xt, hf=hf: e.tensor_tensor(
                    out=xt[:, hf * 512:(hf + 1) * 512], in0=xt[:, hf * 512:(hf + 1) * 512], in1=tf[:, :], op=ALU.add),
                     [tfB, xB], [xB])
            K.dma("sp", out[s, tile * 128:(tile + 1) * 128, :], xt[:, :], [xB], [xd[s][tile]], xd[s][tile])

    def even_layer(layer, li):
        st = ExitStack()

        def sbl(name, shape, dt=F32):
            t = st.enter_context(nc.sbuf_tensor(f"{name}_{layer}", list(shape), dt))
            return t, Buf(name)

        def sbln(name, shape, dt, n):
            return Rot([sbl(f"{name}{i}", shape, dt) for i in range(n)])

        qT = st.enter_context(nc.sbuf_tensor(f"qT_{layer}", [128, 8, 512], BF16))
        qTB = [Buf(f"qT{h}") for h in range(8)]
        qSB = [Buf(f"qS{h}") for h in range(8)]
        ksT = st.enter_context(nc.sbuf_tensor(f"ksT_{layer}", [128, 2, 2048], BF16))
        ksTB = [[Buf(f"ksT{g}_{b}") for b in range(4)] for g in range(2)]
        ebB = Buf("ebig")
        kwT = st.enter_context(nc.sbuf_tensor(f"kwT_{layer}", [64, 2, 1024], BF16))
        kwTB = [[Buf(f"kwT{g}_{r}") for r in range(2)] for g in range(2)]
        kcT, kcTB = sbl("kcT", [128, 528], BF16)
        vcT, vcTB = sbl("vcT", [128, 528], BF16)
        vsA = st.enter_context(nc.sbuf_tensor(f"vsA_{layer}", [128, 16, 2, 65], BF16))
        vsAB = [Buf(f"vsA{k}") for k in range(16)]
        vwA = st.enter_context(nc.sbuf_tensor(f"vwA_{layer}", [128, 8, 2, 65], BF16))
        vwAB = [Buf(f"vwA{k}") for k in range(8)]
        w1k, w1kB = sbl("w1k", [128, 32, 64], BF16)
        w1v, w1vB = sbl("w1v", [128, 32, 64], BF16)
        w2k, w2kB = sbl("w2k", [64, 64], BF16)
        w2v, w2vB = sbl("w2v", [64, 64], BF16)
        pek, pekB = sbl("pek", [32, 64], BF16)
        pev, pevB = sbl("pev", [32, 64], BF16)
        peT, peTB = sbl("peT", [64, 2, 32], BF16)
        pebias, pebiasB = sbl("pebias", [64, 2], F32)
        hidk, hidkB = sbl("hidk", [64, 2, 32], BF16)
        hidv, hidvB = sbl("hidv", [64, 2, 128], BF16)
        kcmpT, kcmpTB = sbl("kcmpT", [64, 2, 128], BF16)
        vcmp, vcmpB = sbl("vcmp", [128, 2, 97], BF16)
        ctab, ctabB = sbl("ctab", [128, 16, 32], F32)
        WsT, WsTB = sbl("WsT", [128, 8, 128], BF16)
        bs, bsB = sbl("bs", [128, 8], F32)
        lng, lngB = sbl("lng", [128, 512], F32)
        lnb, lnbB = sbl("lnb", [128, 512], F32)
        gates = st.enter_context(nc.sbuf_tensor(f"gates_{layer}", [128, 4, 24], F32))
        gatesB = [Buf(f"gates{t}") for t in range(4)]
        sza = st.enter_context(nc.sbuf_tensor(f"sza_{layer}", [128, 4, 512], BF16))
        szaB = [Buf(f"sza{t}") for t in range(4)]
        ug = sbln("ug", [128, 512], BF16, 2)
        szb = sbln("szb", [128, 512], BF16, 2)
        vg = sbln("vg", [128, 512], F32, 2)
        vln = sbln("vln", [128, 512], BF16, 2)
        lnst = sbln("lnst", [128, 16], F32, 2)
        acc_o = st.enter_context(nc.sbuf_tensor(f"acc_o_{layer}", [128, 4, 512], F32))
        acc_oB = [Buf(f"acc_o{h}") for h in range(8)]
        imp = sbln("imp", [128, 4, 32], F32, 2)
        top8 = sbln("top8", [128, 4, 8], F32, 2)
        selpads = [sbl(f"selpad{g}", [128, 4, 96], BF16) for g in range(2)]
        pTs = sbln("pT", [128, 512], BF16, 4)
        rcs = sbln("rc", [128, 16], F32, 4)
        amix = sbln("amix", [128, 512], BF16, 2)
        tmpb = sbln("tmpb", [128, 512], BF16, 3)

        for g in range(2):
            K.dma("pool", ksT[64:96, g, :], c_ebig[:, :], [], [ebB], ebB, max_dma_last_dim=4096)
        for wi, (w1, w1B, src) in enumerate(((w1k, w1kB, e_k_w1), (w1v, w1vB, e_v_w1))):
            srcv = src[li].rearrange("(l d) e -> d l e", d=64)
            for lh in range(2):
                stg, stgB = xa.next()
                stgv = stg[:, :].rearrange("p (l e) -> p l e", l=16)
                for half in range(2):
                    K.dma("sp", stgv[half * 64:(half + 1) * 64, :, :], srcv[:, lh * 16:(lh + 1) * 16, :], [], [stgB], stgB)
                if (wi + lh) % 2 == 0:
                    K.op("dve", lambda e, w1=w1, lh=lh, stgv=stgv: e.tensor_copy(out=w1[:, lh * 16:(lh + 1) * 16, :], in_=stgv),
                         [stgB], [w1B])
                else:
                    K.op("act", lambda e, w1=w1, lh=lh, stgv=stgv: e.copy(out=w1[:, lh * 16:(lh + 1) * 16, :], in_=stgv),
                         [stgB], [w1B])
        K.dma("pool", w2k[:, :], e_k_w2[li], [], [w2kB], w2kB)
        K.dma("pool", w2v[:, :], e_v_w2[li], [], [w2vB], w2vB)
        K.dma("pool", pek[:, :], e_k_pe[li], [], [pekB], pekB)
        K.dma("pool", pev[:, :], e_v_pe[li], [], [pevB], pevB)
        K.dma("sp", ctab[:, :, :], c_ctab[:, :, :], [], [ctabB], ctabB)
        wsst_t, wsstB = xa.next()
        wsst = wsst_t[:, :].rearrange("p (g j) -> p g j", g=8)
        K.dma("sp", wsst, e_sgu_w[li].rearrange("g i j -> i g j"), [], [wsstB], wsstB)
        bst, bstB = xa.next()
        K.dma("sp", bst[0:8, 0:128], e_sgu_b[li], [], [bstB], bstB)
        pt, pB = psA.next()
        K.op("pe", lambda e: e.transpose(pt[:, 0:8], bst[0:8, 0:128], identf[0:8, 0:8]), [bstB, identfB], [pB])
        K.op("dve", lambda e: e.tensor_copy(out=bs[:, :], in_=pt[:, 0:8]), [pB], [bsB])
        K.dma("sp", lng[:, :], e_ln_g[li:li + 1, :].to_broadcast([128, 512]), [], [lngB], lngB)
        K.dma("sp", lnb[:, :], e_ln_b[li:li + 1, :].to_broadcast([128, 512]), [], [lnbB], lnbB)
        K.op("pool", lambda e: e.memset(vsA[:, :, :, 64:65], 1.0), [], vsAB)
        K.op("pool", lambda e: e.memset(vwA[:, :, :, 64:65], 1.0), [], vwAB)
        K.op("pool", lambda e: e.memset(vcmp[:, :, :], 0.0), [], [vcmpB])
        K.op("pool", lambda e: e.memset(vcmp[:, :, 64:65], 1.0), [vcmpB], [vcmpB])
        for g in range(2):
            K.dma("pool", vcmp[0:127, g, 65:97], c_ovl[:, :], [], [vcmpB], vcmpB)
        K.op("pool", lambda e: e.memset(hidv[:, :, :], 0.0), [], [hidvB])
        K.op("pool", lambda e: e.memset(kcmpT[:, :, :], 0.0), [], [kcmpTB])
        for selpad, selpadB in selpads:
            K.op("pool", lambda e, selpad=selpad: e.memset(selpad[:, :, :], 0.0), [], [selpadB])
        K.op("dve", lambda e: e.memset(kcT[:, 0:16], 0.0), [], [kcTB])
        K.op("dve", lambda e: e.memset(vcT[:, 0:16], 0.0), [], [vcTB])
        for g in range(8):
            pt, pB = psA.next()
            K.op("pe", lambda e, g=g, pt=pt: e.transpose(pt[:, 0:128], wsst[:, g, :], identf[:, :]), [wsstB, identfB], [pB])
            K.op("dve", lambda e, g=g, pt=pt: e.tensor_tensor(out=WsT[:, g, :], in0=pt[:, 0:128], in1=trilk[:, :], op=ALU.mult),
                 [pB, trilkB], [WsTB])
        for xi, (pe_, peB_, w1, w1B) in enumerate(((pek, pekB, w1k, w1kB), (pev, pevB, w1v, w1vB))):
            pt, pB = psA.next()
            pb = pt[:].bitcast(BF16)
            K.op("pe", lambda e, pb=pb, pe_=pe_: e.transpose(pb[0:64, 0:32], pe_[:, :], identb[0:32, 0:32]), [peB_, identbB], [pB])
            K.op("dve", lambda e, pb=pb, xi=xi: e.tensor_copy(out=peT[:, xi, :], in_=pb[0:64, 0:32]), [pB], [peTB])
            pt2, pB2 = psA.next()
            for l in range(32):
                K.op("pe", lambda e, l=l, pt2=pt2, w1=w1, xi=xi: e.matmul(pt2[0:64, 0:1], w1[0:64, l, :], peT[:, xi, l:l + 1],
                                                                          start=(l == 0), stop=(l == 31)),
                     [w1B, peTB], [pB2], signal=(l == 31))
            K.op("dve", lambda e, pt2=pt2, xi=xi: e.tensor_copy(out=pebias[:, xi:xi + 1], in_=pt2[0:64, 0:1]), [pB2], [pebiasB])

        def evac_copy(i, out_ap, in_ap, reads, writes, scale=None):
            if i % 2 == 0:
                if scale is None:
                    K.op("act", lambda e: e.copy(out=out_ap, in_=in_ap), reads, writes)
                else:
                    K.op("act", lambda e: e.mul(out=out_ap, in_=in_ap, mul=scale), reads, writes)
            else:
                if scale is None:
                    K.op("dve", lambda e: e.tensor_copy(out=out_ap, in_=in_ap), reads, writes)
                else:
                    K.op("dve", lambda e: e.tensor_scalar(out=out_ap, in0=in_ap, scalar1=scale, scalar2=None, op0=ALU.mult),
                         reads, writes)

        if STOP == "params":
            st.close()
            return
        blocks = [(s_, b_) for s_ in range(NSEQ) for b_ in range(NBLK)]
        K.phase = "evA"
        phase_A(layer, 0, 0)
        for bi, (s, b) in enumerate(blocks):
            if True:
                t0 = b * 512
                K.phase = "evB"
                if STOP == "A":
                    st.close()
                    return
                for h in range(8):
                    pt, pB = fm_proj(C_Q + h * 64, 64)
                    evac_copy(h, qT[0:64, h, :], pt[0:64, 0:512], [pB], [qTB[h]], scale=0.125)
                for g in range(2):
                    pt, pB = fm_proj(C_KS + g * 64, 64)
                    evac_copy(g, ksT[0:64, g, t0:t0 + 512], pt[0:64, 0:512], [pB], [ksTB[g][b]])
                for g in range(2):
                    pt, pB = fm_proj(C_KW + g * 64, 64)
                    r = b % 2
                    evac_copy(g + 1, kwT[0:64, g, r * 512:(r + 1) * 512], pt[0:64, 0:512], [pB], [kwTB[g][r]])
                for i, (cX, XT, XTB) in enumerate(((C_KC, kcT, kcTB), (C_VC, vcT, vcTB))):
                    if b > 0:
                        K.op("pool", lambda e, XT=XT: e.tensor_copy(out=XT[:, 0:16], in_=XT[:, 512:528]), [XTB], [XTB])
                    pt, pB = fm_proj(cX, 128)
                    evac_copy(i, XT[:, 16:528], pt[:, 0:512], [pB], [XTB])
                if STOP == "B":
                    st.close()
                    return
                K.phase = "evC"
                cst = {}
                cst2 = {}

                def c_front(tt):
                    kt = b * 4 + tt
                    pt, pB = tm_proj(tt, C_VS, 408)
                    K.op("dve", lambda e, pt=pt, kt=kt: e.tensor_copy(out=vsA[:, kt, :, 0:64],
                                                                      in_=pt[:, 0:128].rearrange("p (g d) -> p g d", g=2)),
                         [pB], [vsAB[kt]])
                    K.op("dve", lambda e, pt=pt, kt=kt: e.tensor_copy(out=vwA[:, kt % 8, :, 0:64],
                                                                      in_=pt[:, 256:384].rearrange("p (g d) -> p g d", g=2)),
                         [pB], [vwAB[kt % 8]])
                    K.op("act", lambda e, pt=pt, tt=tt: e.activation(out=gates[:, tt, :], in_=pt[:, 384:408], func=AF.Sigmoid),
                         [pB], [gatesB[tt]])
                    pt, pB = tm_proj(tt, C_ZA, 512)
                    K.op("act", lambda e, pt=pt, tt=tt: e.activation(out=sza[:, tt, :], in_=pt[:, 0:512], func=AF.Silu),
                         [pB], [szaB[tt]])
                    pt, pB = tm_proj(tt, C_ZB, 512)
                    zt, ztB = szb.next()
                    K.op("act", lambda e, pt=pt, zt=zt: e.activation(out=zt[:, :], in_=pt[:, 0:512], func=AF.Silu), [pB], [ztB])
                    pt, pB = tm_proj(tt, C_U, 512)
                    ut, utB = ug.next()
                    K.op("act", lambda e, pt=pt, ut=ut: e.activation(out=ut[:, :], in_=pt[:, 0:512], func=AF.Gelu_apprx_tanh),
                         [pB], [utB])
                    pt, pB = tm_proj(tt, C_V, 512)
                    vt, vtB = vg.next()
                    K.op("act", lambda e, pt=pt, vt=vt: e.activation(out=vt[:, :], in_=pt[:, 0:512], func=AF.Gelu_apprx_tanh),
                         [pB], [vtB])
                    K.op("pool", lambda e, ut=ut, zt=zt: e.tensor_tensor(out=ut[:, :], in0=ut[:, :], in1=zt[:, :], op=ALU.mult),
                         [utB, ztB], [utB])
                    ls, lsB = lnst.next()
                    K.op("dve", lambda e, ls=ls, vt=vt: e.bn_stats(out=ls[:, 0:6], in_=vt[:, :]), [vtB], [lsB])
                    K.op("dve", lambda e, ls=ls: e.bn_aggr(out=ls[:, 6:8], in_=ls[:, 0:6]), [lsB], [lsB])
                    K.op("dve", lambda e, ls=ls: e.tensor_scalar(out=ls[:, 8:9], in0=ls[:, 7:8], scalar1=LN_EPS, scalar2=None,
                                                                op0=ALU.add), [lsB], [lsB])
                    K.op("pool", lambda e, ls=ls: e.tensor_tensor(out=ls[:, 9:10], in0=ls[:, 8:9], in1=neghalf[:, 0:1], op=ALU.pow),
                         [lsB, neghalfB], [lsB])
                    K.op("dve", lambda e, ls=ls, vt=vt: e.tensor_scalar(out=vt[:, :], in0=vt[:, :], scalar1=ls[:, 6:7],
                                                                        scalar2=ls[:, 9:10], op0=ALU.subtract, op1=ALU.mult),
                         [lsB, vtB], [vtB])
                    K.op("pool", lambda e, vt=vt: e.tensor_tensor(out=vt[:, :], in0=vt[:, :], in1=lng[:, :], op=ALU.mult),
                         [vtB, lngB], [vtB])
                    vl, vlB = vln.next()
                    K.op("pool", lambda e, vt=vt, vl=vl: e.tensor_tensor(out=vl[:, :], in0=vt[:, :], in1=lnb[:, :], op=ALU.add),
                         [vtB, lnbB], [vlB])
                    cst[tt] = (vl, vlB, ut, utB)

                def c_back(tt):
                    vl, vlB, ut, utB = cst.pop(tt)
                    pt, pB = psB.next()
                    for g in range(8):
                        K.op("pe", lambda e, g=g, pt=pt, vl=vl: e.matmul(pt[:, g * 64:(g + 1) * 64], WsT[:, g, :],
                                                                          vl[:, g * 64:(g + 1) * 64], start=True, stop=True),
                             [WsTB, vlB], [pB], signal=(g == 7))
                    tb, tbB = tmpb.next()
                    K.op("dve", lambda e, pt=pt, tb=tb: e.tensor_tensor(
                        out=tb[:, :].rearrange("p (g d) -> p g d", g=8), in0=pt[:, 0:512].rearrange("p (g d) -> p g d", g=8),
                        in1=bs[:, :].unsqueeze(2).to_broadcast([128, 8, 64]), op=ALU.add), [pB, bsB], [tbB])
                    K.op("pool", lambda e, tb=tb, ut=ut: e.tensor_tensor(out=tb[:, :], in0=tb[:, :], in1=ut[:, :], op=ALU.mult),
                         [tbB, utB], [tbB])
                    cst2[tt] = (tb, tbB)

                def c_back2(tt):
                    tb, tbB = cst2.pop(tt)
                    pt, pB = psA.next()
                    pb = pt[:].bitcast(BF16)
                    for fc in range(4):
                        K.op("pe", lambda e, fc=fc, pb=pb, tb=tb: e.transpose(pb[:, fc * 128:(fc + 1) * 128],
                                                                              tb[:, fc * 128:(fc + 1) * 128], identb[:, :]),
                             [tbB, identbB], [pB], signal=(fc == 3))
                    K.op("dve", lambda e, pb=pb, tt=tt: e.tensor_copy(out=mixT[:, 4:8, tt * 128:(tt + 1) * 128],
                                                                      in_=pb[:, 0:512].rearrange("p (k t) -> p k t", k=4)),
                         [pB], [mixTB[tt]])

                for tt in range(6):
                    if tt < 4:
                        c_front(tt)
                    if 1 <= tt <= 4:
                        c_back(tt - 1)
                    if tt >= 2:
                        c_back2(tt - 2)
                if STOP == "C":
                    st.close()
                    return
                if bi + 1 < len(blocks):
                    K.phase = "evA"
                    phase_A(layer, blocks[bi + 1][0], blocks[bi + 1][1])
                elif layer + 1 < n_layers:
                    load_w_in(layer + 1)
                K.phase = "evD"
                if b == 0:
                    n0, nn, c0 = 0, 31, 16
                else:
                    n0, nn, c0 = 32 * b - 1, 32, 0
                nk = 32 * (b + 1) - 1
                for g in range(2):
                    for xi, (XT, XTB, w1, w1B, hid, hidB, hcol) in enumerate((
                            (kcT, kcTB, w1k, w1kB, hidk, hidkB, 0), (vcT, vcTB, w1v, w1vB, hidv, hidvB, n0))):
                        pt, pB = psA.next()
                        for l in range(32):
                            K.op("pe", lambda e, l=l, pt=pt, XT=XT, w1=w1: e.matmul(
                                pt[0:64, 0:nn], w1[g * 64:(g + 1) * 64, l, :],
                                XT[g * 64:(g + 1) * 64, c0 + l:c0 + l + 16 * (nn - 1) + 1:16], start=(l == 0), stop=(l == 31)),
                                 [w1B, XTB], [pB], signal=(l == 31))
                        K.op("act", lambda e, pt=pt, hid=hid, hcol=hcol, xi=xi: e.activation(
                            out=hid[:, g, hcol:hcol + nn], in_=pt[0:64, 0:nn], func=AF.Silu, bias=pebias[:, xi:xi + 1]),
                             [pB, pebiasB], [hidB])
                    pt, pB = psA.next()
                    K.op("pe", lambda e, pt=pt: e.matmul(pt[0:64, 0:nn], w2k[:, :], hidk[:, g, 0:nn], start=True, stop=True),
                         [w2kB, hidkB], [pB])
                    K.op("dve", lambda e, pt=pt: e.tensor_copy(out=kcmpT[:, g, n0:n0 + nn], in_=pt[0:64, 0:nn]), [pB], [kcmpTB])
                    pt, pB = psA.next()
                    K.op("pe", lambda e, pt=pt: e.matmul(pt[0:nk, 0:64], hidv[:, g, 0:nk], w2v[:, :], start=True, stop=True),
                         [w2vB, hidvB], [pB])
                    K.op("dve", lambda e, pt=pt: e.tensor_copy(out=vcmp[0:nk, g, 0:64], in_=pt[0:nk, 0:64]), [pB], [vcmpB])
                if STOP == "D":
                    st.close()
                    return
                K.phase = "evE"
                for g in range(2):
                    selpad, selpadB = selpads[g]
                    im, imB = imp.next()
                    for r in range(4):
                        h = g * 4 + r
                        pt, pB = psA.next()
                        K.op("pe", lambda e, pt=pt, h=h: e.matmul(pt[0:nk, 0:512], kcmpT[:, g, 0:nk], qT[0:64, h, :],
                                                                  start=True, stop=True), [kcmpTB, qTB[h]], [pB])
                        pT, pTB = pTs.next()
                        K.op("act", lambda e, pt=pt, pT=pT: e.activation(out=pT[0:nk, :], in_=pt[0:nk, 0:512], func=AF.Exp),
                             [pB], [pTB])
                        if STOP == "E1":
                            st.close()
                            return
                        K.op("pool", lambda e, pT=pT: e.affine_select(out=pT[0:nk, :], in_=pT[0:nk, :], pattern=[[1, 512]],
                                                                      compare_op=ALU.is_ge, fill=0.0, base=t0 - 31,
                                                                      channel_multiplier=-16), [pTB], [pTB])
                        if STOP == "E2":
                            st.close()
                            return
                        po, poB = psB.next()
                        po3 = po[:, 0:388].rearrange("p (t c) -> p t c", t=4)
                        for tt in range(4):
                            K.op("pe", lambda e, tt=tt, po=po, pT=pT: e.matmul(po[:, tt * 97:(tt + 1) * 97],
                                                                                pT[0:nk, tt * 128:(tt + 1) * 128], vcmp[0:nk, g, :],
                                                                                start=True, stop=True),
                                 [pTB, vcmpB], [poB], signal=(tt == 3))
                        if STOP == "E3":
                            st.close()
                            return
                        rc, rcB = rcs.next()
                        K.op("dve", lambda e, rc=rc, po3=po3: e.tensor_scalar(out=rc[:, 0:4], in0=po3[:, :, 64], scalar1=1e-30,
                                                                              scalar2=None, op0=ALU.max), [poB], [rcB])
                        K.op("dve", lambda e, rc=rc: e.reciprocal(out=rc[:, 4:8], in_=rc[:, 0:4]), [rcB], [rcB])
                        K.op("dve", lambda e, rc=rc, h=h: e.tensor_tensor(out=rc[:, 8:12], in0=rc[:, 4:8], in1=gates[:, :, h],
                                                                          op=ALU.mult), [rcB] + gatesB, [rcB])
                        for tt in range(4):
                            K.op("dve", lambda e, tt=tt, po3=po3, rc=rc, h=h: e.tensor_scalar(
                                out=acc_o[:, tt, h * 64:(h + 1) * 64], in0=po3[:, tt, 0:64], scalar1=rc[:, 8 + tt:9 + tt],
                                scalar2=None, op0=ALU.mult), [poB, rcB], [acc_oB[h]])
                        for tt in range(4):
                            if r == 0:
                                K.op("dve", lambda e, tt=tt, po3=po3, rc=rc, im=im: e.tensor_scalar(
                                    out=im[:, tt, :], in0=po3[:, tt, 65:97], scalar1=rc[:, 4 + tt:5 + tt], scalar2=None,
                                    op0=ALU.mult), [poB, rcB], [imB])
                            else:
                                K.op("dve", lambda e, tt=tt, po3=po3, rc=rc, im=im: e.scalar_tensor_tensor(
                                    out=im[:, tt, :], in0=po3[:, tt, 65:97], scalar=rc[:, 4 + tt:5 + tt], in1=im[:, tt, :],
                                    op0=ALU.mult, op1=ALU.add), [poB, rcB, imB], [imB])
                    if STOP == "E4":
                        st.close()
                        return
                    K.op("dve", lambda e, im=im: e.tensor_tensor(out=im[:, :, :], in0=im[:, :, :], in1=ctab[:, 4 * b:4 * b + 4, :],
                                                                 op=ALU.add), [imB, ctabB], [imB])
                    t8, t8B = top8.next()
                    for tt in range(4):
                        K.op("dve", lambda e, tt=tt, t8=t8, im=im: e.max(out=t8[:, tt, :], in_=im[:, tt, :]), [imB], [t8B])
                    for tt in range(4):
                        K.op("dve", lambda e, tt=tt, t8=t8, im=im: e.tensor_scalar(
                            out=selpad[:, tt, 64:96], in0=im[:, tt, :], scalar1=t8[:, tt, 7:8], scalar2=-1.0,
                            op0=ALU.is_ge, op1=ALU.add), [imB, t8B], [selpadB])
                    if STOP == "E6":
                        st.close()
                        return
                for g in range(2):
                    selpad, selpadB = selpads[g]
                    pt, pB = psA.next()
                    pb = pt[:].bitcast(BF16)
                    for tt in range(4):
                        K.op("pe", lambda e, tt=tt, pb=pb: e.transpose(pb[0:96, tt * 128:(tt + 1) * 128], selpad[:, tt, :],
                                                                       identb[:, :]), [selpadB, identbB], [pB], signal=(tt == 3))
                    for r in range(4):
                        h = g * 4 + r
                        K.op("dve", lambda e, h=h, pb=pb: e.tensor_copy(out=qT[64:96, h, :], in_=pb[64:96, 0:512]), [pB], [qSB[h]])
                if STOP == "E":
                    st.close()
                    return
                K.phase = "evF"
                items = []
                for h in range(8):
                    for br in (1, 0):
                        kts = list(range(0, 4 * b + 4)) if br == 0 else list(range(max(0, 4 * b - 4), 4 * b + 4))
                        for kt in kts:
                            items.append((h, br, kt, kt == kts[0], kt == kts[-1]))
                LA = 2
                stage = {}
                accs = {}

                def front(idx):
                    h, br, kt, isfirst, islast = items[idx]
                    g = h // 4
                    d = kt - 4 * b
                    lo = max(0, d)
                    hi = 3 if br == 0 else min(3, d + 4)
                    c0_, c1_ = lo * 128, (hi + 1) * 128
                    pt, pB = psA.next()
                    if br == 0:
                        K.op("pe", lambda e: e.matmul(
                            pt[:, c0_:c1_], ksT[0:96, g, kt * 128:(kt + 1) * 128], qT[0:96, h, c0_:c1_],
                            start=True, stop=True), [ksTB[g][kt // 4], ebB, qTB[h], qSB[h]], [pB])
                    else:
                        kr = kt % 8
                        K.op("pe", lambda e: e.matmul(
                            pt[:, c0_:c1_], kwT[0:64, g, kr * 128:(kr + 1) * 128], qT[0:64, h, c0_:c1_],
                            start=True, stop=True), [kwTB[g][kr // 4], qTB[h]], [pB])
                    pT, pTB = pTs.next()
                    K.op("act", lambda e: e.activation(out=pT[:, c0_:c1_], in_=pt[:, c0_:c1_], func=AF.Exp), [pB], [pTB])
                    if d >= 0:
                        K.op("pool", lambda e: e.tensor_tensor(
                            out=pT[:, d * 128:(d + 1) * 128], in0=pT[:, d * 128:(d + 1) * 128], in1=trilk[:, :],
                            op=ALU.mult), [pTB, trilkB], [pTB])
                    if br == 1 and 0 <= d + 4 <= 3:
                        K.op("pool", lambda e: e.tensor_tensor(
                            out=pT[:, (d + 4) * 128:(d + 5) * 128], in0=pT[:, (d + 4) * 128:(d + 5) * 128],
                            in1=fark[:, :], op=ALU.mult), [pTB, farkB], [pTB])
                    stage[idx] = (pT, pTB, lo, hi)

                def back(idx):
                    h, br, kt, isfirst, islast = items[idx]
                    g = h // 4
                    pT, pTB, lo, hi = stage.pop(idx)
                    if isfirst:
                        accs[(h, br)] = psB.next()
                    acc, accB = accs[(h, br)]
                    for tt in range(lo, hi + 1):
                        if br == 0:
                            vB_, rhs = vsAB[kt], vsA[:, kt, g, :]
                        else:
                            vB_, rhs = vwAB[kt % 8], vwA[:, kt % 8, g, :]
                        K.op("pe", lambda e, tt=tt, rhs=rhs: e.matmul(
                            acc[:, tt * 65:(tt + 1) * 65], pT[:, tt * 128:(tt + 1) * 128], rhs,
                            start=(isfirst and tt == lo), stop=True, skip_group_check=True), [pTB, vB_], [accB], signal=(tt == hi))
                    if islast:
                        acc3 = acc[:, 0:260].rearrange("p (t c) -> p t c", t=4)
                        rc, rcB = rcs.next()
                        K.op("dve", lambda e: e.tensor_scalar(out=rc[:, 0:4], in0=acc3[:, :, 64], scalar1=1e-30,
                                                              scalar2=None, op0=ALU.max), [accB], [rcB])
                        K.op("dve", lambda e: e.reciprocal(out=rc[:, 4:8], in_=rc[:, 0:4]), [rcB], [rcB])
                        gi = (1 + br) * 8 + h
                        K.op("dve", lambda e: e.tensor_tensor(out=rc[:, 8:12], in0=rc[:, 4:8], in1=gates[:, :, gi],
                                                              op=ALU.mult), [rcB] + gatesB, [rcB])
                        for tt in range(4):
                            K.op("dve", lambda e, tt=tt: e.scalar_tensor_tensor(
                                out=acc_o[:, tt, h * 64:(h + 1) * 64], in0=acc3[:, tt, 0:64], scalar=rc[:, 8 + tt:9 + tt],
                                in1=acc_o[:, tt, h * 64:(h + 1) * 64], op0=ALU.mult, op1=ALU.add),
                                 [accB, rcB, acc_oB[h]], [acc_oB[h]])
                        del accs[(h, br)]

                for idx in range(len(items) + LA):
                    if idx < len(items):
                        front(idx)
                    if idx - LA >= 0:
                        back(idx - LA)
                for tt in range(4):
                    am, amB = amix.next()
                    K.op("pool", lambda e, tt=tt, am=am: e.tensor_tensor(out=am[:, :], in0=acc_o[:, tt, :], in1=sza[:, tt, :],
                                                                         op=ALU.mult), acc_oB + [szaB[tt]], [amB])
                    pt, pB = psA.next()
                    pb = pt[:].bitcast(BF16)
                    for fc in range(4):
                        K.op("pe", lambda e, fc=fc, pb=pb, am=am: e.transpose(pb[:, fc * 128:(fc + 1) * 128],
                                                                              am[:, fc * 128:(fc + 1) * 128], identb[:, :]),
                             [amB, identbB], [pB], signal=(fc == 3))
                    K.op("dve", lambda e, pb=pb, tt=tt: e.tensor_copy(out=mixT[:, 0:4, tt * 128:(tt + 1) * 128],
                                                                      in_=pb[:, 0:512].rearrange("p (k t) -> p k t", k=4)),
                         [pB], [mixTB[tt]])
                if STOP == "F":
                    st.close()
                    return
                K.phase = "evG"
                phase_G(layer, s, b)
                if STOP == "G":
                    st.close()
                    return
        st.close()

    def odd_layer(layer, li):
        st = ExitStack()

        def sbl(name, shape, dt=F32):
            t = st.enter_context(nc.sbuf_tensor(f"{name}_{layer}", list(shape), dt))
            return t, Buf(name)

        def sbln(name, shape, dt, n):
            return Rot([sbl(f"{name}{i}", shape, dt) for i in range(n)])

        yc = st.enter_context(nc.sbuf_tensor(f"yc_{layer}", [128, 8, 544], BF16))
        ycB = [Buf(f"yc{c}") for c in range(8)]
        szzs = [(st.enter_context(nc.sbuf_tensor(f"szz{i}_{layer}", [128, 8, 512], BF16)), [Buf(f"szz{i}_{c}") for c in range(8)])
                for i in range(2)]
        yv = st.enter_context(nc.sbuf_tensor(f"yv_{layer}", [128, 8, 512], F32))
        yvB = [Buf(f"yv{c}") for c in range(8)]
        diag = sbln("diag", [128, 31, 128], BF16, 3)
        sig = sbln("sig", [128, 512], F32, 2)
        ybf = sbln("ybf", [128, 512], BF16, 3)
        ysq = sbln("ysq", [128, 512], BF16, 3)
        onesb, onesbB = sbl("onesb", [128, 128], BF16)
        mean, meanB = sbl("mean", [128, 512], F32)
        rstdt, rstdtB = sbl("rstdt", [128, 512], F32)
        var, varB = sbl("var", [128, 512], F32)
        t1 = sbln("t1", [128, 512], F32, 2)

        K.op("pool", lambda e: e.memset(onesb[:, :], 1.0 / D), [], [onesbB])

        blocks = [(s_, b_) for s_ in range(NSEQ) for b_ in range(NBLK)]
        K.phase = "odA"
        phase_A(layer, 0, 0)
        def od_proj(bi, mid=None):
            s, b = blocks[bi]
            szz, szzB = szzs[bi % 2]
            K.phase = "odProj"
            for c in range(8):
                if b == 0:
                    K.op("pool", lambda e: e.memset(yc[:, c, 0:32], 0.0), [], [ycB[c]])
                else:
                    K.op("pool", lambda e: e.tensor_copy(out=yc[:, c, 0:32], in_=yc[:, c, 512:544]), [ycB[c]], [ycB[c]])
                pg, pgB = fm_proj(1024 + c * 128, 128)
                sg, sgB = sig.next()
                K.op("act", lambda e: e.activation(out=sg[:, :], in_=pg[:, 0:512], func=AF.Sigmoid), [pgB], [sgB])
                pa, paB = fm_proj(c * 128, 128)
                K.op("dve", lambda e: e.tensor_tensor(out=yc[:, c, 32:544], in0=pa[:, 0:512], in1=sg[:, :], op=ALU.mult),
                     [paB, sgB], [ycB[c]])
            if mid is not None:
                mid()
                K.phase = "odProj"
            for c in range(8):
                pz, pzB = fm_proj(2048 + c * 128, 128)
                K.op("act", lambda e: e.activation(out=szz[:, c, :], in_=pz[:, 0:512], func=AF.Silu), [pzB], [szzB[c]])

        pre_dgs = {}

        def od_conv(bi):
            K.phase = "odConv"
            pm, pmB = psB.next()
            pq, pqB = psB.next()
            dgs = dict(pre_dgs)
            pre_dgs.clear()

            def build_diag(c, into=None):
                dg, dgB = diag.next()
                K.op("pool" if c % 2 == 0 else "dve", lambda e: e.tensor_tensor(
                    out=dg[:, :, :], in0=identb[:, :].unsqueeze(1).to_broadcast([128, 31, 128]),
                    in1=par[:, c, 1:32].unsqueeze(2).to_broadcast([128, 31, 128]), op=ALU.mult), [identbB, parB], [dgB])
                (dgs if into is None else into)[c] = (dg, dgB)

            for c in range(3):
                if c not in dgs:
                    build_diag(c)
            pend_stats = []

            def emit_stats():
                c_, yb_, ybB_, yq_, yqB_ = pend_stats.pop(0)
                K.op("pe", lambda e: e.matmul(pm[:, 0:512], onesb[:, :], yb_[:, :], start=(c_ == 0), stop=(c_ == 7)),
                     [onesbB, ybB_], [pmB], signal=(c_ == 7))
                K.op("pe", lambda e: e.matmul(pq[:, 0:512], onesb[:, :], yq_[:, :], start=(c_ == 0), stop=(c_ == 7)),
                     [onesbB, yqB_], [pqB], signal=(c_ == 7))

            for c in range(8):
                dg, dgB = dgs[c]
                pc, pcB = psA.next()
                for k in range(31):
                    K.op("pe", lambda e, k=k: e.matmul(pc[:, 0:512], dg[:, k, :], yc[:, c, 2 + k:2 + k + 512],
                                                       start=(k == 0), stop=(k == 30)),
                         [dgB, ycB[c]], [pcB], signal=(k == 30))
                if c + 3 < 8:
                    build_diag(c + 3)
                if pend_stats:
                    emit_stats()
                K.op("act", lambda e: e.activation(out=yv[:, c, :], in_=pc[:, 0:512], func=AF.Identity,
                                                   bias=par[:, c, 32:33]), [pcB, parB], [yvB[c]])
                yb, ybB = ybf.next()
                yq, yqB = ysq.next()
                K.op("dve", lambda e: e.tensor_copy(out=yb[:, :], in_=yv[:, c, :]), [yvB[c]], [ybB])
                K.op("pool", lambda e: e.tensor_tensor(out=yq[:, :], in0=yv[:, c, :], in1=yv[:, c, :], op=ALU.mult),
                     [yvB[c]], [yqB])
                pend_stats.append((c, yb, ybB, yq, yqB))
            while pend_stats:
                emit_stats()
            if bi + 1 < len(blocks):
                for c in range(3):
                    build_diag(c, into=pre_dgs)
            K.phase = "odLN"
            K.op("dve", lambda e: e.tensor_copy(out=mean[:, :], in_=pm[:, 0:512]), [pmB], [meanB])
            K.op("dve", lambda e: e.tensor_tensor(out=var[:, :], in0=mean[:, :], in1=mean[:, :], op=ALU.mult), [meanB], [varB])
            K.op("dve", lambda e: e.scalar_tensor_tensor(out=var[:, :], in0=pq[:, 0:512], scalar=LN_EPS, in1=var[:, :],
                                                         op0=ALU.add, op1=ALU.subtract), [pqB, varB], [varB])

        def od_ln_prep2():
            K.phase = "odLN"
            K.op("act", lambda e: e.activation(out=var[:, :], in_=var[:, :], func=AF.Sqrt), [varB], [varB])
            K.op("dve", lambda e: e.reciprocal(out=rstdt[:, :], in_=var[:, :]), [varB], [rstdtB])
            for c in range(8):
                K.op("dve", lambda e: e.tensor_tensor(out=yv[:, c, :], in0=yv[:, c, :], in1=mean[:, :], op=ALU.subtract),
                     [yvB[c], meanB], [yvB[c]])
                K.op("pool", lambda e: e.tensor_tensor(out=yv[:, c, :], in0=yv[:, c, :], in1=rstdt[:, :], op=ALU.mult),
                     [yvB[c], rstdtB], [yvB[c]])

        def od_ln_fin(bi):
            szz, szzB = szzs[bi % 2]
            K.phase = "odLNf"
            for c in range(8):
                ta, taB = t1.next()
                K.op("act", lambda e: e.activation(out=ta[:, :], in_=yv[:, c, :], func=AF.Silu,
                                                   scale=par[:, c, 33:34], bias=par[:, c, 34:35]),
                     [yvB[c], parB], [taB])
                K.op("dve", lambda e: e.tensor_tensor(out=mixT[:, c, :], in0=ta[:, :], in1=szz[:, c, :], op=ALU.mult),
                     [taB, szzB[c]], mixTB)

        od_proj(0)
        for bi, (s, b) in enumerate(blocks):
            if bi + 1 >= len(blocks) and layer + 1 < n_layers:
                load_w_in(layer + 1)
            od_conv(bi)
            if bi + 1 < len(blocks):
                K.phase = "odA"
                phase_A(layer, blocks[bi + 1][0], blocks[bi + 1][1])
                od_proj(bi + 1, mid=od_ln_prep2)
            else:
                od_ln_prep2()
            od_ln_fin(bi)
            K.phase = "odG"
            phase_G(layer, s, b)
        st.close()

    load_w_in(0)
    for layer in range(n_layers):
        li = layer // 2
        load_common(layer)
        if layer > 0:
            K.barrier()
        if layer % 2 == 0:
            even_layer(layer, li)
        else:
            odd_layer(layer, li)

    for s in range(NSEQ):
        for t in range(16):
            b = xd[s][t]
            if b.w is not None:
                sm, v = b.w
                nc.sync.wait_ge(sm.h, v)
    stack.close()
    return nc, K


def make_consts():
    p = np.arange(128)
    ident = np.eye(128, dtype=np.float32)
    tril = (p[:, None] <= p[None, :]).astype(np.float32)
    far = (p[:, None] > p[None, :]).astype(np.float32)
    ctab = np.zeros((128, 16, 32), np.float32)
    j = np.arange(32)
    for tile in range(16):
        t = tile * 128 + p
        cur = t // 64
        forced = (j[None, :] == 0) | (j[None, :] == cur[:, None]) | (j[None, :] == cur[:, None] - 1)
        causal = j[None, :] <= cur[:, None]
        ctab[:, tile, :] = np.where(causal, forced.astype(np.float32) * np.float32(1e4), np.float32(-1e30))
    ebig = ((np.arange(2048)[None, :] // 64) == j[:, None]).astype(np.float32) * np.float32(BIG)
    n_cmp = 127
    tok = np.arange(n_cmp)[:, None] * 16 + np.arange(32)[None, :]
    ovl = ((tok[:, :, None] // 64) == np.arange(32)[None, None, :]).sum(1).astype(np.float32) / np.float32(32)
    return {"c_ident": ident, "c_tril": tril, "c_far": far, "c_ctab": ctab, "c_ebig": ebig, "c_ovl": ovl}


_CACHE = {}


def kernel(**inputs):
    if "nc" not in _CACHE:
        _CACHE["nc"] = build_program(4)[0]
    nc = _CACHE["nc"]
    consts = make_consts()
    x = np.ascontiguousarray(inputs["x"], dtype=np.float32)
    shared = {k: np.ascontiguousarray(v, dtype=np.float32) for k, v in inputs.items() if k != "x"}
    shared.update(consts)
    in_maps = []
    for c in range(NCORES):
        m = dict(shared)
        m["x"] = x[c * NSEQ:(c + 1) * NSEQ]
        in_maps.append(m)
    res = run_bass_kernel_spmd(nc, in_maps, core_ids=list(range(NCORES)))
    return np.concatenate([r["out"] for r in res.results], axis=0).astype(np.float32)
```

```python
from contextlib import ExitStack

import numpy as np
import concourse.bass as bass
import concourse.mybir as mybir
from concourse.bass_utils import run_bass_kernel_spmd

F32 = mybir.dt.float32
BF16 = mybir.dt.bfloat16
AF = mybir.ActivationFunctionType
ALU = mybir.AluOpType

NCORES = 8
SEQ = 2048
D = 1024
NSEQ = 2
NBLK = 4
EVEN_W = 3352
ODD_W = 3072
RMS_EPS = 1e-6
LN_EPS = 1e-5
BIG = 30000.0
import os as _os
STOP = _os.environ.get("KSTOP", "")

C_Q, C_KC, C_VC, C_KS, C_VS, C_KW, C_VW, C_GATE, C_ZA, C_U, C_V, C_ZB = (
    0, 512, 640, 768, 896, 1024, 1152, 1280, 1304, 1816, 2328, 2840)


class Sem:
    __slots__ = ("h", "val", "name")

    def __init__(self, h, name=""):
        self.h = h
        self.val = 0
        self.name = name


class Buf:
    __slots__ = ("name", "w", "r", "dsem")

    def __init__(self, name):
        self.name = name
        self.w = None
        self.r = {}
        self.dsem = None


class Trk:
    def __init__(self, nc, stack):
        self.nc = nc
        self.stack = stack
        self.eng = {"pe": nc.tensor, "act": nc.scalar, "dve": nc.vector, "pool": nc.gpsimd, "sp": nc.sync}
        self.nsem = 0
        self.esem = {e: self.newsem("e_" + e) for e in self.eng}
        self.waited = {e: {} for e in self.eng}
        self.nops = 0
        self.dsems = []
        self.phase = ""
        self.waitlog = {e: [] for e in self.eng}

    def newsem(self, name):
        self.nsem += 1
        return Sem(self.stack.enter_context(self.nc.semaphore(f"{name}_{self.nsem}")), name)

    def _sync(self, E, reads, writes):
        deps = {}
        for b in reads:
            if b.w is not None:
                s, v = b.w
                if deps.get(s, 0) < v:
                    deps[s] = v
        for b in writes:
            if b.w is not None:
                s, v = b.w
                if deps.get(s, 0) < v:
                    deps[s] = v
            for s, v in b.r.items():
                if deps.get(s, 0) < v:
                    deps[s] = v
        eng = self.eng[E]
        w = self.waited[E]
        own = self.esem[E]
        for s, v in deps.items():
            if E == "pe" and s is own:
                continue
            if w.get(s, 0) < v:
                eng.wait_ge(s.h, v)
                w[s] = v
                self.waitlog[E].append((self.phase, s.name))

    def op(self, E, fn, reads=(), writes=(), signal=True):
        self._sync(E, reads, writes)
        ins = fn(self.eng[E])
        s = self.esem[E]
        if signal:
            s.val += 1
            ins.then_inc(s.h, 1)
            tag = (s, s.val)
        else:
            tag = (s, s.val + 1)
        for b in reads:
            if b.r.get(s, 0) < tag[1]:
                b.r[s] = tag[1]
        for b in writes:
            b.w = tag
            b.r = {}
        self.nops += 1
        return ins

    def barrier(self):
        sems = list(self.esem.values()) + self.dsems
        for E, eng in self.eng.items():
            w = self.waited[E]
            for s in sems:
                if s.val > 0 and w.get(s, 0) < s.val:
                    eng.wait_ge(s.h, s.val)
                    w[s] = s.val

    def dma(self, E, out, in_, reads, writes, dbuf, **kw):
        self._sync(E, reads, writes)
        if dbuf.dsem is None:
            dbuf.dsem = self.newsem("d_" + dbuf.name)
            self.dsems.append(dbuf.dsem)
        s = dbuf.dsem
        ins = self.eng[E].dma_start(out=out, in_=in_, **kw)
        s.val += 16
        ins.then_inc(s.h, 16)
        tag = (s, s.val)
        for b in reads:
            if b.r.get(s, 0) < tag[1]:
                b.r[s] = tag[1]
        for b in writes:
            b.w = tag
            b.r = {}
        self.nops += 1
        return ins


class Rot:
    def __init__(self, items):
        self.items = items
        self.i = 0

    def next(self):
        it = self.items[self.i % len(self.items)]
        self.i += 1
        return it


def build_program(n_layers=4):
    nc = bass.Bass("TRN2", target_bir_lowering=False, dynamic_dma_scratch_size=16384)
    stack = ExitStack()
    K = Trk(nc, stack)

    def dram(name, shape, dt=F32, kind="ExternalInput"):
        return nc.dram_tensor(name, list(shape), dt, kind=kind).ap()

    x_in = dram("x", [NSEQ, SEQ, D])
    out = dram("out", [NSEQ, SEQ, D], kind="ExternalOutput")
    norm_pre = dram("norm_pre", [4, D])
    norm_post = dram("norm_post", [4, D])
    e_w_in = dram("even_w_in", [2, D, EVEN_W])
    e_k_pe = dram("even_cmp_k_pe", [2, 32, 64])
    e_k_w1 = dram("even_cmp_k_w1", [2, 2048, 64])
    e_k_w2 = dram("even_cmp_k_w2", [2, 64, 64])
    e_v_pe = dram("even_cmp_v_pe", [2, 32, 64])
    e_v_w1 = dram("even_cmp_v_w1", [2, 2048, 64])
    e_v_w2 = dram("even_cmp_v_w2", [2, 64, 64])
    e_ln_g = dram("even_sgu_ln_g", [2, 512])
    e_ln_b = dram("even_sgu_ln_b", [2, 512])
    e_sgu_w = dram("even_sgu_w", [2, 8, 128, 128])
    e_sgu_b = dram("even_sgu_b", [2, 8, 128])
    e_w_out = dram("even_w_out", [2, D, D])
    o_w_in = dram("odd_w_in", [2, D, ODD_W])
    o_dw_w = dram("odd_dw_w", [2, 31, D])
    o_dw_b = dram("odd_dw_b", [2, D])
    o_ln_g = dram("odd_ln_g", [2, D])
    o_ln_b = dram("odd_ln_b", [2, D])
    o_w_out = dram("odd_w_out", [2, D, D])
    c_ident = dram("c_ident", [128, 128])
    c_tril = dram("c_tril", [128, 128])
    c_far = dram("c_far", [128, 128])
    c_ctab = dram("c_ctab", [128, 16, 32])
    c_ebig = dram("c_ebig", [32, 2048])
    c_ovl = dram("c_ovl", [127, 32])

    def sb(name, shape, dt=F32):
        t = stack.enter_context(nc.sbuf_tensor(name, list(shape), dt))
        return t, Buf(name)

    def sbn(name, shape, dt, n):
        return Rot([sb(f"{name}{i}", shape, dt) for i in range(n)])

    w_in = stack.enter_context(nc.sbuf_tensor("w_in", [128, 8, EVEN_W], BF16))
    w_inB = [Buf(f"w_in{k}") for k in range(8)]
    w_out = stack.enter_context(nc.sbuf_tensor("w_out", [128, 8, D], BF16))
    w_outB = [Buf(f"w_out{k}") for k in range(8)]
    identb, identbB = sb("identb", [128, 128], BF16)
    identf, identfB = sb("identf", [128, 128], F32)
    trilk, trilkB = sb("trilk", [128, 128], BF16)
    fark, farkB = sb("fark", [128, 128], BF16)
    neghalf, neghalfB = sb("neghalf", [128, 8], F32)
    gpost, gpostB = sb("gpost", [128, D], F32)
    xa = sbn("xa", [128, D], F32, 2)
    xr = sbn("xr", [128, D], F32, 2)
    hp = sbn("hp", [128, D], BF16, 2)
    hT = stack.enter_context(nc.sbuf_tensor("hT", [128, 8, 512], BF16))
    hTB = [Buf(f"hT{t}") for t in range(4)]
    mixT = stack.enter_context(nc.sbuf_tensor("mixT", [128, 8, 512], BF16))
    mixTB = [Buf(f"mixT{t}") for t in range(4)]
    junks = sbn("junk", [128, D], BF16, 2)
    ss, _ = sb("ss", [128, 8], F32)
    ssB = [Buf(f"ss{t}") for t in range(8)]
    ms, _ = sb("ms", [128, 8], F32)
    msB = [Buf(f"ms{t}") for t in range(8)]
    rstd, _ = sb("rstd", [128, 8], F32)
    rstdB = [Buf(f"rstd{t}") for t in range(8)]
    ssy = Rot([sb(f"ssy{i}", [128, 4], F32) + (Buf(f"ssy{i}a"), Buf(f"ssy{i}b")) for i in range(2)])
    rsy = sbn("rsy", [128, 4], F32, 2)
    par, parB = sb("par", [128, 8, 36], F32)
    tmpf = sbn("tmpf", [128, 512], F32, 4)

    PS = []
    for i in range(8):
        t = stack.enter_context(nc.psum_tensor(f"ps{i}", [128, 512], F32))
        PS.append((t, Buf(f"ps{i}")))
    psA = Rot(PS[0:4])
    psB = Rot(PS[4:8])

    xd = [[Buf(f"xd{s}_{t}") for t in range(16)] for s in range(NSEQ)]

    K.dma("pool", identb[:, :], c_ident[:, :], [], [identbB], identbB)
    K.dma("sp", identf[:, :], c_ident[:, :], [], [identfB], identfB)
    K.dma("pool", trilk[:, :], c_tril[:, :], [], [trilkB], trilkB)
    K.dma("pool", fark[:, :], c_far[:, :], [], [farkB], farkB)
    K.op("dve", lambda e: e.memset(neghalf[:, :], -0.5), [], [neghalfB])

    def layer_srcs(layer):
        li = layer // 2
        if layer % 2 == 0:
            return e_w_in[li], EVEN_W, e_w_out[li], []
        return (o_w_in[li], ODD_W, o_w_out[li],
                [(o_dw_w[li], 31), (o_dw_b[li:li + 1, :], 1), (o_ln_g[li:li + 1, :], 1), (o_ln_b[li:li + 1, :], 1)])

    def load_w_in(layer):
        w_in_dram, w_in_cols, _, _ = layer_srcs(layer)
        for kc in range(8):
            K.dma("pool", w_in[:, kc, 0:w_in_cols], w_in_dram[kc * 128:(kc + 1) * 128, :], [], [w_inB[kc]], w_inB[kc],
                  max_dma_last_dim=8192)

    def load_common(layer):
        _, _, w_out_dram, rows = layer_srcs(layer)
        stg, stgB = xa.next()
        rows = [(norm_pre[layer:layer + 1, :], 1)] + rows
        if sum(r for _, r in rows) % 2:
            rows = rows + [(norm_post[layer:layer + 1, :], 1)]
        r0 = 0
        for ap, r in rows:
            K.dma("sp", stg[r0:r0 + r, :], ap, [], [stgB], stgB)
            r0 += r
        R = r0
        for c in range(8):
            pt, pB = psA.next()
            K.op("pe", lambda e, c=c, pt=pt: e.transpose(pt[:, 0:R], stg[0:R, c * 128:(c + 1) * 128], identf[0:R, 0:R]),
                 [stgB, identfB], [pB])
            K.op("dve", lambda e, c=c, pt=pt: e.tensor_copy(out=par[:, c, 0:R], in_=pt[:, 0:R]), [pB], [parB])
        for kc in range(8):
            K.dma("pool", w_out[:, kc, :], w_out_dram[kc * 128:(kc + 1) * 128, :], [], [w_outB[kc]], w_outB[kc],
                  max_dma_last_dim=4096)
        K.dma("sp", gpost[:, :], norm_post[layer:layer + 1, :].to_broadcast([128, D]), [], [gpostB], gpostB)

    def phase_A(layer, s, b):
        src = x_in if layer == 0 else out
        xts = []
        for tt in range(4):
            xt, xB = xa.next()
            tile = b * 4 + tt
            K.dma("sp", xt[:, :], src[s, tile * 128:(tile + 1) * 128, :], [xd[s][tile]], [xB], xB)
            jk, jkB = junks.next()
            K.op("act", lambda e, xt=xt, tt=tt, jk=jk: e.activation(out=jk[:, :], in_=xt[:, :], func=AF.Square,
                                                                    accum_out=ss[:, tt:tt + 1]), [xB], [ssB[tt], jkB])
            K.op("dve", lambda e, tt=tt: e.tensor_scalar(out=ms[:, tt:tt + 1], in0=ss[:, tt:tt + 1], scalar1=1.0 / D,
                                                        scalar2=RMS_EPS, op0=ALU.mult, op1=ALU.add), [ssB[tt]], [msB[tt]])
            K.op("pool", lambda e, tt=tt: e.tensor_tensor(out=rstd[:, tt:tt + 1], in0=ms[:, tt:tt + 1],
                                                          in1=neghalf[:, 0:1], op=ALU.pow), [msB[tt], neghalfB], [rstdB[tt]])
            ht, hB = hp.next()
            K.op("dve", lambda e, ht=ht, xt=xt, tt=tt: e.tensor_scalar(out=ht[:, :], in0=xt[:, :], scalar1=rstd[:, tt:tt + 1],
                                                                       scalar2=None, op0=ALU.mult), [xB, rstdB[tt]], [hB])
            pt, pB = psA.next()
            pb = pt[:].bitcast(BF16)
            for kc in range(8):
                K.op("pe", lambda e, kc=kc, pb=pb, ht=ht: e.transpose(pb[:, kc * 128:(kc + 1) * 128],
                                                                      ht[:, kc * 128:(kc + 1) * 128], identb[:, :]),
                     [hB, identbB], [pB], signal=(kc == 7))
            K.op("dve", lambda e, pb=pb, tt=tt: e.tensor_tensor(
                out=hT[:, :, tt * 128:(tt + 1) * 128], in0=pb.rearrange("p (k t) -> p k t", k=8),
                in1=par[:, :, 0:1].to_broadcast([128, 8, 128]), op=ALU.mult), [pB, parB], [hTB[tt]])

    def fm_proj(col0, M):
        pt, pB = psA.next()
        for kc in range(8):
            K.op("pe", lambda e, kc=kc, pt=pt: e.matmul(pt[0:M, 0:512], w_in[:, kc, col0:col0 + M], hT[:, kc, :],
                                                        start=(kc == 0), stop=(kc == 7)),
                 [w_inB[kc]] + hTB, [pB], signal=(kc == 7))
        return pt, pB

    def tm_proj(tt, col0, N):
        pt, pB = psA.next()
        for kc in range(8):
            K.op("pe", lambda e, kc=kc, pt=pt: e.matmul(pt[:, 0:N], hT[:, kc, tt * 128:(tt + 1) * 128],
                                                        w_in[:, kc, col0:col0 + N], start=(kc == 0), stop=(kc == 7)),
                 [w_inB[kc], hTB[tt]], [pB], signal=(kc == 7))
        return pt, pB

    def phase_G(layer, s, b):
        src = x_in if layer == 0 else out
        for tt in range(4):
            tile = b * 4 + tt
            xt, xB = xr.next()
            K.dma("sp", xt[:, :], src[s, tile * 128:(tile + 1) * 128, :], [xd[s][tile]], [xB], xB)
            halves = []
            sy, syB, syB0, syB1 = ssy.next()
            ry, ryB = rsy.next()
            for hf in range(2):
                pt, pB = psB.next()
                for fc in range(8):
                    K.op("pe", lambda e, fc=fc, pt=pt, hf=hf: e.matmul(pt[:, 0:512], mixT[:, fc, tt * 128:(tt + 1) * 128],
                                                                        w_out[:, fc, hf * 512:(hf + 1) * 512],
                                                                        start=(fc == 0), stop=(fc == 7)),
                         [mixTB[tt], w_outB[fc]], [pB], signal=(fc == 7))
                halves.append((pt, pB))
            ysbs = []
            for hf, sB_ in ((0, syB0), (1, syB1)):
                pt, pB = halves[hf]
                tf, tfB = tmpf.next()
                if hf == 0:
                    K.op("act", lambda e, pt=pt, tf=tf: e.copy(out=tf[:, :], in_=pt[:, 0:512]), [pB], [tfB])
                else:
                    K.op("dve", lambda e, pt=pt, tf=tf: e.tensor_copy(out=tf[:, :], in_=pt[:, 0:512]), [pB], [tfB])
                jk, jkB = junks.next()
                K.op("act", lambda e, tf=tf, hf=hf, sy=sy, jk=jk: e.activation(out=jk[:, 0:512], in_=tf[:, :], func=AF.Square,
                                                                               accum_out=sy[:, hf:hf + 1]), [tfB], [sB_, jkB])
                ysbs.append((tf, tfB))
            K.op("dve", lambda e, sy=sy: e.tensor_tensor(out=sy[:, 2:3], in0=sy[:, 0:1], in1=sy[:, 1:2], op=ALU.add),
                 [syB0, syB1, syB], [syB])
            K.op("dve", lambda e, sy=sy: e.tensor_scalar(out=sy[:, 3:4], in0=sy[:, 2:3], scalar1=1.0 / D, scalar2=RMS_EPS,
                                                        op0=ALU.mult, op1=ALU.add), [syB], [syB])
            K.op("pool", lambda e, sy=sy, ry=ry: e.tensor_tensor(out=ry[:, 0:1], in0=sy[:, 3:4], in1=neghalf[:, 0:1],
                                                                 op=ALU.pow), [syB, neghalfB], [ryB])
            for hf in range(2):
                tf, tfB = ysbs[hf]
                K.op("dve", lambda e, tf=tf, hf=hf, ry=ry: e.scalar_tensor_tensor(
                    out=tf[:, :], in0=tf[:, :], scalar=ry[:, 0:1], in1=gpost[:, hf * 512:(hf + 1) * 512],
                    op0=ALU.mult, op1=ALU.mult), [tfB, ryB, gpostB], [tfB])
                K.op("pool", lambda e, tf=tf, xt=xt, hf=hf: e.tensor_tensor(
                    out=xt[:, hf * 512:(hf + 1) * 512], in0=xt[:, hf * 512:(hf + 1) * 512], in1=tf[:, :], op=ALU.add),
                     [tfB, xB], [xB])
            K.dma("sp", out[s, tile * 128:(tile + 1) * 128, :], xt[:, :], [xB], [xd[s][tile]], xd[s][tile])

    def even_layer(layer, li):
        st = ExitStack()

        def sbl(name, shape, dt=F32):
            t = st.enter_context(nc.sbuf_tensor(f"{name}_{layer}", list(shape), dt))
            return t, Buf(name)

        def sbln(name, shape, dt, n):
            return Rot([sbl(f"{name}{i}", shape, dt) for i in range(n)])

        qT = st.enter_context(nc.sbuf_tensor(f"qT_{layer}", [128, 8, 512], BF16))
        qTB = [Buf(f"qT{h}") for h in range(8)]
        qSB = [Buf(f"qS{h}") for h in range(8)]
        ksT = st.enter_context(nc.sbuf_tensor(f"ksT_{layer}", [128, 2, 2048], BF16))
        ksTB = [[Buf(f"ksT{g}_{b}") for b in range(4)] for g in range(2)]
        ebB = Buf("ebig")
        kwT = st.enter_context(nc.sbuf_tensor(f"kwT_{layer}", [64, 2, 1024], BF16))
        kwTB = [[Buf(f"kwT{g}_{r}") for r in range(2)] for g in range(2)]
        kcT, kcTB = sbl("kcT", [128, 528], BF16)
        vcT, vcTB = sbl("vcT", [128, 528], BF16)
        vsA = st.enter_context(nc.sbuf_tensor(f"vsA_{layer}", [128, 16, 2, 65], BF16))
        vsAB = [Buf(f"vsA{k}") for k in range(16)]
        vwA = st.enter_context(nc.sbuf_tensor(f"vwA_{layer}", [128, 8, 2, 65], BF16))
        vwAB = [Buf(f"vwA{k}") for k in range(8)]
        w1k, w1kB = sbl("w1k", [128, 32, 64], BF16)
        w1v, w1vB = sbl("w1v", [128, 32, 64], BF16)
        w2k, w2kB = sbl("w2k", [64, 64], BF16)
        w2v, w2vB = sbl("w2v", [64, 64], BF16)
        pek, pekB = sbl("pek", [32, 64], BF16)
        pev, pevB = sbl("pev", [32, 64], BF16)
        peT, peTB = sbl("peT", [64, 2, 32], BF16)
        pebias, pebiasB = sbl("pebias", [64, 2], F32)
        hidk, hidkB = sbl("hidk", [64, 2, 32], BF16)
        hidv, hidvB = sbl("hidv", [64, 2, 128], BF16)
        kcmpT, kcmpTB = sbl("kcmpT", [64, 2, 128], BF16)
        vcmp, vcmpB = sbl("vcmp", [128, 2, 97], BF16)
        ctab, ctabB = sbl("ctab", [128, 16, 32], F32)
        WsT, WsTB = sbl("WsT", [128, 8, 128], BF16)
        bs, bsB = sbl("bs", [128, 8], F32)
        lng, lngB = sbl("lng", [128, 512], F32)
        lnb, lnbB = sbl("lnb", [128, 512], F32)
        gates = st.enter_context(nc.sbuf_tensor(f"gates_{layer}", [128, 4, 24], F32))
        gatesB = [Buf(f"gates{t}") for t in range(4)]
        sza = st.enter_context(nc.sbuf_tensor(f"sza_{layer}", [128, 4, 512], BF16))
        szaB = [Buf(f"sza{t}") for t in range(4)]
        ug = sbln("ug", [128, 512], BF16, 2)
        szb = sbln("szb", [128, 512], BF16, 2)
        vg = sbln("vg", [128, 512], F32, 2)
        vln = sbln("vln", [128, 512], BF16, 2)
        lnst = sbln("lnst", [128, 16], F32, 2)
        acc_o = st.enter_context(nc.sbuf_tensor(f"acc_o_{layer}", [128, 4, 512], F32))
        acc_oB = [Buf(f"acc_o{h}") for h in range(8)]
        imp = sbln("imp", [128, 4, 32], F32, 2)
        top8 = sbln("top8", [128, 4, 8], F32, 2)
        selpads = [sbl(f"selpad{g}", [128, 4, 96], BF16) for g in range(2)]
        pTs = sbln("pT", [128, 512], BF16, 4)
        rcs = sbln("rc", [128, 16], F32, 4)
        amix = sbln("amix", [128, 512], BF16, 2)
        tmpb = sbln("tmpb", [128, 512], BF16, 3)

        for g in range(2):
            K.dma("pool", ksT[64:96, g, :], c_ebig[:, :], [], [ebB], ebB, max_dma_last_dim=4096)
        for wi, (w1, w1B, src) in enumerate(((w1k, w1kB, e_k_w1), (w1v, w1vB, e_v_w1))):
            srcv = src[li].rearrange("(l d) e -> d l e", d=64)
            for lh in range(2):
                stg, stgB = xa.next()
                stgv = stg[:, :].rearrange("p (l e) -> p l e", l=16)
                for half in range(2):
                    K.dma("sp", stgv[half * 64:(half + 1) * 64, :, :], srcv[:, lh * 16:(lh + 1) * 16, :], [], [stgB], stgB)
                if (wi + lh) % 2 == 0:
                    K.op("dve", lambda e, w1=w1, lh=lh, stgv=stgv: e.tensor_copy(out=w1[:, lh * 16:(lh + 1) * 16, :], in_=stgv),
                         [stgB], [w1B])
                else:
                    K.op("act", lambda e, w1=w1, lh=lh, stgv=stgv: e.copy(out=w1[:, lh * 16:(lh + 1) * 16, :], in_=stgv),
                         [stgB], [w1B])
        K.dma("pool", w2k[:, :], e_k_w2[li], [], [w2kB], w2kB)
        K.dma("pool", w2v[:, :], e_v_w2[li], [], [w2vB], w2vB)
        K.dma("pool", pek[:, :], e_k_pe[li], [], [pekB], pekB)
        K.dma("pool", pev[:, :], e_v_pe[li], [], [pevB], pevB)
        K.dma("sp", ctab[:, :, :], c_ctab[:, :, :], [], [ctabB], ctabB)
        wsst_t, wsstB = xa.next()
        wsst = wsst_t[:, :].rearrange("p (g j) -> p g j", g=8)
        K.dma("sp", wsst, e_sgu_w[li].rearrange("g i j -> i g j"), [], [wsstB], wsstB)
        bst, bstB = xa.next()
        K.dma("sp", bst[0:8, 0:128], e_sgu_b[li], [], [bstB], bstB)
        pt, pB = psA.next()
        K.op("pe", lambda e: e.transpose(pt[:, 0:8], bst[0:8, 0:128], identf[0:8, 0:8]), [bstB, identfB], [pB])
        K.op("dve", lambda e: e.tensor_copy(out=bs[:, :], in_=pt[:, 0:8]), [pB], [bsB])
        K.dma("sp", lng[:, :], e_ln_g[li:li + 1, :].to_broadcast([128, 512]), [], [lngB], lngB)
        K.dma("sp", lnb[:, :], e_ln_b[li:li + 1, :].to_broadcast([128, 512]), [], [lnbB], lnbB)
        K.op("pool", lambda e: e.memset(vsA[:, :, :, 64:65], 1.0), [], vsAB)
        K.op("pool", lambda e: e.memset(vwA[:, :, :, 64:65], 1.0), [], vwAB)
        K.op("pool", lambda e: e.memset(vcmp[:, :, :], 0.0), [], [vcmpB])
        K.op("pool", lambda e: e.memset(vcmp[:, :, 64:65], 1.0), [vcmpB], [vcmpB])
        for g in range(2):
            K.dma("pool", vcmp[0:127, g, 65:97], c_ovl[:, :], [], [vcmpB], vcmpB)
        K.op("pool", lambda e: e.memset(hidv[:, :, :], 0.0), [], [hidvB])
        K.op("pool", lambda e: e.memset(kcmpT[:, :, :], 0.0), [], [kcmpTB])
        for selpad, selpadB in selpads:
            K.op("pool", lambda e, selpad=selpad: e.memset(selpad[:, :, :], 0.0), [], [selpadB])
        K.op("dve", lambda e: e.memset(kcT[:, 0:16], 0.0), [], [kcTB])
        K.op("dve", lambda e: e.memset(vcT[:, 0:16], 0.0), [], [vcTB])
        for g in range(8):
            pt, pB = psA.next()
            K.op("pe", lambda e, g=g, pt=pt: e.transpose(pt[:, 0:128], wsst[:, g, :], identf[:, :]), [wsstB, identfB], [pB])
            K.op("dve", lambda e, g=g, pt=pt: e.tensor_tensor(out=WsT[:, g, :], in0=pt[:, 0:128], in1=trilk[:, :], op=ALU.mult),
                 [pB, trilkB], [WsTB])
        for xi, (pe_, peB_, w1, w1B) in enumerate(((pek, pekB, w1k, w1kB), (pev, pevB, w1v, w1vB))):
            pt, pB = psA.next()
            pb = pt[:].bitcast(BF16)
            K.op("pe", lambda e, pb=pb, pe_=pe_: e.transpose(pb[0:64, 0:32], pe_[:, :], identb[0:32, 0:32]), [peB_, identbB], [pB])
            K.op("dve", lambda e, pb=pb, xi=xi: e.tensor_copy(out=peT[:, xi, :], in_=pb[0:64, 0:32]), [pB], [peTB])
            pt2, pB2 = psA.next()
            for l in range(32):
                K.op("pe", lambda e, l=l, pt2=pt2, w1=w1, xi=xi: e.matmul(pt2[0:64, 0:1], w1[0:64, l, :], peT[:, xi, l:l + 1],
                                                                          start=(l == 0), stop=(l == 31)),
                     [w1B, peTB], [pB2], signal=(l == 31))
            K.op("dve", lambda e, pt2=pt2, xi=xi: e.tensor_copy(out=pebias[:, xi:xi + 1], in_=pt2[0:64, 0:1]), [pB2], [pebiasB])

        def evac_copy(i, out_ap, in_ap, reads, writes, scale=None):
            if i % 2 == 0:
                if scale is None:
                    K.op("act", lambda e: e.copy(out=out_ap, in_=in_ap), reads, writes)
                else:
                    K.op("act", lambda e: e.mul(out=out_ap, in_=in_ap, mul=scale), reads, writes)
            else:
                if scale is None:
                    K.op("dve", lambda e: e.tensor_copy(out=out_ap, in_=in_ap), reads, writes)
                else:
                    K.op("dve", lambda e: e.tensor_scalar(out=out_ap, in0=in_ap, scalar1=scale, scalar2=None, op0=ALU.mult),
                         reads, writes)

        if STOP == "params":
            st.close()
            return
        blocks = [(s_, b_) for s_ in range(NSEQ) for b_ in range(NBLK)]
        K.phase = "evA"
        phase_A(layer, 0, 0)
        for bi, (s, b) in enumerate(blocks):
            if True:
                t0 = b * 512
                K.phase = "evB"
                if STOP == "A":
                    st.close()
                    return
                for h in range(8):
                    pt, pB = fm_proj(C_Q + h * 64, 64)
                    evac_copy(h, qT[0:64, h, :], pt[0:64, 0:512], [pB], [qTB[h]], scale=0.125)
                for g in range(2):
                    pt, pB = fm_proj(C_KS + g * 64, 64)
                    evac_copy(g, ksT[0:64, g, t0:t0 + 512], pt[0:64, 0:512], [pB], [ksTB[g][b]])
                for g in range(2):
                    pt, pB = fm_proj(C_KW + g * 64, 64)
                    r = b % 2
                    evac_copy(g + 1, kwT[0:64, g, r * 512:(r + 1) * 512], pt[0:64, 0:512], [pB], [kwTB[g][r]])
                for i, (cX, XT, XTB) in enumerate(((C_KC, kcT, kcTB), (C_VC, vcT, vcTB))):
                    if b > 0:
                        K.op("pool", lambda e, XT=XT: e.tensor_copy(out=XT[:, 0:16], in_=XT[:, 512:528]), [XTB], [XTB])
                    pt, pB = fm_proj(cX, 128)
                    evac_copy(i, XT[:, 16:528], pt[:, 0:512], [pB], [XTB])
                if STOP == "B":
                    st.close()
                    return
                K.phase = "evC"
                cst = {}
                cst2 = {}

                def c_front(tt):
                    kt = b * 4 + tt
                    pt, pB = tm_proj(tt, C_VS, 408)
                    K.op("dve", lambda e, pt=pt, kt=kt: e.tensor_copy(out=vsA[:, kt, :, 0:64],
                                                                      in_=pt[:, 0:128].rearrange("p (g d) -> p g d", g=2)),
                         [pB], [vsAB[kt]])
                    K.op("dve", lambda e, pt=pt, kt=kt: e.tensor_copy(out=vwA[:, kt % 8, :, 0:64],
                                                                      in_=pt[:, 256:384].rearrange("p (g d) -> p g d", g=2)),
                         [pB], [vwAB[kt % 8]])
                    K.op("act", lambda e, pt=pt, tt=tt: e.activation(out=gates[:, tt, :], in_=pt[:, 384:408], func=AF.Sigmoid),
                         [pB], [gatesB[tt]])
                    pt, pB = tm_proj(tt, C_ZA, 512)
                    K.op("act", lambda e, pt=pt, tt=tt: e.activation(out=sza[:, tt, :], in_=pt[:, 0:512], func=AF.Silu),
                         [pB], [szaB[tt]])
                    pt, pB = tm_proj(tt, C_ZB, 512)
                    zt, ztB = szb.next()
                    K.op("act", lambda e, pt=pt, zt=zt: e.activation(out=zt[:, :], in_=pt[:, 0:512], func=AF.Silu), [pB], [ztB])
                    pt, pB = tm_proj(tt, C_U, 512)
                    ut, utB = ug.next()
                    K.op("act", lambda e, pt=pt, ut=ut: e.activation(out=ut[:, :], in_=pt[:, 0:512], func=AF.Gelu_apprx_tanh),
                         [pB], [utB])
                    pt, pB = tm_proj(tt, C_V, 512)
                    vt, vtB = vg.next()
                    K.op("act", lambda e, pt=pt, vt=vt: e.activation(out=vt[:, :], in_=pt[:, 0:512], func=AF.Gelu_apprx_tanh),
                         [pB], [vtB])
                    K.op("pool", lambda e, ut=ut, zt=zt: e.tensor_tensor(out=ut[:, :], in0=ut[:, :], in1=zt[:, :], op=ALU.mult),
                         [utB, ztB], [utB])
                    ls, lsB = lnst.next()
                    K.op("dve", lambda e, ls=ls, vt=vt: e.bn_stats(out=ls[:, 0:6], in_=vt[:, :]), [vtB], [lsB])
                    K.op("dve", lambda e, ls=ls: e.bn_aggr(out=ls[:, 6:8], in_=ls[:, 0:6]), [lsB], [lsB])
                    K.op("dve", lambda e, ls=ls: e.tensor_scalar(out=ls[:, 8:9], in0=ls[:, 7:8], scalar1=LN_EPS, scalar2=None,
                                                                op0=ALU.add), [lsB], [lsB])
                    K.op("pool", lambda e, ls=ls: e.tensor_tensor(out=ls[:, 9:10], in0=ls[:, 8:9], in1=neghalf[:, 0:1], op=ALU.pow),
                         [lsB, neghalfB], [lsB])
                    K.op("dve", lambda e, ls=ls, vt=vt: e.tensor_scalar(out=vt[:, :], in0=vt[:, :], scalar1=ls[:, 6:7],
                                                                        scalar2=ls[:, 9:10], op0=ALU.subtract, op1=ALU.mult),
                         [lsB, vtB], [vtB])
                    K.op("pool", lambda e, vt=vt: e.tensor_tensor(out=vt[:, :], in0=vt[:, :], in1=lng[:, :], op=ALU.mult),
                         [vtB, lngB], [vtB])
                    vl, vlB = vln.next()
                    K.op("pool", lambda e, vt=vt, vl=vl: e.tensor_tensor(out=vl[:, :], in0=vt[:, :], in1=lnb[:, :], op=ALU.add),
                         [vtB, lnbB], [vlB])
                    cst[tt] = (vl, vlB, ut, utB)

                def c_back(tt):
                    vl, vlB, ut, utB = cst.pop(tt)
                    pt, pB = psB.next()
                    for g in range(8):
                        K.op("pe", lambda e, g=g, pt=pt, vl=vl: e.matmul(pt[:, g * 64:(g + 1) * 64], WsT[:, g, :],
                                                                          vl[:, g * 64:(g + 1) * 64], start=True, stop=True),
                             [WsTB, vlB], [pB], signal=(g == 7))
                    tb, tbB = tmpb.next()
                    K.op("dve", lambda e, pt=pt, tb=tb: e.tensor_tensor(
                        out=tb[:, :].rearrange("p (g d) -> p g d", g=8), in0=pt[:, 0:512].rearrange("p (g d) -> p g d", g=8),
                        in1=bs[:, :].unsqueeze(2).to_broadcast([128, 8, 64]), op=ALU.add), [pB, bsB], [tbB])
                    K.op("pool", lambda e, tb=tb, ut=ut: e.tensor_tensor(out=tb[:, :], in0=tb[:, :], in1=ut[:, :], op=ALU.mult),
                         [tbB, utB], [tbB])
                    cst2[tt] = (tb, tbB)

                def c_back2(tt):
                    tb, tbB = cst2.pop(tt)
                    pt, pB = psA.next()
                    pb = pt[:].bitcast(BF16)
                    for fc in range(4):
                        K.op("pe", lambda e, fc=fc, pb=pb, tb=tb: e.transpose(pb[:, fc * 128:(fc + 1) * 128],
                                                                              tb[:, fc * 128:(fc + 1) * 128], identb[:, :]),
                             [tbB, identbB], [pB], signal=(fc == 3))
                    K.op("dve", lambda e, pb=pb, tt=tt: e.tensor_copy(out=mixT[:, 4:8, tt * 128:(tt + 1) * 128],
                                                                      in_=pb[:, 0:512].rearrange("p (k t) -> p k t", k=4)),
                         [pB], [mixTB[tt]])

                for tt in range(6):
                    if tt < 4:
                        c_front(tt)
                    if 1 <= tt <= 4:
                        c_back(tt - 1)
                    if tt >= 2:
                        c_back2(tt - 2)
                if STOP == "C":
                    st.close()
                    return
                if bi + 1 < len(blocks):
                    K.phase = "evA"
                    phase_A(layer, blocks[bi + 1][0], blocks[bi + 1][1])
                elif layer + 1 < n_layers:
                    load_w_in(layer + 1)
                K.phase = "evD"
                if b == 0:
                    n0, nn, c0 = 0, 31, 16
                else:
                    n0, nn, c0 = 32 * b - 1, 32, 0
                nk = 32 * (b + 1) - 1
                for g in range(2):
                    for xi, (XT, XTB, w1, w1B, hid, hidB, hcol) in enumerate((
                            (kcT, kcTB, w1k, w1kB, hidk, hidkB, 0), (vcT, vcTB, w1v, w1vB, hidv, hidvB, n0))):
                        pt, pB = psA.next()
                        for l in range(32):
                            K.op("pe", lambda e, l=l, pt=pt, XT=XT, w1=w1: e.matmul(
                                pt[0:64, 0:nn], w1[g * 64:(g + 1) * 64, l, :],
                                XT[g * 64:(g + 1) * 64, c0 + l:c0 + l + 16 * (nn - 1) + 1:16], start=(l == 0), stop=(l == 31)),
                                 [w1B, XTB], [pB], signal=(l == 31))
                        K.op("act", lambda e, pt=pt, hid=hid, hcol=hcol, xi=xi: e.activation(
                            out=hid[:, g, hcol:hcol + nn], in_=pt[0:64, 0:nn], func=AF.Silu, bias=pebias[:, xi:xi + 1]),
                             [pB, pebiasB], [hidB])
                    pt, pB = psA.next()
                    K.op("pe", lambda e, pt=pt: e.matmul(pt[0:64, 0:nn], w2k[:, :], hidk[:, g, 0:nn], start=True, stop=True),
                         [w2kB, hidkB], [pB])
                    K.op("dve", lambda e, pt=pt: e.tensor_copy(out=kcmpT[:, g, n0:n0 + nn], in_=pt[0:64, 0:nn]), [pB], [kcmpTB])
                    pt, pB = psA.next()
                    K.op("pe", lambda e, pt=pt: e.matmul(pt[0:nk, 0:64], hidv[:, g, 0:nk], w2v[:, :], start=True, stop=True),
                         [w2vB, hidvB], [pB])
                    K.op("dve", lambda e, pt=pt: e.tensor_copy(out=vcmp[0:nk, g, 0:64], in_=pt[0:nk, 0:64]), [pB], [vcmpB])
                if STOP == "D":
                    st.close()
                    return
                K.phase = "evE"
                for g in range(2):
                    selpad, selpadB = selpads[g]
                    im, imB = imp.next()
                    for r in range(4):
                        h = g * 4 + r
                        pt, pB = psA.next()
                        K.op("pe", lambda e, pt=pt, h=h: e.matmul(pt[0:nk, 0:512], kcmpT[:, g, 0:nk], qT[0:64, h, :],
                                                                  start=True, stop=True), [kcmpTB, qTB[h]], [pB])
                        pT, pTB = pTs.next()
                        K.op("act", lambda e, pt=pt, pT=pT: e.activation(out=pT[0:nk, :], in_=pt[0:nk, 0:512], func=AF.Exp),
                             [pB], [pTB])
                        if STOP == "E1":
                            st.close()
                            return
                        K.op("pool", lambda e, pT=pT: e.affine_select(out=pT[0:nk, :], in_=pT[0:nk, :], pattern=[[1, 512]],
                                                                      compare_op=ALU.is_ge, fill=0.0, base=t0 - 31,
                                                                      channel_multiplier=-16), [pTB], [pTB])
                        if STOP == "E2":
                            st.close()
                            return
                        po, poB = psB.next()
                        po3 = po[:, 0:388].rearrange("p (t c) -> p t c", t=4)
                        for tt in range(4):
                            K.op("pe", lambda e, tt=tt, po=po, pT=pT: e.matmul(po[:, tt * 97:(tt + 1) * 97],
                                                                                pT[0:nk, tt * 128:(tt + 1) * 128], vcmp[0:nk, g, :],
                                                                                start=True, stop=True),
                                 [pTB, vcmpB], [poB], signal=(tt == 3))
                        if STOP == "E3":
                            st.close()
                            return
                        rc, rcB = rcs.next()
                        K.op("dve", lambda e, rc=rc, po3=po3: e.tensor_scalar(out=rc[:, 0:4], in0=po3[:, :, 64], scalar1=1e-30,
                                                                              scalar2=None, op0=ALU.max), [poB], [rcB])
                        K.op("dve", lambda e, rc=rc: e.reciprocal(out=rc[:, 4:8], in_=rc[:, 0:4]), [rcB], [rcB])
                        K.op("dve", lambda e, rc=rc, h=h: e.tensor_tensor(out=rc[:, 8:12], in0=rc[:, 4:8], in1=gates[:, :, h],
                                                                          op=ALU.mult), [rcB] + gatesB, [rcB])
                        for tt in range(4):
                            K.op("dve", lambda e, tt=tt, po3=po3, rc=rc, h=h: e.tensor_scalar(
                                out=acc_o[:, tt, h * 64:(h + 1) * 64], in0=po3[:, tt, 0:64], scalar1=rc[:, 8 + tt:9 + tt],
                                scalar2=None, op0=ALU.mult), [poB, rcB], [acc_oB[h]])
                        for tt in range(4):
                            if r == 0:
                                K.op("dve", lambda e, tt=tt, po3=po3, rc=rc, im=im: e.tensor_scalar(
                                    out=im[:, tt, :], in0=po3[:, tt, 65:97], scalar1=rc[:, 4 + tt:5 + tt], scalar2=None,
                                    op0=ALU.mult), [poB, rcB], [imB])
                            else:
                                K.op("dve", lambda e, tt=tt, po3=po3, rc=rc, im=im: e.scalar_tensor_tensor(
                                    out=im[:, tt, :], in0=po3[:, tt, 65:97], scalar=rc[:, 4 + tt:5 + tt], in1=im[:, tt, :],
                                    op0=ALU.mult, op1=ALU.add), [poB, rcB, imB], [imB])
                    if STOP == "E4":
                        st.close()
                        return
                    K.op("dve", lambda e, im=im: e.tensor_tensor(out=im[:, :, :], in0=im[:, :, :], in1=ctab[:, 4 * b:4 * b + 4, :],
                                                                 op=ALU.add), [imB, ctabB], [imB])
                    t8, t8B = top8.next()
                    for tt in range(4):
                        K.op("dve", lambda e, tt=tt, t8=t8, im=im: e.max(out=t8[:, tt, :], in_=im[:, tt, :]), [imB], [t8B])
                    for tt in range(4):
                        K.op("dve", lambda e, tt=tt, t8=t8, im=im: e.tensor_scalar(
                            out=selpad[:, tt, 64:96], in0=im[:, tt, :], scalar1=t8[:, tt, 7:8], scalar2=-1.0,
                            op0=ALU.is_ge, op1=ALU.add), [imB, t8B], [selpadB])
                    if STOP == "E6":
                        st.close()
                        return
                for g in range(2):
                    selpad, selpadB = selpads[g]
                    pt, pB = psA.next()
                    pb = pt[:].bitcast(BF16)
                    for tt in range(4):
                        K.op("pe", lambda e, tt=tt, pb=pb: e.transpose(pb[0:96, tt * 128:(tt + 1) * 128], selpad[:, tt, :],
                                                                       identb[:, :]), [selpadB, identbB], [pB], signal=(tt == 3))
                    for r in range(4):
                        h = g * 4 + r
                        K.op("dve", lambda e, h=h, pb=pb: e.tensor_copy(out=qT[64:96, h, :], in_=pb[64:96, 0:512]), [pB], [qSB[h]])
                if STOP == "E":
                    st.close()
                    return
                K.phase = "evF"
                items = []
                for h in range(8):
                    for br in (1, 0):
                        kts = list(range(0, 4 * b + 4)) if br == 0 else list(range(max(0, 4 * b - 4), 4 * b + 4))
                        for kt in kts:
                            items.append((h, br, kt, kt == kts[0], kt == kts[-1]))
                LA = 2
                stage = {}
                accs = {}

                def front(idx):
                    h, br, kt, isfirst, islast = items[idx]
                    g = h // 4
                    d = kt - 4 * b
                    lo = max(0, d)
                    hi = 3 if br == 0 else min(3, d + 4)
                    c0_, c1_ = lo * 128, (hi + 1) * 128
                    pt, pB = psA.next()
                    if br == 0:
                        K.op("pe", lambda e: e.matmul(
                            pt[:, c0_:c1_], ksT[0:96, g, kt * 128:(kt + 1) * 128], qT[0:96, h, c0_:c1_],
                            start=True, stop=True), [ksTB[g][kt // 4], ebB, qTB[h], qSB[h]], [pB])
                    else:
                        kr = kt % 8
                        K.op("pe", lambda e: e.matmul(
                            pt[:, c0_:c1_], kwT[0:64, g, kr * 128:(kr + 1) * 128], qT[0:64, h, c0_:c1_],
                            start=True, stop=True), [kwTB[g][kr // 4], qTB[h]], [pB])
                    pT, pTB = pTs.next()
                    K.op("act", lambda e: e.activation(out=pT[:, c0_:c1_], in_=pt[:, c0_:c1_], func=AF.Exp), [pB], [pTB])
                    if d >= 0:
                        K.op("pool", lambda e: e.tensor_tensor(
                            out=pT[:, d * 128:(d + 1) * 128], in0=pT[:, d * 128:(d + 1) * 128], in1=trilk[:, :],
                            op=ALU.mult), [pTB, trilkB], [pTB])
                    if br == 1 and 0 <= d + 4 <= 3:
                        K.op("pool", lambda e: e.tensor_tensor(
                            out=pT[:, (d + 4) * 128:(d + 5) * 128], in0=pT[:, (d + 4) * 128:(d + 5) * 128],
                            in1=fark[:, :], op=ALU.mult), [pTB, farkB], [pTB])
                    stage[idx] = (pT, pTB, lo, hi)

                def back(idx):
                    h, br, kt, isfirst, islast = items[idx]
                    g = h // 4
                    pT, pTB, lo, hi = stage.pop(idx)
                    if isfirst:
                        accs[(h, br)] = psB.next()
                    acc, accB = accs[(h, br)]
                    for tt in range(lo, hi + 1):
                        if br == 0:
                            vB_, rhs = vsAB[kt], vsA[:, kt, g, :]
                        else:
                            vB_, rhs = vwAB[kt % 8], vwA[:, kt % 8, g, :]
                        K.op("pe", lambda e, tt=tt, rhs=rhs: e.matmul(
                            acc[:, tt * 65:(tt + 1) * 65], pT[:, tt * 128:(tt + 1) * 128], rhs,
                            start=(isfirst and tt == lo), stop=True, skip_group_check=True), [pTB, vB_], [accB], signal=(tt == hi))
                    if islast:
                        acc3 = acc[:, 0:260].rearrange("p (t c) -> p t c", t=4)
                        rc, rcB = rcs.next()
                        K.op("dve", lambda e: e.tensor_scalar(out=rc[:, 0:4], in0=acc3[:, :, 64], scalar1=1e-30,
                                                              scalar2=None, op0=ALU.max), [accB], [rcB])
                        K.op("dve", lambda e: e.reciprocal(out=rc[:, 4:8], in_=rc[:, 0:4]), [rcB], [rcB])
                        gi = (1 + br) * 8 + h
                        K.op("dve", lambda e: e.tensor_tensor(out=rc[:, 8:12], in0=rc[:, 4:8], in1=gates[:, :, gi],
                                                              op=ALU.mult), [rcB] + gatesB, [rcB])
                        for tt in range(4):
                            K.op("dve", lambda e, tt=tt: e.scalar_tensor_tensor(
                                out=acc_o[:, tt, h * 64:(h + 1) * 64], in0=acc3[:, tt, 0:64], scalar=rc[:, 8 + tt:9 + tt],
                                in1=acc_o[:, tt, h * 64:(h + 1) * 64], op0=ALU.mult, op1=ALU.add),
                                 [accB, rcB, acc_oB[h]], [acc_oB[h]])
                        del accs[(h, br)]

                for idx in range(len(items) + LA):
                    if idx < len(items):
                        front(idx)
                    if idx - LA >= 0:
                        back(idx - LA)
                for tt in range(4):
                    am, amB = amix.next()
                    K.op("pool", lambda e, tt=tt, am=am: e.tensor_tensor(out=am[:, :], in0=acc_o[:, tt, :], in1=sza[:, tt, :],
                                                                         op=ALU.mult), acc_oB + [szaB[tt]], [amB])
                    pt, pB = psA.next()
                    pb = pt[:].bitcast(BF16)
                    for fc in range(4):
                        K.op("pe", lambda e, fc=fc, pb=pb, am=am: e.transpose(pb[:, fc * 128:(fc + 1) * 128],
                                                                              am[:, fc * 128:(fc + 1) * 128], identb[:, :]),
                             [amB, identbB], [pB], signal=(fc == 3))
                    K.op("dve", lambda e, pb=pb, tt=tt: e.tensor_copy(out=mixT[:, 0:4, tt * 128:(tt + 1) * 128],
                                                                      in_=pb[:, 0:512].rearrange("p (k t) -> p k t", k=4)),
                         [pB], [mixTB[tt]])
                if STOP == "F":
                    st.close()
                    return
                K.phase = "evG"
                phase_G(layer, s, b)
                if STOP == "G":
                    st.close()
                    return
        st.close()

    def odd_layer(layer, li):
        st = ExitStack()

        def sbl(name, shape, dt=F32):
            t = st.enter_context(nc.sbuf_tensor(f"{name}_{layer}", list(shape), dt))
            return t, Buf(name)

        def sbln(name, shape, dt, n):
            return Rot([sbl(f"{name}{i}", shape, dt) for i in range(n)])

        yc = st.enter_context(nc.sbuf_tensor(f"yc_{layer}", [128, 8, 544], BF16))
        ycB = [Buf(f"yc{c}") for c in range(8)]
        szzs = [(st.enter_context(nc.sbuf_tensor(f"szz{i}_{layer}", [128, 8, 512], BF16)), [Buf(f"szz{i}_{c}") for c in range(8)])
                for i in range(2)]
        yv = st.enter_context(nc.sbuf_tensor(f"yv_{layer}", [128, 8, 512], F32))
        yvB = [Buf(f"yv{c}") for c in range(8)]
        diag = sbln("diag", [128, 31, 128], BF16, 3)
        sig = sbln("sig", [128, 512], F32, 2)
        ybf = sbln("ybf", [128, 512], BF16, 3)
        ysq = sbln("ysq", [128, 512], BF16, 3)
        onesb, onesbB = sbl("onesb", [128, 128], BF16)
        mean, meanB = sbl("mean", [128, 512], F32)
        rstdt, rstdtB = sbl("rstdt", [128, 512], F32)
        var, varB = sbl("var", [128, 512], F32)
        t1 = sbln("t1", [128, 512], F32, 2)

        K.op("pool", lambda e: e.memset(onesb[:, :], 1.0 / D), [], [onesbB])

        blocks = [(s_, b_) for s_ in range(NSEQ) for b_ in range(NBLK)]
        K.phase = "odA"
        phase_A(layer, 0, 0)
        def od_proj(bi, mid=None):
            s, b = blocks[bi]
            szz, szzB = szzs[bi % 2]
            K.phase = "odProj"
            for c in range(8):
                if b == 0:
                    K.op("pool", lambda e: e.memset(yc[:, c, 0:32], 0.0), [], [ycB[c]])
                else:
                    K.op("pool", lambda e: e.tensor_copy(out=yc[:, c, 0:32], in_=yc[:, c, 512:544]), [ycB[c]], [ycB[c]])
                pg, pgB = fm_proj(1024 + c * 128, 128)
                sg, sgB = sig.next()
                K.op("act", lambda e: e.activation(out=sg[:, :], in_=pg[:, 0:512], func=AF.Sigmoid), [pgB], [sgB])
                pa, paB = fm_proj(c * 128, 128)
                K.op("dve", lambda e: e.tensor_tensor(out=yc[:, c, 32:544], in0=pa[:, 0:512], in1=sg[:, :], op=ALU.mult),
                     [paB, sgB], [ycB[c]])
            if mid is not None:
                mid()
                K.phase = "odProj"
            for c in range(8):
                pz, pzB = fm_proj(2048 + c * 128, 128)
                K.op("act", lambda e: e.activation(out=szz[:, c, :], in_=pz[:, 0:512], func=AF.Silu), [pzB], [szzB[c]])

        pre_dgs = {}
        hooks = {}

        def od_conv(bi):
            K.phase = "odConv"
            pm, pmB = psB.next()
            pq, pqB = psB.next()
            dgs = dict(pre_dgs)
            pre_dgs.clear()

            def build_diag(c, into=None, eng=None):
                dg, dgB = diag.next()
                K.op(eng if eng is not None else ("pool" if c % 2 == 0 else "dve"), lambda e: e.tensor_tensor(
                    out=dg[:, :, :], in0=identb[:, :].unsqueeze(1).to_broadcast([128, 31, 128]),
                    in1=par[:, c, 1:32].unsqueeze(2).to_broadcast([128, 31, 128]), op=ALU.mult), [identbB, parB], [dgB])
                (dgs if into is None else into)[c] = (dg, dgB)

            for c in range(3):
                if c not in dgs:
                    build_diag(c)
            hooks["build_diag"] = build_diag
            pend_stats = []

            def emit_stats():
                c_, yb_, ybB_, yq_, yqB_ = pend_stats.pop(0)
                K.op("pe", lambda e: e.matmul(pm[:, 0:512], onesb[:, :], yb_[:, :], start=(c_ == 0), stop=(c_ == 7)),
                     [onesbB, ybB_], [pmB], signal=(c_ == 7))
                K.op("pe", lambda e: e.matmul(pq[:, 0:512], onesb[:, :], yq_[:, :], start=(c_ == 0), stop=(c_ == 7)),
                     [onesbB, yqB_], [pqB], signal=(c_ == 7))

            for c in range(8):
                dg, dgB = dgs[c]
                pc, pcB = psA.next()
                for k in range(31):
                    K.op("pe", lambda e, k=k: e.matmul(pc[:, 0:512], dg[:, k, :], yc[:, c, 2 + k:2 + k + 512],
                                                       start=(k == 0), stop=(k == 30)),
                         [dgB, ycB[c]], [pcB], signal=(k == 30))
                if c + 3 < 8:
                    build_diag(c + 3)
                if pend_stats:
                    emit_stats()
                K.op("act", lambda e: e.activation(out=yv[:, c, :], in_=pc[:, 0:512], func=AF.Identity,
                                                   bias=par[:, c, 32:33]), [pcB, parB], [yvB[c]])
                yb, ybB = ybf.next()
                yq, yqB = ysq.next()
                K.op("dve", lambda e: e.tensor_copy(out=yb[:, :], in_=yv[:, c, :]), [yvB[c]], [ybB])
                K.op("pool", lambda e: e.tensor_tensor(out=yq[:, :], in0=yv[:, c, :], in1=yv[:, c, :], op=ALU.mult),
                     [yvB[c]], [yqB])
                pend_stats.append((c, yb, ybB, yq, yqB))
            while pend_stats:
                emit_stats()
            K.phase = "odLN"
            K.op("dve", lambda e: e.tensor_copy(out=mean[:, :], in_=pm[:, 0:512]), [pmB], [meanB])
            K.op("dve", lambda e: e.tensor_tensor(out=var[:, :], in0=mean[:, :], in1=mean[:, :], op=ALU.mult), [meanB], [varB])
            K.op("dve", lambda e: e.scalar_tensor_tensor(out=var[:, :], in0=pq[:, 0:512], scalar=LN_EPS, in1=var[:, :],
                                                         op0=ALU.add, op1=ALU.subtract), [pqB, varB], [varB])

        def od_ln_prep2():
            K.phase = "odLN"
            K.op("act", lambda e: e.activation(out=var[:, :], in_=var[:, :], func=AF.Sqrt), [varB], [varB])
            K.op("dve", lambda e: e.reciprocal(out=rstdt[:, :], in_=var[:, :]), [varB], [rstdtB])
            for c in range(8):
                K.op("dve", lambda e: e.tensor_tensor(out=yv[:, c, :], in0=yv[:, c, :], in1=mean[:, :], op=ALU.subtract),
                     [yvB[c], meanB], [yvB[c]])
                K.op("pool" if c % 2 == 0 else "dve", lambda e: e.tensor_tensor(out=yv[:, c, :], in0=yv[:, c, :], in1=rstdt[:, :],
                                                                              op=ALU.mult), [yvB[c], rstdtB], [yvB[c]])

        def od_ln_fin(bi):
            szz, szzB = szzs[bi % 2]
            K.phase = "odLNf"
            for c in range(8):
                ta, taB = t1.next()
                K.op("act", lambda e: e.activation(out=ta[:, :], in_=yv[:, c, :], func=AF.Silu,
                                                   scale=par[:, c, 33:34], bias=par[:, c, 34:35]),
                     [yvB[c], parB], [taB])
                K.op("dve", lambda e: e.tensor_tensor(out=mixT[:, c, :], in0=ta[:, :], in1=szz[:, c, :], op=ALU.mult),
                     [taB, szzB[c]], mixTB)

        od_proj(0)
        for bi, (s, b) in enumerate(blocks):
            if bi + 1 >= len(blocks) and layer + 1 < n_layers:
                load_w_in(layer + 1)
            od_conv(bi)
            if bi + 1 < len(blocks):
                K.phase = "odA"
                phase_A(layer, blocks[bi + 1][0], blocks[bi + 1][1])
                K.phase = "odConv"
                for c in range(3):
                    hooks["build_diag"](c, into=pre_dgs, eng="pool")
                od_proj(bi + 1, mid=od_ln_prep2)
            else:
                od_ln_prep2()
            od_ln_fin(bi)
            K.phase = "odG"
            phase_G(layer, s, b)
        st.close()

    load_w_in(0)
    for layer in range(n_layers):
        li = layer // 2
        load_common(layer)
        if layer > 0:
            K.barrier()
        if layer % 2 == 0:
            even_layer(layer, li)
        else:
            odd_layer(layer, li)

    for s in range(NSEQ):
        for t in range(16):
            b = xd[s][t]
            if b.w is not None:
                sm, v = b.w
                nc.sync.wait_ge(sm.h, v)
    stack.close()
    return nc, K


def make_consts():
    p = np.arange(128)
    ident = np.eye(128, dtype=np.float32)
    tril = (p[:, None] <= p[None, :]).astype(np.float32)
    far = (p[:, None] > p[None, :]).astype(np.float32)
    ctab = np.zeros((128, 16, 32), np.float32)
    j = np.arange(32)
    for tile in range(16):
        t = tile * 128 + p
        cur = t // 64
        forced = (j[None, :] == 0) | (j[None, :] == cur[:, None]) | (j[None, :] == cur[:, None] - 1)
        causal = j[None, :] <= cur[:, None]
        ctab[:, tile, :] = np.where(causal, forced.astype(np.float32) * np.float32(1e4), np.float32(-1e30))
    ebig = ((np.arange(2048)[None, :] // 64) == j[:, None]).astype(np.float32) * np.float32(BIG)
    n_cmp = 127
    tok = np.arange(n_cmp)[:, None] * 16 + np.arange(32)[None, :]
    ovl = ((tok[:, :, None] // 64) == np.arange(32)[None, None, :]).sum(1).astype(np.float32) / np.float32(32)
    return {"c_ident": ident, "c_tril": tril, "c_far": far, "c_ctab": ctab, "c_ebig": ebig, "c_ovl": ovl}


_CACHE = {}


def kernel(**inputs):
    if "nc" not in _CACHE:
        _CACHE["nc"] = build_program(4)[0]
    nc = _CACHE["nc"]
    consts = make_consts()
    x = np.ascontiguousarray(inputs["x"], dtype=np.float32)
    shared = {k: np.ascontiguousarray(v, dtype=np.float32) for k, v in inputs.items() if k != "x"}
    shared.update(consts)
    in_maps = []
    for c in range(NCORES):
        m = dict(shared)
        m["x"] = x[c * NSEQ:(c + 1) * NSEQ]
        in_maps.append(m)
    res = run_bass_kernel_spmd(nc, in_maps, core_ids=list(range(NCORES)))
    return np.concatenate([r["out"] for r in res.results], axis=0).astype(np.float32)
```
